# Optimizing a Trainium2 kernel written in Bass

```python
import jax
import jax.numpy as jnp
from jax import lax
import numpy as np

D_MODEL = 1024
BATCH = 16
SEQ = 2048
DEPTH = 4

HEAD_DIM = 64
N_HEADS_SB = 4
N_HEADS_GM = 4
N_HEADS_RW = 4
N_HEADS_FX = 4
W_SB = N_HEADS_SB * HEAD_DIM
W_GM = N_HEADS_GM * HEAD_DIM
W_RW = N_HEADS_RW * HEAD_DIM
W_FX = N_HEADS_FX * HEAD_DIM
D_MIX = W_SB + W_GM + W_RW + W_FX
BLOCK_Q = 128
CHUNK = 128
RW_DECAY_LORA = 64
RW_AAA_LORA = 64
RW_GATE_LORA = 128
SB_COLS = 3 * W_SB
GM_COLS = 2 * W_GM
RW_COLS = 3 * W_RW + RW_DECAY_LORA + RW_AAA_LORA + RW_GATE_LORA
FX_COLS = 3 * W_FX + N_HEADS_FX
D_IN_PROJ = SB_COLS + GM_COLS + RW_COLS + FX_COLS
N_EXPERT_GROUPS = 4
EXPERTS_PER_GROUP = 8
N_EXPERTS = N_EXPERT_GROUPS * EXPERTS_PER_GROUP
TOP_K = 2
EXPERT_HIDDEN = 512
MOE_BLOCK = 128
RMS_EPS = 1e-6
LN_EPS = 1e-5
GN_EPS = 64e-5
L2_EPS = 1e-12

kernel_name = 'hymba_style_sb_gmlp_rwkv7_fox_hiermoe'


def rms_norm(x, g):
    xf = x.astype(jnp.float32)
    y = xf * lax.rsqrt(jnp.mean(xf * xf, axis=-1, keepdims=True) + RMS_EPS)
    return (y * g.astype(jnp.float32)).astype(x.dtype)


def split_heads(t, n_heads):
    bsz, seq, _ = t.shape
    return t.reshape(bsz, seq, n_heads, HEAD_DIM).transpose(0, 2, 1, 3)


def merge_heads(t):
    bsz, n_heads, seq, dh = t.shape
    return t.transpose(0, 2, 1, 3).reshape(bsz, seq, n_heads * dh)


def qkv_heads(p, n_heads, width):
    q = split_heads(p[..., 0:width], n_heads)
    k = split_heads(p[..., width:2 * width], n_heads)
    v = split_heads(p[..., 2 * width:3 * width], n_heads)
    return q, k, v


def stick_breaking_attention(q, k, v):
    seq = q.shape[2]
    scale = HEAD_DIM ** -0.5
    outs = []
    for q0 in range(0, seq, BLOCK_Q):
        k_end = q0 + BLOCK_Q
        z = jnp.einsum('bhqd,bhkd->bhqk', q[:, :, q0:k_end], k[:, :, :k_end]).astype(jnp.float32) * scale
        strict = jnp.arange(k_end)[None, :] < (q0 + jnp.arange(BLOCK_Q))[:, None]
        log_one_minus = jnp.where(strict, jax.nn.log_sigmoid(-z), 0.0)
        between = lax.cumsum(log_one_minus, axis=3, reverse=True) - log_one_minus
        attn = jnp.where(strict, jnp.exp(jax.nn.log_sigmoid(z) + between), 0.0)
        outs.append(jnp.einsum('bhqk,bhkd->bhqd', attn.astype(v.dtype), v[:, :, :k_end]))
    return jnp.concatenate(outs, axis=2)


def forgetting_attention(q, k, v, log_f):
    seq = q.shape[2]
    scale = HEAD_DIM ** -0.5
    cum = lax.cumsum(log_f, axis=2)
    outs = []
    for q0 in range(0, seq, BLOCK_Q):
        k_end = q0 + BLOCK_Q
        logits = jnp.einsum('bhqd,bhkd->bhqk', q[:, :, q0:k_end], k[:, :, :k_end]).astype(jnp.float32) * scale
        logits = logits + cum[:, :, q0:k_end, None] - cum[:, :, None, :k_end]
        causal = jnp.arange(k_end)[None, :] <= (q0 + jnp.arange(BLOCK_Q))[:, None]
        probs = jax.nn.softmax(jnp.where(causal, logits, -jnp.inf), axis=-1)
        outs.append(jnp.einsum('bhqk,bhkd->bhqd', probs.astype(v.dtype), v[:, :, :k_end]))
    return jnp.concatenate(outs, axis=2)


def chunked_spatial_gating(p, w_s, b_s):
    bsz, seq, _ = p.shape
    hid = jax.nn.gelu(p)
    u, v = hid[..., :W_GM], hid[..., W_GM:]
    v = v.reshape(bsz, seq // CHUNK, CHUNK, N_HEADS_GM, HEAD_DIM).astype(jnp.float32)
    mean = jnp.mean(v, axis=-1, keepdims=True)
    var = jnp.mean(jnp.square(v - mean), axis=-1, keepdims=True)
    v = (v - mean) * lax.rsqrt(var + LN_EPS)
    w_causal = jnp.tril(w_s.astype(jnp.float32))
    mixed = jnp.einsum('gts,bcsgd->bctgd', w_causal, v) + b_s.astype(jnp.float32).T[None, None, :, :, None]
    return u * mixed.reshape(bsz, seq, W_GM).astype(p.dtype)


def rwkv7_time_mix(p, mu, w0, w2, a0, a2, g2, k_k, k_a, r_k, gn_w, gn_b):
    bsz, seq, _ = p.shape
    f32 = jnp.float32
    p = p.astype(f32)
    prev = jnp.pad(p, ((0, 0), (1, 0), (0, 0)))[:, :-1]
    p = p + (prev - p) * mu.astype(f32)
    r = p[..., 0:W_RW]
    k = p[..., W_RW:2 * W_RW]
    v = p[..., 2 * W_RW:3 * W_RW]
    o = 3 * W_RW
    xw = p[..., o:o + RW_DECAY_LORA]
    o = o + RW_DECAY_LORA
    xa = p[..., o:o + RW_AAA_LORA]
    o = o + RW_AAA_LORA
    xg = p[..., o:o + RW_GATE_LORA]
    w = -jax.nn.softplus(-(w0.astype(f32) + jnp.tanh(xw) @ w2.astype(f32))) - 0.5
    decay = jnp.exp(-jnp.exp(w))
    a = jax.nn.sigmoid(a0.astype(f32) + xa @ a2.astype(f32))
    g = jax.nn.sigmoid(xg) @ g2.astype(f32)
    kk = (k * k_k.astype(f32)).reshape(bsz, seq, N_HEADS_RW, HEAD_DIM)
    kk = kk / jnp.maximum(jnp.sqrt(jnp.sum(kk * kk, axis=-1, keepdims=True)), L2_EPS)
    k = k * (1.0 + (a - 1.0) * k_a.astype(f32))
    shp = (bsz, seq, N_HEADS_RW, HEAD_DIM)
    r_h, k_h, v_h, a_h, d_h = r.reshape(shp), k.reshape(shp), v.reshape(shp), a.reshape(shp), decay.reshape(shp)

    def step(state, inp):
        r_t, d_t, k_t, v_t, kk_t, a_t = inp
        sa = jnp.einsum('bhvk,bhk->bhv', state, -kk_t)
        state = (state * d_t[:, :, None, :]
                 + sa[..., None] * (kk_t * a_t)[:, :, None, :]
                 + v_t[..., None] * k_t[:, :, None, :])
        return state, jnp.einsum('bhvk,bhk->bhv', state, r_t)

    xs = tuple(jnp.swapaxes(t, 0, 1) for t in (r_h, d_h, k_h, v_h, kk, a_h))
    state0 = jnp.zeros((bsz, N_HEADS_RW, HEAD_DIM, HEAD_DIM), f32)
    _, y = lax.scan(step, state0, xs)
    y = jnp.swapaxes(y, 0, 1)
    mean = jnp.mean(y, axis=-1, keepdims=True)
    var = jnp.mean(jnp.square(y - mean), axis=-1, keepdims=True)
    y = ((y - mean) * lax.rsqrt(var + GN_EPS)).reshape(bsz, seq, W_RW)
    y = y * gn_w.astype(f32) + gn_b.astype(f32)
    bonus = (jnp.sum(r_h * k_h * r_k.astype(f32), axis=-1, keepdims=True) * v_h).reshape(bsz, seq, W_RW)
    return (y + bonus) * g


def routed_experts(hf, expert_idx, gates, w_gate, w_up, w_down):
    n_tok, d = hf.shape
    n_assign = n_tok * TOP_K
    flat_e = expert_idx.reshape(n_assign)
    order = jnp.argsort(flat_e)
    sorted_e = flat_e[order]
    counts = jnp.bincount(flat_e, length=N_EXPERTS)
    padded = (counts + MOE_BLOCK - 1) // MOE_BLOCK * MOE_BLOCK
    pad_end = jnp.cumsum(padded)
    pad_start = pad_end - padded
    seg_start = jnp.cumsum(counts) - counts
    slot = pad_start[sorted_e] + jnp.arange(n_assign) - seg_start[sorted_e]
    n_blocks = -(-n_assign // MOE_BLOCK) + N_EXPERTS
    n_slots = n_blocks * MOE_BLOCK
    slot_token = jnp.full((n_slots,), n_tok, dtype=jnp.int32).at[slot].set((order // TOP_K).astype(jnp.int32))
    block_expert = jnp.minimum(
        jnp.searchsorted(pad_end, jnp.arange(n_blocks) * MOE_BLOCK, side='right'), N_EXPERTS - 1)
    h_pad = jnp.concatenate([hf, jnp.zeros((1, d), hf.dtype)], axis=0)
    xb = h_pad[slot_token].reshape(n_blocks, MOE_BLOCK, d)

    def expert_block(args):
        xe, e = args
        return (jax.nn.silu(xe @ w_gate[e]) * (xe @ w_up[e])) @ w_down[e]

    yb = lax.map(expert_block, (xb, block_expert)).reshape(n_slots, d)
    slot_of_assign = jnp.zeros((n_assign,), slot.dtype).at[order].set(slot)
    y_assign = yb[slot_of_assign].reshape(n_tok, TOP_K, d)
    return jnp.einsum('nkd,nk->nd', y_assign, gates.astype(yb.dtype))


def hierarchical_moe(h, wg, bg, we, be, w_gate, w_up, w_down):
    bsz, seq, d = h.shape
    n_tok = bsz * seq
    hf = h.reshape(n_tok, d)
    rows = jnp.arange(n_tok)
    group_logits = (hf @ wg + bg).astype(jnp.float32)
    group_prob = jax.nn.softmax(group_logits, axis=-1)
    g_sel = jnp.argmax(group_logits, axis=-1)
    g_gate = group_prob[rows, g_sel]
    expert_logits = (hf @ we + be).astype(jnp.float32).reshape(n_tok, N_EXPERT_GROUPS, EXPERTS_PER_GROUP)
    in_group = expert_logits[rows, g_sel]
    top_logits, top_local = lax.top_k(in_group, TOP_K)
    gates = jax.nn.softmax(top_logits, axis=-1) * g_gate[:, None]
    expert_idx = g_sel[:, None] * EXPERTS_PER_GROUP + top_local
    y = routed_experts(hf, expert_idx, gates, w_gate, w_up, w_down)
    return y.reshape(bsz, seq, d).astype(h.dtype)


def setup_inputs(seed: int = 0) -> dict:
    key = jax.random.key(seed)
    ks = jax.random.split(key, 32)
    f32 = jnp.float32

    def nrm(k, shape, scale):
        return jax.random.normal(k, shape, f32) * scale

    resid_scale = (2 * DEPTH) ** -0.5
    return {
        'x': nrm(ks[0], (BATCH, SEQ, D_MODEL), 1.0),
        'norm1_g': 1.0 + nrm(ks[1], (DEPTH, D_MODEL), 0.02),
        'w_in': nrm(ks[2], (DEPTH, D_MODEL, D_IN_PROJ), D_MODEL ** -0.5),
        'gm_w_s': nrm(ks[3], (DEPTH, N_HEADS_GM, CHUNK, CHUNK), CHUNK ** -0.5),
        'gm_b': nrm(ks[4], (DEPTH, N_HEADS_GM, CHUNK), 0.02),
        'rw_mu': jax.random.uniform(ks[5], (DEPTH, RW_COLS), f32),
        'rw_w0': nrm(ks[6], (DEPTH, W_RW), 0.5),
        'rw_w2': nrm(ks[7], (DEPTH, RW_DECAY_LORA, W_RW), 0.5 * RW_DECAY_LORA ** -0.5),
        'rw_a0': nrm(ks[8], (DEPTH, W_RW), 0.1),
        'rw_a2': nrm(ks[9], (DEPTH, RW_AAA_LORA, W_RW), 0.5 * RW_AAA_LORA ** -0.5),
        'rw_g2': nrm(ks[10], (DEPTH, RW_GATE_LORA, W_RW), RW_GATE_LORA ** -0.5),
        'rw_k_k': 0.85 + nrm(ks[11], (DEPTH, W_RW), 0.02),
        'rw_k_a': 1.0 + nrm(ks[12], (DEPTH, W_RW), 0.02),
        'rw_r_k': nrm(ks[13], (DEPTH, N_HEADS_RW, HEAD_DIM), 0.1),
        'rw_gn_w': 1.0 + nrm(ks[14], (DEPTH, W_RW), 0.02),
        'rw_gn_b': nrm(ks[15], (DEPTH, W_RW), 0.02),
        'fx_b_f': nrm(ks[16], (DEPTH, N_HEADS_FX), 0.1),
        'w_out': nrm(ks[17], (DEPTH, D_MIX, D_MODEL), D_MIX ** -0.5 * resid_scale),
        'norm2_g': 1.0 + nrm(ks[18], (DEPTH, D_MODEL), 0.02),
        'router_group_w': nrm(ks[19], (DEPTH, D_MODEL, N_EXPERT_GROUPS), D_MODEL ** -0.5),
        'router_group_b': nrm(ks[20], (DEPTH, N_EXPERT_GROUPS), 0.01),
        'router_expert_w': nrm(ks[21], (DEPTH, D_MODEL, N_EXPERTS), D_MODEL ** -0.5),
        'router_expert_b': nrm(ks[22], (DEPTH, N_EXPERTS), 0.01),
        'exp_w_gate': nrm(ks[23], (DEPTH, N_EXPERTS, D_MODEL, EXPERT_HIDDEN), D_MODEL ** -0.5),
        'exp_w_up': nrm(ks[24], (DEPTH, N_EXPERTS, D_MODEL, EXPERT_HIDDEN), D_MODEL ** -0.5),
        'exp_w_down': nrm(ks[25], (DEPTH, N_EXPERTS, EXPERT_HIDDEN, D_MODEL), EXPERT_HIDDEN ** -0.5 * resid_scale),
        'final_norm_g': 1.0 + nrm(ks[26], (D_MODEL,), 0.02),
    }


def reference(x, norm1_g, w_in, gm_w_s, gm_b, rw_mu, rw_w0, rw_w2, rw_a0, rw_a2, rw_g2,
              rw_k_k, rw_k_a, rw_r_k, rw_gn_w, rw_gn_b, fx_b_f, w_out, norm2_g,
              router_group_w, router_group_b, router_expert_w, router_expert_b,
              exp_w_gate, exp_w_up, exp_w_down, final_norm_g):
    splits = [SB_COLS, SB_COLS + GM_COLS, SB_COLS + GM_COLS + RW_COLS]
    for layer in range(DEPTH):
        h = rms_norm(x, norm1_g[layer])
        proj = h @ w_in[layer]
        p_sb, p_gm, p_rw, p_fx = jnp.split(proj, splits, axis=-1)

        q, k, v = qkv_heads(p_sb, N_HEADS_SB, W_SB)
        y_sb = merge_heads(stick_breaking_attention(q, k, v))

        y_gm = chunked_spatial_gating(p_gm, gm_w_s[layer], gm_b[layer])

        y_rw = rwkv7_time_mix(p_rw, rw_mu[layer], rw_w0[layer], rw_w2[layer], rw_a0[layer],
                              rw_a2[layer], rw_g2[layer], rw_k_k[layer], rw_k_a[layer],
                              rw_r_k[layer], rw_gn_w[layer], rw_gn_b[layer]).astype(x.dtype)

        q, k, v = qkv_heads(p_fx, N_HEADS_FX, W_FX)
        log_f = jax.nn.log_sigmoid(p_fx[..., 3 * W_FX:].astype(jnp.float32)
                                   + fx_b_f[layer].astype(jnp.float32))
        y_fx = merge_heads(forgetting_attention(q, k, v, jnp.transpose(log_f, (0, 2, 1))))

        mix = jnp.concatenate([y_sb, y_gm, y_rw, y_fx], axis=-1)
        x = x + mix @ w_out[layer]

        h2 = rms_norm(x, norm2_g[layer])
        x = x + hierarchical_moe(h2, router_group_w[layer], router_group_b[layer],
                                 router_expert_w[layer], router_expert_b[layer],
                                 exp_w_gate[layer], exp_w_up[layer], exp_w_down[layer])
    return rms_norm(x, final_norm_g)
```

```python
import numpy as np
import concourse.bass as bass
import concourse.mybir as mybir
from concourse.bass_utils import run_bass_kernel_spmd

F32 = mybir.dt.float32
BF16 = mybir.dt.bfloat16
AF = mybir.ActivationFunctionType
ALU = mybir.AluOpType
AX = mybir.AxisListType

T = 2048
D = 1024
NT = 16
DIN = 3076
NE = 32
EH = 512
NDMA_SEMS = 12
RMS_EPS = 1e-6
LN_EPS = 1e-5
GN_EPS = 64e-5
NEGM = -30000.0
import os
MAXOPS = int(os.environ.get("KB_MAXOPS", "1000000000"))


class KB:
    def __init__(self):
        self.nc = bass.Bass("TRN2", target_bir_lowering=False)
        nc = self.nc
        self.engs = {"pe": nc.tensor, "act": nc.scalar, "dve": nc.vector,
                     "pool": nc.gpsimd, "sp": nc.sync}
        self.sem = {}
        self.cnt = {}
        for e in self.engs:
            self.sem[e] = nc.alloc_semaphore("s_" + e)
            self.cnt[e] = 0
        self.dsem = {}
        self.dcnt = {}
        self.drr = {}
        for q in ("sp", "pool"):
            self.dsem[q] = [nc.alloc_semaphore("d_%s%d" % (q, i)) for i in range(NDMA_SEMS)]
            self.dcnt[q] = [0] * NDMA_SEMS
            self.drr[q] = 0
        self.waited = {e: {} for e in self.engs}
        self.lastw = {}
        self.readers = {}
        self.nins = 0

    def _semof(self, sk):
        if sk[0] == "c":
            return self.sem[sk[1]]
        return self.dsem[sk[1]][sk[2]]

    def _wait(self, eng, tok):
        sk, val = tok
        if eng == "pe" and sk == ("c", "pe"):
            return
        w = self.waited[eng]
        if w.get(sk, 0) >= val:
            return
        self.engs[eng].wait_ge(self._semof(sk), val)
        w[sk] = val
        self.nins += 1

    def _deps(self, eng, reads, writes):
        for k in reads:
            t = self.lastw.get(k)
            if t is not None:
                self._wait(eng, t)
        for k in writes:
            t = self.lastw.get(k)
            if t is not None:
                self._wait(eng, t)
            for t in self.readers.get(k, ()):
                self._wait(eng, t)

    def _record(self, tok, reads, writes):
        for k in reads:
            lst = self.readers.setdefault(k, [])
            lst[:] = [t for t in lst if t[0] != tok[0]]
            lst.append(tok)
        for k in writes:
            self.lastw[k] = tok
            self.readers[k] = []

    def op(self, eng, fn, reads=(), writes=()):
        self.nops = getattr(self, "nops", 0) + 1
        if self.nops > MAXOPS:
            return None
        pr = [k for k in reads if k.startswith("pb")]
        if pr:
            reads = [k for k in reads if not k.startswith("pb")]
            writes = list(writes) + pr
        self._deps(eng, reads, writes)
        ins = fn(self.engs[eng])
        self.cnt[eng] += 1
        ins.then_inc(self.sem[eng], 1)
        tok = (("c", eng), self.cnt[eng])
        self._record(tok, reads, writes)
        self.nins += 1
        return tok

    def dma(self, out, in_, reads=(), writes=(), q="sp", **kw):
        self.nops = getattr(self, "nops", 0) + 1
        if self.nops > MAXOPS:
            return None
        i = self.drr[q]
        self.drr[q] = (i + 1) % NDMA_SEMS
        sk = ("d", q, i)
        if self.dcnt[q][i] > 0:
            self._wait(q, (sk, self.dcnt[q][i]))
        self._deps(q, reads, writes)
        ins = self.engs[q].dma_start(out=out, in_=in_, **kw)
        self.dcnt[q][i] += 16
        ins.then_inc(self.dsem[q][i], 16)
        tok = (sk, self.dcnt[q][i])
        self._record(tok, reads, writes)
        self.nins += 1
        return tok

    def barrier(self):
        toks = [(("c", e), self.cnt[e]) for e in self.engs if self.cnt[e] > 0]
        for q in self.dsem:
            for i in range(NDMA_SEMS):
                if self.dcnt[q][i] > 0:
                    toks.append((("d", q, i), self.dcnt[q][i]))
        for e in self.engs:
            for t in toks:
                if t[0] == ("c", e):
                    continue
                self._wait(e, t)
        self.lastw = {}
        self.readers = {}

    def dram(self, name, shape, dt=F32, kind="Internal"):
        return self.nc.dram_tensor(name, list(shape), dt, kind=kind).ap()


def _dtsize(dt):
    return 2 if dt == BF16 else 4


class Arena:
    def __init__(self, base_ap_f32, start, end):
        self.A = base_ap_f32
        self.cur = start
        self.end = end

    def t(self, shape, dt=F32, parts=128):
        n = 1
        for s in shape[1:]:
            n *= s
        nbytes = (n * _dtsize(dt) + 31) // 32 * 32
        off = self.cur
        self.cur += nbytes
        assert self.cur <= self.end, ("arena overflow", self.cur, self.end)
        v = self.A[0:shape[0], off // 4:(off + nbytes) // 4]
        if dt == BF16:
            v = v.bitcast(BF16)
        v = v[:, 0:n]
        if len(shape) == 3:
            v = v.rearrange("p (a b) -> p a b", a=shape[1])
        elif len(shape) == 4:
            v = v.rearrange("p (a b c) -> p a b c", a=shape[1], b=shape[2])
        return v


def make_consts():
    j = np.arange(128)[:, None]
    t = np.arange(128)[None, :]
    c = np.zeros((128, 8, 128), np.float32)
    c[:, 0] = np.eye(128)
    c[:, 1] = np.where(j >= t, -1.0, 0.0)
    c[:, 2] = -1.0
    c[:, 3] = np.where(j < t, 0.0, NEGM)
    c[:, 4] = np.where(j <= t, 0.0, NEGM)
    c[:, 5] = np.where(j < t, 1.0, 0.0)
    c[:, 6] = np.where(j <= t, 1.0, 0.0)
    c[:, 7] = np.where(t < j, 1.0, 0.0)
    return c.reshape(128, 1024)


class Prog:
    def __init__(self, NL=4, NB=2, dbg=(), stop=None, moe_experts=NE):
        self.NL, self.NB = NL, NB
        self.stop = stop
        self.moe_experts = moe_experts
        kb = self.kb = KB()
        nc = self.nc = kb.nc
        dk = lambda n: ("ExternalOutput" if n in dbg else "Internal")
        I = lambda n, s: kb.dram(n, s, F32, kind="ExternalInput")
        self.x_in = I("x", [NB, T, D])
        self.consts_d = I("consts", [128, 1024])
        self.norm1_g = I("norm1_g", [NL, D])
        self.w_in = I("w_in", [NL, D, DIN])
        self.gm_wT = I("gm_wT", [NL, 4, 128, 128])
        self.gm_bT = I("gm_bT", [NL, 128, 4])
        self.rw_mu = I("rw_mu", [NL, 1024])
        self.rw_pv = I("rw_pv", [NL, 64, 32])
        self.rw_w2 = I("rw_w2", [NL, 64, 256])
        self.rw_a2 = I("rw_a2", [NL, 64, 256])
        self.rw_g2 = I("rw_g2", [NL, 128, 256])
        self.rw_gn_w = I("rw_gn_w", [NL, 256])
        self.rw_gn_b = I("rw_gn_b", [NL, 256])
        self.fx_b_f = I("fx_b_f", [NL, 4])
        self.w_out = I("w_out", [NL, D, D])
        self.norm2_g = I("norm2_g", [NL, D])
        self.router_w = I("router_w", [NL, D, 36])
        self.router_b = I("router_b", [NL, 36])
        self.w_gate = I("exp_w_gate", [NL, NE, D, EH])
        self.w_up = I("exp_w_up", [NL, NE, D, EH])
        self.w_down = I("exp_w_down", [NL, NE, EH, D])
        self.final_g = I("final_norm_g", [1, D])
        self.out = kb.dram("out", [NB, T, D], F32, kind="ExternalOutput")
        S = lambda n, s, dt=F32: kb.dram(n, s, dt, kind=dk(n))
        self.xres = S("xres", [NB, T, D])
        self.qkT = S("qkT", [NB, 16, 64, T], BF16)
        self.vtok = S("vtok", [NB, T, 512], BF16)
        self.rwT = S("rwT", [NB, 1024, T])
        self.fgT = S("fgT", [NB, 4, T])
        self.cumd = S("cumd", [NB, 4, T])
        self.mixT = S("mixT", [NB, 1024, T], BF16)
        self.h2T = S("h2T", [NB, 1024, T], BF16)
        self.gates = S("gates", [NB, T, 32])
        arena = nc.alloc_sbuf_tensor("arena", [128, 52000], F32)
        self.arena_ap = arena[:]
        self.pb = [nc.alloc_psum_tensor("pb%d" % i, [128, 512], F32)[:] for i in range(8)]
        self.R1 = Arena(self.arena_ap, 0, 40000)
        self.setup_consts()

    def P(self, eng, meth, *a, r=(), w=(), **kw):
        return self.kb.op(eng, lambda e: getattr(e, meth)(*a, **kw), r, w)

    def mm(self, out, lhsT, rhs, start, stop, r=(), w=()):
        return self.kb.op("pe", lambda e: e.matmul(out, lhsT, rhs, start=start, stop=stop), r, w)

    def actf(self, out, in_, func, r=(), w=(), eng="act", **kw):
        return self.kb.op("act", lambda e: e.activation(out, in_, func, **kw), r, w)

    def col(self, dram_row):
        return dram_row.rearrange("(p o) -> p o", o=1)

    def setup_consts(self):
        kb = self.kb
        a = self.R1
        self.cst = a.t([128, 8, 128])
        kb.dma(self.cst.rearrange("p a b -> p (a b)"), self.consts_d, writes=["cst"])
        c = self.cst
        self.ident = c[:, 0, :]
        self.ntri = c[:, 1, :]
        self.negones = c[:, 2, :]
        self.mask2 = c[:, 5:7, :]
        self.m_incl = c[:, 6, :]
        self.m_lows = c[:, 7, :]
        self.ident_bf = a.t([128, 128], BF16)
        self.mbs_bf = a.t([128, 128], BF16)
        self.mbi_bf = a.t([128, 128], BF16)
        self.P("dve", "tensor_copy", self.ident_bf, c[:, 0, :], r=["cst"], w=["ident_bf"])
        self.P("dve", "tensor_copy", self.mbs_bf, c[:, 3, :], r=["cst"], w=["mbs_bf"])
        self.P("dve", "tensor_copy", self.mbi_bf, c[:, 4, :], r=["cst"], w=["mbi_bf"])
        self.ones_f = a.t([128, 128])
        self.P("pool", "memset", self.ones_f, 1.0, w=["ones_f"])
        self.ones_bf = a.t([128, 64], BF16)
        self.P("pool", "memset", self.ones_bf, 1.0, w=["ones_bf"])
        self.reset = a.t([64, 512])
        self.P("pool", "memset", self.reset, 1.0, w=["reset"])
        self.P("pool", "memset", self.reset.rearrange("p (c t) -> p c t", c=4)[:, :, 0:1], 0.0, w=["reset"])
        self.CK = ["cst", "ident_bf", "mbs_bf", "mbi_bf", "ones_f", "ones_bf", "reset", "ones4"]
        self.g1bc = a.t([128, D])
        self.g2bc = a.t([128, D])
        self.rbbc = a.t([128, 36])
        self.wr = a.t([128, 8, 36])
        self.gmw = a.t([128, 4, 128])
        self.gmb = a.t([128, 4])
        self.gnw = a.t([128, 256])
        self.gnb = a.t([128, 256])
        self.w2s = a.t([64, 256])
        self.a2s = a.t([64, 256])
        self.g2s = a.t([128, 256])
        self.pv = a.t([64, 40])
        self.mu_w = a.t([64, 1])
        self.mu_a = a.t([64, 1])
        self.mu_g = a.t([128, 1])
        self.fbf = a.t([4, 1])
        self.nfbf = a.t([4, 1])
        self.Mst = a.t([64, 4, 64])
        self.r1_end = a.cur

    def keepconsts(self):
        pass

    def load_layer(self, l):
        kb = self.kb
        bc = lambda row: row.partition_broadcast(128)
        kb.dma(self.g1bc, bc(self.norm1_g[l:l + 1, :]), writes=["g1bc"])
        kb.dma(self.g2bc, bc(self.norm2_g[l:l + 1, :]), writes=["g2bc"])
        kb.dma(self.rbbc, bc(self.router_b[l:l + 1, :]), writes=["rbbc"])
        kb.dma(self.wr, self.router_w[l].rearrange("(k p) n -> p k n", p=128), writes=["wr"])
        kb.dma(self.gmw, self.gm_wT[l].rearrange("g s t -> s g t"), writes=["gmw"])
        self.P("dve", "tensor_tensor", self.gmw, self.gmw, self.m_incl.unsqueeze(1).to_broadcast([128, 4, 128]),
               ALU.mult, r=["gmw"], w=["gmw"])
        kb.dma(self.gmb, self.gm_bT[l], writes=["gmb"])
        kb.dma(self.gnw, bc(self.rw_gn_w[l:l + 1, :]), writes=["gnw"])
        kb.dma(self.gnb, bc(self.rw_gn_b[l:l + 1, :]), writes=["gnb"])
        kb.dma(self.w2s, self.rw_w2[l], writes=["w2s"])
        kb.dma(self.a2s, self.rw_a2[l], writes=["a2s"])
        kb.dma(self.g2s, self.rw_g2[l], writes=["g2s"])
        kb.dma(self.pv[:, 0:32], self.rw_pv[l], writes=["pv"])
        self.P("dve", "tensor_scalar", self.pv[:, 32:36], self.pv[:, 24:28], -1.0, 1.0, ALU.mult, ALU.add,
               r=["pv"], w=["pv"])
        kb.dma(self.mu_w, self.col(self.rw_mu[l, 768:832]), writes=["mu_w"])
        kb.dma(self.mu_a, self.col(self.rw_mu[l, 832:896]), writes=["mu_a"])
        kb.dma(self.mu_g, self.col(self.rw_mu[l, 896:1024]), writes=["mu_g"])
        kb.dma(self.fbf, self.col(self.fx_b_f[l]), writes=["fbf"])
        self.P("dve", "tensor_single_scalar", self.nfbf, self.fbf, -1.0, ALU.mult, r=["fbf"], w=["nfbf"])

    def rmsnorm(self, xt, xk, gbc, gk, out, outk, tmp, tmpk, st, stk):
        self.actf(tmp, xt, AF.Square, r=[xk], w=[tmpk, stk], accum_out=st[:, 0:1])
        self.actf(st[:, 1:2], st[:, 0:1], AF.Ln, r=[stk], w=[stk], scale=1.0 / D, bias=RMS_EPS)
        self.actf(st[:, 2:3], st[:, 1:2], AF.Exp, r=[stk], w=[stk], scale=-0.5)
        self.P("dve", "scalar_tensor_tensor", out, xt, st[:, 2:3], gbc, ALU.mult, ALU.mult,
               r=[xk, stk, gk], w=[outk])

    def phaseA(self, l, b, src):
        kb = self.kb
        a = Arena(self.arena_ap, self.r1_end, 208000)
        win = a.t([128, 8, DIN], BF16)
        for kc in range(8):
            kb.dma(win[:, kc, :], self.w_in[l, kc * 128:(kc + 1) * 128, :], writes=["win%d" % kc], q="pool")
        WK = ["win%d" % kc for kc in range(8)]
        xt = [a.t([128, D]) for _ in range(2)]
        junk = a.t([128, D])
        hb = [a.t([128, D], BF16) for _ in range(2)]
        hT4 = [a.t([128, 8, 512], BF16) for _ in range(2)]
        st = [a.t([128, 4]) for _ in range(2)]
        vsb = [a.t([128, 512], BF16) for _ in range(2)]
        xg = a.t([128, 512]); x2 = a.t([128, 512]); u1 = a.t([128, 512]); sg = a.t([128, 512]); hid = a.t([128, 512])
        gst = a.t([128, 16])
        vc = a.t([128, 256]); sq = a.t([128, 256]); vn = a.t([128, 256]); tmpg = a.t([128, 256]); ygm = a.t([128, 256])
        ygT = [a.t([128, 2, 128], BF16) for _ in range(2)]
        stg_bf = [a.t([64, 512], BF16) for _ in range(4)]
        stg_f = [a.t([128, 512]) for _ in range(4)]
        pb = self.pb
        pbT = pb[7][:, 0:512].bitcast(BF16)
        fm = []
        for h in range(4):
            fm.append((h * 64, 64, "qk", h, 1.0))
            fm.append((256 + h * 64, 64, "qk", 4 + h, 0.125))
            fm.append((2304 + h * 64, 64, "qk", 8 + h, 1.0))
            fm.append((2560 + h * 64, 64, "qk", 12 + h, 0.125))
        for j in range(12):
            fm.append((1280 + j * 64, 64, "rw", j * 64, 1.0))
        fm.append((2048, 64, "rw", 768, 1.0))
        fm.append((2112, 64, "rw", 832, 1.0))
        fm.append((2176, 128, "rw", 896, 1.0))
        fm.append((3072, 4, "fg", 0, 1.0))
        ci = 0
        for g4 in range(4):
            hT = hT4[g4 % 2]
            hTk = "hT4_%d" % (g4 % 2)
            for tt in range(4):
                t = g4 * 4 + tt
                p = t % 2
                rows = slice(t * 128, (t + 1) * 128)
                kb.dma(xt[p], src[rows, :], writes=["xt%d" % p])
                self.rmsnorm(xt[p], "xt%d" % p, self.g1bc, "g1bc", hb[p], "hb%d" % p, junk, "junk", st[p], "st%d" % p)
                for kc in range(8):
                    self.P("pe", "transpose", pbT[:, kc * 128:(kc + 1) * 128], hb[p][:, kc * 128:(kc + 1) * 128],
                           self.ident_bf, r=["hb%d" % p, "ident_bf"], w=["pb7"])
                self.actf(hT[:, :, tt * 128:(tt + 1) * 128], pbT.rearrange("p (k n) -> p k n", k=8), AF.Copy,
                          r=["pb7"], w=[hTk])
                for (pbi, pc0, c0, n) in ((0, 0, 512, 512), (1, 0, 1024, 256), (1, 256, 2816, 256)):
                    for kc in range(8):
                        self.mm(pb[pbi][:, pc0:pc0 + n], hT[:, kc, tt * 128:(tt + 1) * 128], win[:, kc, c0:c0 + n],
                                kc == 0, kc == 7, r=[hTk, WK[kc]], w=["pb%d" % pbi])
                self.actf(vsb[p][:, 0:256], pb[0][:, 0:256], AF.Copy, r=["pb0"], w=["vsb%d" % p])
                self.actf(vsb[p][:, 256:512], pb[1][:, 256:512], AF.Copy, r=["pb1"], w=["vsb%d" % p])
                kb.dma(self.vtok[b, rows, :], vsb[p], reads=["vsb%d" % p], writes=["vtok"])
                self.P("dve", "tensor_copy", xg[:, 0:256], pb[0][:, 256:512], r=["pb0"], w=["xg"])
                self.P("dve", "tensor_copy", xg[:, 256:512], pb[1][:, 0:256], r=["pb1"], w=["xg"])
                self.actf(x2, xg, AF.Square, r=["xg"], w=["x2"])
                self.P("dve", "tensor_scalar", u1, x2, 0.044715, 1.0, ALU.mult, ALU.add, r=["x2"], w=["u1"])
                self.P("dve", "tensor_tensor", u1, u1, xg, ALU.mult, r=["u1", "xg"], w=["u1"])
                self.actf(sg, u1, AF.Sigmoid, r=["u1"], w=["sg"], scale=1.5957691216057308)
                self.P("dve", "tensor_tensor", hid, sg, xg, ALU.mult, r=["sg", "xg"], w=["hid"])
                hv = hid[:, 256:512].rearrange("p (g d) -> p g d", g=4)
                v3 = lambda ap: ap.rearrange("p (g d) -> p g d", g=4)
                bc4 = lambda ap: ap.unsqueeze(2).to_broadcast([128, 4, 64])
                self.P("dve", "tensor_reduce", gst[:, 0:4], hv, AX.X, ALU.add, r=["hid"], w=["gst"])
                self.P("dve", "tensor_single_scalar", gst[:, 4:8], gst[:, 0:4], -1.0 / 64, ALU.mult, r=["gst"], w=["gst"])
                self.P("dve", "tensor_tensor", v3(vc), hv, bc4(gst[:, 4:8]), ALU.add, r=["hid", "gst"], w=["vc"])
                self.actf(sq, vc, AF.Square, r=["vc"], w=["sq"])
                self.P("dve", "tensor_reduce", gst[:, 8:12], v3(sq), AX.X, ALU.add, r=["sq"], w=["gst"])
                self.actf(gst[:, 8:12], gst[:, 8:12], AF.Ln, r=["gst"], w=["gst"], scale=1.0 / 64, bias=LN_EPS)
                self.actf(gst[:, 12:16], gst[:, 8:12], AF.Exp, r=["gst"], w=["gst"], scale=-0.5)
                self.P("dve", "tensor_tensor", v3(vn), v3(vc), bc4(gst[:, 12:16]), ALU.mult, r=["vc", "gst"], w=["vn"])
                for g in range(4):
                    self.mm(pb[2][:, g * 64:(g + 1) * 64], self.gmw[:, g, :], vn[:, g * 64:(g + 1) * 64], True, True,
                            r=["gmw", "vn"], w=["pb2"])
                self.P("dve", "tensor_tensor", v3(tmpg), v3(pb[2][:, 0:256]), bc4(self.gmb), ALU.add,
                       r=["pb2", "gmb"], w=["tmpg"])
                self.P("dve", "tensor_tensor", ygm, tmpg, hid[:, 0:256], ALU.mult, r=["tmpg", "hid"], w=["ygm"])
                for c in range(2):
                    self.P("pe", "transpose", pb[2][:, 256 + c * 128:256 + (c + 1) * 128], ygm[:, c * 128:(c + 1) * 128],
                           self.ident, r=["ygm", "cst"], w=["pb2"])
                self.actf(ygT[p].rearrange("p c n -> p (c n)"), pb[2][:, 256:512], AF.Copy, r=["pb2"], w=["ygT%d" % p])
                kb.dma(self.mixT[b, 256:512, rows].rearrange("(c p) n -> p c n", p=128), ygT[p],
                       reads=["ygT%d" % p], writes=["mixT"])
            cols = slice(g4 * 512, (g4 + 1) * 512)
            for (c0, M, kind, dest, scale) in fm:
                pi = 3 + ci % 4
                si = ci % 4
                ci += 1
                pk = "pb%d" % pi
                for kc in range(8):
                    self.mm(pb[pi][0:M, :], win[:, kc, c0:c0 + M], hT[:, kc, :], kc == 0, kc == 7,
                            r=[hTk, WK[kc]], w=[pk])
                if kind == "qk":
                    self.actf(stg_bf[si], pb[pi][0:64, :], AF.Copy, r=[pk], w=["stgb%d" % si], scale=scale)
                    kb.dma(self.qkT[b, dest, :, cols], stg_bf[si], reads=["stgb%d" % si], writes=["qkT"])
                elif kind == "rw":
                    self.P("dve", "tensor_copy", stg_f[si][0:M, :], pb[pi][0:M, :], r=[pk], w=["stgf%d" % si])
                    kb.dma(self.rwT[b, dest:dest + M, cols], stg_f[si][0:M, :], reads=["stgf%d" % si], writes=["rwT"])
                else:
                    self.P("dve", "tensor_copy", stg_f[si][0:M, :], pb[pi][0:M, :], r=[pk], w=["stgf%d" % si])
                    kb.dma(self.fgT[b, :, cols], stg_f[si][0:M, :], reads=["stgf%d" % si], writes=["fgT"])

    def phaseB_attn(self, l, b):
        kb = self.kb
        pball = self.pb
        a = Arena(self.arena_ap, self.r1_end, 208000)
        fg = a.t([4, T]); fe = a.t([4, T]); cum = a.t([4, T])
        self.ones4 = a.t([4, T])
        self.P("pool", "memset", self.ones4, 1.0, w=["ones4"])
        ncum_all = a.t([128, 16, 4])
        NS = 2
        slots = []
        for s in range(NS):
            B = dict(qT=a.t([64, T], BF16), kT=a.t([64, T], BF16), V=a.t([128, 16, 64], BF16),
                     E=[a.t([128, 512]) for _ in range(2)], Lp=[a.t([128, 512]) for _ in range(2)],
                     At=[a.t([128, 512], BF16) for _ in range(2)], Slp=a.t([128, 512]),
                     ob=[a.t([64, 512], BF16) for _ in range(2)], rec=a.t([64, 512]), cumrow=a.t([1, T]))
            slots.append(B)
        kb.dma(fg, self.fgT[b], reads=["fgT"], writes=["fg"])
        self.actf(fe, fg, AF.Exp, r=["fg", "nfbf"], w=["fe"], scale=-1.0, bias=self.nfbf)
        self.actf(fe, fe, AF.Ln, r=["fe"], w=["fe"], bias=1.0)
        self.P("dve", "tensor_tensor_scan", cum, self.ones4, fe, 0.0, ALU.mult, ALU.subtract,
               r=["fe", "ones4"], w=["cum"])
        kb.dma(self.cumd[b], cum, reads=["cum"], writes=["cumd"])
        for kbk in range(16):
            self.P("pe", "transpose", pball[6][:, kbk * 4:(kbk + 1) * 4], cum[0:4, kbk * 128:(kbk + 1) * 128],
                   self.ident[0:4, 0:4], r=["cum", "cst"], w=["pb6"])
        self.actf(ncum_all.rearrange("p a b -> p (a b)"), pball[6][:, 0:64], AF.Copy, r=["pb6"], w=["ncum"], scale=-1.0)

        def head_gen(kind, h, s):
            B = slots[s]
            K = lambda n: "%s_s%d" % (n, s)
            pb = pball[4 * s:4 * s + 4]
            pk = ["pb%d" % (4 * s + i) for i in range(4)]
            qT, kT, V, Slp, rec, cumrow = B["qT"], B["kT"], B["V"], B["Slp"], B["rec"], B["cumrow"]
            qk_q = h if kind == "sb" else 8 + h
            qk_k = 4 + h if kind == "sb" else 12 + h
            vc0 = h * 64 if kind == "sb" else 256 + h * 64
            mrow = h * 64 if kind == "sb" else 768 + h * 64
            kb.dma(qT, self.qkT[b, qk_q], reads=["qkT"], writes=[K("qT")])
            kb.dma(kT, self.qkT[b, qk_k], reads=["qkT"], writes=[K("kT")])
            kb.dma(V, self.vtok[b, :, vc0:vc0 + 64].rearrange("(kb p) d -> p kb d", p=128),
                   reads=["vtok"], writes=[K("V")])
            if kind == "fx":
                kb.dma(cumrow, self.cumd[b, h:h + 1, :], reads=["cumd"], writes=[K("cumrow")])
            yield
            it = 0
            for c in range(4):
                Q0 = c * 512
                last = 4 * c + 3
                if kind == "sb":
                    self.P("pool", "memset", Slp, 0.0, w=[K("Slp")])
                    order = list(range(last, -1, -1))
                else:
                    order = list(range(0, last + 1))
                first_pv = True
                for kbk in order:
                    r_ = kbk - 4 * c
                    diag = r_ >= 0
                    col0 = max(r_, 0) * 128
                    i2 = it % 2
                    it += 1
                    E, Lp, At = B["E"][i2], B["Lp"][i2], B["At"][i2]
                    Ek, Lk, Ak = K("E%d" % i2), K("Lp%d" % i2), K("At%d" % i2)
                    kblk = kT[:, kbk * 128:(kbk + 1) * 128]
                    qs = lambda c_a, c_b: qT[:, Q0 + c_a:Q0 + c_b]
                    blocks = []
                    if diag:
                        blocks.append((col0, col0 + 128, True))
                        if col0 + 128 < 512:
                            blocks.append((col0 + 128, 512, False))
                    else:
                        blocks.append((0, 512, False))
                    if kind == "sb":
                        pz, pzk = (pb[0], pk[0]) if i2 == 0 else (pb[3], pk[3])
                        pa, pak = pb[1], pk[1]
                        for (lo, hi, dg) in blocks:
                            if dg:
                                self.mm(pz[:, lo:hi], self.ident_bf, self.mbs_bf, True, False, r=["ident_bf", "mbs_bf"], w=[pzk])
                            self.mm(pz[:, lo:hi], kblk, qs(lo, hi), not dg, True, r=[K("kT"), K("qT")], w=[pzk])
                        yield
                        self.actf(E[:, col0:512], pz[:, col0:512], AF.Exp, r=[pzk], w=[Ek])
                        yield
                        self.actf(Lp[:, col0:512], E[:, col0:512], AF.Ln, r=[Ek], w=[Lk], bias=1.0)
                        yield
                        for (lo, hi, dg) in blocks:
                            if dg:
                                self.mm(pa[:, lo:hi], self.ident_bf, self.mbs_bf, True, False, r=["ident_bf", "mbs_bf"], w=[pak])
                            self.mm(pa[:, lo:hi], kblk, qs(lo, hi), not dg, False, r=[K("kT"), K("qT")], w=[pak])
                            self.mm(pa[:, lo:hi], self.ntri, Lp[:, lo:hi], False, False, r=["cst", Lk], w=[pak])
                            self.mm(pa[:, lo:hi], self.negones, Slp[:, lo:hi], False, True, r=["cst", K("Slp")], w=[pak])
                        yield
                        self.actf(At[:, col0:512], pa[:, col0:512], AF.Exp, r=[pak], w=[Ak])
                        self.P("pool", "tensor_tensor", Slp[:, col0:512], Slp[:, col0:512], Lp[:, col0:512], ALU.add,
                               r=[K("Slp"), Lk], w=[K("Slp")])
                        yield
                        for (lo, hi, dg) in blocks:
                            self.mm(pb[2][0:64, lo:hi], V[:, kbk, :], At[:, lo:hi], first_pv, kbk == 0,
                                    r=[K("V"), Ak], w=[pk[2]])
                            first_pv = False
                        yield
                    else:
                        pz, pzk = (pb[0], pk[0]) if i2 == 0 else (pb[1], pk[1])
                        for (lo, hi, dg) in blocks:
                            if dg:
                                self.mm(pz[:, lo:hi], self.ident_bf, self.mbi_bf, True, False, r=["ident_bf", "mbi_bf"], w=[pzk])
                            self.mm(pz[:, lo:hi], kblk, qs(lo, hi), not dg, False, r=[K("kT"), K("qT")], w=[pzk])
                            self.mm(pz[:, lo:hi], self.ones_f[0:1, :], cumrow[0:1, Q0 + lo:Q0 + hi], False, True,
                                    r=["ones_f", K("cumrow")], w=[pzk])
                        yield
                        self.actf(At[:, col0:512], pz[:, col0:512], AF.Exp, r=[pzk, "ncum"], w=[Ak],
                                  bias=ncum_all[:, kbk, h:h + 1])
                        yield
                        for (lo, hi, dg) in blocks:
                            self.mm(pb[2][0:64, lo:hi], V[:, kbk, :], At[:, lo:hi], first_pv, dg,
                                    r=[K("V"), Ak], w=[pk[2]])
                            self.mm(pb[3][0:64, lo:hi], self.ones_bf, At[:, lo:hi], first_pv, dg,
                                    r=["ones_bf", Ak], w=[pk[3]])
                            first_pv = False
                        yield
                oi = c % 2
                ob, obk = B["ob"][oi], K("ob%d" % oi)
                if kind == "sb":
                    self.actf(ob, pb[2][0:64, :], AF.Copy, r=[pk[2]], w=[obk])
                else:
                    self.P("dve", "reciprocal", rec, pb[3][0:64, :], r=[pk[3]], w=[K("rec")])
                    yield
                    self.P("dve", "tensor_tensor", ob, pb[2][0:64, :], rec, ALU.mult, r=[pk[2], K("rec")], w=[obk])
                yield
                kb.dma(self.mixT[b, mrow:mrow + 64, Q0:Q0 + 512], ob, reads=[obk], writes=["mixT"])

        for kind in ("sb", "fx"):
            for hp in range(2):
                gens = [head_gen(kind, 2 * hp + s, s) for s in range(NS)]
                while gens:
                    for g in list(gens):
                        try:
                            next(g)
                        except StopIteration:
                            gens.remove(g)

    def phaseB_rwkv(self, l, b):
        kb = self.kb
        pball = self.pb
        a = Arena(self.arena_ap, self.r1_end, 208000)
        F = lambda parts=64: a.t([parts, 512])
        xw, xwp, xa, xap = F(), F(), F(), F()
        xgt, xgp = F(128), F(128)
        pv = self.pv
        NS = 2
        slots = []
        for s in range(NS):
            B = {}
            for nm in ("raw0", "raw1", "raw2", "prv0", "prv1", "prv2", "rm", "km", "vm", "sgd", "logd", "G", "av",
                       "kk", "kk2", "nrm", "kap", "tmpa", "kp", "bbv", "eG", "eGn", "eGx", "ktT", "btT", "kdT", "rtT", "rkr"):
                B[nm] = F()
            B["tok3"] = [a.t([128, 3, 64]) for _ in range(2)]
            B["sgt"] = [a.t([128, 72]) for _ in range(2)]
            B["ABm"] = [a.t([128, 4, 128]) for _ in range(2)]
            B["X"] = [a.t([128, 128]) for _ in range(2)]
            B["Y"] = [a.t([128, 128]) for _ in range(2)]
            B["R"] = [a.t([128, 128]) for _ in range(2)]
            for nm in ("Wn", "U", "ysb", "yn", "yj"):
                B[nm] = a.t([128, 64])
            B["mt"] = a.t([64, 64])
            B["yst"] = a.t([128, 8])
            B["ymix"] = [a.t([64, 512], BF16) for _ in range(2)]
            slots.append(B)
        for h in range(4):
            self.P("pool", "memset", self.Mst[:, h, :], 0.0, w=["M%d" % h])

        def load_shift(g4, dst, dstp, rows, nm, parts):
            c0 = g4 * 512
            kb.dma(dst[0:parts, :], self.rwT[b, rows, c0:c0 + 512], reads=["rwT"], writes=[nm])
            if g4 == 0:
                self.P("pool", "memset", dstp[0:parts, 0:1], 0.0, w=[nm + "p"])
                kb.dma(dstp[0:parts, 1:512], self.rwT[b, rows, 0:511], reads=["rwT"], writes=[nm + "p"])
            else:
                kb.dma(dstp[0:parts, :], self.rwT[b, rows, c0 - 1:c0 + 511], reads=["rwT"], writes=[nm + "p"])

        def mix(dst, x_, xp_, mu, nm, dk, parts=64):
            self.P("pool", "tensor_tensor", xp_[0:parts, :], xp_[0:parts, :], x_[0:parts, :], ALU.subtract,
                   r=[nm, nm + "p"], w=[nm + "p"])
            self.P("dve", "scalar_tensor_tensor", dst[0:parts, :], xp_[0:parts, :], mu, x_[0:parts, :], ALU.mult, ALU.add,
                   r=[nm, nm + "p", "pv", "mu_w", "mu_a", "mu_g"], w=[dk])

        def head_gen(g4, h, s):
            B = slots[s]
            K = lambda n: "%s_s%d" % (n, s)
            pb = pball[4 * s:4 * s + 4]
            pk = ["pb%d" % (4 * s + i) for i in range(4)]
            c0 = g4 * 512
            cols = slice(c0, c0 + 512)
            hc = slice(h * 64, (h + 1) * 64)
            Mh = self.Mst[:, h, :]
            Mk = "M%d" % h
            rm, km, vm = B["rm"], B["km"], B["vm"]
            sgd, logd, G, av = B["sgd"], B["logd"], B["G"], B["av"]
            kk, kk2, nrm, kap, tmpa, kp, bbv = B["kk"], B["kk2"], B["nrm"], B["kap"], B["tmpa"], B["kp"], B["bbv"]
            eG, eGn, eGx = B["eG"], B["eGn"], B["eGx"]
            ktT, btT, kdT, rtT, rkr = B["ktT"], B["btT"], B["kdT"], B["rtT"], B["rkr"]
            Wn, U, ysb, yn, yj, mt, yst = B["Wn"], B["U"], B["ysb"], B["yn"], B["yj"], B["mt"], B["yst"]
            X, Y, R = B["X"], B["Y"], B["R"]
            for j, (nm, dst) in enumerate((("r", rm), ("k", km), ("v", vm))):
                rows = slice((j * 4 + h) * 64, (j * 4 + h + 1) * 64)
                load_shift(g4, B["raw%d" % j], B["prv%d" % j], rows, K("raw%d" % j), 64)
                mix(dst, B["raw%d" % j], B["prv%d" % j], pv[:, j * 4 + h:j * 4 + h + 1], K("raw%d" % j), K(nm + "m"))
            yield
            self.mm(pb[0][0:64, :], self.w2s[:, hc], xw, True, True, r=["w2s", "xw"], w=[pk[0]])
            self.mm(pb[1][0:64, :], self.a2s[:, hc], xa, True, True, r=["a2s", "xa"], w=[pk[1]])
            self.P("dve", "tensor_single_scalar", kk, km, pv[:, 20 + h:21 + h], ALU.mult, r=[K("km"), "pv"], w=[K("kk")])
            self.P("pool", "tensor_tensor", kk2, kk, kk, ALU.mult, r=[K("kk")], w=[K("kk2")])
            yield
            self.actf(sgd, pb[0][0:64, :], AF.Sigmoid, r=[pk[0], "pv"], w=[K("sgd")], bias=pv[:, 12 + h:13 + h])
            self.actf(av, pb[1][0:64, :], AF.Sigmoid, r=[pk[1], "pv"], w=[K("av")], bias=pv[:, 16 + h:17 + h])
            self.mm(pb[2][0:64, :], self.ones_f[0:64, 0:64], kk2, True, True, r=["ones_f", K("kk2")], w=[pk[2]])
            yield
            self.P("dve", "tensor_single_scalar", logd, sgd, -0.6065306597126334, ALU.mult, r=[K("sgd")], w=[K("logd")])
            self.actf(nrm, pb[2][0:64, :], AF.Sqrt, r=[pk[2]], w=[K("nrm")])
            yield
            self.P("dve", "tensor_tensor_scan", G, self.reset, logd, 0.0, ALU.mult, ALU.add, r=["reset", K("logd")], w=[K("G")])
            self.P("dve", "tensor_single_scalar", nrm, nrm, 1e-12, ALU.max, r=[K("nrm")], w=[K("nrm")])
            yield
            self.P("dve", "reciprocal", nrm, nrm, r=[K("nrm")], w=[K("nrm")])
            self.actf(eG, G, AF.Exp, r=[K("G")], w=[K("eG")])
            self.actf(eGn, G, AF.Exp, r=[K("G")], w=[K("eGn")], scale=-1.0)
            self.P("dve", "tensor_tensor", logd, G, logd, ALU.subtract, r=[K("G"), K("logd")], w=[K("logd")])
            yield
            self.P("dve", "tensor_tensor", kap, kk, nrm, ALU.mult, r=[K("kk"), K("nrm")], w=[K("kap")])
            self.actf(eGx, logd, AF.Exp, r=[K("logd")], w=[K("eGx")])
            self.P("dve", "tensor_scalar", tmpa, av, pv[:, 24 + h:25 + h], pv[:, 32 + h:33 + h], ALU.mult, ALU.add,
                   r=[K("av"), "pv"], w=[K("tmpa")])
            yield
            self.P("pool", "tensor_tensor", kp, km, tmpa, ALU.mult, r=[K("km"), K("tmpa")], w=[K("kp")])
            self.P("pool", "tensor_tensor", bbv, kap, av, ALU.mult, r=[K("kap"), K("av")], w=[K("bbv")])
            self.P("dve", "tensor_tensor", ktT, kap, eGx, ALU.mult, r=[K("kap"), K("eGx")], w=[K("ktT")])
            self.P("pool", "tensor_tensor", rtT, rm, eG, ALU.mult, r=[K("rm"), K("eG")], w=[K("rtT")])
            yield
            self.P("pool", "tensor_tensor", btT, bbv, eGn, ALU.mult, r=[K("bbv"), K("eGn")], w=[K("btT")])
            self.P("dve", "tensor_tensor", kdT, kp, eGn, ALU.mult, r=[K("kp"), K("eGn")], w=[K("kdT")])
            self.P("dve", "scalar_tensor_tensor", rkr, rm, pv[:, 28 + h:29 + h], kp, ALU.mult, ALU.mult,
                   r=[K("rm"), K("kp"), "pv"], w=[K("rkr")])
            yield
            for cc in range(4):
                u2 = cc % 2
                cs = slice(cc * 128, (cc + 1) * 128)
                t3, t3k = B["tok3"][u2], K("tok3_%d" % u2)
                sgtk = K("sgt%d" % u2)
                for j, (srcT, sk) in enumerate(((btT, K("btT")), (kdT, K("kdT")), (vm, K("vm")))):
                    self.P("pe", "transpose", pb[0][:, j * 64:(j + 1) * 64], srcT[:, cs], self.ident[0:64, 0:64],
                           r=[sk, "cst"], w=[pk[0]])
                self.mm(pb[0][:, 192:193], rkr[:, cs], self.ones_f[0:64, 0:1], True, True, r=[K("rkr"), "ones_f"], w=[pk[0]])
                self.mm(pb[0][:, 200:264], xgt[:, cs], self.g2s[:, hc], True, True, r=["xg", "g2s"], w=[pk[0]])
                for j, (lT, lk, rT, rk_) in enumerate(((btT, K("btT"), ktT, K("ktT")), (btT, K("btT"), rtT, K("rtT")),
                                                       (kdT, K("kdT"), ktT, K("ktT")), (kdT, K("kdT"), rtT, K("rtT")))):
                    self.mm(pb[1][:, j * 128:(j + 1) * 128], lT[:, cs], rT[:, cs], True, True, r=[lk, rk_], w=[pk[1]])
                self.mm(pb[2][:, 0:128], ktT[:, cs], btT[:, cs], True, True, r=[K("ktT"), K("btT")], w=[pk[2]])
                yield
                self.actf(t3.rearrange("p a b -> p (a b)"), pb[0][:, 0:192], AF.Copy, r=[pk[0]], w=[t3k])
                self.P("dve", "tensor_copy", B["sgt"][u2], pb[0][:, 192:264], r=[pk[0]], w=[sgtk])
                bt_tok, kd_tok, v_tok = t3[:, 0, :], t3[:, 1, :], t3[:, 2, :]
                s_tok = B["sgt"][u2][:, 0:1]
                g_tok = B["sgt"][u2][:, 8:72]
                AB, ABk = B["ABm"][u2], K("AB%d" % u2)
                self.P("dve", "tensor_tensor", AB.rearrange("p (x y) t -> p x y t", x=2),
                       pb[1].rearrange("p (x y t) -> p x y t", x=2, y=2),
                       self.mask2.unsqueeze(1).to_broadcast([128, 2, 2, 128]), ALU.mult, r=[pk[1], "cst"], w=[ABk])
                Nm, BbT, AkT, BkT = AB[:, 0, :], AB[:, 1, :], AB[:, 2, :], AB[:, 3, :]
                self.P("dve", "tensor_tensor", Y[0], pb[2][:, 0:128], self.m_lows, ALU.mult, r=[pk[2], "cst"], w=[K("Y0")])
                yield
                self.P("pool", "tensor_tensor", R[0], self.ident, Nm, ALU.subtract, r=["cst", ABk], w=[K("R0")])
                Xc, Xk = Nm, ABk
                Yc, Yk = Y[0], K("Y0")
                Rc, Rk = R[0], K("R0")
                for i in range(1, 7):
                    xi = i % 2
                    if i < 6:
                        self.mm(pb[2][:, 128:256], Yc, Xc, True, True, r=[Yk, Xk], w=[pk[2]])
                    self.mm(pb[2][:, 256:384], Xc, Yc, True, True, r=[Xk, Yk], w=[pk[2]])
                    yield
                    if i < 6:
                        self.actf(X[xi], pb[2][:, 128:256], AF.Copy, r=[pk[2]], w=[K("X%d" % xi)])
                    self.P("dve", "tensor_copy", Y[xi], pb[2][:, 256:384], r=[pk[2]], w=[K("Y%d" % xi)])
                    if i < 6:
                        Xc, Xk = X[xi], K("X%d" % xi)
                    Yc, Yk = Y[xi], K("Y%d" % xi)
                    yield
                    self.mm(pb[3][:, 0:128], Yc, Rc, True, True, r=[Yk, Rk], w=[pk[3]])
                    yield
                    self.P("dve", "tensor_tensor", R[xi], pb[3][:, 0:128], Rc, ALU.add, r=[pk[3], Rk], w=[K("R%d" % xi)])
                    Rc, Rk = R[xi], K("R%d" % xi)
                    yield
                self.mm(pb[3][:, 128:192], ktT[:, cs], Mh, True, False, r=[K("ktT"), Mk], w=[pk[3]])
                self.mm(pb[3][:, 128:192], AkT, v_tok, False, True, r=[ABk, t3k], w=[pk[3]])
                yield
                self.actf(Wn, pb[3][:, 128:192], AF.Copy, r=[pk[3]], w=[K("Wn")], scale=-1.0)
                yield
                self.mm(pb[3][:, 192:256], Rc, Wn, True, True, r=[Rk, K("Wn")], w=[pk[3]])
                yield
                self.actf(U, pb[3][:, 192:256], AF.Copy, r=[pk[3]], w=[K("U")])
                yield
                self.mm(pb[3][:, 256:320], rtT[:, cs], Mh, True, False, r=[K("rtT"), Mk], w=[pk[3]])
                self.mm(pb[3][:, 256:320], BbT, U, False, False, r=[ABk, K("U")], w=[pk[3]])
                self.mm(pb[3][:, 256:320], BkT, v_tok, False, True, r=[ABk, t3k], w=[pk[3]])
                self.mm(pb[3][0:64, 320:384], bt_tok, U, True, False, r=[t3k, K("U")], w=[pk[3]])
                self.mm(pb[3][0:64, 320:384], kd_tok, v_tok, False, True, r=[t3k], w=[pk[3]])
                yield
                self.P("dve", "tensor_tensor", mt, pb[3][0:64, 320:384], Mh, ALU.add, r=[pk[3], Mk], w=[K("mt")])
                self.actf(ysb, pb[3][:, 256:320], AF.Identity, r=[pk[3]], w=[K("ysb"), K("yst")], accum_out=yst[:, 0:1])
                yield
                self.P("dve", "tensor_single_scalar", Mh, mt, eG[:, cc * 128 + 127:cc * 128 + 128], ALU.mult,
                       r=[K("mt"), K("eG")], w=[Mk])
                self.P("dve", "tensor_single_scalar", yst[:, 1:2], yst[:, 0:1], -1.0 / 64, ALU.mult, r=[K("yst")], w=[K("yst")])
                yield
                self.actf(yj, ysb, AF.Square, r=[K("ysb"), K("yst")], w=[K("yj"), K("yst")], bias=yst[:, 1:2], accum_out=yst[:, 2:3])
                yield
                self.actf(yst[:, 3:4], yst[:, 2:3], AF.Ln, r=[K("yst")], w=[K("yst")], scale=1.0 / 64, bias=GN_EPS)
                yield
                self.actf(yst[:, 4:5], yst[:, 3:4], AF.Exp, r=[K("yst")], w=[K("yst")], scale=-0.5)
                yield
                self.P("dve", "tensor_scalar", yn, ysb, yst[:, 1:2], yst[:, 4:5], ALU.add, ALU.mult, r=[K("ysb"), K("yst")], w=[K("yn")])
                yield
                self.P("dve", "tensor_tensor", yn, yn, self.gnw[:, hc], ALU.mult, r=[K("yn"), "gnw"], w=[K("yn")])
                yield
                self.P("dve", "tensor_tensor", yn, yn, self.gnb[:, hc], ALU.add, r=[K("yn"), "gnb"], w=[K("yn")])
                yield
                self.P("dve", "scalar_tensor_tensor", yn, v_tok, s_tok, yn, ALU.mult, ALU.add,
                       r=[K("yn"), t3k, sgtk], w=[K("yn")])
                yield
                self.P("dve", "tensor_tensor", yn, yn, g_tok, ALU.mult, r=[K("yn"), sgtk], w=[K("yn")])
                yield
                self.P("pe", "transpose", pb[0][0:64, 384:512], yn, self.ident, r=[K("yn"), "cst"], w=[pk[0]])
                yield
                ym, ymk = B["ymix"][g4 % 2], K("ymix%d" % (g4 % 2))
                self.actf(ym[:, cs], pb[0][0:64, 384:512], AF.Copy, r=[pk[0]], w=[ymk])
                yield
            kb.dma(self.mixT[b, 512 + h * 64:512 + (h + 1) * 64, cols], ym, reads=[ymk], writes=["mixT"])

        for g4 in range(4):
            load_shift(g4, xw, xwp, slice(768, 832), "xw", 64)
            load_shift(g4, xa, xap, slice(832, 896), "xa", 64)
            load_shift(g4, xgt, xgp, slice(896, 1024), "xg", 128)
            mix(xw, xw, xwp, self.mu_w, "xw", "xw")
            mix(xa, xa, xap, self.mu_a, "xa", "xa")
            mix(xgt, xgt, xgp, self.mu_g, "xg", "xg", 128)
            self.actf(xw, xw, AF.Tanh, r=["xw"], w=["xw"])
            self.actf(xgt, xgt, AF.Sigmoid, r=["xg"], w=["xg"])
            for hp in range(2):
                gens = [head_gen(g4, 2 * hp + s, s) for s in range(NS)]
                while gens:
                    for g in list(gens):
                        try:
                            next(g)
                        except StopIteration:
                            gens.remove(g)

    def phaseD(self, l, b, src):
        kb = self.kb
        pb = self.pb
        a = Arena(self.arena_ap, self.r1_end, 208000)
        wo = a.t([128, 8, D], BF16)
        for kc in range(8):
            kb.dma(wo[:, kc, :], self.w_out[l, kc * 128:(kc + 1) * 128, :], writes=["wo%d" % kc], q="pool")
        mx = [a.t([128, 8, 128], BF16) for _ in range(2)]
        xt = [a.t([128, D]) for _ in range(2)]
        xn = [a.t([128, D]) for _ in range(2)]
        h2 = [a.t([128, D]) for _ in range(2)]
        junk = a.t([128, D])
        st = [a.t([128, 4]) for _ in range(2)]
        hTf = a.t([128, 8, 128])
        hTb = [a.t([128, 8, 128], BF16) for _ in range(2)]
        lg = a.t([128, 36]); lm = a.t([128, 32]); l2 = a.t([128, 32]); oh1 = a.t([128, 32]); oh2 = a.t([128, 32])
        ohg = a.t([128, 4]); ge = a.t([128, 4]); rs = a.t([128, 16])
        gt = [a.t([128, 32]) for _ in range(2)]
        for t in range(NT):
            p = t % 2
            rows = slice(t * 128, (t + 1) * 128)
            kb.dma(mx[p], self.mixT[b, :, rows].rearrange("(k p) n -> p k n", p=128), reads=["mixT"], writes=["mx%d" % p])
            kb.dma(xt[p], src[rows, :], reads=["xres"], writes=["xt%d" % p])
            for hf in range(2):
                for kc in range(8):
                    self.mm(pb[hf], mx[p][:, kc, :], wo[:, kc, hf * 512:(hf + 1) * 512], kc == 0, kc == 7,
                            r=["mx%d" % p, "wo%d" % kc], w=["pb%d" % hf])
                self.P("dve", "tensor_tensor", xn[p][:, hf * 512:(hf + 1) * 512], pb[hf], xt[p][:, hf * 512:(hf + 1) * 512],
                       ALU.add, r=["pb%d" % hf, "xt%d" % p], w=["xn%d" % p])
            kb.dma(self.xres[b, rows, :], xn[p], reads=["xn%d" % p], writes=["xres_w"])
            self.rmsnorm(xn[p], "xn%d" % p, self.g2bc, "g2bc", h2[p], "h2%d" % p, junk, "junk", st[p], "st%d" % p)
            for kc in range(8):
                pbi = 2 + kc // 4
                self.P("pe", "transpose", pb[pbi][:, (kc % 4) * 128:(kc % 4 + 1) * 128], h2[p][:, kc * 128:(kc + 1) * 128],
                       self.ident, r=["h2%d" % p, "cst"], w=["pb%d" % pbi])
            for hh in range(2):
                self.actf(hTf[:, hh * 4:(hh + 1) * 4, :].rearrange("p k n -> p (k n)"), pb[2 + hh], AF.Copy,
                          r=["pb%d" % (2 + hh)], w=["hTf"])
                self.P("dve", "tensor_copy", hTb[p][:, hh * 4:(hh + 1) * 4, :].rearrange("p k n -> p (k n)"), pb[2 + hh],
                       r=["pb%d" % (2 + hh)], w=["hTb%d" % p])
            kb.dma(self.h2T[b, :, rows].rearrange("(k p) n -> p k n", p=128), hTb[p], reads=["hTb%d" % p], writes=["h2T"])
            for kc in range(8):
                self.mm(pb[4][:, 0:36], hTf[:, kc, :], self.wr[:, kc, :], kc == 0, kc == 7, r=["hTf", "wr"], w=["pb4"])
            self.P("dve", "tensor_tensor", lg, pb[4][:, 0:36], self.rbbc, ALU.add, r=["pb4", "rbbc"], w=["lg"])
            self.P("dve", "tensor_reduce", rs[:, 0:1], lg[:, 0:4], AX.X, ALU.max, r=["lg"], w=["rs"])
            self.P("dve", "tensor_scalar", ohg, lg[:, 0:4], rs[:, 0:1], None, ALU.is_equal, r=["lg", "rs"], w=["ohg"])
            self.P("dve", "tensor_single_scalar", rs[:, 1:2], rs[:, 0:1], -1.0, ALU.mult, r=["rs"], w=["rs"])
            self.actf(ge, lg[:, 0:4], AF.Exp, r=["lg", "rs"], w=["ge", "rs"], bias=rs[:, 1:2], accum_out=rs[:, 2:3])
            self.P("dve", "reciprocal", rs[:, 3:4], rs[:, 2:3], r=["rs"], w=["rs"])
            self.P("dve", "tensor_scalar", ohg, ohg, -1.0, 1e30, ALU.add, ALU.mult, r=["ohg"], w=["ohg"])
            self.P("dve", "tensor_tensor", lm.rearrange("p (g e) -> p g e", g=4), lg[:, 4:36].rearrange("p (g e) -> p g e", g=4),
                   ohg.unsqueeze(2).to_broadcast([128, 4, 8]), ALU.add, r=["lg", "ohg"], w=["lm"])
            self.P("dve", "tensor_reduce", rs[:, 4:5], lm, AX.X, ALU.max, r=["lm"], w=["rs"])
            self.P("dve", "tensor_scalar", oh1, lm, rs[:, 4:5], None, ALU.is_equal, r=["lm", "rs"], w=["oh1"])
            self.P("dve", "scalar_tensor_tensor", l2, oh1, -1e30, lm, ALU.mult, ALU.add, r=["oh1", "lm"], w=["l2"])
            self.P("dve", "tensor_reduce", rs[:, 5:6], l2, AX.X, ALU.max, r=["l2"], w=["rs"])
            self.P("dve", "tensor_scalar", oh2, l2, rs[:, 5:6], None, ALU.is_equal, r=["l2", "rs"], w=["oh2"])
            self.P("dve", "tensor_tensor", rs[:, 6:7], rs[:, 5:6], rs[:, 4:5], ALU.subtract, r=["rs"], w=["rs"])
            self.actf(rs[:, 7:8], rs[:, 6:7], AF.Exp, r=["rs"], w=["rs"])
            self.P("dve", "tensor_single_scalar", rs[:, 8:9], rs[:, 7:8], 1.0, ALU.add, r=["rs"], w=["rs"])
            self.P("dve", "reciprocal", rs[:, 9:10], rs[:, 8:9], r=["rs"], w=["rs"])
            self.P("dve", "tensor_tensor", rs[:, 10:11], rs[:, 9:10], rs[:, 7:8], ALU.mult, r=["rs"], w=["rs"])
            self.P("dve", "tensor_tensor", rs[:, 11:12], rs[:, 9:10], rs[:, 3:4], ALU.mult, r=["rs"], w=["rs"])
            self.P("dve", "tensor_tensor", rs[:, 12:13], rs[:, 10:11], rs[:, 3:4], ALU.mult, r=["rs"], w=["rs"])
            self.P("dve", "tensor_single_scalar", gt[p], oh1, rs[:, 11:12], ALU.mult, r=["oh1", "rs"], w=["gt%d" % p])
            self.P("dve", "scalar_tensor_tensor", gt[p], oh2, rs[:, 12:13], gt[p], ALU.mult, ALU.add,
                   r=["oh2", "rs", "gt%d" % p], w=["gt%d" % p])
            kb.dma(self.gates[b, rows, :], gt[p], reads=["gt%d" % p], writes=["gates"])

    def phaseE(self, l, b, final):
        kb = self.kb
        pb = self.pb
        a = Arena(self.arena_ap, self.r1_end, 208000)
        yacc = a.t([128, NT, D])
        hT = a.t([128, 8, T], BF16)
        gts = a.t([128, NT, 32])
        wg = [a.t([128, 8, EH], BF16) for _ in range(2)]
        wu = [a.t([128, 8, EH], BF16) for _ in range(2)]
        wd = [a.t([128, 4, D], BF16) for _ in range(2)]
        sl = [a.t([128, 512]) for _ in range(2)]
        act = [a.t([128, 4, 512], BF16) for _ in range(2)]
        for t in range(NT):
            kb.dma(yacc[:, t, :], self.xres[b, t * 128:(t + 1) * 128, :], writes=["y%d" % t])
        for kc in range(8):
            kb.dma(hT[:, kc, :], self.h2T[b, kc * 128:(kc + 1) * 128, :], writes=["hT"])
        kb.dma(gts, self.gates[b].rearrange("(t p) e -> p t e", p=128), writes=["gts"])
        it = 0
        nstage = 0
        stg = [a.t([128, 4, 512]) for _ in range(2)]
        for e in range(self.moe_experts):
            p = e % 2
            for wi, (wsrc, wdst, wkey, nh) in enumerate(((self.w_gate, wg[p], "wg%d" % p, 2), (self.w_up, wu[p], "wu%d" % p, 2),
                                                           (self.w_down, wd[p], "wd%d" % p, 1))):
                for hh in range(2):
                    si = nstage % 2
                    nstage += 1
                    if nh == 2:
                        src_ap = wsrc[l, e, hh * 512:(hh + 1) * 512, :].rearrange("(k p) n -> p k n", p=128)
                        dst_ap = wdst[:, hh * 4:(hh + 1) * 4, :]
                        stv = stg[si]
                    else:
                        src_ap = wsrc[l, e, hh * 256:(hh + 1) * 256, :].rearrange("(k p) n -> p k n", p=128)
                        dst_ap = wdst[:, hh * 2:(hh + 1) * 2, :]
                        stv = stg[si].rearrange("p k n -> p (k n)").rearrange("p (k n) -> p k n", k=2)
                    kb.dma(stv, src_ap, writes=["stg%d" % si])
                    if nstage % 2 == 0:
                        self.actf(dst_ap, stv, AF.Copy, r=["stg%d" % si], w=[wkey + "_%d" % hh])
                    else:
                        self.P("pool", "tensor_copy", dst_ap, stv, r=["stg%d" % si], w=[wkey + "_%d" % hh])
            for g in range(4):
                ap_ = it % 2
                it += 1
                tcols = slice(g * 512, (g + 1) * 512)
                for m in range(4):
                    pg = pb[(2 * m) % 4]; pgk = "pb%d" % ((2 * m) % 4)
                    pu = pb[(2 * m) % 4 + 1]; puk = "pb%d" % ((2 * m) % 4 + 1)
                    for kc in range(8):
                        self.mm(pg, wg[p][:, kc, m * 128:(m + 1) * 128], hT[:, kc, tcols], kc == 0, kc == 7,
                                r=["wg%d_0" % p, "wg%d_1" % p, "hT"], w=[pgk])
                    for kc in range(8):
                        self.mm(pu, wu[p][:, kc, m * 128:(m + 1) * 128], hT[:, kc, tcols], kc == 0, kc == 7,
                                r=["wu%d_0" % p, "wu%d_1" % p, "hT"], w=[puk])
                    s_ = sl[m % 2]; sk = "sl%d" % (m % 2)
                    self.actf(s_, pg, AF.Silu, r=[pgk], w=[sk])
                    self.P("dve", "tensor_tensor", act[ap_][:, m, :], s_, pu, ALU.mult, r=[sk, puk], w=["act%d_%d" % (ap_, m)])
                for tt in range(4):
                    t = g * 4 + tt
                    for hf in range(2):
                        po = pb[4 + (tt * 2 + hf) % 4]; pok = "pb%d" % (4 + (tt * 2 + hf) % 4)
                        for m in range(4):
                            self.mm(po, act[ap_][:, m, tt * 128:(tt + 1) * 128], wd[p][:, m, hf * 512:(hf + 1) * 512],
                                    m == 0, m == 3, r=["act%d_%d" % (ap_, m), "wd%d_0" % p, "wd%d_1" % p], w=[pok])
                        ys = yacc[:, t, hf * 512:(hf + 1) * 512]
                        self.P("dve", "scalar_tensor_tensor", ys, po, gts[:, t, e:e + 1], ys, ALU.mult, ALU.add,
                               r=[pok, "gts", "y%d" % t], w=["y%d" % t])
        if not final:
            for t in range(NT):
                kb.dma(self.xres[b, t * 128:(t + 1) * 128, :], yacc[:, t, :], reads=["y%d" % t], writes=["xres"])
        else:
            kb.barrier()
            gf = wg[0].rearrange("p k n -> p (k n)").bitcast(F32)[:, 0:D]
            junk = wu[0].rearrange("p k n -> p (k n)").bitcast(F32)[:, 0:D]
            ot = [wd[0].rearrange("p k n -> p (k n)").bitcast(F32)[:, 0:D], wd[1].rearrange("p k n -> p (k n)").bitcast(F32)[:, 0:D]]
            stf = sl[0]
            kb.dma(gf, self.final_g.partition_broadcast(128), writes=["wg0_0", "wg0_1", "gf"])
            for t in range(NT):
                p = t % 2
                self.rmsnorm(yacc[:, t, :], "y%d" % t, gf, "gf", ot[p], "ot%d" % p, junk, "fjunk",
                             stf[:, 4 * t:4 * t + 4], "sl0")
                kb.dma(self.out[b, t * 128:(t + 1) * 128, :], ot[p], reads=["ot%d" % p], writes=["out"])

    def build(self):
        kb = self.kb
        for l in range(self.NL):
            self.load_layer(l)
            for b in range(self.NB):
                src = self.x_in[b] if l == 0 else self.xres[b]
                self.phaseA(l, b, src)
                kb.barrier()
                if self.stop == "A":
                    return self.finish()
                self.phaseB_attn(l, b)
                kb.barrier()
                if self.stop == "B1":
                    return self.finish()
                self.phaseB_rwkv(l, b)
                kb.barrier()
                if self.stop == "B2":
                    return self.finish()
                self.phaseD(l, b, src)
                kb.barrier()
                if self.stop == "D":
                    return self.finish()
                self.phaseE(l, b, final=(l == self.NL - 1))
                kb.barrier()
        return self.finish()

    def finish(self):
        self.kb.barrier()
        return self


def prep_inputs(inp, b0, nb, NL=4):
    f = lambda a: np.ascontiguousarray(a, dtype=np.float32)
    m = {
        "x": f(inp["x"][b0:b0 + nb]),
        "consts": make_consts(),
        "norm1_g": f(inp["norm1_g"][:NL]),
        "w_in": f(inp["w_in"][:NL]),
        "gm_wT": f(np.transpose(inp["gm_w_s"][:NL], (0, 1, 3, 2))),
        "gm_bT": f(np.transpose(inp["gm_b"][:NL], (0, 2, 1))),
        "rw_mu": f(inp["rw_mu"][:NL]),
        "rw_pv": f(np.concatenate([
            np.transpose(np.reshape(inp["rw_mu"][:NL, 0:768], (NL, 12, 64)), (0, 2, 1))] + [
            np.transpose(np.reshape(np.reshape(inp[k][:NL], (NL, 256)), (NL, 4, 64)), (0, 2, 1))
            for k in ("rw_w0", "rw_a0", "rw_k_k", "rw_k_a", "rw_r_k")], axis=2)),
        "rw_w2": f(inp["rw_w2"][:NL]),
        "rw_a2": f(inp["rw_a2"][:NL]),
        "rw_g2": f(inp["rw_g2"][:NL]),
        "rw_gn_w": f(inp["rw_gn_w"][:NL]),
        "rw_gn_b": f(inp["rw_gn_b"][:NL]),
        "fx_b_f": f(inp["fx_b_f"][:NL]),
        "w_out": f(inp["w_out"][:NL]),
        "norm2_g": f(inp["norm2_g"][:NL]),
        "router_w": f(np.concatenate([inp["router_group_w"][:NL], inp["router_expert_w"][:NL]], axis=2)),
        "router_b": f(np.concatenate([inp["router_group_b"][:NL], inp["router_expert_b"][:NL]], axis=1)),
        "exp_w_gate": f(inp["exp_w_gate"][:NL]),
        "exp_w_up": f(inp["exp_w_up"][:NL]),
        "exp_w_down": f(inp["exp_w_down"][:NL]),
        "final_norm_g": f(np.reshape(inp["final_norm_g"], (1, D))),
    }
    return m


def kernel(**inputs):
    inp = {k: np.asarray(v) for k, v in inputs.items()}
    n = 8
    nb = 2
    prog = Prog(NL=4, NB=nb).build()
    shared = prep_inputs(inp, 0, nb)
    in_maps = []
    for c in range(n):
        m = dict(shared)
        m["x"] = np.ascontiguousarray(inp["x"][c * nb:(c + 1) * nb], dtype=np.float32)
        in_maps.append(m)
    res = run_bass_kernel_spmd(prog.nc, in_maps, core_ids=list(range(n)))
    out = np.concatenate([np.asarray(r["out"]) for r in res.results], axis=0)
    return out.astype(np.float32)
```

```python
import numpy as np
import concourse.bass as bass
import concourse.mybir as mybir
from concourse.bass_utils import run_bass_kernel_spmd

F32 = mybir.dt.float32
BF16 = mybir.dt.bfloat16
AF = mybir.ActivationFunctionType
ALU = mybir.AluOpType
AX = mybir.AxisListType

T = 2048
D = 1024
NT = 16
DIN = 3076
NE = 32
EH = 512
NDMA_SEMS = 12
RMS_EPS = 1e-6
LN_EPS = 1e-5
GN_EPS = 64e-5
NEGM = -30000.0
import os
MAXOPS = int(os.environ.get("KB_MAXOPS", "1000000000"))


class KB:
    def __init__(self):
        self.nc = bass.Bass("TRN2", target_bir_lowering=False)
        nc = self.nc
        self.engs = {"pe": nc.tensor, "act": nc.scalar, "dve": nc.vector,
                     "pool": nc.gpsimd, "sp": nc.sync}
        self.sem = {}
        self.cnt = {}
        for e in self.engs:
            self.sem[e] = nc.alloc_semaphore("s_" + e)
            self.cnt[e] = 0
        self.dsem = {}
        self.dcnt = {}
        self.drr = {}
        for q in ("sp", "pool"):
            self.dsem[q] = [nc.alloc_semaphore("d_%s%d" % (q, i)) for i in range(NDMA_SEMS)]
            self.dcnt[q] = [0] * NDMA_SEMS
            self.drr[q] = 0
        self.waited = {e: {} for e in self.engs}
        self.lastw = {}
        self.readers = {}
        self.nins = 0

    def _semof(self, sk):
        if sk[0] == "c":
            return self.sem[sk[1]]
        return self.dsem[sk[1]][sk[2]]

    def _wait(self, eng, tok):
        sk, val = tok
        if eng == "pe" and sk == ("c", "pe"):
            return
        w = self.waited[eng]
        if w.get(sk, 0) >= val:
            return
        self.engs[eng].wait_ge(self._semof(sk), val)
        w[sk] = val
        self.nins += 1

    def _deps(self, eng, reads, writes):
        for k in reads:
            t = self.lastw.get(k)
            if t is not None:
                self._wait(eng, t)
        for k in writes:
            t = self.lastw.get(k)
            if t is not None:
                self._wait(eng, t)
            for t in self.readers.get(k, ()):
                self._wait(eng, t)

    def _record(self, tok, reads, writes):
        for k in reads:
            lst = self.readers.setdefault(k, [])
            lst[:] = [t for t in lst if t[0] != tok[0]]
            lst.append(tok)
        for k in writes:
            self.lastw[k] = tok
            self.readers[k] = []

    def op(self, eng, fn, reads=(), writes=()):
        self.nops = getattr(self, "nops", 0) + 1
        if self.nops > MAXOPS:
            return None
        pr = [k for k in reads if k.startswith("pb")]
        if pr:
            reads = [k for k in reads if not k.startswith("pb")]
            writes = list(writes) + pr
        self._deps(eng, reads, writes)
        ins = fn(self.engs[eng])
        self.cnt[eng] += 1
        ins.then_inc(self.sem[eng], 1)
        tok = (("c", eng), self.cnt[eng])
        self._record(tok, reads, writes)
        self.nins += 1
        return tok

    def dma(self, out, in_, reads=(), writes=(), q="sp", **kw):
        self.nops = getattr(self, "nops", 0) + 1
        if self.nops > MAXOPS:
            return None
        i = self.drr[q]
        self.drr[q] = (i + 1) % NDMA_SEMS
        sk = ("d", q, i)
        if self.dcnt[q][i] > 0:
            self._wait(q, (sk, self.dcnt[q][i]))
        self._deps(q, reads, writes)
        ins = self.engs[q].dma_start(out=out, in_=in_, **kw)
        self.dcnt[q][i] += 16
        ins.then_inc(self.dsem[q][i], 16)
        tok = (sk, self.dcnt[q][i])
        self._record(tok, reads, writes)
        self.nins += 1
        return tok

    def idma(self, out, out_off, in_, in_off, reads=(), writes=()):
        q = "pool"
        self.nops = getattr(self, "nops", 0) + 1
        if self.nops > MAXOPS:
            return None
        i = self.drr[q]
        self.drr[q] = (i + 1) % NDMA_SEMS
        sk = ("d", q, i)
        if self.dcnt[q][i] > 0:
            self._wait(q, (sk, self.dcnt[q][i]))
        self._deps(q, reads, writes)
        ins = self.nc.gpsimd.indirect_dma_start(out=out, out_offset=out_off, in_=in_, in_offset=in_off)
        self.dcnt[q][i] += 16
        ins.then_inc(self.dsem[q][i], 16)
        tok = (sk, self.dcnt[q][i])
        self._record(tok, reads, writes)
        self.nins += 1
        return tok

    def barrier(self):
        toks = [(("c", e), self.cnt[e]) for e in self.engs if self.cnt[e] > 0]
        for q in self.dsem:
            for i in range(NDMA_SEMS):
                if self.dcnt[q][i] > 0:
                    toks.append((("d", q, i), self.dcnt[q][i]))
        for e in self.engs:
            for t in toks:
                if t[0] == ("c", e):
                    continue
                self._wait(e, t)
        self.lastw = {}
        self.readers = {}

    def dram(self, name, shape, dt=F32, kind="Internal"):
        return self.nc.dram_tensor(name, list(shape), dt, kind=kind).ap()


def _dtsize(dt):
    return 2 if dt == BF16 else 4


class Arena:
    def __init__(self, base_ap_f32, start, end):
        self.A = base_ap_f32
        self.cur = start
        self.end = end

    def t(self, shape, dt=F32, parts=128):
        n = 1
        for s in shape[1:]:
            n *= s
        nbytes = (n * _dtsize(dt) + 31) // 32 * 32
        off = self.cur
        self.cur += nbytes
        assert self.cur <= self.end, ("arena overflow", self.cur, self.end)
        v = self.A[0:shape[0], off // 4:(off + nbytes) // 4]
        if dt != F32:
            v = v.bitcast(dt)
        v = v[:, 0:n]
        if len(shape) == 3:
            v = v.rearrange("p (a b) -> p a b", a=shape[1])
        elif len(shape) == 4:
            v = v.rearrange("p (a b c) -> p a b c", a=shape[1], b=shape[2])
        return v


def make_consts():
    j = np.arange(128)[:, None]
    t = np.arange(128)[None, :]
    c = np.zeros((128, 8, 128), np.float32)
    c[:, 0] = np.eye(128)
    c[:, 1] = np.where(j >= t, -1.0, 0.0)
    c[:, 2] = -1.0
    c[:, 3] = np.where(j < t, 0.0, NEGM)
    c[:, 4] = np.where(j <= t, 0.0, NEGM)
    c[:, 5] = np.where(j < t, 1.0, 0.0)
    c[:, 6] = np.where(j <= t, 1.0, 0.0)
    c[:, 7] = np.where(t < j, 1.0, 0.0)
    ex = np.zeros((128, 256), np.float32)
    ex[:, 0:128] = 128.0 * np.arange(128)[None, :]
    ex[:, 128] = np.arange(128)
    return np.concatenate([c.reshape(128, 1024), ex], axis=1)


class Prog:
    def __init__(self, NL=4, NB=2, dbg=(), stop=None, moe_experts=NE, sparse=True):
        self.NL, self.NB = NL, NB
        self.stop = stop
        self.moe_experts = moe_experts
        self.sparse = sparse
        self.NTT = NB * NT
        self.NBLK = NB * 32 + 32
        kb = self.kb = KB()
        nc = self.nc = kb.nc
        dk = lambda n: ("ExternalOutput" if n in dbg else "Internal")
        I = lambda n, s: kb.dram(n, s, F32, kind="ExternalInput")
        self.x_in = I("x", [NB, T, D])
        self.consts_d = I("consts", [128, 1280])
        self.norm1_g = I("norm1_g", [NL, D])
        self.w_in = I("w_in", [NL, D, DIN])
        self.gm_wT = I("gm_wT", [NL, 4, 128, 128])
        self.gm_bT = I("gm_bT", [NL, 128, 4])
        self.rw_mu = I("rw_mu", [NL, 1024])
        self.rw_pv = I("rw_pv", [NL, 64, 32])
        self.rw_w2 = I("rw_w2", [NL, 64, 256])
        self.rw_a2 = I("rw_a2", [NL, 64, 256])
        self.rw_g2 = I("rw_g2", [NL, 128, 256])
        self.rw_gn_w = I("rw_gn_w", [NL, 256])
        self.rw_gn_b = I("rw_gn_b", [NL, 256])
        self.fx_b_f = I("fx_b_f", [NL, 4])
        self.w_out = I("w_out", [NL, D, D])
        self.norm2_g = I("norm2_g", [NL, D])
        self.router_w = I("router_w", [NL, D, 36])
        self.router_b = I("router_b", [NL, 36])
        if sparse:
            self.w_gate = I("exp_w_gate", [NL, NE * 128, 4096])
            self.w_up = I("exp_w_up", [NL, NE * 128, 4096])
            self.w_down = I("exp_w_down", [NL, NE * 128, 4096])
        else:
            self.w_gate = I("exp_w_gate", [NL, NE, D, EH])
            self.w_up = I("exp_w_up", [NL, NE, D, EH])
            self.w_down = I("exp_w_down", [NL, NE, EH, D])
        self.final_g = I("final_norm_g", [1, D])
        self.out = kb.dram("out", [NB, T, D], F32, kind="ExternalOutput")
        S = lambda n, s, dt=F32: kb.dram(n, s, dt, kind=dk(n))
        self.xres = S("xres", [NB, T, D])
        self.qkT = S("qkT", [NB, 16, 64, T], BF16)
        self.vtok = S("vtok", [NB, T, 512], BF16)
        self.rwT = S("rwT", [NB, 1024, T])
        self.fgT = S("fgT", [NB, 4, T])
        self.cumd = S("cumd", [NB, 4, T])
        self.mixT = S("mixT", [NB, 1024, T], BF16)
        self.h2T = S("h2T", [NB, 1024, T], BF16)
        self.gates = S("gates", [NB, T, 32])
        self.h2tok = S("h2tok", [NB, T, D], BF16)
        self.route = S("route", [NB, T, 66])
        self.xb = S("xb", [self.NBLK * 128, D], BF16)
        self.yb = S("yb", [self.NBLK * 128, D])
        self.dbg_sl = S("dbg_sl", [128, self.NTT * 2], mybir.dt.int32)
        self.dbg_widx = S("dbg_widx", [128, self.NBLK], mybir.dt.int32)
        self.dbg_misc = S("dbg_misc", [128, 4, 32])
        arena = nc.alloc_sbuf_tensor("arena", [128, 52000], F32)
        self.arena_ap = arena[:]
        self.pb = [nc.alloc_psum_tensor("pb%d" % i, [128, 512], F32)[:] for i in range(8)]
        self.R1 = Arena(self.arena_ap, 0, 40000)
        self.setup_consts()

    def P(self, eng, meth, *a, r=(), w=(), **kw):
        return self.kb.op(eng, lambda e: getattr(e, meth)(*a, **kw), r, w)

    def mm(self, out, lhsT, rhs, start, stop, r=(), w=()):
        return self.kb.op("pe", lambda e: e.matmul(out, lhsT, rhs, start=start, stop=stop), r, w)

    def actf(self, out, in_, func, r=(), w=(), eng="act", **kw):
        return self.kb.op("act", lambda e: e.activation(out, in_, func, **kw), r, w)

    def col(self, dram_row):
        return dram_row.rearrange("(p o) -> p o", o=1)

    def setup_consts(self):
        kb = self.kb
        a = self.R1
        self.cst = a.t([128, 8, 128])
        kb.dma(self.cst.rearrange("p a b -> p (a b)"), self.consts_d[:, 0:1024], writes=["cst"])
        self.cex = a.t([128, 256])
        kb.dma(self.cex, self.consts_d[:, 1024:1280], writes=["cex"])
        self.thr = self.cex[:, 0:128]
        self.iota_p = self.cex[:, 128:129]
        self.m_strict = self.cst[:, 5, :]
        c = self.cst
        self.ident = c[:, 0, :]
        self.ntri = c[:, 1, :]
        self.negones = c[:, 2, :]
        self.mask2 = c[:, 5:7, :]
        self.m_incl = c[:, 6, :]
        self.m_lows = c[:, 7, :]
        self.ident_bf = a.t([128, 128], BF16)
        self.mbs_bf = a.t([128, 128], BF16)
        self.mbi_bf = a.t([128, 128], BF16)
        self.P("dve", "tensor_copy", self.ident_bf, c[:, 0, :], r=["cst"], w=["ident_bf"])
        self.P("dve", "tensor_copy", self.mbs_bf, c[:, 3, :], r=["cst"], w=["mbs_bf"])
        self.P("dve", "tensor_copy", self.mbi_bf, c[:, 4, :], r=["cst"], w=["mbi_bf"])
        self.ones_f = a.t([128, 128])
        self.P("pool", "memset", self.ones_f, 1.0, w=["ones_f"])
        self.ones_bf = a.t([128, 64], BF16)
        self.P("pool", "memset", self.ones_bf, 1.0, w=["ones_bf"])
        self.reset = a.t([64, 512])
        self.P("pool", "memset", self.reset, 1.0, w=["reset"])
        self.P("pool", "memset", self.reset.rearrange("p (c t) -> p c t", c=4)[:, :, 0:1], 0.0, w=["reset"])
        self.CK = ["cst", "ident_bf", "mbs_bf", "mbi_bf", "ones_f", "ones_bf", "reset", "ones4"]
        self.g1bc = a.t([128, D])
        self.g2bc = a.t([128, D])
        self.rbbc = a.t([128, 36])
        self.wr = a.t([128, 8, 36])
        self.gmw = a.t([128, 4, 128])
        self.gmb = a.t([128, 4])
        self.gnw = a.t([128, 256])
        self.gnb = a.t([128, 256])
        self.w2s = a.t([64, 256])
        self.a2s = a.t([64, 256])
        self.g2s = a.t([128, 256])
        self.pv = a.t([64, 40])
        self.mu_w = a.t([64, 1])
        self.mu_a = a.t([64, 1])
        self.mu_g = a.t([128, 1])
        self.fbf = a.t([4, 1])
        self.nfbf = a.t([4, 1])
        self.Mst = a.t([64, 4, 64])
        self.r1_end = a.cur

    def keepconsts(self):
        pass

    def load_layer(self, l):
        kb = self.kb
        bc = lambda row: row.partition_broadcast(128)
        kb.dma(self.g1bc, bc(self.norm1_g[l:l + 1, :]), writes=["g1bc"])
        kb.dma(self.g2bc, bc(self.norm2_g[l:l + 1, :]), writes=["g2bc"])
        kb.dma(self.rbbc, bc(self.router_b[l:l + 1, :]), writes=["rbbc"])
        kb.dma(self.wr, self.router_w[l].rearrange("(k p) n -> p k n", p=128), writes=["wr"])
        kb.dma(self.gmw, self.gm_wT[l].rearrange("g s t -> s g t"), writes=["gmw"])
        self.P("dve", "tensor_tensor", self.gmw, self.gmw, self.m_incl.unsqueeze(1).to_broadcast([128, 4, 128]),
               ALU.mult, r=["gmw"], w=["gmw"])
        kb.dma(self.gmb, self.gm_bT[l], writes=["gmb"])
        kb.dma(self.gnw, bc(self.rw_gn_w[l:l + 1, :]), writes=["gnw"])
        kb.dma(self.gnb, bc(self.rw_gn_b[l:l + 1, :]), writes=["gnb"])
        kb.dma(self.w2s, self.rw_w2[l], writes=["w2s"])
        kb.dma(self.a2s, self.rw_a2[l], writes=["a2s"])
        kb.dma(self.g2s, self.rw_g2[l], writes=["g2s"])
        kb.dma(self.pv[:, 0:32], self.rw_pv[l], writes=["pv"])
        self.P("dve", "tensor_scalar", self.pv[:, 32:36], self.pv[:, 24:28], -1.0, 1.0, ALU.mult, ALU.add,
               r=["pv"], w=["pv"])
        kb.dma(self.mu_w, self.col(self.rw_mu[l, 768:832]), writes=["mu_w"])
        kb.dma(self.mu_a, self.col(self.rw_mu[l, 832:896]), writes=["mu_a"])
        kb.dma(self.mu_g, self.col(self.rw_mu[l, 896:1024]), writes=["mu_g"])
        kb.dma(self.fbf, self.col(self.fx_b_f[l]), writes=["fbf"])
        self.P("dve", "tensor_single_scalar", self.nfbf, self.fbf, -1.0, ALU.mult, r=["fbf"], w=["nfbf"])

    def rmsnorm(self, xt, xk, gbc, gk, out, outk, tmp, tmpk, st, stk):
        self.actf(tmp, xt, AF.Square, r=[xk], w=[tmpk, stk], accum_out=st[:, 0:1])
        self.actf(st[:, 1:2], st[:, 0:1], AF.Ln, r=[stk], w=[stk], scale=1.0 / D, bias=RMS_EPS)
        self.actf(st[:, 2:3], st[:, 1:2], AF.Exp, r=[stk], w=[stk], scale=-0.5)
        self.P("dve", "scalar_tensor_tensor", out, xt, st[:, 2:3], gbc, ALU.mult, ALU.mult,
               r=[xk, stk, gk], w=[outk])

    def phaseA(self, l, b, src):
        kb = self.kb
        a = Arena(self.arena_ap, self.r1_end, 208000)
        win = a.t([128, 8, DIN], BF16)
        for kc in range(8):
            kb.dma(win[:, kc, :], self.w_in[l, kc * 128:(kc + 1) * 128, :], writes=["win%d" % kc], q="pool")
        WK = ["win%d" % kc for kc in range(8)]
        xt = [a.t([128, D]) for _ in range(2)]
        junk = a.t([128, D])
        hb = [a.t([128, D], BF16) for _ in range(2)]
        hT4 = [a.t([128, 8, 512], BF16) for _ in range(2)]
        st = [a.t([128, 4]) for _ in range(2)]
        vsb = [a.t([128, 512], BF16) for _ in range(2)]
        xg = a.t([128, 512]); x2 = a.t([128, 512]); u1 = a.t([128, 512]); sg = a.t([128, 512]); hid = a.t([128, 512])
        gst = a.t([128, 16])
        vc = a.t([128, 256]); sq = a.t([128, 256]); vn = a.t([128, 256]); tmpg = a.t([128, 256]); ygm = a.t([128, 256])
        ygT = [a.t([128, 2, 128], BF16) for _ in range(2)]
        stg_bf = [a.t([64, 512], BF16) for _ in range(4)]
        stg_f = [a.t([128, 512]) for _ in range(4)]
        pb = self.pb
        pbT = pb[7][:, 0:512].bitcast(BF16)
        fm = []
        for h in range(4):
            fm.append((h * 64, 64, "qk", h, 1.0))
            fm.append((256 + h * 64, 64, "qk", 4 + h, 0.125))
            fm.append((2304 + h * 64, 64, "qk", 8 + h, 1.0))
            fm.append((2560 + h * 64, 64, "qk", 12 + h, 0.125))
        for j in range(12):
            fm.append((1280 + j * 64, 64, "rw", j * 64, 1.0))
        fm.append((2048, 64, "rw", 768, 1.0))
        fm.append((2112, 64, "rw", 832, 1.0))
        fm.append((2176, 128, "rw", 896, 1.0))
        fm.append((3072, 4, "fg", 0, 1.0))
        ci = 0
        for g4 in range(4):
            hT = hT4[g4 % 2]
            hTk = "hT4_%d" % (g4 % 2)
            for tt in range(4):
                t = g4 * 4 + tt
                p = t % 2
                rows = slice(t * 128, (t + 1) * 128)
                kb.dma(xt[p], src[rows, :], writes=["xt%d" % p])
                self.rmsnorm(xt[p], "xt%d" % p, self.g1bc, "g1bc", hb[p], "hb%d" % p, junk, "junk", st[p], "st%d" % p)
                for kc in range(8):
                    self.P("pe", "transpose", pbT[:, kc * 128:(kc + 1) * 128], hb[p][:, kc * 128:(kc + 1) * 128],
                           self.ident_bf, r=["hb%d" % p, "ident_bf"], w=["pb7"])
                self.actf(hT[:, :, tt * 128:(tt + 1) * 128], pbT.rearrange("p (k n) -> p k n", k=8), AF.Copy,
                          r=["pb7"], w=[hTk])
                for (pbi, pc0, c0, n) in ((0, 0, 512, 512), (1, 0, 1024, 256), (1, 256, 2816, 256)):
                    for kc in range(8):
                        self.mm(pb[pbi][:, pc0:pc0 + n], hT[:, kc, tt * 128:(tt + 1) * 128], win[:, kc, c0:c0 + n],
                                kc == 0, kc == 7, r=[hTk, WK[kc]], w=["pb%d" % pbi])
                self.actf(vsb[p][:, 0:256], pb[0][:, 0:256], AF.Copy, r=["pb0"], w=["vsb%d" % p])
                self.actf(vsb[p][:, 256:512], pb[1][:, 256:512], AF.Copy, r=["pb1"], w=["vsb%d" % p])
                kb.dma(self.vtok[b, rows, :], vsb[p], reads=["vsb%d" % p], writes=["vtok"])
                self.P("dve", "tensor_copy", xg[:, 0:256], pb[0][:, 256:512], r=["pb0"], w=["xg"])
                self.P("dve", "tensor_copy", xg[:, 256:512], pb[1][:, 0:256], r=["pb1"], w=["xg"])
                self.actf(x2, xg, AF.Square, r=["xg"], w=["x2"])
                self.P("dve", "tensor_scalar", u1, x2, 0.044715, 1.0, ALU.mult, ALU.add, r=["x2"], w=["u1"])
                self.P("dve", "tensor_tensor", u1, u1, xg, ALU.mult, r=["u1", "xg"], w=["u1"])
                self.actf(sg, u1, AF.Sigmoid, r=["u1"], w=["sg"], scale=1.5957691216057308)
                self.P("dve", "tensor_tensor", hid, sg, xg, ALU.mult, r=["sg", "xg"], w=["hid"])
                hv = hid[:, 256:512].rearrange("p (g d) -> p g d", g=4)
                v3 = lambda ap: ap.rearrange("p (g d) -> p g d", g=4)
                bc4 = lambda ap: ap.unsqueeze(2).to_broadcast([128, 4, 64])
                self.P("dve", "tensor_reduce", gst[:, 0:4], hv, AX.X, ALU.add, r=["hid"], w=["gst"])
                self.P("dve", "tensor_single_scalar", gst[:, 4:8], gst[:, 0:4], -1.0 / 64, ALU.mult, r=["gst"], w=["gst"])
                self.P("dve", "tensor_tensor", v3(vc), hv, bc4(gst[:, 4:8]), ALU.add, r=["hid", "gst"], w=["vc"])
                self.actf(sq, vc, AF.Square, r=["vc"], w=["sq"])
                self.P("dve", "tensor_reduce", gst[:, 8:12], v3(sq), AX.X, ALU.add, r=["sq"], w=["gst"])
                self.actf(gst[:, 8:12], gst[:, 8:12], AF.Ln, r=["gst"], w=["gst"], scale=1.0 / 64, bias=LN_EPS)
                self.actf(gst[:, 12:16], gst[:, 8:12], AF.Exp, r=["gst"], w=["gst"], scale=-0.5)
                self.P("dve", "tensor_tensor", v3(vn), v3(vc), bc4(gst[:, 12:16]), ALU.mult, r=["vc", "gst"], w=["vn"])
                for g in range(4):
                    self.mm(pb[2][:, g * 64:(g + 1) * 64], self.gmw[:, g, :], vn[:, g * 64:(g + 1) * 64], True, True,
                            r=["gmw", "vn"], w=["pb2"])
                self.P("dve", "tensor_tensor", v3(tmpg), v3(pb[2][:, 0:256]), bc4(self.gmb), ALU.add,
                       r=["pb2", "gmb"], w=["tmpg"])
                self.P("dve", "tensor_tensor", ygm, tmpg, hid[:, 0:256], ALU.mult, r=["tmpg", "hid"], w=["ygm"])
                for c in range(2):
                    self.P("pe", "transpose", pb[2][:, 256 + c * 128:256 + (c + 1) * 128], ygm[:, c * 128:(c + 1) * 128],
                           self.ident, r=["ygm", "cst"], w=["pb2"])
                self.actf(ygT[p].rearrange("p c n -> p (c n)"), pb[2][:, 256:512], AF.Copy, r=["pb2"], w=["ygT%d" % p])
                kb.dma(self.mixT[b, 256:512, rows].rearrange("(c p) n -> p c n", p=128), ygT[p],
                       reads=["ygT%d" % p], writes=["mixT"])
            cols = slice(g4 * 512, (g4 + 1) * 512)
            for (c0, M, kind, dest, scale) in fm:
                pi = 3 + ci % 4
                si = ci % 4
                ci += 1
                pk = "pb%d" % pi
                for kc in range(8):
                    self.mm(pb[pi][0:M, :], win[:, kc, c0:c0 + M], hT[:, kc, :], kc == 0, kc == 7,
                            r=[hTk, WK[kc]], w=[pk])
                if kind == "qk":
                    self.actf(stg_bf[si], pb[pi][0:64, :], AF.Copy, r=[pk], w=["stgb%d" % si], scale=scale)
                    kb.dma(self.qkT[b, dest, :, cols], stg_bf[si], reads=["stgb%d" % si], writes=["qkT"])
                elif kind == "rw":
                    self.P("dve", "tensor_copy", stg_f[si][0:M, :], pb[pi][0:M, :], r=[pk], w=["stgf%d" % si])
                    kb.dma(self.rwT[b, dest:dest + M, cols], stg_f[si][0:M, :], reads=["stgf%d" % si], writes=["rwT"])
                else:
                    self.P("dve", "tensor_copy", stg_f[si][0:M, :], pb[pi][0:M, :], r=[pk], w=["stgf%d" % si])
                    kb.dma(self.fgT[b, :, cols], stg_f[si][0:M, :], reads=["stgf%d" % si], writes=["fgT"])

    def phaseB_attn(self, l, b):
        kb = self.kb
        pball = self.pb
        a = Arena(self.arena_ap, self.r1_end, 208000)
        fg = a.t([4, T]); fe = a.t([4, T]); cum = a.t([4, T])
        self.ones4 = a.t([4, T])
        self.P("pool", "memset", self.ones4, 1.0, w=["ones4"])
        ncum_all = a.t([128, 16, 4])
        NS = 2
        slots = []
        for s in range(NS):
            B = dict(qT=a.t([64, T], BF16), kT=a.t([64, T], BF16), V=a.t([128, 16, 64], BF16),
                     E=[a.t([128, 512]) for _ in range(2)], Lp=[a.t([128, 512]) for _ in range(2)],
                     At=[a.t([128, 512], BF16) for _ in range(2)], Slp=a.t([128, 512]),
                     ob=[a.t([64, 512], BF16) for _ in range(2)], rec=a.t([64, 512]), cumrow=a.t([1, T]))
            slots.append(B)
        kb.dma(fg, self.fgT[b], reads=["fgT"], writes=["fg"])
        self.actf(fe, fg, AF.Exp, r=["fg", "nfbf"], w=["fe"], scale=-1.0, bias=self.nfbf)
        self.actf(fe, fe, AF.Ln, r=["fe"], w=["fe"], bias=1.0)
        self.P("dve", "tensor_tensor_scan", cum, self.ones4, fe, 0.0, ALU.mult, ALU.subtract,
               r=["fe", "ones4"], w=["cum"])
        kb.dma(self.cumd[b], cum, reads=["cum"], writes=["cumd"])
        for kbk in range(16):
            self.P("pe", "transpose", pball[6][:, kbk * 4:(kbk + 1) * 4], cum[0:4, kbk * 128:(kbk + 1) * 128],
                   self.ident[0:4, 0:4], r=["cum", "cst"], w=["pb6"])
        self.actf(ncum_all.rearrange("p a b -> p (a b)"), pball[6][:, 0:64], AF.Copy, r=["pb6"], w=["ncum"], scale=-1.0)

        def head_gen(kind, h, s):
            B = slots[s]
            K = lambda n: "%s_s%d" % (n, s)
            pb = pball[4 * s:4 * s + 4]
            pk = ["pb%d" % (4 * s + i) for i in range(4)]
            qT, kT, V, Slp, rec, cumrow = B["qT"], B["kT"], B["V"], B["Slp"], B["rec"], B["cumrow"]
            qk_q = h if kind == "sb" else 8 + h
            qk_k = 4 + h if kind == "sb" else 12 + h
            vc0 = h * 64 if kind == "sb" else 256 + h * 64
            mrow = h * 64 if kind == "sb" else 768 + h * 64
            kb.dma(qT, self.qkT[b, qk_q], reads=["qkT"], writes=[K("qT")])
            kb.dma(kT, self.qkT[b, qk_k], reads=["qkT"], writes=[K("kT")])
            kb.dma(V, self.vtok[b, :, vc0:vc0 + 64].rearrange("(kb p) d -> p kb d", p=128),
                   reads=["vtok"], writes=[K("V")])
            if kind == "fx":
                kb.dma(cumrow, self.cumd[b, h:h + 1, :], reads=["cumd"], writes=[K("cumrow")])
            yield
            it = 0
            for c in range(4):
                Q0 = c * 512
                last = 4 * c + 3
                if kind == "sb":
                    self.P("pool", "memset", Slp, 0.0, w=[K("Slp")])
                    order = list(range(last, -1, -1))
                else:
                    order = list(range(0, last + 1))
                first_pv = True
                for kbk in order:
                    r_ = kbk - 4 * c
                    diag = r_ >= 0
                    col0 = max(r_, 0) * 128
                    i2 = it % 2
                    it += 1
                    E, Lp, At = B["E"][i2], B["Lp"][i2], B["At"][i2]
                    Ek, Lk, Ak = K("E%d" % i2), K("Lp%d" % i2), K("At%d" % i2)
                    kblk = kT[:, kbk * 128:(kbk + 1) * 128]
                    qs = lambda c_a, c_b: qT[:, Q0 + c_a:Q0 + c_b]
                    blocks = []
                    if diag:
                        blocks.append((col0, col0 + 128, True))
                        if col0 + 128 < 512:
                            blocks.append((col0 + 128, 512, False))
                    else:
                        blocks.append((0, 512, False))
                    if kind == "sb":
                        pz, pzk = (pb[0], pk[0]) if i2 == 0 else (pb[3], pk[3])
                        pa, pak = pb[1], pk[1]
                        for (lo, hi, dg) in blocks:
                            if dg:
                                self.mm(pz[:, lo:hi], self.ident_bf, self.mbs_bf, True, False, r=["ident_bf", "mbs_bf"], w=[pzk])
                            self.mm(pz[:, lo:hi], kblk, qs(lo, hi), not dg, True, r=[K("kT"), K("qT")], w=[pzk])
                        yield
                        self.actf(E[:, col0:512], pz[:, col0:512], AF.Exp, r=[pzk], w=[Ek])
                        yield
                        self.actf(Lp[:, col0:512], E[:, col0:512], AF.Ln, r=[Ek], w=[Lk], bias=1.0)
                        yield
                        for (lo, hi, dg) in blocks:
                            if dg:
                                self.mm(pa[:, lo:hi], self.ident_bf, self.mbs_bf, True, False, r=["ident_bf", "mbs_bf"], w=[pak])
                            self.mm(pa[:, lo:hi], kblk, qs(lo, hi), not dg, False, r=[K("kT"), K("qT")], w=[pak])
                            self.mm(pa[:, lo:hi], self.ntri, Lp[:, lo:hi], False, False, r=["cst", Lk], w=[pak])
                            self.mm(pa[:, lo:hi], self.negones, Slp[:, lo:hi], False, True, r=["cst", K("Slp")], w=[pak])
                        yield
                        self.actf(At[:, col0:512], pa[:, col0:512], AF.Exp, r=[pak], w=[Ak])
                        self.P("pool", "tensor_tensor", Slp[:, col0:512], Slp[:, col0:512], Lp[:, col0:512], ALU.add,
                               r=[K("Slp"), Lk], w=[K("Slp")])
                        yield
                        for (lo, hi, dg) in blocks:
                            self.mm(pb[2][0:64, lo:hi], V[:, kbk, :], At[:, lo:hi], first_pv, kbk == 0,
                                    r=[K("V"), Ak], w=[pk[2]])
                            first_pv = False
                        yield
                    else:
                        pz, pzk = (pb[0], pk[0]) if i2 == 0 else (pb[1], pk[1])
                        for (lo, hi, dg) in blocks:
                            if dg:
                                self.mm(pz[:, lo:hi], self.ident_bf, self.mbi_bf, True, False, r=["ident_bf", "mbi_bf"], w=[pzk])
                            self.mm(pz[:, lo:hi], kblk, qs(lo, hi), not dg, False, r=[K("kT"), K("qT")], w=[pzk])
                            self.mm(pz[:, lo:hi], self.ones_f[0:1, :], cumrow[0:1, Q0 + lo:Q0 + hi], False, True,
                                    r=["ones_f", K("cumrow")], w=[pzk])
                        yield
                        self.actf(At[:, col0:512], pz[:, col0:512], AF.Exp, r=[pzk, "ncum"], w=[Ak],
                                  bias=ncum_all[:, kbk, h:h + 1])
                        yield
                        for (lo, hi, dg) in blocks:
                            self.mm(pb[2][0:64, lo:hi], V[:, kbk, :], At[:, lo:hi], first_pv, dg,
                                    r=[K("V"), Ak], w=[pk[2]])
                            self.mm(pb[3][0:64, lo:hi], self.ones_bf, At[:, lo:hi], first_pv, dg,
                                    r=["ones_bf", Ak], w=[pk[3]])
                            first_pv = False
                        yield
                oi = c % 2
                ob, obk = B["ob"][oi], K("ob%d" % oi)
                if kind == "sb":
                    self.actf(ob, pb[2][0:64, :], AF.Copy, r=[pk[2]], w=[obk])
                else:
                    self.P("dve", "reciprocal", rec, pb[3][0:64, :], r=[pk[3]], w=[K("rec")])
                    yield
                    self.P("dve", "tensor_tensor", ob, pb[2][0:64, :], rec, ALU.mult, r=[pk[2], K("rec")], w=[obk])
                yield
                kb.dma(self.mixT[b, mrow:mrow + 64, Q0:Q0 + 512], ob, reads=[obk], writes=["mixT"])

        for kind in ("sb", "fx"):
            for hp in range(2):
                gens = [head_gen(kind, 2 * hp + s, s) for s in range(NS)]
                while gens:
                    for g in list(gens):
                        try:
                            next(g)
                        except StopIteration:
                            gens.remove(g)

    def phaseB_rwkv(self, l, b):
        kb = self.kb
        pball = self.pb
        a = Arena(self.arena_ap, self.r1_end, 208000)
        F = lambda parts=64: a.t([parts, 512])
        xw, xwp, xa, xap = F(), F(), F(), F()
        xgt, xgp = F(128), F(128)
        pv = self.pv
        NS = 2
        slots = []
        for s in range(NS):
            B = {}
            for nm in ("raw0", "raw1", "raw2", "prv0", "prv1", "prv2", "rm", "km", "vm", "sgd", "logd", "G", "av",
                       "kk", "kk2", "nrm", "kap", "tmpa", "kp", "bbv", "eG", "eGn", "eGx", "ktT", "btT", "kdT", "rtT", "rkr"):
                B[nm] = F()
            B["tok3"] = [a.t([128, 3, 64]) for _ in range(2)]
            B["sgt"] = [a.t([128, 72]) for _ in range(2)]
            B["ABm"] = [a.t([128, 4, 128]) for _ in range(2)]
            B["X"] = [a.t([128, 128]) for _ in range(2)]
            B["Y"] = [a.t([128, 128]) for _ in range(2)]
            B["R"] = [a.t([128, 128]) for _ in range(2)]
            for nm in ("Wn", "U", "ysb", "yn", "yj"):
                B[nm] = a.t([128, 64])
            B["mt"] = a.t([64, 64])
            B["yst"] = a.t([128, 8])
            B["ymix"] = [a.t([64, 512], BF16) for _ in range(2)]
            slots.append(B)
        for h in range(4):
            self.P("pool", "memset", self.Mst[:, h, :], 0.0, w=["M%d" % h])

        def load_shift(g4, dst, dstp, rows, nm, parts):
            c0 = g4 * 512
            kb.dma(dst[0:parts, :], self.rwT[b, rows, c0:c0 + 512], reads=["rwT"], writes=[nm])
            if g4 == 0:
                self.P("pool", "memset", dstp[0:parts, 0:1], 0.0, w=[nm + "p"])
                kb.dma(dstp[0:parts, 1:512], self.rwT[b, rows, 0:511], reads=["rwT"], writes=[nm + "p"])
            else:
                kb.dma(dstp[0:parts, :], self.rwT[b, rows, c0 - 1:c0 + 511], reads=["rwT"], writes=[nm + "p"])

        def mix(dst, x_, xp_, mu, nm, dk, parts=64):
            self.P("pool", "tensor_tensor", xp_[0:parts, :], xp_[0:parts, :], x_[0:parts, :], ALU.subtract,
                   r=[nm, nm + "p"], w=[nm + "p"])
            self.P("dve", "scalar_tensor_tensor", dst[0:parts, :], xp_[0:parts, :], mu, x_[0:parts, :], ALU.mult, ALU.add,
                   r=[nm, nm + "p", "pv", "mu_w", "mu_a", "mu_g"], w=[dk])

        def head_gen(g4, h, s):
            B = slots[s]
            K = lambda n: "%s_s%d" % (n, s)
            pb = pball[4 * s:4 * s + 4]
            pk = ["pb%d" % (4 * s + i) for i in range(4)]
            c0 = g4 * 512
            cols = slice(c0, c0 + 512)
            hc = slice(h * 64, (h + 1) * 64)
            Mh = self.Mst[:, h, :]
            Mk = "M%d" % h
            rm, km, vm = B["rm"], B["km"], B["vm"]
            sgd, logd, G, av = B["sgd"], B["logd"], B["G"], B["av"]
            kk, kk2, nrm, kap, tmpa, kp, bbv = B["kk"], B["kk2"], B["nrm"], B["kap"], B["tmpa"], B["kp"], B["bbv"]
            eG, eGn, eGx = B["eG"], B["eGn"], B["eGx"]
            ktT, btT, kdT, rtT, rkr = B["ktT"], B["btT"], B["kdT"], B["rtT"], B["rkr"]
            Wn, U, ysb, yn, yj, mt, yst = B["Wn"], B["U"], B["ysb"], B["yn"], B["yj"], B["mt"], B["yst"]
            X, Y, R = B["X"], B["Y"], B["R"]
            for j, (nm, dst) in enumerate((("r", rm), ("k", km), ("v", vm))):
                rows = slice((j * 4 + h) * 64, (j * 4 + h + 1) * 64)
                load_shift(g4, B["raw%d" % j], B["prv%d" % j], rows, K("raw%d" % j), 64)
                mix(dst, B["raw%d" % j], B["prv%d" % j], pv[:, j * 4 + h:j * 4 + h + 1], K("raw%d" % j), K(nm + "m"))
            yield
            self.mm(pb[0][0:64, :], self.w2s[:, hc], xw, True, True, r=["w2s", "xw"], w=[pk[0]])
            self.mm(pb[1][0:64, :], self.a2s[:, hc], xa, True, True, r=["a2s", "xa"], w=[pk[1]])
            self.P("dve", "tensor_single_scalar", kk, km, pv[:, 20 + h:21 + h], ALU.mult, r=[K("km"), "pv"], w=[K("kk")])
            self.P("pool", "tensor_tensor", kk2, kk, kk, ALU.mult, r=[K("kk")], w=[K("kk2")])
            yield
            self.actf(sgd, pb[0][0:64, :], AF.Sigmoid, r=[pk[0], "pv"], w=[K("sgd")], bias=pv[:, 12 + h:13 + h])
            self.actf(av, pb[1][0:64, :], AF.Sigmoid, r=[pk[1], "pv"], w=[K("av")], bias=pv[:, 16 + h:17 + h])
            self.mm(pb[2][0:64, :], self.ones_f[0:64, 0:64], kk2, True, True, r=["ones_f", K("kk2")], w=[pk[2]])
            yield
            self.P("dve", "tensor_single_scalar", logd, sgd, -0.6065306597126334, ALU.mult, r=[K("sgd")], w=[K("logd")])
            self.actf(nrm, pb[2][0:64, :], AF.Sqrt, r=[pk[2]], w=[K("nrm")])
            yield
            self.P("dve", "tensor_tensor_scan", G, self.reset, logd, 0.0, ALU.mult, ALU.add, r=["reset", K("logd")], w=[K("G")])
            self.P("dve", "tensor_single_scalar", nrm, nrm, 1e-12, ALU.max, r=[K("nrm")], w=[K("nrm")])
            yield
            self.P("dve", "reciprocal", nrm, nrm, r=[K("nrm")], w=[K("nrm")])
            self.actf(eG, G, AF.Exp, r=[K("G")], w=[K("eG")])
            self.actf(eGn, G, AF.Exp, r=[K("G")], w=[K("eGn")], scale=-1.0)
            self.P("dve", "tensor_tensor", logd, G, logd, ALU.subtract, r=[K("G"), K("logd")], w=[K("logd")])
            yield
            self.P("dve", "tensor_tensor", kap, kk, nrm, ALU.mult, r=[K("kk"), K("nrm")], w=[K("kap")])
            self.actf(eGx, logd, AF.Exp, r=[K("logd")], w=[K("eGx")])
            self.P("dve", "tensor_scalar", tmpa, av, pv[:, 24 + h:25 + h], pv[:, 32 + h:33 + h], ALU.mult, ALU.add,
                   r=[K("av"), "pv"], w=[K("tmpa")])
            yield
            self.P("pool", "tensor_tensor", kp, km, tmpa, ALU.mult, r=[K("km"), K("tmpa")], w=[K("kp")])
            self.P("pool", "tensor_tensor", bbv, kap, av, ALU.mult, r=[K("kap"), K("av")], w=[K("bbv")])
            self.P("dve", "tensor_tensor", ktT, kap, eGx, ALU.mult, r=[K("kap"), K("eGx")], w=[K("ktT")])
            self.P("pool", "tensor_tensor", rtT, rm, eG, ALU.mult, r=[K("rm"), K("eG")], w=[K("rtT")])
            yield
            self.P("pool", "tensor_tensor", btT, bbv, eGn, ALU.mult, r=[K("bbv"), K("eGn")], w=[K("btT")])
            self.P("dve", "tensor_tensor", kdT, kp, eGn, ALU.mult, r=[K("kp"), K("eGn")], w=[K("kdT")])
            self.P("dve", "scalar_tensor_tensor", rkr, rm, pv[:, 28 + h:29 + h], kp, ALU.mult, ALU.mult,
                   r=[K("rm"), K("kp"), "pv"], w=[K("rkr")])
            yield
            for cc in range(4):
                u2 = cc % 2
                cs = slice(cc * 128, (cc + 1) * 128)
                t3, t3k = B["tok3"][u2], K("tok3_%d" % u2)
                sgtk = K("sgt%d" % u2)
                for j, (srcT, sk) in enumerate(((btT, K("btT")), (kdT, K("kdT")), (vm, K("vm")))):
                    self.P("pe", "transpose", pb[0][:, j * 64:(j + 1) * 64], srcT[:, cs], self.ident[0:64, 0:64],
                           r=[sk, "cst"], w=[pk[0]])
                self.mm(pb[0][:, 192:193], rkr[:, cs], self.ones_f[0:64, 0:1], True, True, r=[K("rkr"), "ones_f"], w=[pk[0]])
                self.mm(pb[0][:, 200:264], xgt[:, cs], self.g2s[:, hc], True, True, r=["xg", "g2s"], w=[pk[0]])
                for j, (lT, lk, rT, rk_) in enumerate(((btT, K("btT"), ktT, K("ktT")), (btT, K("btT"), rtT, K("rtT")),
                                                       (kdT, K("kdT"), ktT, K("ktT")), (kdT, K("kdT"), rtT, K("rtT")))):
                    self.mm(pb[1][:, j * 128:(j + 1) * 128], lT[:, cs], rT[:, cs], True, True, r=[lk, rk_], w=[pk[1]])
                self.mm(pb[2][:, 0:128], ktT[:, cs], btT[:, cs], True, True, r=[K("ktT"), K("btT")], w=[pk[2]])
                yield
                self.actf(t3.rearrange("p a b -> p (a b)"), pb[0][:, 0:192], AF.Copy, r=[pk[0]], w=[t3k])
                self.P("dve", "tensor_copy", B["sgt"][u2], pb[0][:, 192:264], r=[pk[0]], w=[sgtk])
                bt_tok, kd_tok, v_tok = t3[:, 0, :], t3[:, 1, :], t3[:, 2, :]
                s_tok = B["sgt"][u2][:, 0:1]
                g_tok = B["sgt"][u2][:, 8:72]
                AB, ABk = B["ABm"][u2], K("AB%d" % u2)
                self.P("dve", "tensor_tensor", AB.rearrange("p (x y) t -> p x y t", x=2),
                       pb[1].rearrange("p (x y t) -> p x y t", x=2, y=2),
                       self.mask2.unsqueeze(1).to_broadcast([128, 2, 2, 128]), ALU.mult, r=[pk[1], "cst"], w=[ABk])
                Nm, BbT, AkT, BkT = AB[:, 0, :], AB[:, 1, :], AB[:, 2, :], AB[:, 3, :]
                self.P("dve", "tensor_tensor", Y[0], pb[2][:, 0:128], self.m_lows, ALU.mult, r=[pk[2], "cst"], w=[K("Y0")])
                yield
                self.P("pool", "tensor_tensor", R[0], self.ident, Nm, ALU.subtract, r=["cst", ABk], w=[K("R0")])
                Xc, Xk = Nm, ABk
                Yc, Yk = Y[0], K("Y0")
                Rc, Rk = R[0], K("R0")
                for i in range(1, 7):
                    xi = i % 2
                    if i < 6:
                        self.mm(pb[2][:, 128:256], Yc, Xc, True, True, r=[Yk, Xk], w=[pk[2]])
                    self.mm(pb[2][:, 256:384], Xc, Yc, True, True, r=[Xk, Yk], w=[pk[2]])
                    yield
                    if i < 6:
                        self.actf(X[xi], pb[2][:, 128:256], AF.Copy, r=[pk[2]], w=[K("X%d" % xi)])
                    self.P("dve", "tensor_copy", Y[xi], pb[2][:, 256:384], r=[pk[2]], w=[K("Y%d" % xi)])
                    if i < 6:
                        Xc, Xk = X[xi], K("X%d" % xi)
                    Yc, Yk = Y[xi], K("Y%d" % xi)
                    yield
                    self.mm(pb[3][:, 0:128], Yc, Rc, True, True, r=[Yk, Rk], w=[pk[3]])
                    yield
                    self.P("dve", "tensor_tensor", R[xi], pb[3][:, 0:128], Rc, ALU.add, r=[pk[3], Rk], w=[K("R%d" % xi)])
                    Rc, Rk = R[xi], K("R%d" % xi)
                    yield
                self.mm(pb[3][:, 128:192], ktT[:, cs], Mh, True, False, r=[K("ktT"), Mk], w=[pk[3]])
                self.mm(pb[3][:, 128:192], AkT, v_tok, False, True, r=[ABk, t3k], w=[pk[3]])
                yield
                self.actf(Wn, pb[3][:, 128:192], AF.Copy, r=[pk[3]], w=[K("Wn")], scale=-1.0)
                yield
                self.mm(pb[3][:, 192:256], Rc, Wn, True, True, r=[Rk, K("Wn")], w=[pk[3]])
                yield
                self.actf(U, pb[3][:, 192:256], AF.Copy, r=[pk[3]], w=[K("U")])
                yield
                self.mm(pb[3][:, 256:320], rtT[:, cs], Mh, True, False, r=[K("rtT"), Mk], w=[pk[3]])
                self.mm(pb[3][:, 256:320], BbT, U, False, False, r=[ABk, K("U")], w=[pk[3]])
                self.mm(pb[3][:, 256:320], BkT, v_tok, False, True, r=[ABk, t3k], w=[pk[3]])
                self.mm(pb[3][0:64, 320:384], bt_tok, U, True, False, r=[t3k, K("U")], w=[pk[3]])
                self.mm(pb[3][0:64, 320:384], kd_tok, v_tok, False, True, r=[t3k], w=[pk[3]])
                yield
                self.P("dve", "tensor_tensor", mt, pb[3][0:64, 320:384], Mh, ALU.add, r=[pk[3], Mk], w=[K("mt")])
                self.actf(ysb, pb[3][:, 256:320], AF.Identity, r=[pk[3]], w=[K("ysb"), K("yst")], accum_out=yst[:, 0:1])
                yield
                self.P("dve", "tensor_single_scalar", Mh, mt, eG[:, cc * 128 + 127:cc * 128 + 128], ALU.mult,
                       r=[K("mt"), K("eG")], w=[Mk])
                self.P("dve", "tensor_single_scalar", yst[:, 1:2], yst[:, 0:1], -1.0 / 64, ALU.mult, r=[K("yst")], w=[K("yst")])
                yield
                self.actf(yj, ysb, AF.Square, r=[K("ysb"), K("yst")], w=[K("yj"), K("yst")], bias=yst[:, 1:2], accum_out=yst[:, 2:3])
                yield
                self.actf(yst[:, 3:4], yst[:, 2:3], AF.Ln, r=[K("yst")], w=[K("yst")], scale=1.0 / 64, bias=GN_EPS)
                yield
                self.actf(yst[:, 4:5], yst[:, 3:4], AF.Exp, r=[K("yst")], w=[K("yst")], scale=-0.5)
                yield
                self.P("dve", "tensor_scalar", yn, ysb, yst[:, 1:2], yst[:, 4:5], ALU.add, ALU.mult, r=[K("ysb"), K("yst")], w=[K("yn")])
                yield
                self.P("dve", "tensor_tensor", yn, yn, self.gnw[:, hc], ALU.mult, r=[K("yn"), "gnw"], w=[K("yn")])
                yield
                self.P("dve", "tensor_tensor", yn, yn, self.gnb[:, hc], ALU.add, r=[K("yn"), "gnb"], w=[K("yn")])
                yield
                self.P("dve", "scalar_tensor_tensor", yn, v_tok, s_tok, yn, ALU.mult, ALU.add,
                       r=[K("yn"), t3k, sgtk], w=[K("yn")])
                yield
                self.P("dve", "tensor_tensor", yn, yn, g_tok, ALU.mult, r=[K("yn"), sgtk], w=[K("yn")])
                yield
                self.P("pe", "transpose", pb[0][0:64, 384:512], yn, self.ident, r=[K("yn"), "cst"], w=[pk[0]])
                yield
                ym, ymk = B["ymix"][g4 % 2], K("ymix%d" % (g4 % 2))
                self.actf(ym[:, cs], pb[0][0:64, 384:512], AF.Copy, r=[pk[0]], w=[ymk])
                yield
            kb.dma(self.mixT[b, 512 + h * 64:512 + (h + 1) * 64, cols], ym, reads=[ymk], writes=["mixT"])

        for g4 in range(4):
            load_shift(g4, xw, xwp, slice(768, 832), "xw", 64)
            load_shift(g4, xa, xap, slice(832, 896), "xa", 64)
            load_shift(g4, xgt, xgp, slice(896, 1024), "xg", 128)
            mix(xw, xw, xwp, self.mu_w, "xw", "xw")
            mix(xa, xa, xap, self.mu_a, "xa", "xa")
            mix(xgt, xgt, xgp, self.mu_g, "xg", "xg", 128)
            self.actf(xw, xw, AF.Tanh, r=["xw"], w=["xw"])
            self.actf(xgt, xgt, AF.Sigmoid, r=["xg"], w=["xg"])
            for hp in range(2):
                gens = [head_gen(g4, 2 * hp + s, s) for s in range(NS)]
                while gens:
                    for g in list(gens):
                        try:
                            next(g)
                        except StopIteration:
                            gens.remove(g)

    def phaseD(self, l, b, src):
        kb = self.kb
        pb = self.pb
        a = Arena(self.arena_ap, self.r1_end, 208000)
        wo = a.t([128, 8, D], BF16)
        for kc in range(8):
            kb.dma(wo[:, kc, :], self.w_out[l, kc * 128:(kc + 1) * 128, :], writes=["wo%d" % kc], q="pool")
        mx = [a.t([128, 8, 128], BF16) for _ in range(2)]
        xt = [a.t([128, D]) for _ in range(2)]
        xn = [a.t([128, D]) for _ in range(2)]
        h2 = [a.t([128, D]) for _ in range(2)]
        junk = a.t([128, D])
        st = [a.t([128, 4]) for _ in range(2)]
        hTf = a.t([128, 8, 128])
        hTb = [a.t([128, 8, 128], BF16) for _ in range(2)]
        lg = a.t([128, 36]); lm = a.t([128, 32]); l2 = a.t([128, 32]); oh1 = a.t([128, 32]); oh2 = a.t([128, 32])
        ohg = a.t([128, 4]); ge = a.t([128, 4]); rs = a.t([128, 16])
        gt = [a.t([128, 32]) for _ in range(2)]
        rt = [a.t([128, 66]) for _ in range(2)]
        h2b = [a.t([128, D], BF16) for _ in range(2)]
        for t in range(NT):
            p = t % 2
            rows = slice(t * 128, (t + 1) * 128)
            kb.dma(mx[p], self.mixT[b, :, rows].rearrange("(k p) n -> p k n", p=128), reads=["mixT"], writes=["mx%d" % p])
            kb.dma(xt[p], src[rows, :], reads=["xres"], writes=["xt%d" % p])
            for hf in range(2):
                for kc in range(8):
                    self.mm(pb[hf], mx[p][:, kc, :], wo[:, kc, hf * 512:(hf + 1) * 512], kc == 0, kc == 7,
                            r=["mx%d" % p, "wo%d" % kc], w=["pb%d" % hf])
                self.P("dve", "tensor_tensor", xn[p][:, hf * 512:(hf + 1) * 512], pb[hf], xt[p][:, hf * 512:(hf + 1) * 512],
                       ALU.add, r=["pb%d" % hf, "xt%d" % p], w=["xn%d" % p])
            kb.dma(self.xres[b, rows, :], xn[p], reads=["xn%d" % p], writes=["xres_w"])
            self.rmsnorm(xn[p], "xn%d" % p, self.g2bc, "g2bc", h2[p], "h2%d" % p, junk, "junk", st[p], "st%d" % p)
            for kc in range(8):
                pbi = 2 + kc // 4
                self.P("pe", "transpose", pb[pbi][:, (kc % 4) * 128:(kc % 4 + 1) * 128], h2[p][:, kc * 128:(kc + 1) * 128],
                       self.ident, r=["h2%d" % p, "cst"], w=["pb%d" % pbi])
            for hh in range(2):
                self.actf(hTf[:, hh * 4:(hh + 1) * 4, :].rearrange("p k n -> p (k n)"), pb[2 + hh], AF.Copy,
                          r=["pb%d" % (2 + hh)], w=["hTf"])
                self.P("dve", "tensor_copy", hTb[p][:, hh * 4:(hh + 1) * 4, :].rearrange("p k n -> p (k n)"), pb[2 + hh],
                       r=["pb%d" % (2 + hh)], w=["hTb%d" % p])
            if not self.sparse:
                kb.dma(self.h2T[b, :, rows].rearrange("(k p) n -> p k n", p=128), hTb[p], reads=["hTb%d" % p], writes=["h2T"])
            else:
                self.P("pool", "tensor_copy", h2b[p], h2[p], r=["h2%d" % p], w=["h2b%d" % p])
                kb.dma(self.h2tok[b, rows, :], h2b[p], reads=["h2b%d" % p], writes=["h2tok"])
            for kc in range(8):
                self.mm(pb[4][:, 0:36], hTf[:, kc, :], self.wr[:, kc, :], kc == 0, kc == 7, r=["hTf", "wr"], w=["pb4"])
            self.P("dve", "tensor_tensor", lg, pb[4][:, 0:36], self.rbbc, ALU.add, r=["pb4", "rbbc"], w=["lg"])
            self.P("dve", "tensor_reduce", rs[:, 0:1], lg[:, 0:4], AX.X, ALU.max, r=["lg"], w=["rs"])
            self.P("dve", "tensor_scalar", ohg, lg[:, 0:4], rs[:, 0:1], None, ALU.is_equal, r=["lg", "rs"], w=["ohg"])
            self.P("dve", "tensor_single_scalar", rs[:, 1:2], rs[:, 0:1], -1.0, ALU.mult, r=["rs"], w=["rs"])
            self.actf(ge, lg[:, 0:4], AF.Exp, r=["lg", "rs"], w=["ge", "rs"], bias=rs[:, 1:2], accum_out=rs[:, 2:3])
            self.P("dve", "reciprocal", rs[:, 3:4], rs[:, 2:3], r=["rs"], w=["rs"])
            self.P("dve", "tensor_scalar", ohg, ohg, -1.0, 1e30, ALU.add, ALU.mult, r=["ohg"], w=["ohg"])
            self.P("dve", "tensor_tensor", lm.rearrange("p (g e) -> p g e", g=4), lg[:, 4:36].rearrange("p (g e) -> p g e", g=4),
                   ohg.unsqueeze(2).to_broadcast([128, 4, 8]), ALU.add, r=["lg", "ohg"], w=["lm"])
            self.P("dve", "tensor_reduce", rs[:, 4:5], lm, AX.X, ALU.max, r=["lm"], w=["rs"])
            self.P("dve", "tensor_scalar", oh1, lm, rs[:, 4:5], None, ALU.is_equal, r=["lm", "rs"], w=["oh1"])
            self.P("dve", "scalar_tensor_tensor", l2, oh1, -1e30, lm, ALU.mult, ALU.add, r=["oh1", "lm"], w=["l2"])
            self.P("dve", "tensor_reduce", rs[:, 5:6], l2, AX.X, ALU.max, r=["l2"], w=["rs"])
            self.P("dve", "tensor_scalar", oh2, l2, rs[:, 5:6], None, ALU.is_equal, r=["l2", "rs"], w=["oh2"])
            self.P("dve", "tensor_tensor", rs[:, 6:7], rs[:, 5:6], rs[:, 4:5], ALU.subtract, r=["rs"], w=["rs"])
            self.actf(rs[:, 7:8], rs[:, 6:7], AF.Exp, r=["rs"], w=["rs"])
            self.P("dve", "tensor_single_scalar", rs[:, 8:9], rs[:, 7:8], 1.0, ALU.add, r=["rs"], w=["rs"])
            self.P("dve", "reciprocal", rs[:, 9:10], rs[:, 8:9], r=["rs"], w=["rs"])
            self.P("dve", "tensor_tensor", rs[:, 10:11], rs[:, 9:10], rs[:, 7:8], ALU.mult, r=["rs"], w=["rs"])
            self.P("dve", "tensor_tensor", rs[:, 11:12], rs[:, 9:10], rs[:, 3:4], ALU.mult, r=["rs"], w=["rs"])
            self.P("dve", "tensor_tensor", rs[:, 12:13], rs[:, 10:11], rs[:, 3:4], ALU.mult, r=["rs"], w=["rs"])
            self.P("dve", "tensor_single_scalar", gt[p], oh1, rs[:, 11:12], ALU.mult, r=["oh1", "rs"], w=["gt%d" % p])
            self.P("dve", "scalar_tensor_tensor", gt[p], oh2, rs[:, 12:13], gt[p], ALU.mult, ALU.add,
                   r=["oh2", "rs", "gt%d" % p], w=["gt%d" % p])
            if not self.sparse:
                kb.dma(self.gates[b, rows, :], gt[p], reads=["gt%d" % p], writes=["gates"])
            else:
                self.P("dve", "tensor_copy", rt[p][:, 0:32], oh1, r=["oh1"], w=["rt%d" % p])
                self.P("dve", "tensor_copy", rt[p][:, 32:64], oh2, r=["oh2"], w=["rt%d" % p])
                self.P("dve", "tensor_copy", rt[p][:, 64:66], rs[:, 11:13], r=["rs"], w=["rt%d" % p])
                kb.dma(self.route[b, rows, :], rt[p], reads=["rt%d" % p], writes=["route"])

    def phaseE(self, l, b, final):
        kb = self.kb
        pb = self.pb
        a = Arena(self.arena_ap, self.r1_end, 208000)
        yacc = a.t([128, NT, D])
        hT = a.t([128, 8, T], BF16)
        gts = a.t([128, NT, 32])
        wg = [a.t([128, 8, EH], BF16) for _ in range(2)]
        wu = [a.t([128, 8, EH], BF16) for _ in range(2)]
        wd = [a.t([128, 4, D], BF16) for _ in range(2)]
        sl = [a.t([128, 512]) for _ in range(2)]
        act = [a.t([128, 4, 512], BF16) for _ in range(2)]
        for t in range(NT):
            kb.dma(yacc[:, t, :], self.xres[b, t * 128:(t + 1) * 128, :], writes=["y%d" % t])
        for kc in range(8):
            kb.dma(hT[:, kc, :], self.h2T[b, kc * 128:(kc + 1) * 128, :], writes=["hT"])
        kb.dma(gts, self.gates[b].rearrange("(t p) e -> p t e", p=128), writes=["gts"])
        it = 0
        nstage = 0
        stg = [a.t([128, 4, 512]) for _ in range(2)]
        for e in range(self.moe_experts):
            p = e % 2
            for wi, (wsrc, wdst, wkey, nh) in enumerate(((self.w_gate, wg[p], "wg%d" % p, 2), (self.w_up, wu[p], "wu%d" % p, 2),
                                                           (self.w_down, wd[p], "wd%d" % p, 1))):
                for hh in range(2):
                    si = nstage % 2
                    nstage += 1
                    if nh == 2:
                        src_ap = wsrc[l, e, hh * 512:(hh + 1) * 512, :].rearrange("(k p) n -> p k n", p=128)
                        dst_ap = wdst[:, hh * 4:(hh + 1) * 4, :]
                        stv = stg[si]
                    else:
                        src_ap = wsrc[l, e, hh * 256:(hh + 1) * 256, :].rearrange("(k p) n -> p k n", p=128)
                        dst_ap = wdst[:, hh * 2:(hh + 1) * 2, :]
                        stv = stg[si].rearrange("p k n -> p (k n)").rearrange("p (k n) -> p k n", k=2)
                    kb.dma(stv, src_ap, writes=["stg%d" % si])
                    if nstage % 2 == 0:
                        self.actf(dst_ap, stv, AF.Copy, r=["stg%d" % si], w=[wkey + "_%d" % hh])
                    else:
                        self.P("pool", "tensor_copy", dst_ap, stv, r=["stg%d" % si], w=[wkey + "_%d" % hh])
            for g in range(4):
                ap_ = it % 2
                it += 1
                tcols = slice(g * 512, (g + 1) * 512)
                for m in range(4):
                    pg = pb[(2 * m) % 4]; pgk = "pb%d" % ((2 * m) % 4)
                    pu = pb[(2 * m) % 4 + 1]; puk = "pb%d" % ((2 * m) % 4 + 1)
                    for kc in range(8):
                        self.mm(pg, wg[p][:, kc, m * 128:(m + 1) * 128], hT[:, kc, tcols], kc == 0, kc == 7,
                                r=["wg%d_0" % p, "wg%d_1" % p, "hT"], w=[pgk])
                    for kc in range(8):
                        self.mm(pu, wu[p][:, kc, m * 128:(m + 1) * 128], hT[:, kc, tcols], kc == 0, kc == 7,
                                r=["wu%d_0" % p, "wu%d_1" % p, "hT"], w=[puk])
                    s_ = sl[m % 2]; sk = "sl%d" % (m % 2)
                    self.actf(s_, pg, AF.Silu, r=[pgk], w=[sk])
                    self.P("dve", "tensor_tensor", act[ap_][:, m, :], s_, pu, ALU.mult, r=[sk, puk], w=["act%d_%d" % (ap_, m)])
                for tt in range(4):
                    t = g * 4 + tt
                    for hf in range(2):
                        po = pb[4 + (tt * 2 + hf) % 4]; pok = "pb%d" % (4 + (tt * 2 + hf) % 4)
                        for m in range(4):
                            self.mm(po, act[ap_][:, m, tt * 128:(tt + 1) * 128], wd[p][:, m, hf * 512:(hf + 1) * 512],
                                    m == 0, m == 3, r=["act%d_%d" % (ap_, m), "wd%d_0" % p, "wd%d_1" % p], w=[pok])
                        ys = yacc[:, t, hf * 512:(hf + 1) * 512]
                        self.P("dve", "scalar_tensor_tensor", ys, po, gts[:, t, e:e + 1], ys, ALU.mult, ALU.add,
                               r=[pok, "gts", "y%d" % t], w=["y%d" % t])
        if not final:
            for t in range(NT):
                kb.dma(self.xres[b, t * 128:(t + 1) * 128, :], yacc[:, t, :], reads=["y%d" % t], writes=["xres"])
        else:
            kb.barrier()
            gf = wg[0].rearrange("p k n -> p (k n)").bitcast(F32)[:, 0:D]
            junk = wu[0].rearrange("p k n -> p (k n)").bitcast(F32)[:, 0:D]
            ot = [wd[0].rearrange("p k n -> p (k n)").bitcast(F32)[:, 0:D], wd[1].rearrange("p k n -> p (k n)").bitcast(F32)[:, 0:D]]
            stf = sl[0]
            kb.dma(gf, self.final_g.partition_broadcast(128), writes=["wg0_0", "wg0_1", "gf"])
            for t in range(NT):
                p = t % 2
                self.rmsnorm(yacc[:, t, :], "y%d" % t, gf, "gf", ot[p], "ot%d" % p, junk, "fjunk",
                             stf[:, 4 * t:4 * t + 4], "sl0")
                kb.dma(self.out[b, t * 128:(t + 1) * 128, :], ot[p], reads=["ot%d" % p], writes=["out"])

    def phaseE_sparse(self, l, final):
        kb = self.kb
        pb = self.pb
        I32 = mybir.dt.int32
        NTT, NBLK = self.NTT, self.NBLK
        IO = bass.IndirectOffsetOnAxis
        a = Arena(self.arena_ap, self.r1_end, 208000)
        R = a.t([128, NTT, 66])
        OHs = a.t([128, NTT, 32])
        rk = a.t([128, NTT, 32])
        tmp = a.t([128, NTT, 32])
        cnt = a.t([128, 32]); nblk = a.t([128, 32]); pend = a.t([128, 32]); pstart = a.t([128, 32])
        cmp_ = a.t([128, max(NBLK, 32), 32])
        sl_f = a.t([128, NTT, 2])
        sl_i = a.t([128, NTT, 2], I32)
        be_f = a.t([128, NBLK])
        widx = a.t([128, NBLK], I32)
        stg = [a.t([128, 4096]) for _ in range(3)]
        wgb = [a.t([128, 8, EH], BF16) for _ in range(2)]
        wub = [a.t([128, 8, EH], BF16) for _ in range(2)]
        wdb = [a.t([128, 4, D], BF16) for _ in range(2)]
        xblk = [a.t([128, D], BF16) for _ in range(2)]
        xT = [a.t([128, 8, 128], BF16) for _ in range(2)]
        ssb = a.t([128, 512]); actb = a.t([128, 512], BF16); actT = a.t([128, 4, 128], BF16)
        yblk = [a.t([128, D]) for _ in range(2)]
        pbT = pb[7][:, 0:512].bitcast(BF16)
        for b in range(self.NB):
            kb.dma(R[:, b * NT:(b + 1) * NT, :], self.route[b].rearrange("(t p) c -> p t c", p=128), writes=["R"])
        self.P("dve", "tensor_tensor", OHs, R[:, :, 0:32], R[:, :, 32:64], ALU.add, r=["R"], w=["OHs"])
        for t in range(NTT):
            bank = pb[t // 16]
            bk = "pb%d" % (t // 16)
            cs = slice((t % 16) * 32, (t % 16 + 1) * 32)
            for t2 in range(t):
                self.mm(bank[:, cs], self.ones_f, OHs[:, t2, :], t2 == 0, False, r=["ones_f", "OHs"], w=[bk])
            self.mm(bank[:, cs], self.m_strict, OHs[:, t, :], t == 0, True, r=["cst", "OHs"], w=[bk])
        for t in range(NTT):
            self.mm(pb[2][:, 0:32], self.ones_f, OHs[:, t, :], t == 0, t == NTT - 1, r=["ones_f", "OHs"], w=["pb2"])
        for g in range(NTT // 16):
            self.actf(rk[:, g * 16:(g + 1) * 16, :].rearrange("p t e -> p (t e)"), pb[g], AF.Copy, r=["pb%d" % g], w=["rk"])
        self.P("dve", "tensor_copy", cnt, pb[2][:, 0:32], r=["pb2"], w=["cnt"])
        self.P("dve", "tensor_tensor", cmp_[:, 0:32, :], cnt.unsqueeze(2).to_broadcast([128, 32, 32]),
               self.thr[:, 0:32].unsqueeze(1).to_broadcast([128, 32, 32]), ALU.is_gt, r=["cnt", "cex"], w=["cmp"])
        self.P("dve", "tensor_reduce", nblk, cmp_[:, 0:32, :], AX.X, ALU.add, r=["cmp"], w=["nblk"])
        self.P("dve", "tensor_single_scalar", nblk, nblk, 128.0, ALU.mult, r=["nblk"], w=["nblk"])
        self.P("dve", "tensor_tensor_scan", pend, self.ones_f[:, 0:32], nblk, 0.0, ALU.mult, ALU.add, r=["ones_f", "nblk"], w=["pend"])
        self.P("dve", "tensor_tensor", pstart, pend, nblk, ALU.subtract, r=["pend", "nblk"], w=["pstart"])
        self.P("dve", "tensor_tensor", rk, rk, pstart.unsqueeze(1).to_broadcast([128, NTT, 32]), ALU.add, r=["rk", "pstart"], w=["rk"])
        for k in range(2):
            self.P("dve", "tensor_tensor", tmp, rk, R[:, :, k * 32:(k + 1) * 32], ALU.mult, r=["rk", "R"], w=["tmp"])
            self.P("dve", "tensor_reduce", sl_f[:, :, k], tmp, AX.X, ALU.add, r=["tmp"], w=["sl_f"])
        self.P("dve", "tensor_copy", sl_i, sl_f, r=["sl_f"], w=["sl_i"])
        self.P("dve", "tensor_tensor", cmp_[:, 0:NBLK, :], self.thr[:, 0:NBLK].unsqueeze(2).to_broadcast([128, NBLK, 32]),
               pend.unsqueeze(1).to_broadcast([128, NBLK, 32]), ALU.is_ge, r=["cex", "pend"], w=["cmp"])
        self.P("dve", "tensor_reduce", be_f, cmp_[:, 0:NBLK, :], AX.X, ALU.add, r=["cmp"], w=["be_f"])
        self.P("dve", "tensor_scalar", be_f, be_f, 31.0, 128.0, ALU.min, ALU.mult, r=["be_f"], w=["be_f"])
        self.P("dve", "tensor_scalar", be_f, be_f, self.iota_p, float(l * NE * 128), ALU.add, ALU.add, r=["be_f", "cex"], w=["be_f"])
        self.P("dve", "tensor_copy", widx, be_f, r=["be_f"], w=["widx"])
        if self.stop in ("E0", "E1", "E2"):
            kb.dma(self.dbg_sl, sl_i.rearrange("p t k -> p (t k)"), reads=["sl_i"], writes=["dbg_sl"])
            kb.dma(self.dbg_widx, widx, reads=["widx"], writes=["dbg_widx"])
            for i_, (tl, tk) in enumerate(((cnt, "cnt"), (nblk, "nblk"), (pend, "pend"), (pstart, "pstart"))):
                kb.dma(self.dbg_misc[:, i_, :], tl, reads=[tk], writes=["dbg_misc"])
        if self.stop == "E0":
            return
        for t in range(NTT):
            b, tt = t // NT, t % NT
            p = t % 2
            kb.dma(xblk[p], self.h2tok[b, tt * 128:(tt + 1) * 128, :], writes=["xblk%d" % p])
            for k in range(2):
                kb.idma(self.xb, IO(ap=sl_i[:, t, k:k + 1], axis=0), xblk[p], None,
                        reads=["xblk%d" % p, "sl_i"], writes=["xb"])
        if self.stop == "E1":
            return
        for j in range(NBLK):
            p = j % 2
            for wi, (wsrc, wdst, wk) in enumerate(((self.w_gate, wgb[p], "wgb%d" % p), (self.w_up, wub[p], "wub%d" % p),
                                                   (self.w_down, wdb[p], "wdb%d" % p))):
                kb.idma(stg[wi], None, wsrc.rearrange("l r c -> (l r) c"), IO(ap=widx[:, j:j + 1], axis=0), reads=["widx"], writes=["stg%d" % wi])
                dstv = wdst.rearrange("p k n -> p (k n)")
                if wi == 0:
                    self.actf(dstv, stg[wi], AF.Copy, r=["stg%d" % wi], w=[wk])
                elif wi == 1:
                    self.P("dve", "tensor_copy", dstv, stg[wi], r=["stg%d" % wi], w=[wk])
                else:
                    self.actf(dstv[:, 0:2048], stg[wi][:, 0:2048], AF.Copy, r=["stg%d" % wi], w=[wk])
                    self.P("dve", "tensor_copy", dstv[:, 2048:4096], stg[wi][:, 2048:4096], r=["stg%d" % wi], w=[wk])
            kb.dma(xblk[p], self.xb[j * 128:(j + 1) * 128, :], reads=["xb"], writes=["xblk%d" % p])
            for kc in range(8):
                self.P("pe", "transpose", pbT[:, kc * 128:(kc + 1) * 128], xblk[p][:, kc * 128:(kc + 1) * 128],
                       self.ident_bf, r=["xblk%d" % p, "ident_bf"], w=["pb7"])
            self.actf(xT[p].rearrange("p k n -> p (k n)"), pbT, AF.Copy, r=["pb7"], w=["xT%d" % p])
            pg, pu = pb[0 + 2 * p], pb[1 + 2 * p]
            pgk, puk = "pb%d" % (2 * p), "pb%d" % (1 + 2 * p)
            for kc in range(8):
                self.mm(pg, xT[p][:, kc, :], wgb[p][:, kc, :], kc == 0, kc == 7, r=["xT%d" % p, "wgb%d" % p], w=[pgk])
            for kc in range(8):
                self.mm(pu, xT[p][:, kc, :], wub[p][:, kc, :], kc == 0, kc == 7, r=["xT%d" % p, "wub%d" % p], w=[puk])
            self.actf(ssb, pg, AF.Silu, r=[pgk], w=["ssb"])
            self.P("dve", "tensor_tensor", actb, ssb, pu, ALU.mult, r=["ssb", puk], w=["actb"])
            for m in range(4):
                self.P("pe", "transpose", pbT[:, m * 128:(m + 1) * 128], actb[:, m * 128:(m + 1) * 128],
                       self.ident_bf, r=["actb", "ident_bf"], w=["pb7"])
            self.actf(actT.rearrange("p k n -> p (k n)"), pbT[:, 0:512], AF.Copy, r=["pb7"], w=["actT"])
            for hf in range(2):
                po, pok = pb[4 + hf], "pb%d" % (4 + hf)
                for m in range(4):
                    self.mm(po, actT[:, m, :], wdb[p][:, m, hf * 512:(hf + 1) * 512], m == 0, m == 3,
                            r=["actT", "wdb%d" % p], w=[pok])
                if hf == 0:
                    self.actf(yblk[p][:, 0:512], po, AF.Copy, r=[pok], w=["yblk%d" % p])
                else:
                    self.P("dve", "tensor_copy", yblk[p][:, 512:1024], po, r=[pok], w=["yblk%d" % p])
            kb.dma(self.yb[j * 128:(j + 1) * 128, :], yblk[p], reads=["yblk%d" % p], writes=["yb"])
        if self.stop == "E2":
            return
        xt = [stg[0][:, 0:D], stg[0][:, D:2 * D]]
        y1 = [stg[1][:, 0:D], stg[1][:, D:2 * D]]
        y2 = [stg[2][:, 0:D], stg[2][:, D:2 * D]]
        ot = [stg[0][:, 2 * D:3 * D], stg[0][:, 3 * D:4 * D]]
        junk = stg[1][:, 2 * D:3 * D]
        gf = stg[2][:, 2 * D:3 * D]
        stf = a.t([128, 4 * NTT])
        kb.barrier()
        if final:
            kb.dma(gf, self.final_g.partition_broadcast(128), writes=["gf"])
        for t in range(NTT):
            b, tt = t // NT, t % NT
            p = t % 2
            rows = slice(tt * 128, (tt + 1) * 128)
            kb.dma(xt[p], self.xres[b, rows, :], writes=["cxt%d" % p])
            kb.idma(y1[p], None, self.yb, IO(ap=sl_i[:, t, 0:1], axis=0), reads=["sl_i"], writes=["cy1%d" % p])
            kb.idma(y2[p], None, self.yb, IO(ap=sl_i[:, t, 1:2], axis=0), reads=["sl_i"], writes=["cy2%d" % p])
            self.P("dve", "scalar_tensor_tensor", xt[p], y1[p], R[:, t, 64:65], xt[p], ALU.mult, ALU.add,
                   r=["cy1%d" % p, "R", "cxt%d" % p], w=["cxt%d" % p])
            self.P("dve", "scalar_tensor_tensor", xt[p], y2[p], R[:, t, 65:66], xt[p], ALU.mult, ALU.add,
                   r=["cy2%d" % p, "R", "cxt%d" % p], w=["cxt%d" % p])
            if not final:
                kb.dma(self.xres[b, rows, :], xt[p], reads=["cxt%d" % p], writes=["xres"])
            else:
                self.rmsnorm(xt[p], "cxt%d" % p, gf, "gf", ot[p], "cot%d" % p, junk, "cjunk", stf[:, 4 * t:4 * t + 4], "stf")
                kb.dma(self.out[b, rows, :], ot[p], reads=["cot%d" % p], writes=["out"])

    def build(self):
        kb = self.kb
        if self.sparse:
            z = self.arena_ap[:, 45000:45000 + 512].bitcast(BF16)
            self.P("pool", "memset", z, 0.0, w=["zz"])
            for j in range(self.NBLK):
                kb.dma(self.xb[j * 128:(j + 1) * 128, :], z, reads=["zz"], writes=["xb"])
            kb.barrier()
        for l in range(self.NL):
            self.load_layer(l)
            for b in range(self.NB):
                src = self.x_in[b] if l == 0 else self.xres[b]
                self.phaseA(l, b, src)
                kb.barrier()
                if self.stop == "A":
                    return self.finish()
                self.phaseB_attn(l, b)
                kb.barrier()
                if self.stop == "B1":
                    return self.finish()
                self.phaseB_rwkv(l, b)
                kb.barrier()
                if self.stop == "B2":
                    return self.finish()
                self.phaseD(l, b, src)
                kb.barrier()
                if self.stop == "D":
                    return self.finish()
                if not self.sparse:
                    self.phaseE(l, b, final=(l == self.NL - 1))
                    kb.barrier()
            if self.sparse:
                self.phaseE_sparse(l, final=(l == self.NL - 1))
                kb.barrier()
        return self.finish()

    def finish(self):
        self.kb.barrier()
        return self


def prep_inputs(inp, b0, nb, NL=4):
    f = lambda a: np.ascontiguousarray(a, dtype=np.float32)

    def pm(w, k, n):
        L = w.shape[0]
        w5 = np.reshape(np.asarray(w, dtype=np.float32), (L, NE, k, 128, n))
        return np.ascontiguousarray(np.transpose(w5, (0, 1, 3, 2, 4))).reshape(L, NE * 128, k * n)
    m = {
        "x": f(inp["x"][b0:b0 + nb]),
        "consts": make_consts(),
        "norm1_g": f(inp["norm1_g"][:NL]),
        "w_in": f(inp["w_in"][:NL]),
        "gm_wT": f(np.transpose(inp["gm_w_s"][:NL], (0, 1, 3, 2))),
        "gm_bT": f(np.transpose(inp["gm_b"][:NL], (0, 2, 1))),
        "rw_mu": f(inp["rw_mu"][:NL]),
        "rw_pv": f(np.concatenate([
            np.transpose(np.reshape(inp["rw_mu"][:NL, 0:768], (NL, 12, 64)), (0, 2, 1))] + [
            np.transpose(np.reshape(np.reshape(inp[k][:NL], (NL, 256)), (NL, 4, 64)), (0, 2, 1))
            for k in ("rw_w0", "rw_a0", "rw_k_k", "rw_k_a", "rw_r_k")], axis=2)),
        "rw_w2": f(inp["rw_w2"][:NL]),
        "rw_a2": f(inp["rw_a2"][:NL]),
        "rw_g2": f(inp["rw_g2"][:NL]),
        "rw_gn_w": f(inp["rw_gn_w"][:NL]),
        "rw_gn_b": f(inp["rw_gn_b"][:NL]),
        "fx_b_f": f(inp["fx_b_f"][:NL]),
        "w_out": f(inp["w_out"][:NL]),
        "norm2_g": f(inp["norm2_g"][:NL]),
        "router_w": f(np.concatenate([inp["router_group_w"][:NL], inp["router_expert_w"][:NL]], axis=2)),
        "router_b": f(np.concatenate([inp["router_group_b"][:NL], inp["router_expert_b"][:NL]], axis=1)),
        "exp_w_gate": pm(inp["exp_w_gate"][:NL], 8, EH),
        "exp_w_up": pm(inp["exp_w_up"][:NL], 8, EH),
        "exp_w_down": pm(inp["exp_w_down"][:NL], 4, D),
        "final_norm_g": f(np.reshape(inp["final_norm_g"], (1, D))),
    }
    return m


def kernel(**inputs):
    inp = {k: np.asarray(v) for k, v in inputs.items()}
    n = 8
    nb = 2
    prog = Prog(NL=4, NB=nb).build()
    shared = prep_inputs(inp, 0, nb)
    in_maps = []
    for c in range(n):
        m = dict(shared)
        m["x"] = np.ascontiguousarray(inp["x"][c * nb:(c + 1) * nb], dtype=np.float32)
        in_maps.append(m)
    res = run_bass_kernel_spmd(prog.nc, in_maps, core_ids=list(range(n)))
    out = np.concatenate([np.asarray(r["out"]) for r in res.results], axis=0)
    return out.astype(np.float32)
```

```python
import numpy as np
import concourse.bass as bass
import concourse.mybir as mybir
from concourse.bass_utils import run_bass_kernel_spmd

F32 = mybir.dt.float32
BF16 = mybir.dt.bfloat16
AF = mybir.ActivationFunctionType
ALU = mybir.AluOpType
AX = mybir.AxisListType

T = 2048
D = 1024
NT = 16
DIN = 3076
NE = 32
EH = 512
NDMA_SEMS = 12
RMS_EPS = 1e-6
LN_EPS = 1e-5
GN_EPS = 64e-5
NEGM = -30000.0
import os
MAXOPS = int(os.environ.get("KB_MAXOPS", "1000000000"))


class KB:
    def __init__(self):
        self.nc = bass.Bass("TRN2", target_bir_lowering=False)
        nc = self.nc
        self.engs = {"pe": nc.tensor, "act": nc.scalar, "dve": nc.vector,
                     "pool": nc.gpsimd, "sp": nc.sync}
        self.sem = {}
        self.cnt = {}
        for e in self.engs:
            self.sem[e] = nc.alloc_semaphore("s_" + e)
            self.cnt[e] = 0
        self.dsem = {}
        self.dcnt = {}
        self.drr = {}
        for q in ("sp", "pool"):
            self.dsem[q] = [nc.alloc_semaphore("d_%s%d" % (q, i)) for i in range(NDMA_SEMS)]
            self.dcnt[q] = [0] * NDMA_SEMS
            self.drr[q] = 0
        self.waited = {e: {} for e in self.engs}
        self.lastw = {}
        self.readers = {}
        self.nins = 0

    def _semof(self, sk):
        if sk[0] == "c":
            return self.sem[sk[1]]
        return self.dsem[sk[1]][sk[2]]

    def _wait(self, eng, tok):
        sk, val = tok
        if eng == "pe" and sk == ("c", "pe"):
            return
        w = self.waited[eng]
        if w.get(sk, 0) >= val:
            return
        self.engs[eng].wait_ge(self._semof(sk), val)
        w[sk] = val
        self.nins += 1

    def _deps(self, eng, reads, writes):
        for k in reads:
            t = self.lastw.get(k)
            if t is not None:
                self._wait(eng, t)
        for k in writes:
            t = self.lastw.get(k)
            if t is not None:
                self._wait(eng, t)
            for t in self.readers.get(k, ()):
                self._wait(eng, t)

    def _record(self, tok, reads, writes):
        for k in reads:
            lst = self.readers.setdefault(k, [])
            lst[:] = [t for t in lst if t[0] != tok[0]]
            lst.append(tok)
        for k in writes:
            self.lastw[k] = tok
            self.readers[k] = []

    def op(self, eng, fn, reads=(), writes=()):
        self.nops = getattr(self, "nops", 0) + 1
        if self.nops > MAXOPS:
            return None
        pr = [k for k in reads if k.startswith("pb")]
        if pr:
            reads = [k for k in reads if not k.startswith("pb")]
            writes = list(writes) + pr
        self._deps(eng, reads, writes)
        ins = fn(self.engs[eng])
        self.cnt[eng] += 1
        ins.then_inc(self.sem[eng], 1)
        tok = (("c", eng), self.cnt[eng])
        self._record(tok, reads, writes)
        self.nins += 1
        return tok

    def dma(self, out, in_, reads=(), writes=(), q="sp", **kw):
        self.nops = getattr(self, "nops", 0) + 1
        if self.nops > MAXOPS:
            return None
        i = self.drr[q]
        self.drr[q] = (i + 1) % NDMA_SEMS
        sk = ("d", q, i)
        if self.dcnt[q][i] > 0:
            self._wait(q, (sk, self.dcnt[q][i]))
        self._deps(q, reads, writes)
        ins = self.engs[q].dma_start(out=out, in_=in_, **kw)
        self.dcnt[q][i] += 16
        ins.then_inc(self.dsem[q][i], 16)
        tok = (sk, self.dcnt[q][i])
        self._record(tok, reads, writes)
        self.nins += 1
        return tok

    def idma(self, out, out_off, in_, in_off, reads=(), writes=()):
        q = "pool"
        self.nops = getattr(self, "nops", 0) + 1
        if self.nops > MAXOPS:
            return None
        i = self.drr[q]
        self.drr[q] = (i + 1) % NDMA_SEMS
        sk = ("d", q, i)
        if self.dcnt[q][i] > 0:
            self._wait(q, (sk, self.dcnt[q][i]))
        self._deps(q, reads, writes)
        ins = self.nc.gpsimd.indirect_dma_start(out=out, out_offset=out_off, in_=in_, in_offset=in_off)
        self.dcnt[q][i] += 16
        ins.then_inc(self.dsem[q][i], 16)
        tok = (sk, self.dcnt[q][i])
        self._record(tok, reads, writes)
        self.nins += 1
        return tok

    def barrier(self):
        toks = [(("c", e), self.cnt[e]) for e in self.engs if self.cnt[e] > 0]
        for q in self.dsem:
            for i in range(NDMA_SEMS):
                if self.dcnt[q][i] > 0:
                    toks.append((("d", q, i), self.dcnt[q][i]))
        for e in self.engs:
            for t in toks:
                if t[0] == ("c", e):
                    continue
                self._wait(e, t)
        self.lastw = {}
        self.readers = {}

    def dram(self, name, shape, dt=F32, kind="Internal"):
        return self.nc.dram_tensor(name, list(shape), dt, kind=kind).ap()


def _dtsize(dt):
    return 2 if dt == BF16 else 4


class Arena:
    def __init__(self, base_ap_f32, start, end):
        self.A = base_ap_f32
        self.cur = start
        self.end = end

    def t(self, shape, dt=F32, parts=128):
        n = 1
        for s in shape[1:]:
            n *= s
        nbytes = (n * _dtsize(dt) + 31) // 32 * 32
        off = self.cur
        self.cur += nbytes
        assert self.cur <= self.end, ("arena overflow", self.cur, self.end)
        v = self.A[0:shape[0], off // 4:(off + nbytes) // 4]
        if dt != F32:
            v = v.bitcast(dt)
        v = v[:, 0:n]
        if len(shape) == 3:
            v = v.rearrange("p (a b) -> p a b", a=shape[1])
        elif len(shape) == 4:
            v = v.rearrange("p (a b c) -> p a b c", a=shape[1], b=shape[2])
        return v


def make_consts():
    j = np.arange(128)[:, None]
    t = np.arange(128)[None, :]
    c = np.zeros((128, 8, 128), np.float32)
    c[:, 0] = np.eye(128)
    c[:, 1] = np.where(j >= t, -1.0, 0.0)
    c[:, 2] = -1.0
    c[:, 3] = np.where(j < t, 0.0, NEGM)
    c[:, 4] = np.where(j <= t, 0.0, NEGM)
    c[:, 5] = np.where(j < t, 1.0, 0.0)
    c[:, 6] = np.where(j <= t, 1.0, 0.0)
    c[:, 7] = np.where(t < j, 1.0, 0.0)
    ex = np.zeros((128, 256), np.float32)
    ex[:, 0:128] = 128.0 * np.arange(128)[None, :]
    ex[:, 128] = np.arange(128)
    return np.concatenate([c.reshape(128, 1024), ex], axis=1)


class Prog:
    def __init__(self, NL=4, NB=2, dbg=(), stop=None, moe_experts=NE, sparse=True):
        self.NL, self.NB = NL, NB
        self.stop = stop
        self.moe_experts = moe_experts
        self.sparse = sparse
        self.NTT = NB * NT
        self.S = 256
        self.NBLK = NB * T * 2 // self.S + 32
        kb = self.kb = KB()
        nc = self.nc = kb.nc
        dk = lambda n: ("ExternalOutput" if n in dbg else "Internal")
        I = lambda n, s: kb.dram(n, s, F32, kind="ExternalInput")
        self.x_in = I("x", [NB, T, D])
        self.consts_d = I("consts", [128, 1280])
        self.norm1_g = I("norm1_g", [NL, D])
        self.w_in = I("w_in", [NL, D, DIN])
        self.gm_wT = I("gm_wT", [NL, 4, 128, 128])
        self.gm_bT = I("gm_bT", [NL, 128, 4])
        self.rw_mu = I("rw_mu", [NL, 1024])
        self.rw_pv = I("rw_pv", [NL, 64, 32])
        self.rw_w2 = I("rw_w2", [NL, 64, 256])
        self.rw_a2 = I("rw_a2", [NL, 64, 256])
        self.rw_g2 = I("rw_g2", [NL, 128, 256])
        self.rw_gn_w = I("rw_gn_w", [NL, 256])
        self.rw_gn_b = I("rw_gn_b", [NL, 256])
        self.fx_b_f = I("fx_b_f", [NL, 4])
        self.w_out = I("w_out", [NL, D, D])
        self.norm2_g = I("norm2_g", [NL, D])
        self.router_w = I("router_w", [NL, D, 36])
        self.router_b = I("router_b", [NL, 36])
        if sparse:
            self.w_gate = I("exp_w_gate", [NL, NE * 128, 4096])
            self.w_up = I("exp_w_up", [NL, NE * 128, 4096])
            self.w_down = I("exp_w_down", [NL, NE * 128, 4096])
        else:
            self.w_gate = I("exp_w_gate", [NL, NE, D, EH])
            self.w_up = I("exp_w_up", [NL, NE, D, EH])
            self.w_down = I("exp_w_down", [NL, NE, EH, D])
        self.final_g = I("final_norm_g", [1, D])
        self.out = kb.dram("out", [NB, T, D], F32, kind="ExternalOutput")
        S = lambda n, s, dt=F32: kb.dram(n, s, dt, kind=dk(n))
        self.xres = S("xres", [NB, T, D])
        self.qkT = S("qkT", [NB, 16, 64, T], BF16)
        self.vtok = S("vtok", [NB, T, 512], BF16)
        self.rwT = S("rwT", [NB, 1024, T])
        self.fgT = S("fgT", [NB, 4, T])
        self.cumd = S("cumd", [NB, 4, T])
        self.mixT = S("mixT", [NB, 1024, T], BF16)
        self.h2T = S("h2T", [NB, 1024, T], BF16)
        self.gates = S("gates", [NB, T, 32])
        self.h2tok = S("h2tok", [NB, T, D], BF16)
        self.route = S("route", [NB, T, 66])
        self.xb = S("xb", [self.NBLK * self.S, D], BF16)
        self.yb = S("yb", [self.NBLK * self.S, D])
        self.dbg_sl = S("dbg_sl", [128, self.NTT * 2], mybir.dt.int32)
        self.dbg_widx = S("dbg_widx", [128, self.NBLK], mybir.dt.int32)
        self.dbg_misc = S("dbg_misc", [128, 4, 32])
        arena = nc.alloc_sbuf_tensor("arena", [128, 52000], F32)
        self.arena_ap = arena[:]
        self.pb = [nc.alloc_psum_tensor("pb%d" % i, [128, 512], F32)[:] for i in range(8)]
        self.R1 = Arena(self.arena_ap, 0, 40000)
        self.setup_consts()

    def P(self, eng, meth, *a, r=(), w=(), **kw):
        return self.kb.op(eng, lambda e: getattr(e, meth)(*a, **kw), r, w)

    def mm(self, out, lhsT, rhs, start, stop, r=(), w=()):
        return self.kb.op("pe", lambda e: e.matmul(out, lhsT, rhs, start=start, stop=stop), r, w)

    def actf(self, out, in_, func, r=(), w=(), eng="act", **kw):
        return self.kb.op("act", lambda e: e.activation(out, in_, func, **kw), r, w)

    def col(self, dram_row):
        return dram_row.rearrange("(p o) -> p o", o=1)

    def setup_consts(self):
        kb = self.kb
        a = self.R1
        self.cst = a.t([128, 8, 128])
        kb.dma(self.cst.rearrange("p a b -> p (a b)"), self.consts_d[:, 0:1024], writes=["cst"])
        self.cex = a.t([128, 256])
        kb.dma(self.cex, self.consts_d[:, 1024:1280], writes=["cex"])
        self.thr = self.cex[:, 0:128]
        self.iota_p = self.cex[:, 128:129]
        self.m_strict = self.cst[:, 5, :]
        c = self.cst
        self.ident = c[:, 0, :]
        self.ntri = c[:, 1, :]
        self.negones = c[:, 2, :]
        self.mask2 = c[:, 5:7, :]
        self.m_incl = c[:, 6, :]
        self.m_lows = c[:, 7, :]
        self.ident_bf = a.t([128, 128], BF16)
        self.mbs_bf = a.t([128, 128], BF16)
        self.mbi_bf = a.t([128, 128], BF16)
        self.P("dve", "tensor_copy", self.ident_bf, c[:, 0, :], r=["cst"], w=["ident_bf"])
        self.P("dve", "tensor_copy", self.mbs_bf, c[:, 3, :], r=["cst"], w=["mbs_bf"])
        self.P("dve", "tensor_copy", self.mbi_bf, c[:, 4, :], r=["cst"], w=["mbi_bf"])
        self.ones_f = a.t([128, 128])
        self.P("pool", "memset", self.ones_f, 1.0, w=["ones_f"])
        self.ones_bf = a.t([128, 64], BF16)
        self.P("pool", "memset", self.ones_bf, 1.0, w=["ones_bf"])
        self.reset = a.t([64, 512])
        self.P("pool", "memset", self.reset, 1.0, w=["reset"])
        self.P("pool", "memset", self.reset.rearrange("p (c t) -> p c t", c=4)[:, :, 0:1], 0.0, w=["reset"])
        self.CK = ["cst", "ident_bf", "mbs_bf", "mbi_bf", "ones_f", "ones_bf", "reset", "ones4"]
        self.g1bc = a.t([128, D])
        self.g2bc = a.t([128, D])
        self.rbbc = a.t([128, 36])
        self.wr = a.t([128, 8, 36])
        self.gmw = a.t([128, 4, 128])
        self.gmb = a.t([128, 4])
        self.gnw = a.t([128, 256])
        self.gnb = a.t([128, 256])
        self.w2s = a.t([64, 256])
        self.a2s = a.t([64, 256])
        self.g2s = a.t([128, 256])
        self.pv = a.t([64, 40])
        self.mu_w = a.t([64, 1])
        self.mu_a = a.t([64, 1])
        self.mu_g = a.t([128, 1])
        self.fbf = a.t([4, 1])
        self.nfbf = a.t([4, 1])
        self.Mst = a.t([64, 4, 64])
        self.r1_end = a.cur

    def keepconsts(self):
        pass

    def load_layer(self, l):
        kb = self.kb
        bc = lambda row: row.partition_broadcast(128)
        kb.dma(self.g1bc, bc(self.norm1_g[l:l + 1, :]), writes=["g1bc"])
        kb.dma(self.g2bc, bc(self.norm2_g[l:l + 1, :]), writes=["g2bc"])
        kb.dma(self.rbbc, bc(self.router_b[l:l + 1, :]), writes=["rbbc"])
        kb.dma(self.wr, self.router_w[l].rearrange("(k p) n -> p k n", p=128), writes=["wr"])
        kb.dma(self.gmw, self.gm_wT[l].rearrange("g s t -> s g t"), writes=["gmw"])
        self.P("dve", "tensor_tensor", self.gmw, self.gmw, self.m_incl.unsqueeze(1).to_broadcast([128, 4, 128]),
               ALU.mult, r=["gmw"], w=["gmw"])
        kb.dma(self.gmb, self.gm_bT[l], writes=["gmb"])
        kb.dma(self.gnw, bc(self.rw_gn_w[l:l + 1, :]), writes=["gnw"])
        kb.dma(self.gnb, bc(self.rw_gn_b[l:l + 1, :]), writes=["gnb"])
        kb.dma(self.w2s, self.rw_w2[l], writes=["w2s"])
        kb.dma(self.a2s, self.rw_a2[l], writes=["a2s"])
        kb.dma(self.g2s, self.rw_g2[l], writes=["g2s"])
        kb.dma(self.pv[:, 0:32], self.rw_pv[l], writes=["pv"])
        self.P("dve", "tensor_scalar", self.pv[:, 32:36], self.pv[:, 24:28], -1.0, 1.0, ALU.mult, ALU.add,
               r=["pv"], w=["pv"])
        kb.dma(self.mu_w, self.col(self.rw_mu[l, 768:832]), writes=["mu_w"])
        kb.dma(self.mu_a, self.col(self.rw_mu[l, 832:896]), writes=["mu_a"])
        kb.dma(self.mu_g, self.col(self.rw_mu[l, 896:1024]), writes=["mu_g"])
        kb.dma(self.fbf, self.col(self.fx_b_f[l]), writes=["fbf"])
        self.P("dve", "tensor_single_scalar", self.nfbf, self.fbf, -1.0, ALU.mult, r=["fbf"], w=["nfbf"])

    def rmsnorm(self, xt, xk, gbc, gk, out, outk, tmp, tmpk, st, stk):
        self.actf(tmp, xt, AF.Square, r=[xk], w=[tmpk, stk], accum_out=st[:, 0:1])
        self.actf(st[:, 1:2], st[:, 0:1], AF.Ln, r=[stk], w=[stk], scale=1.0 / D, bias=RMS_EPS)
        self.actf(st[:, 2:3], st[:, 1:2], AF.Exp, r=[stk], w=[stk], scale=-0.5)
        self.P("dve", "scalar_tensor_tensor", out, xt, st[:, 2:3], gbc, ALU.mult, ALU.mult,
               r=[xk, stk, gk], w=[outk])

    def phaseA(self, l, b, src):
        kb = self.kb
        a = Arena(self.arena_ap, self.r1_end, 208000)
        win = a.t([128, 8, DIN], BF16)
        for kc in range(8):
            kb.dma(win[:, kc, :], self.w_in[l, kc * 128:(kc + 1) * 128, :], writes=["win%d" % kc], q="pool")
        WK = ["win%d" % kc for kc in range(8)]
        xt = [a.t([128, D]) for _ in range(2)]
        junk = a.t([128, D])
        hb = [a.t([128, D], BF16) for _ in range(2)]
        hT4 = [a.t([128, 8, 512], BF16) for _ in range(2)]
        st = [a.t([128, 4]) for _ in range(2)]
        vsb = [a.t([128, 512], BF16) for _ in range(2)]
        xg = a.t([128, 512]); x2 = a.t([128, 512]); u1 = a.t([128, 512]); sg = a.t([128, 512]); hid = a.t([128, 512])
        gst = a.t([128, 16])
        vc = a.t([128, 256]); sq = a.t([128, 256]); vn = a.t([128, 256]); tmpg = a.t([128, 256]); ygm = a.t([128, 256])
        ygT = [a.t([128, 2, 128], BF16) for _ in range(2)]
        stg_bf = [a.t([64, 512], BF16) for _ in range(4)]
        stg_f = [a.t([128, 512]) for _ in range(4)]
        pb = self.pb
        pbT = pb[7][:, 0:512].bitcast(BF16)
        fm = []
        for h in range(4):
            fm.append((h * 64, 64, "qk", h, 1.0))
            fm.append((256 + h * 64, 64, "qk", 4 + h, 0.125))
            fm.append((2304 + h * 64, 64, "qk", 8 + h, 1.0))
            fm.append((2560 + h * 64, 64, "qk", 12 + h, 0.125))
        for j in range(12):
            fm.append((1280 + j * 64, 64, "rw", j * 64, 1.0))
        fm.append((2048, 64, "rw", 768, 1.0))
        fm.append((2112, 64, "rw", 832, 1.0))
        fm.append((2176, 128, "rw", 896, 1.0))
        fm.append((3072, 4, "fg", 0, 1.0))
        ci = 0
        for g4 in range(4):
            hT = hT4[g4 % 2]
            hTk = "hT4_%d" % (g4 % 2)
            for tt in range(4):
                t = g4 * 4 + tt
                p = t % 2
                rows = slice(t * 128, (t + 1) * 128)
                kb.dma(xt[p], src[rows, :], writes=["xt%d" % p])
                self.rmsnorm(xt[p], "xt%d" % p, self.g1bc, "g1bc", hb[p], "hb%d" % p, junk, "junk", st[p], "st%d" % p)
                for kc in range(8):
                    self.P("pe", "transpose", pbT[:, kc * 128:(kc + 1) * 128], hb[p][:, kc * 128:(kc + 1) * 128],
                           self.ident_bf, r=["hb%d" % p, "ident_bf"], w=["pb7"])
                self.actf(hT[:, :, tt * 128:(tt + 1) * 128], pbT.rearrange("p (k n) -> p k n", k=8), AF.Copy,
                          r=["pb7"], w=[hTk])
                for (pbi, pc0, c0, n) in ((0, 0, 512, 512), (1, 0, 1024, 256), (1, 256, 2816, 256)):
                    for kc in range(8):
                        self.mm(pb[pbi][:, pc0:pc0 + n], hT[:, kc, tt * 128:(tt + 1) * 128], win[:, kc, c0:c0 + n],
                                kc == 0, kc == 7, r=[hTk, WK[kc]], w=["pb%d" % pbi])
                self.actf(vsb[p][:, 0:256], pb[0][:, 0:256], AF.Copy, r=["pb0"], w=["vsb%d" % p])
                self.actf(vsb[p][:, 256:512], pb[1][:, 256:512], AF.Copy, r=["pb1"], w=["vsb%d" % p])
                kb.dma(self.vtok[b, rows, :], vsb[p], reads=["vsb%d" % p], writes=["vtok"])
                self.P("dve", "tensor_copy", xg[:, 0:256], pb[0][:, 256:512], r=["pb0"], w=["xg"])
                self.P("dve", "tensor_copy", xg[:, 256:512], pb[1][:, 0:256], r=["pb1"], w=["xg"])
                self.actf(x2, xg, AF.Square, r=["xg"], w=["x2"])
                self.P("dve", "tensor_scalar", u1, x2, 0.044715, 1.0, ALU.mult, ALU.add, r=["x2"], w=["u1"])
                self.P("dve", "tensor_tensor", u1, u1, xg, ALU.mult, r=["u1", "xg"], w=["u1"])
                self.actf(sg, u1, AF.Sigmoid, r=["u1"], w=["sg"], scale=1.5957691216057308)
                self.P("dve", "tensor_tensor", hid, sg, xg, ALU.mult, r=["sg", "xg"], w=["hid"])
                hv = hid[:, 256:512].rearrange("p (g d) -> p g d", g=4)
                v3 = lambda ap: ap.rearrange("p (g d) -> p g d", g=4)
                bc4 = lambda ap: ap.unsqueeze(2).to_broadcast([128, 4, 64])
                self.P("dve", "tensor_reduce", gst[:, 0:4], hv, AX.X, ALU.add, r=["hid"], w=["gst"])
                self.P("dve", "tensor_single_scalar", gst[:, 4:8], gst[:, 0:4], -1.0 / 64, ALU.mult, r=["gst"], w=["gst"])
                self.P("dve", "tensor_tensor", v3(vc), hv, bc4(gst[:, 4:8]), ALU.add, r=["hid", "gst"], w=["vc"])
                self.actf(sq, vc, AF.Square, r=["vc"], w=["sq"])
                self.P("dve", "tensor_reduce", gst[:, 8:12], v3(sq), AX.X, ALU.add, r=["sq"], w=["gst"])
                self.actf(gst[:, 8:12], gst[:, 8:12], AF.Ln, r=["gst"], w=["gst"], scale=1.0 / 64, bias=LN_EPS)
                self.actf(gst[:, 12:16], gst[:, 8:12], AF.Exp, r=["gst"], w=["gst"], scale=-0.5)
                self.P("dve", "tensor_tensor", v3(vn), v3(vc), bc4(gst[:, 12:16]), ALU.mult, r=["vc", "gst"], w=["vn"])
                for g in range(4):
                    self.mm(pb[2][:, g * 64:(g + 1) * 64], self.gmw[:, g, :], vn[:, g * 64:(g + 1) * 64], True, True,
                            r=["gmw", "vn"], w=["pb2"])
                self.P("dve", "tensor_tensor", v3(tmpg), v3(pb[2][:, 0:256]), bc4(self.gmb), ALU.add,
                       r=["pb2", "gmb"], w=["tmpg"])
                self.P("dve", "tensor_tensor", ygm, tmpg, hid[:, 0:256], ALU.mult, r=["tmpg", "hid"], w=["ygm"])
                for c in range(2):
                    self.P("pe", "transpose", pb[2][:, 256 + c * 128:256 + (c + 1) * 128], ygm[:, c * 128:(c + 1) * 128],
                           self.ident, r=["ygm", "cst"], w=["pb2"])
                self.actf(ygT[p].rearrange("p c n -> p (c n)"), pb[2][:, 256:512], AF.Copy, r=["pb2"], w=["ygT%d" % p])
                kb.dma(self.mixT[b, 256:512, rows].rearrange("(c p) n -> p c n", p=128), ygT[p],
                       reads=["ygT%d" % p], writes=["mixT"])
            cols = slice(g4 * 512, (g4 + 1) * 512)
            for (c0, M, kind, dest, scale) in fm:
                pi = 3 + ci % 4
                si = ci % 4
                ci += 1
                pk = "pb%d" % pi
                for kc in range(8):
                    self.mm(pb[pi][0:M, :], win[:, kc, c0:c0 + M], hT[:, kc, :], kc == 0, kc == 7,
                            r=[hTk, WK[kc]], w=[pk])
                if kind == "qk":
                    self.actf(stg_bf[si], pb[pi][0:64, :], AF.Copy, r=[pk], w=["stgb%d" % si], scale=scale)
                    kb.dma(self.qkT[b, dest, :, cols], stg_bf[si], reads=["stgb%d" % si], writes=["qkT"])
                elif kind == "rw":
                    self.P("dve", "tensor_copy", stg_f[si][0:M, :], pb[pi][0:M, :], r=[pk], w=["stgf%d" % si])
                    kb.dma(self.rwT[b, dest:dest + M, cols], stg_f[si][0:M, :], reads=["stgf%d" % si], writes=["rwT"])
                else:
                    self.P("dve", "tensor_copy", stg_f[si][0:M, :], pb[pi][0:M, :], r=[pk], w=["stgf%d" % si])
                    kb.dma(self.fgT[b, :, cols], stg_f[si][0:M, :], reads=["stgf%d" % si], writes=["fgT"])

    def phaseB_attn(self, l, b):
        kb = self.kb
        pball = self.pb
        a = Arena(self.arena_ap, self.r1_end, 208000)
        fg = a.t([4, T]); fe = a.t([4, T]); cum = a.t([4, T])
        self.ones4 = a.t([4, T])
        self.P("pool", "memset", self.ones4, 1.0, w=["ones4"])
        ncum_all = a.t([128, 16, 4])
        NS = 2
        slots = []
        for s in range(NS):
            B = dict(qT=a.t([64, T], BF16), kT=a.t([64, T], BF16), V=a.t([128, 16, 64], BF16),
                     E=[a.t([128, 512]) for _ in range(2)], Lp=[a.t([128, 512]) for _ in range(2)],
                     At=[a.t([128, 512], BF16) for _ in range(2)], Slp=a.t([128, 512]),
                     ob=[a.t([64, 512], BF16) for _ in range(2)], rec=a.t([64, 512]), cumrow=a.t([1, T]))
            slots.append(B)
        kb.dma(fg, self.fgT[b], reads=["fgT"], writes=["fg"])
        self.actf(fe, fg, AF.Exp, r=["fg", "nfbf"], w=["fe"], scale=-1.0, bias=self.nfbf)
        self.actf(fe, fe, AF.Ln, r=["fe"], w=["fe"], bias=1.0)
        self.P("dve", "tensor_tensor_scan", cum, self.ones4, fe, 0.0, ALU.mult, ALU.subtract,
               r=["fe", "ones4"], w=["cum"])
        kb.dma(self.cumd[b], cum, reads=["cum"], writes=["cumd"])
        for kbk in range(16):
            self.P("pe", "transpose", pball[6][:, kbk * 4:(kbk + 1) * 4], cum[0:4, kbk * 128:(kbk + 1) * 128],
                   self.ident[0:4, 0:4], r=["cum", "cst"], w=["pb6"])
        self.actf(ncum_all.rearrange("p a b -> p (a b)"), pball[6][:, 0:64], AF.Copy, r=["pb6"], w=["ncum"], scale=-1.0)

        def head_gen(kind, h, s):
            B = slots[s]
            K = lambda n: "%s_s%d" % (n, s)
            pb = pball[4 * s:4 * s + 4]
            pk = ["pb%d" % (4 * s + i) for i in range(4)]
            qT, kT, V, Slp, rec, cumrow = B["qT"], B["kT"], B["V"], B["Slp"], B["rec"], B["cumrow"]
            qk_q = h if kind == "sb" else 8 + h
            qk_k = 4 + h if kind == "sb" else 12 + h
            vc0 = h * 64 if kind == "sb" else 256 + h * 64
            mrow = h * 64 if kind == "sb" else 768 + h * 64
            kb.dma(qT, self.qkT[b, qk_q], reads=["qkT"], writes=[K("qT")])
            kb.dma(kT, self.qkT[b, qk_k], reads=["qkT"], writes=[K("kT")])
            kb.dma(V, self.vtok[b, :, vc0:vc0 + 64].rearrange("(kb p) d -> p kb d", p=128),
                   reads=["vtok"], writes=[K("V")])
            if kind == "fx":
                kb.dma(cumrow, self.cumd[b, h:h + 1, :], reads=["cumd"], writes=[K("cumrow")])
            yield
            it = 0
            for c in range(4):
                Q0 = c * 512
                last = 4 * c + 3
                if kind == "sb":
                    self.P("pool", "memset", Slp, 0.0, w=[K("Slp")])
                    order = list(range(last, -1, -1))
                else:
                    order = list(range(0, last + 1))
                first_pv = True
                for kbk in order:
                    r_ = kbk - 4 * c
                    diag = r_ >= 0
                    col0 = max(r_, 0) * 128
                    i2 = it % 2
                    it += 1
                    E, Lp, At = B["E"][i2], B["Lp"][i2], B["At"][i2]
                    Ek, Lk, Ak = K("E%d" % i2), K("Lp%d" % i2), K("At%d" % i2)
                    kblk = kT[:, kbk * 128:(kbk + 1) * 128]
                    qs = lambda c_a, c_b: qT[:, Q0 + c_a:Q0 + c_b]
                    blocks = []
                    if diag:
                        blocks.append((col0, col0 + 128, True))
                        if col0 + 128 < 512:
                            blocks.append((col0 + 128, 512, False))
                    else:
                        blocks.append((0, 512, False))
                    if kind == "sb":
                        pz, pzk = (pb[0], pk[0]) if i2 == 0 else (pb[3], pk[3])
                        pa, pak = pb[1], pk[1]
                        for (lo, hi, dg) in blocks:
                            if dg:
                                self.mm(pz[:, lo:hi], self.ident_bf, self.mbs_bf, True, False, r=["ident_bf", "mbs_bf"], w=[pzk])
                            self.mm(pz[:, lo:hi], kblk, qs(lo, hi), not dg, True, r=[K("kT"), K("qT")], w=[pzk])
                        yield
                        self.actf(E[:, col0:512], pz[:, col0:512], AF.Exp, r=[pzk], w=[Ek])
                        yield
                        self.actf(Lp[:, col0:512], E[:, col0:512], AF.Ln, r=[Ek], w=[Lk], bias=1.0)
                        yield
                        for (lo, hi, dg) in blocks:
                            if dg:
                                self.mm(pa[:, lo:hi], self.ident_bf, self.mbs_bf, True, False, r=["ident_bf", "mbs_bf"], w=[pak])
                            self.mm(pa[:, lo:hi], kblk, qs(lo, hi), not dg, False, r=[K("kT"), K("qT")], w=[pak])
                            self.mm(pa[:, lo:hi], self.ntri, Lp[:, lo:hi], False, False, r=["cst", Lk], w=[pak])
                            self.mm(pa[:, lo:hi], self.negones, Slp[:, lo:hi], False, True, r=["cst", K("Slp")], w=[pak])
                        yield
                        self.actf(At[:, col0:512], pa[:, col0:512], AF.Exp, r=[pak], w=[Ak])
                        self.P("pool", "tensor_tensor", Slp[:, col0:512], Slp[:, col0:512], Lp[:, col0:512], ALU.add,
                               r=[K("Slp"), Lk], w=[K("Slp")])
                        yield
                        for (lo, hi, dg) in blocks:
                            self.mm(pb[2][0:64, lo:hi], V[:, kbk, :], At[:, lo:hi], first_pv, kbk == 0,
                                    r=[K("V"), Ak], w=[pk[2]])
                            first_pv = False
                        yield
                    else:
                        pz, pzk = (pb[0], pk[0]) if i2 == 0 else (pb[1], pk[1])
                        for (lo, hi, dg) in blocks:
                            if dg:
                                self.mm(pz[:, lo:hi], self.ident_bf, self.mbi_bf, True, False, r=["ident_bf", "mbi_bf"], w=[pzk])
                            self.mm(pz[:, lo:hi], kblk, qs(lo, hi), not dg, False, r=[K("kT"), K("qT")], w=[pzk])
                            self.mm(pz[:, lo:hi], self.ones_f[0:1, :], cumrow[0:1, Q0 + lo:Q0 + hi], False, True,
                                    r=["ones_f", K("cumrow")], w=[pzk])
                        yield
                        self.actf(At[:, col0:512], pz[:, col0:512], AF.Exp, r=[pzk, "ncum"], w=[Ak],
                                  bias=ncum_all[:, kbk, h:h + 1])
                        yield
                        for (lo, hi, dg) in blocks:
                            self.mm(pb[2][0:64, lo:hi], V[:, kbk, :], At[:, lo:hi], first_pv, dg,
                                    r=[K("V"), Ak], w=[pk[2]])
                            self.mm(pb[3][0:64, lo:hi], self.ones_bf, At[:, lo:hi], first_pv, dg,
                                    r=["ones_bf", Ak], w=[pk[3]])
                            first_pv = False
                        yield
                oi = c % 2
                ob, obk = B["ob"][oi], K("ob%d" % oi)
                if kind == "sb":
                    self.actf(ob, pb[2][0:64, :], AF.Copy, r=[pk[2]], w=[obk])
                else:
                    self.P("dve", "reciprocal", rec, pb[3][0:64, :], r=[pk[3]], w=[K("rec")])
                    yield
                    self.P("dve", "tensor_tensor", ob, pb[2][0:64, :], rec, ALU.mult, r=[pk[2], K("rec")], w=[obk])
                yield
                kb.dma(self.mixT[b, mrow:mrow + 64, Q0:Q0 + 512], ob, reads=[obk], writes=["mixT"])

        for kind in ("sb", "fx"):
            for hp in range(2):
                gens = [head_gen(kind, 2 * hp + s, s) for s in range(NS)]
                while gens:
                    for g in list(gens):
                        try:
                            next(g)
                        except StopIteration:
                            gens.remove(g)

    def phaseB_rwkv(self, l, b):
        kb = self.kb
        pball = self.pb
        a = Arena(self.arena_ap, self.r1_end, 208000)
        F = lambda parts=64: a.t([parts, 512])
        xw, xwp, xa, xap = F(), F(), F(), F()
        xgt, xgp = F(128), F(128)
        pv = self.pv
        NS = 2
        slots = []
        for s in range(NS):
            B = {}
            for nm in ("raw0", "raw1", "raw2", "prv0", "prv1", "prv2", "rm", "km", "vm", "sgd", "logd", "G", "av",
                       "kk", "kk2", "nrm", "kap", "tmpa", "kp", "bbv", "eG", "eGn", "eGx", "ktT", "btT", "kdT", "rtT", "rkr"):
                B[nm] = F()
            B["tok3"] = [a.t([128, 3, 64]) for _ in range(2)]
            B["sgt"] = [a.t([128, 72]) for _ in range(2)]
            B["ABm"] = [a.t([128, 4, 128]) for _ in range(2)]
            B["X"] = [a.t([128, 128]) for _ in range(2)]
            B["Y"] = [a.t([128, 128]) for _ in range(2)]
            B["R"] = [a.t([128, 128]) for _ in range(2)]
            for nm in ("Wn", "U", "ysb", "yn", "yj"):
                B[nm] = a.t([128, 64])
            B["mt"] = a.t([64, 64])
            B["yst"] = a.t([128, 8])
            B["ymix"] = [a.t([64, 512], BF16) for _ in range(2)]
            slots.append(B)
        for h in range(4):
            self.P("pool", "memset", self.Mst[:, h, :], 0.0, w=["M%d" % h])

        def load_shift(g4, dst, dstp, rows, nm, parts):
            c0 = g4 * 512
            kb.dma(dst[0:parts, :], self.rwT[b, rows, c0:c0 + 512], reads=["rwT"], writes=[nm])
            if g4 == 0:
                self.P("pool", "memset", dstp[0:parts, 0:1], 0.0, w=[nm + "p"])
                kb.dma(dstp[0:parts, 1:512], self.rwT[b, rows, 0:511], reads=["rwT"], writes=[nm + "p"])
            else:
                kb.dma(dstp[0:parts, :], self.rwT[b, rows, c0 - 1:c0 + 511], reads=["rwT"], writes=[nm + "p"])

        def mix(dst, x_, xp_, mu, nm, dk, parts=64):
            self.P("pool", "tensor_tensor", xp_[0:parts, :], xp_[0:parts, :], x_[0:parts, :], ALU.subtract,
                   r=[nm, nm + "p"], w=[nm + "p"])
            self.P("dve", "scalar_tensor_tensor", dst[0:parts, :], xp_[0:parts, :], mu, x_[0:parts, :], ALU.mult, ALU.add,
                   r=[nm, nm + "p", "pv", "mu_w", "mu_a", "mu_g"], w=[dk])

        def head_gen(g4, h, s):
            B = slots[s]
            K = lambda n: "%s_s%d" % (n, s)
            pb = pball[4 * s:4 * s + 4]
            pk = ["pb%d" % (4 * s + i) for i in range(4)]
            c0 = g4 * 512
            cols = slice(c0, c0 + 512)
            hc = slice(h * 64, (h + 1) * 64)
            Mh = self.Mst[:, h, :]
            Mk = "M%d" % h
            rm, km, vm = B["rm"], B["km"], B["vm"]
            sgd, logd, G, av = B["sgd"], B["logd"], B["G"], B["av"]
            kk, kk2, nrm, kap, tmpa, kp, bbv = B["kk"], B["kk2"], B["nrm"], B["kap"], B["tmpa"], B["kp"], B["bbv"]
            eG, eGn, eGx = B["eG"], B["eGn"], B["eGx"]
            ktT, btT, kdT, rtT, rkr = B["ktT"], B["btT"], B["kdT"], B["rtT"], B["rkr"]
            Wn, U, ysb, yn, yj, mt, yst = B["Wn"], B["U"], B["ysb"], B["yn"], B["yj"], B["mt"], B["yst"]
            X, Y, R = B["X"], B["Y"], B["R"]
            for j, (nm, dst) in enumerate((("r", rm), ("k", km), ("v", vm))):
                rows = slice((j * 4 + h) * 64, (j * 4 + h + 1) * 64)
                load_shift(g4, B["raw%d" % j], B["prv%d" % j], rows, K("raw%d" % j), 64)
                mix(dst, B["raw%d" % j], B["prv%d" % j], pv[:, j * 4 + h:j * 4 + h + 1], K("raw%d" % j), K(nm + "m"))
            yield
            self.mm(pb[0][0:64, :], self.w2s[:, hc], xw, True, True, r=["w2s", "xw"], w=[pk[0]])
            self.mm(pb[1][0:64, :], self.a2s[:, hc], xa, True, True, r=["a2s", "xa"], w=[pk[1]])
            self.P("dve", "tensor_single_scalar", kk, km, pv[:, 20 + h:21 + h], ALU.mult, r=[K("km"), "pv"], w=[K("kk")])
            self.P("pool", "tensor_tensor", kk2, kk, kk, ALU.mult, r=[K("kk")], w=[K("kk2")])
            yield
            self.actf(sgd, pb[0][0:64, :], AF.Sigmoid, r=[pk[0], "pv"], w=[K("sgd")], bias=pv[:, 12 + h:13 + h])
            self.actf(av, pb[1][0:64, :], AF.Sigmoid, r=[pk[1], "pv"], w=[K("av")], bias=pv[:, 16 + h:17 + h])
            self.mm(pb[2][0:64, :], self.ones_f[0:64, 0:64], kk2, True, True, r=["ones_f", K("kk2")], w=[pk[2]])
            yield
            self.P("dve", "tensor_single_scalar", logd, sgd, -0.6065306597126334, ALU.mult, r=[K("sgd")], w=[K("logd")])
            self.actf(nrm, pb[2][0:64, :], AF.Sqrt, r=[pk[2]], w=[K("nrm")])
            yield
            self.P("dve", "tensor_tensor_scan", G, self.reset, logd, 0.0, ALU.mult, ALU.add, r=["reset", K("logd")], w=[K("G")])
            self.P("dve", "tensor_single_scalar", nrm, nrm, 1e-12, ALU.max, r=[K("nrm")], w=[K("nrm")])
            yield
            self.P("dve", "reciprocal", nrm, nrm, r=[K("nrm")], w=[K("nrm")])
            self.actf(eG, G, AF.Exp, r=[K("G")], w=[K("eG")])
            self.actf(eGn, G, AF.Exp, r=[K("G")], w=[K("eGn")], scale=-1.0)
            self.P("dve", "tensor_tensor", logd, G, logd, ALU.subtract, r=[K("G"), K("logd")], w=[K("logd")])
            yield
            self.P("dve", "tensor_tensor", kap, kk, nrm, ALU.mult, r=[K("kk"), K("nrm")], w=[K("kap")])
            self.actf(eGx, logd, AF.Exp, r=[K("logd")], w=[K("eGx")])
            self.P("dve", "tensor_scalar", tmpa, av, pv[:, 24 + h:25 + h], pv[:, 32 + h:33 + h], ALU.mult, ALU.add,
                   r=[K("av"), "pv"], w=[K("tmpa")])
            yield
            self.P("pool", "tensor_tensor", kp, km, tmpa, ALU.mult, r=[K("km"), K("tmpa")], w=[K("kp")])
            self.P("pool", "tensor_tensor", bbv, kap, av, ALU.mult, r=[K("kap"), K("av")], w=[K("bbv")])
            self.P("dve", "tensor_tensor", ktT, kap, eGx, ALU.mult, r=[K("kap"), K("eGx")], w=[K("ktT")])
            self.P("pool", "tensor_tensor", rtT, rm, eG, ALU.mult, r=[K("rm"), K("eG")], w=[K("rtT")])
            yield
            self.P("pool", "tensor_tensor", btT, bbv, eGn, ALU.mult, r=[K("bbv"), K("eGn")], w=[K("btT")])
            self.P("dve", "tensor_tensor", kdT, kp, eGn, ALU.mult, r=[K("kp"), K("eGn")], w=[K("kdT")])
            self.P("dve", "scalar_tensor_tensor", rkr, rm, pv[:, 28 + h:29 + h], kp, ALU.mult, ALU.mult,
                   r=[K("rm"), K("kp"), "pv"], w=[K("rkr")])
            yield
            for cc in range(4):
                u2 = cc % 2
                cs = slice(cc * 128, (cc + 1) * 128)
                t3, t3k = B["tok3"][u2], K("tok3_%d" % u2)
                sgtk = K("sgt%d" % u2)
                for j, (srcT, sk) in enumerate(((btT, K("btT")), (kdT, K("kdT")), (vm, K("vm")))):
                    self.P("pe", "transpose", pb[0][:, j * 64:(j + 1) * 64], srcT[:, cs], self.ident[0:64, 0:64],
                           r=[sk, "cst"], w=[pk[0]])
                self.mm(pb[0][:, 192:193], rkr[:, cs], self.ones_f[0:64, 0:1], True, True, r=[K("rkr"), "ones_f"], w=[pk[0]])
                self.mm(pb[0][:, 200:264], xgt[:, cs], self.g2s[:, hc], True, True, r=["xg", "g2s"], w=[pk[0]])
                for j, (lT, lk, rT, rk_) in enumerate(((btT, K("btT"), ktT, K("ktT")), (btT, K("btT"), rtT, K("rtT")),
                                                       (kdT, K("kdT"), ktT, K("ktT")), (kdT, K("kdT"), rtT, K("rtT")))):
                    self.mm(pb[1][:, j * 128:(j + 1) * 128], lT[:, cs], rT[:, cs], True, True, r=[lk, rk_], w=[pk[1]])
                self.mm(pb[2][:, 0:128], ktT[:, cs], btT[:, cs], True, True, r=[K("ktT"), K("btT")], w=[pk[2]])
                yield
                self.actf(t3.rearrange("p a b -> p (a b)"), pb[0][:, 0:192], AF.Copy, r=[pk[0]], w=[t3k])
                self.P("dve", "tensor_copy", B["sgt"][u2], pb[0][:, 192:264], r=[pk[0]], w=[sgtk])
                bt_tok, kd_tok, v_tok = t3[:, 0, :], t3[:, 1, :], t3[:, 2, :]
                s_tok = B["sgt"][u2][:, 0:1]
                g_tok = B["sgt"][u2][:, 8:72]
                AB, ABk = B["ABm"][u2], K("AB%d" % u2)
                self.P("dve", "tensor_tensor", AB.rearrange("p (x y) t -> p x y t", x=2),
                       pb[1].rearrange("p (x y t) -> p x y t", x=2, y=2),
                       self.mask2.unsqueeze(1).to_broadcast([128, 2, 2, 128]), ALU.mult, r=[pk[1], "cst"], w=[ABk])
                Nm, BbT, AkT, BkT = AB[:, 0, :], AB[:, 1, :], AB[:, 2, :], AB[:, 3, :]
                self.P("dve", "tensor_tensor", Y[0], pb[2][:, 0:128], self.m_lows, ALU.mult, r=[pk[2], "cst"], w=[K("Y0")])
                yield
                self.P("pool", "tensor_tensor", R[0], self.ident, Nm, ALU.subtract, r=["cst", ABk], w=[K("R0")])
                Xc, Xk = Nm, ABk
                Yc, Yk = Y[0], K("Y0")
                Rc, Rk = R[0], K("R0")
                for i in range(1, 7):
                    xi = i % 2
                    if i < 6:
                        self.mm(pb[2][:, 128:256], Yc, Xc, True, True, r=[Yk, Xk], w=[pk[2]])
                    self.mm(pb[2][:, 256:384], Xc, Yc, True, True, r=[Xk, Yk], w=[pk[2]])
                    yield
                    if i < 6:
                        self.actf(X[xi], pb[2][:, 128:256], AF.Copy, r=[pk[2]], w=[K("X%d" % xi)])
                    self.P("dve", "tensor_copy", Y[xi], pb[2][:, 256:384], r=[pk[2]], w=[K("Y%d" % xi)])
                    if i < 6:
                        Xc, Xk = X[xi], K("X%d" % xi)
                    Yc, Yk = Y[xi], K("Y%d" % xi)
                    yield
                    self.mm(pb[3][:, 0:128], Yc, Rc, True, True, r=[Yk, Rk], w=[pk[3]])
                    yield
                    self.P("dve", "tensor_tensor", R[xi], pb[3][:, 0:128], Rc, ALU.add, r=[pk[3], Rk], w=[K("R%d" % xi)])
                    Rc, Rk = R[xi], K("R%d" % xi)
                    yield
                self.mm(pb[3][:, 128:192], ktT[:, cs], Mh, True, False, r=[K("ktT"), Mk], w=[pk[3]])
                self.mm(pb[3][:, 128:192], AkT, v_tok, False, True, r=[ABk, t3k], w=[pk[3]])
                yield
                self.actf(Wn, pb[3][:, 128:192], AF.Copy, r=[pk[3]], w=[K("Wn")], scale=-1.0)
                yield
                self.mm(pb[3][:, 192:256], Rc, Wn, True, True, r=[Rk, K("Wn")], w=[pk[3]])
                yield
                self.actf(U, pb[3][:, 192:256], AF.Copy, r=[pk[3]], w=[K("U")])
                yield
                self.mm(pb[3][:, 256:320], rtT[:, cs], Mh, True, False, r=[K("rtT"), Mk], w=[pk[3]])
                self.mm(pb[3][:, 256:320], BbT, U, False, False, r=[ABk, K("U")], w=[pk[3]])
                self.mm(pb[3][:, 256:320], BkT, v_tok, False, True, r=[ABk, t3k], w=[pk[3]])
                self.mm(pb[3][0:64, 320:384], bt_tok, U, True, False, r=[t3k, K("U")], w=[pk[3]])
                self.mm(pb[3][0:64, 320:384], kd_tok, v_tok, False, True, r=[t3k], w=[pk[3]])
                yield
                self.P("dve", "tensor_tensor", mt, pb[3][0:64, 320:384], Mh, ALU.add, r=[pk[3], Mk], w=[K("mt")])
                self.actf(ysb, pb[3][:, 256:320], AF.Identity, r=[pk[3]], w=[K("ysb"), K("yst")], accum_out=yst[:, 0:1])
                yield
                self.P("dve", "tensor_single_scalar", Mh, mt, eG[:, cc * 128 + 127:cc * 128 + 128], ALU.mult,
                       r=[K("mt"), K("eG")], w=[Mk])
                self.P("dve", "tensor_single_scalar", yst[:, 1:2], yst[:, 0:1], -1.0 / 64, ALU.mult, r=[K("yst")], w=[K("yst")])
                yield
                self.actf(yj, ysb, AF.Square, r=[K("ysb"), K("yst")], w=[K("yj"), K("yst")], bias=yst[:, 1:2], accum_out=yst[:, 2:3])
                yield
                self.actf(yst[:, 3:4], yst[:, 2:3], AF.Ln, r=[K("yst")], w=[K("yst")], scale=1.0 / 64, bias=GN_EPS)
                yield
                self.actf(yst[:, 4:5], yst[:, 3:4], AF.Exp, r=[K("yst")], w=[K("yst")], scale=-0.5)
                yield
                self.P("dve", "tensor_scalar", yn, ysb, yst[:, 1:2], yst[:, 4:5], ALU.add, ALU.mult, r=[K("ysb"), K("yst")], w=[K("yn")])
                yield
                self.P("dve", "tensor_tensor", yn, yn, self.gnw[:, hc], ALU.mult, r=[K("yn"), "gnw"], w=[K("yn")])
                yield
                self.P("dve", "tensor_tensor", yn, yn, self.gnb[:, hc], ALU.add, r=[K("yn"), "gnb"], w=[K("yn")])
                yield
                self.P("dve", "scalar_tensor_tensor", yn, v_tok, s_tok, yn, ALU.mult, ALU.add,
                       r=[K("yn"), t3k, sgtk], w=[K("yn")])
                yield
                self.P("dve", "tensor_tensor", yn, yn, g_tok, ALU.mult, r=[K("yn"), sgtk], w=[K("yn")])
                yield
                self.P("pe", "transpose", pb[0][0:64, 384:512], yn, self.ident, r=[K("yn"), "cst"], w=[pk[0]])
                yield
                ym, ymk = B["ymix"][g4 % 2], K("ymix%d" % (g4 % 2))
                self.actf(ym[:, cs], pb[0][0:64, 384:512], AF.Copy, r=[pk[0]], w=[ymk])
                yield
            kb.dma(self.mixT[b, 512 + h * 64:512 + (h + 1) * 64, cols], ym, reads=[ymk], writes=["mixT"])

        for g4 in range(4):
            load_shift(g4, xw, xwp, slice(768, 832), "xw", 64)
            load_shift(g4, xa, xap, slice(832, 896), "xa", 64)
            load_shift(g4, xgt, xgp, slice(896, 1024), "xg", 128)
            mix(xw, xw, xwp, self.mu_w, "xw", "xw")
            mix(xa, xa, xap, self.mu_a, "xa", "xa")
            mix(xgt, xgt, xgp, self.mu_g, "xg", "xg", 128)
            self.actf(xw, xw, AF.Tanh, r=["xw"], w=["xw"])
            self.actf(xgt, xgt, AF.Sigmoid, r=["xg"], w=["xg"])
            for hp in range(2):
                gens = [head_gen(g4, 2 * hp + s, s) for s in range(NS)]
                while gens:
                    for g in list(gens):
                        try:
                            next(g)
                        except StopIteration:
                            gens.remove(g)

    def phaseD(self, l, b, src):
        kb = self.kb
        pb = self.pb
        a = Arena(self.arena_ap, self.r1_end, 208000)
        wo = a.t([128, 8, D], BF16)
        for kc in range(8):
            kb.dma(wo[:, kc, :], self.w_out[l, kc * 128:(kc + 1) * 128, :], writes=["wo%d" % kc], q="pool")
        mx = [a.t([128, 8, 128], BF16) for _ in range(2)]
        xt = [a.t([128, D]) for _ in range(2)]
        xn = [a.t([128, D]) for _ in range(2)]
        h2 = [a.t([128, D]) for _ in range(2)]
        junk = a.t([128, D])
        st = [a.t([128, 4]) for _ in range(2)]
        hTf = a.t([128, 8, 128])
        hTb = [a.t([128, 8, 128], BF16) for _ in range(2)]
        lg = a.t([128, 36]); lm = a.t([128, 32]); l2 = a.t([128, 32]); oh1 = a.t([128, 32]); oh2 = a.t([128, 32])
        ohg = a.t([128, 4]); ge = a.t([128, 4]); rs = a.t([128, 16])
        gt = [a.t([128, 32]) for _ in range(2)]
        rt = [a.t([128, 66]) for _ in range(2)]
        h2b = [a.t([128, D], BF16) for _ in range(2)]
        for t in range(NT):
            p = t % 2
            rows = slice(t * 128, (t + 1) * 128)
            kb.dma(mx[p], self.mixT[b, :, rows].rearrange("(k p) n -> p k n", p=128), reads=["mixT"], writes=["mx%d" % p])
            kb.dma(xt[p], src[rows, :], reads=["xres"], writes=["xt%d" % p])
            for hf in range(2):
                for kc in range(8):
                    self.mm(pb[hf], mx[p][:, kc, :], wo[:, kc, hf * 512:(hf + 1) * 512], kc == 0, kc == 7,
                            r=["mx%d" % p, "wo%d" % kc], w=["pb%d" % hf])
                self.P("dve", "tensor_tensor", xn[p][:, hf * 512:(hf + 1) * 512], pb[hf], xt[p][:, hf * 512:(hf + 1) * 512],
                       ALU.add, r=["pb%d" % hf, "xt%d" % p], w=["xn%d" % p])
            kb.dma(self.xres[b, rows, :], xn[p], reads=["xn%d" % p], writes=["xres_w"])
            self.rmsnorm(xn[p], "xn%d" % p, self.g2bc, "g2bc", h2[p], "h2%d" % p, junk, "junk", st[p], "st%d" % p)
            for kc in range(8):
                pbi = 2 + kc // 4
                self.P("pe", "transpose", pb[pbi][:, (kc % 4) * 128:(kc % 4 + 1) * 128], h2[p][:, kc * 128:(kc + 1) * 128],
                       self.ident, r=["h2%d" % p, "cst"], w=["pb%d" % pbi])
            for hh in range(2):
                self.actf(hTf[:, hh * 4:(hh + 1) * 4, :].rearrange("p k n -> p (k n)"), pb[2 + hh], AF.Copy,
                          r=["pb%d" % (2 + hh)], w=["hTf"])
                self.P("dve", "tensor_copy", hTb[p][:, hh * 4:(hh + 1) * 4, :].rearrange("p k n -> p (k n)"), pb[2 + hh],
                       r=["pb%d" % (2 + hh)], w=["hTb%d" % p])
            if not self.sparse:
                kb.dma(self.h2T[b, :, rows].rearrange("(k p) n -> p k n", p=128), hTb[p], reads=["hTb%d" % p], writes=["h2T"])
            else:
                self.P("pool", "tensor_copy", h2b[p], h2[p], r=["h2%d" % p], w=["h2b%d" % p])
                kb.dma(self.h2tok[b, rows, :], h2b[p], reads=["h2b%d" % p], writes=["h2tok"])
            for kc in range(8):
                self.mm(pb[4][:, 0:36], hTf[:, kc, :], self.wr[:, kc, :], kc == 0, kc == 7, r=["hTf", "wr"], w=["pb4"])
            self.P("dve", "tensor_tensor", lg, pb[4][:, 0:36], self.rbbc, ALU.add, r=["pb4", "rbbc"], w=["lg"])
            self.P("dve", "tensor_reduce", rs[:, 0:1], lg[:, 0:4], AX.X, ALU.max, r=["lg"], w=["rs"])
            self.P("dve", "tensor_scalar", ohg, lg[:, 0:4], rs[:, 0:1], None, ALU.is_equal, r=["lg", "rs"], w=["ohg"])
            self.P("dve", "tensor_single_scalar", rs[:, 1:2], rs[:, 0:1], -1.0, ALU.mult, r=["rs"], w=["rs"])
            self.actf(ge, lg[:, 0:4], AF.Exp, r=["lg", "rs"], w=["ge", "rs"], bias=rs[:, 1:2], accum_out=rs[:, 2:3])
            self.P("dve", "reciprocal", rs[:, 3:4], rs[:, 2:3], r=["rs"], w=["rs"])
            self.P("dve", "tensor_scalar", ohg, ohg, -1.0, 1e30, ALU.add, ALU.mult, r=["ohg"], w=["ohg"])
            self.P("dve", "tensor_tensor", lm.rearrange("p (g e) -> p g e", g=4), lg[:, 4:36].rearrange("p (g e) -> p g e", g=4),
                   ohg.unsqueeze(2).to_broadcast([128, 4, 8]), ALU.add, r=["lg", "ohg"], w=["lm"])
            self.P("dve", "tensor_reduce", rs[:, 4:5], lm, AX.X, ALU.max, r=["lm"], w=["rs"])
            self.P("dve", "tensor_scalar", oh1, lm, rs[:, 4:5], None, ALU.is_equal, r=["lm", "rs"], w=["oh1"])
            self.P("dve", "scalar_tensor_tensor", l2, oh1, -1e30, lm, ALU.mult, ALU.add, r=["oh1", "lm"], w=["l2"])
            self.P("dve", "tensor_reduce", rs[:, 5:6], l2, AX.X, ALU.max, r=["l2"], w=["rs"])
            self.P("dve", "tensor_scalar", oh2, l2, rs[:, 5:6], None, ALU.is_equal, r=["l2", "rs"], w=["oh2"])
            self.P("dve", "tensor_tensor", rs[:, 6:7], rs[:, 5:6], rs[:, 4:5], ALU.subtract, r=["rs"], w=["rs"])
            self.actf(rs[:, 7:8], rs[:, 6:7], AF.Exp, r=["rs"], w=["rs"])
            self.P("dve", "tensor_single_scalar", rs[:, 8:9], rs[:, 7:8], 1.0, ALU.add, r=["rs"], w=["rs"])
            self.P("dve", "reciprocal", rs[:, 9:10], rs[:, 8:9], r=["rs"], w=["rs"])
            self.P("dve", "tensor_tensor", rs[:, 10:11], rs[:, 9:10], rs[:, 7:8], ALU.mult, r=["rs"], w=["rs"])
            self.P("dve", "tensor_tensor", rs[:, 11:12], rs[:, 9:10], rs[:, 3:4], ALU.mult, r=["rs"], w=["rs"])
            self.P("dve", "tensor_tensor", rs[:, 12:13], rs[:, 10:11], rs[:, 3:4], ALU.mult, r=["rs"], w=["rs"])
            self.P("dve", "tensor_single_scalar", gt[p], oh1, rs[:, 11:12], ALU.mult, r=["oh1", "rs"], w=["gt%d" % p])
            self.P("dve", "scalar_tensor_tensor", gt[p], oh2, rs[:, 12:13], gt[p], ALU.mult, ALU.add,
                   r=["oh2", "rs", "gt%d" % p], w=["gt%d" % p])
            if not self.sparse:
                kb.dma(self.gates[b, rows, :], gt[p], reads=["gt%d" % p], writes=["gates"])
            else:
                self.P("dve", "tensor_copy", rt[p][:, 0:32], oh1, r=["oh1"], w=["rt%d" % p])
                self.P("dve", "tensor_copy", rt[p][:, 32:64], oh2, r=["oh2"], w=["rt%d" % p])
                self.P("dve", "tensor_copy", rt[p][:, 64:66], rs[:, 11:13], r=["rs"], w=["rt%d" % p])
                kb.dma(self.route[b, rows, :], rt[p], reads=["rt%d" % p], writes=["route"])

    def phaseE(self, l, b, final):
        kb = self.kb
        pb = self.pb
        a = Arena(self.arena_ap, self.r1_end, 208000)
        yacc = a.t([128, NT, D])
        hT = a.t([128, 8, T], BF16)
        gts = a.t([128, NT, 32])
        wg = [a.t([128, 8, EH], BF16) for _ in range(2)]
        wu = [a.t([128, 8, EH], BF16) for _ in range(2)]
        wd = [a.t([128, 4, D], BF16) for _ in range(2)]
        sl = [a.t([128, 512]) for _ in range(2)]
        act = [a.t([128, 4, 512], BF16) for _ in range(2)]
        for t in range(NT):
            kb.dma(yacc[:, t, :], self.xres[b, t * 128:(t + 1) * 128, :], writes=["y%d" % t])
        for kc in range(8):
            kb.dma(hT[:, kc, :], self.h2T[b, kc * 128:(kc + 1) * 128, :], writes=["hT"])
        kb.dma(gts, self.gates[b].rearrange("(t p) e -> p t e", p=128), writes=["gts"])
        it = 0
        nstage = 0
        stg = [a.t([128, 4, 512]) for _ in range(2)]
        for e in range(self.moe_experts):
            p = e % 2
            for wi, (wsrc, wdst, wkey, nh) in enumerate(((self.w_gate, wg[p], "wg%d" % p, 2), (self.w_up, wu[p], "wu%d" % p, 2),
                                                           (self.w_down, wd[p], "wd%d" % p, 1))):
                for hh in range(2):
                    si = nstage % 2
                    nstage += 1
                    if nh == 2:
                        src_ap = wsrc[l, e, hh * 512:(hh + 1) * 512, :].rearrange("(k p) n -> p k n", p=128)
                        dst_ap = wdst[:, hh * 4:(hh + 1) * 4, :]
                        stv = stg[si]
                    else:
                        src_ap = wsrc[l, e, hh * 256:(hh + 1) * 256, :].rearrange("(k p) n -> p k n", p=128)
                        dst_ap = wdst[:, hh * 2:(hh + 1) * 2, :]
                        stv = stg[si].rearrange("p k n -> p (k n)").rearrange("p (k n) -> p k n", k=2)
                    kb.dma(stv, src_ap, writes=["stg%d" % si])
                    if nstage % 2 == 0:
                        self.actf(dst_ap, stv, AF.Copy, r=["stg%d" % si], w=[wkey + "_%d" % hh])
                    else:
                        self.P("pool", "tensor_copy", dst_ap, stv, r=["stg%d" % si], w=[wkey + "_%d" % hh])
            for g in range(4):
                ap_ = it % 2
                it += 1
                tcols = slice(g * 512, (g + 1) * 512)
                for m in range(4):
                    pg = pb[(2 * m) % 4]; pgk = "pb%d" % ((2 * m) % 4)
                    pu = pb[(2 * m) % 4 + 1]; puk = "pb%d" % ((2 * m) % 4 + 1)
                    for kc in range(8):
                        self.mm(pg, wg[p][:, kc, m * 128:(m + 1) * 128], hT[:, kc, tcols], kc == 0, kc == 7,
                                r=["wg%d_0" % p, "wg%d_1" % p, "hT"], w=[pgk])
                    for kc in range(8):
                        self.mm(pu, wu[p][:, kc, m * 128:(m + 1) * 128], hT[:, kc, tcols], kc == 0, kc == 7,
                                r=["wu%d_0" % p, "wu%d_1" % p, "hT"], w=[puk])
                    s_ = sl[m % 2]; sk = "sl%d" % (m % 2)
                    self.actf(s_, pg, AF.Silu, r=[pgk], w=[sk])
                    self.P("dve", "tensor_tensor", act[ap_][:, m, :], s_, pu, ALU.mult, r=[sk, puk], w=["act%d_%d" % (ap_, m)])
                for tt in range(4):
                    t = g * 4 + tt
                    for hf in range(2):
                        po = pb[4 + (tt * 2 + hf) % 4]; pok = "pb%d" % (4 + (tt * 2 + hf) % 4)
                        for m in range(4):
                            self.mm(po, act[ap_][:, m, tt * 128:(tt + 1) * 128], wd[p][:, m, hf * 512:(hf + 1) * 512],
                                    m == 0, m == 3, r=["act%d_%d" % (ap_, m), "wd%d_0" % p, "wd%d_1" % p], w=[pok])
                        ys = yacc[:, t, hf * 512:(hf + 1) * 512]
                        self.P("dve", "scalar_tensor_tensor", ys, po, gts[:, t, e:e + 1], ys, ALU.mult, ALU.add,
                               r=[pok, "gts", "y%d" % t], w=["y%d" % t])
        if not final:
            for t in range(NT):
                kb.dma(self.xres[b, t * 128:(t + 1) * 128, :], yacc[:, t, :], reads=["y%d" % t], writes=["xres"])
        else:
            kb.barrier()
            gf = wg[0].rearrange("p k n -> p (k n)").bitcast(F32)[:, 0:D]
            junk = wu[0].rearrange("p k n -> p (k n)").bitcast(F32)[:, 0:D]
            ot = [wd[0].rearrange("p k n -> p (k n)").bitcast(F32)[:, 0:D], wd[1].rearrange("p k n -> p (k n)").bitcast(F32)[:, 0:D]]
            stf = sl[0]
            kb.dma(gf, self.final_g.partition_broadcast(128), writes=["wg0_0", "wg0_1", "gf"])
            for t in range(NT):
                p = t % 2
                self.rmsnorm(yacc[:, t, :], "y%d" % t, gf, "gf", ot[p], "ot%d" % p, junk, "fjunk",
                             stf[:, 4 * t:4 * t + 4], "sl0")
                kb.dma(self.out[b, t * 128:(t + 1) * 128, :], ot[p], reads=["ot%d" % p], writes=["out"])

    def phaseE_sparse(self, l, final):
        kb = self.kb
        pb = self.pb
        I32 = mybir.dt.int32
        NTT, NBLK = self.NTT, self.NBLK
        IO = bass.IndirectOffsetOnAxis
        a = Arena(self.arena_ap, self.r1_end, 208000)
        R = a.t([128, NTT, 66])
        OHs = a.t([128, NTT, 32])
        rk = a.t([128, NTT, 32])
        tmp = a.t([128, NTT, 32])
        cnt = a.t([128, 32]); nblk = a.t([128, 32]); pend = a.t([128, 32]); pstart = a.t([128, 32])
        cmp_ = a.t([128, max(NBLK, 32), 32])
        sl_f = a.t([128, NTT, 2])
        sl_i = a.t([128, NTT, 2], I32)
        be_f = a.t([128, NBLK])
        widx = a.t([128, NBLK], I32)
        stg = [a.t([128, 4096]) for _ in range(3)]
        wgb = [a.t([128, 8, EH], BF16) for _ in range(2)]
        wub = [a.t([128, 8, EH], BF16) for _ in range(2)]
        wdb = [a.t([128, 4, D], BF16) for _ in range(2)]
        xblk = [a.t([128, D], BF16) for _ in range(2)]
        xT = [a.t([128, 8, 128], BF16) for _ in range(2)]
        ssb = a.t([128, 512]); actb = a.t([128, 512], BF16); actT = a.t([128, 4, 128], BF16)
        yblk = [a.t([128, D]) for _ in range(2)]
        pbT = pb[7][:, 0:512].bitcast(BF16)
        for b in range(self.NB):
            kb.dma(R[:, b * NT:(b + 1) * NT, :], self.route[b].rearrange("(t p) c -> p t c", p=128), writes=["R"])
        self.P("dve", "tensor_tensor", OHs, R[:, :, 0:32], R[:, :, 32:64], ALU.add, r=["R"], w=["OHs"])
        for t in range(NTT):
            bank = pb[t // 16]
            bk = "pb%d" % (t // 16)
            cs = slice((t % 16) * 32, (t % 16 + 1) * 32)
            for t2 in range(t):
                self.mm(bank[:, cs], self.ones_f, OHs[:, t2, :], t2 == 0, False, r=["ones_f", "OHs"], w=[bk])
            self.mm(bank[:, cs], self.m_strict, OHs[:, t, :], t == 0, True, r=["cst", "OHs"], w=[bk])
        for t in range(NTT):
            self.mm(pb[2][:, 0:32], self.ones_f, OHs[:, t, :], t == 0, t == NTT - 1, r=["ones_f", "OHs"], w=["pb2"])
        for g in range(NTT // 16):
            self.actf(rk[:, g * 16:(g + 1) * 16, :].rearrange("p t e -> p (t e)"), pb[g], AF.Copy, r=["pb%d" % g], w=["rk"])
        self.P("dve", "tensor_copy", cnt, pb[2][:, 0:32], r=["pb2"], w=["cnt"])
        SB_ = self.S
        self.P("dve", "tensor_single_scalar", cnt, cnt, 128.0 / SB_, ALU.mult, r=["cnt"], w=["cnt"])
        self.P("dve", "tensor_tensor", cmp_[:, 0:32, :], cnt.unsqueeze(2).to_broadcast([128, 32, 32]),
               self.thr[:, 0:32].unsqueeze(1).to_broadcast([128, 32, 32]), ALU.is_gt, r=["cnt", "cex"], w=["cmp"])
        self.P("dve", "tensor_reduce", nblk, cmp_[:, 0:32, :], AX.X, ALU.add, r=["cmp"], w=["nblk"])
        self.P("dve", "tensor_single_scalar", nblk, nblk, float(SB_), ALU.mult, r=["nblk"], w=["nblk"])
        self.P("dve", "tensor_tensor_scan", pend, self.ones_f[:, 0:32], nblk, 0.0, ALU.mult, ALU.add, r=["ones_f", "nblk"], w=["pend"])
        self.P("dve", "tensor_tensor", pstart, pend, nblk, ALU.subtract, r=["pend", "nblk"], w=["pstart"])
        self.P("dve", "tensor_tensor", rk, rk, pstart.unsqueeze(1).to_broadcast([128, NTT, 32]), ALU.add, r=["rk", "pstart"], w=["rk"])
        for k in range(2):
            self.P("dve", "tensor_tensor", tmp, rk, R[:, :, k * 32:(k + 1) * 32], ALU.mult, r=["rk", "R"], w=["tmp"])
            self.P("dve", "tensor_reduce", sl_f[:, :, k], tmp, AX.X, ALU.add, r=["tmp"], w=["sl_f"])
        self.P("dve", "tensor_copy", sl_i, sl_f, r=["sl_f"], w=["sl_i"])
        self.P("dve", "tensor_single_scalar", pstart, pend, 128.0 / SB_, ALU.mult, r=["pend", "rk"], w=["pstart"])
        self.P("dve", "tensor_tensor", cmp_[:, 0:NBLK, :], self.thr[:, 0:NBLK].unsqueeze(2).to_broadcast([128, NBLK, 32]),
               pstart.unsqueeze(1).to_broadcast([128, NBLK, 32]), ALU.is_ge, r=["cex", "pstart"], w=["cmp"])
        self.P("dve", "tensor_reduce", be_f, cmp_[:, 0:NBLK, :], AX.X, ALU.add, r=["cmp"], w=["be_f"])
        self.P("dve", "tensor_scalar", be_f, be_f, 31.0, 128.0, ALU.min, ALU.mult, r=["be_f"], w=["be_f"])
        self.P("dve", "tensor_scalar", be_f, be_f, self.iota_p, float(l * NE * 128), ALU.add, ALU.add, r=["be_f", "cex"], w=["be_f"])
        self.P("dve", "tensor_copy", widx, be_f, r=["be_f"], w=["widx"])
        if self.stop in ("E0", "E1", "E2"):
            kb.dma(self.dbg_sl, sl_i.rearrange("p t k -> p (t k)"), reads=["sl_i"], writes=["dbg_sl"])
            kb.dma(self.dbg_widx, widx, reads=["widx"], writes=["dbg_widx"])
            for i_, (tl, tk) in enumerate(((cnt, "cnt"), (nblk, "nblk"), (pend, "pend"), (pstart, "pstart"))):
                kb.dma(self.dbg_misc[:, i_, :], tl, reads=[tk], writes=["dbg_misc"])
        if self.stop == "E0":
            return
        for t in range(NTT):
            b, tt = t // NT, t % NT
            p = t % 2
            kb.dma(xblk[p], self.h2tok[b, tt * 128:(tt + 1) * 128, :], writes=["xblk%d" % p])
            for k in range(2):
                kb.idma(self.xb, IO(ap=sl_i[:, t, k:k + 1], axis=0), xblk[p], None,
                        reads=["xblk%d" % p, "sl_i"], writes=["xb"])
        if self.stop == "E1":
            return
        ssb2 = [ssb, a.t([128, 512])]
        actb2 = [actb, a.t([128, 512], BF16)]
        actT2 = [actT, a.t([128, 4, 128], BF16)]

        def load_weights(j):
            p = j % 2
            for wi, (wsrc, wdst, wk) in enumerate(((self.w_gate, wgb[p], "wgb%d" % p), (self.w_up, wub[p], "wub%d" % p),
                                                   (self.w_down, wdb[p], "wdb%d" % p))):
                kb.idma(stg[wi], None, wsrc.rearrange("l r c -> (l r) c"), IO(ap=widx[:, j:j + 1], axis=0), reads=["widx"], writes=["stg%d" % wi])
                dstv = wdst.rearrange("p k n -> p (k n)")
                if wi == 0:
                    self.actf(dstv, stg[wi], AF.Copy, r=["stg%d" % wi], w=[wk])
                elif wi == 1:
                    self.P("dve", "tensor_copy", dstv, stg[wi], r=["stg%d" % wi], w=[wk])
                else:
                    self.actf(dstv[:, 0:2048], stg[wi][:, 0:2048], AF.Copy, r=["stg%d" % wi], w=[wk])
                    self.P("dve", "tensor_copy", dstv[:, 2048:4096], stg[wi][:, 2048:4096], r=["stg%d" % wi], w=[wk])

        def sub_gen(j, sub, q):
            p = j % 2
            row0 = j * self.S + sub * 128
            pT = pb[6 + q][:, 0:512].bitcast(BF16)
            pTk = "pb%d" % (6 + q)
            pg, pu, po = pb[2 * q], pb[2 * q + 1], pb[4 + q]
            pgk, puk, pok = "pb%d" % (2 * q), "pb%d" % (2 * q + 1), "pb%d" % (4 + q)
            kb.dma(xblk[q], self.xb[row0:row0 + 128, :], reads=["xb"], writes=["xblk%d" % q])
            yield
            for kc in range(8):
                self.P("pe", "transpose", pT[:, kc * 128:(kc + 1) * 128], xblk[q][:, kc * 128:(kc + 1) * 128],
                       self.ident_bf, r=["xblk%d" % q, "ident_bf"], w=[pTk])
            yield
            self.actf(xT[q].rearrange("p k n -> p (k n)"), pT, AF.Copy, r=[pTk], w=["xT%d" % q])
            yield
            for kc in range(8):
                self.mm(pg, xT[q][:, kc, :], wgb[p][:, kc, :], kc == 0, kc == 7, r=["xT%d" % q, "wgb%d" % p], w=[pgk])
            for kc in range(8):
                self.mm(pu, xT[q][:, kc, :], wub[p][:, kc, :], kc == 0, kc == 7, r=["xT%d" % q, "wub%d" % p], w=[puk])
            yield
            self.actf(ssb2[q], pg, AF.Silu, r=[pgk], w=["ssb%d" % q])
            yield
            self.P("dve", "tensor_tensor", actb2[q], ssb2[q], pu, ALU.mult, r=["ssb%d" % q, puk], w=["actb%d" % q])
            yield
            for m in range(4):
                self.P("pe", "transpose", pT[:, m * 128:(m + 1) * 128], actb2[q][:, m * 128:(m + 1) * 128],
                       self.ident_bf, r=["actb%d" % q, "ident_bf"], w=[pTk])
            yield
            self.actf(actT2[q].rearrange("p k n -> p (k n)"), pT[:, 0:512], AF.Copy, r=[pTk], w=["actT%d" % q])
            yield
            for hf in range(2):
                for m in range(4):
                    self.mm(po, actT2[q][:, m, :], wdb[p][:, m, hf * 512:(hf + 1) * 512], m == 0, m == 3,
                            r=["actT%d" % q, "wdb%d" % p], w=[pok])
                yield
                if hf == 0:
                    self.actf(yblk[q][:, 0:512], po, AF.Copy, r=[pok], w=["yblk%d" % q])
                else:
                    self.P("dve", "tensor_copy", yblk[q][:, 512:1024], po, r=[pok], w=["yblk%d" % q])
                yield
            kb.dma(self.yb[row0:row0 + 128, :], yblk[q], reads=["yblk%d" % q], writes=["yb"])

        def run2(gens):
            gens = list(gens)
            while gens:
                for g in list(gens):
                    try:
                        next(g)
                    except StopIteration:
                        gens.remove(g)

        load_weights(0)
        nsub = self.S // 128
        for j in range(NBLK):
            for pr in range(nsub // 2):
                run2([sub_gen(j, 2 * pr + q, q) for q in range(2)])
                if pr == 0 and j + 1 < NBLK:
                    load_weights(j + 1)
        if self.stop == "E2":
            return
        xt = [stg[0][:, 0:D], stg[0][:, D:2 * D]]
        y1 = [stg[1][:, 0:D], stg[1][:, D:2 * D]]
        y2 = [stg[2][:, 0:D], stg[2][:, D:2 * D]]
        ot = [stg[0][:, 2 * D:3 * D], stg[0][:, 3 * D:4 * D]]
        junk = stg[1][:, 2 * D:3 * D]
        gf = stg[2][:, 2 * D:3 * D]
        stf = a.t([128, 4 * NTT])
        kb.barrier()
        if final:
            kb.dma(gf, self.final_g.partition_broadcast(128), writes=["gf"])
        for t in range(NTT):
            b, tt = t // NT, t % NT
            p = t % 2
            rows = slice(tt * 128, (tt + 1) * 128)
            kb.dma(xt[p], self.xres[b, rows, :], writes=["cxt%d" % p])
            kb.idma(y1[p], None, self.yb, IO(ap=sl_i[:, t, 0:1], axis=0), reads=["sl_i"], writes=["cy1%d" % p])
            kb.idma(y2[p], None, self.yb, IO(ap=sl_i[:, t, 1:2], axis=0), reads=["sl_i"], writes=["cy2%d" % p])
            self.P("dve", "scalar_tensor_tensor", xt[p], y1[p], R[:, t, 64:65], xt[p], ALU.mult, ALU.add,
                   r=["cy1%d" % p, "R", "cxt%d" % p], w=["cxt%d" % p])
            self.P("dve", "scalar_tensor_tensor", xt[p], y2[p], R[:, t, 65:66], xt[p], ALU.mult, ALU.add,
                   r=["cy2%d" % p, "R", "cxt%d" % p], w=["cxt%d" % p])
            if not final:
                kb.dma(self.xres[b, rows, :], xt[p], reads=["cxt%d" % p], writes=["xres"])
            else:
                self.rmsnorm(xt[p], "cxt%d" % p, gf, "gf", ot[p], "cot%d" % p, junk, "cjunk", stf[:, 4 * t:4 * t + 4], "stf")
                kb.dma(self.out[b, rows, :], ot[p], reads=["cot%d" % p], writes=["out"])

    def build(self):
        kb = self.kb
        if self.sparse:
            z = self.arena_ap[:, 45000:45000 + 512].bitcast(BF16)
            self.P("pool", "memset", z, 0.0, w=["zz"])
            for j in range(self.NBLK * self.S // 128):
                kb.dma(self.xb[j * 128:(j + 1) * 128, :], z, reads=["zz"], writes=["xb"])
            kb.barrier()
        for l in range(self.NL):
            self.load_layer(l)
            for b in range(self.NB):
                src = self.x_in[b] if l == 0 else self.xres[b]
                self.phaseA(l, b, src)
                kb.barrier()
                if self.stop == "A":
                    return self.finish()
                self.phaseB_attn(l, b)
                kb.barrier()
                if self.stop == "B1":
                    return self.finish()
                self.phaseB_rwkv(l, b)
                kb.barrier()
                if self.stop == "B2":
                    return self.finish()
                self.phaseD(l, b, src)
                kb.barrier()
                if self.stop == "D":
                    return self.finish()
                if not self.sparse:
                    self.phaseE(l, b, final=(l == self.NL - 1))
                    kb.barrier()
            if self.sparse:
                self.phaseE_sparse(l, final=(l == self.NL - 1))
                kb.barrier()
        return self.finish()

    def finish(self):
        self.kb.barrier()
        return self


def prep_inputs(inp, b0, nb, NL=4):
    f = lambda a: np.ascontiguousarray(a, dtype=np.float32)

    def pm(w, k, n):
        L = w.shape[0]
        w5 = np.reshape(np.asarray(w, dtype=np.float32), (L, NE, k, 128, n))
        return np.ascontiguousarray(np.transpose(w5, (0, 1, 3, 2, 4))).reshape(L, NE * 128, k * n)
    m = {
        "x": f(inp["x"][b0:b0 + nb]),
        "consts": make_consts(),
        "norm1_g": f(inp["norm1_g"][:NL]),
        "w_in": f(inp["w_in"][:NL]),
        "gm_wT": f(np.transpose(inp["gm_w_s"][:NL], (0, 1, 3, 2))),
        "gm_bT": f(np.transpose(inp["gm_b"][:NL], (0, 2, 1))),
        "rw_mu": f(inp["rw_mu"][:NL]),
        "rw_pv": f(np.concatenate([
            np.transpose(np.reshape(inp["rw_mu"][:NL, 0:768], (NL, 12, 64)), (0, 2, 1))] + [
            np.transpose(np.reshape(np.reshape(inp[k][:NL], (NL, 256)), (NL, 4, 64)), (0, 2, 1))
            for k in ("rw_w0", "rw_a0", "rw_k_k", "rw_k_a", "rw_r_k")], axis=2)),
        "rw_w2": f(inp["rw_w2"][:NL]),
        "rw_a2": f(inp["rw_a2"][:NL]),
        "rw_g2": f(inp["rw_g2"][:NL]),
        "rw_gn_w": f(inp["rw_gn_w"][:NL]),
        "rw_gn_b": f(inp["rw_gn_b"][:NL]),
        "fx_b_f": f(inp["fx_b_f"][:NL]),
        "w_out": f(inp["w_out"][:NL]),
        "norm2_g": f(inp["norm2_g"][:NL]),
        "router_w": f(np.concatenate([inp["router_group_w"][:NL], inp["router_expert_w"][:NL]], axis=2)),
        "router_b": f(np.concatenate([inp["router_group_b"][:NL], inp["router_expert_b"][:NL]], axis=1)),
        "exp_w_gate": pm(inp["exp_w_gate"][:NL], 8, EH),
        "exp_w_up": pm(inp["exp_w_up"][:NL], 8, EH),
        "exp_w_down": pm(inp["exp_w_down"][:NL], 4, D),
        "final_norm_g": f(np.reshape(inp["final_norm_g"], (1, D))),
    }
    return m


def kernel(**inputs):
    inp = {k: np.asarray(v) for k, v in inputs.items()}
    n = 8
    nb = 2
    prog = Prog(NL=4, NB=nb).build()
    shared = prep_inputs(inp, 0, nb)
    in_maps = []
    for c in range(n):
        m = dict(shared)
        m["x"] = np.ascontiguousarray(inp["x"][c * nb:(c + 1) * nb], dtype=np.float32)
        in_maps.append(m)
    res = run_bass_kernel_spmd(prog.nc, in_maps, core_ids=list(range(n)))
    out = np.concatenate([np.asarray(r["out"]) for r in res.results], axis=0)
    return out.astype(np.float32)
```

```python
import numpy as np
import concourse.bass as bass
import concourse.mybir as mybir
from concourse.bass_utils import run_bass_kernel_spmd

F32 = mybir.dt.float32
BF16 = mybir.dt.bfloat16
AF = mybir.ActivationFunctionType
ALU = mybir.AluOpType
AX = mybir.AxisListType

T = 2048
D = 1024
NT = 16
DIN = 3076
NE = 32
EH = 512
NDMA_SEMS = 12
RMS_EPS = 1e-6
LN_EPS = 1e-5
GN_EPS = 64e-5
NEGM = -30000.0
import os
MAXOPS = int(os.environ.get("KB_MAXOPS", "1000000000"))


class KB:
    def __init__(self):
        self.nc = bass.Bass("TRN2", target_bir_lowering=False)
        nc = self.nc
        self.engs = {"pe": nc.tensor, "act": nc.scalar, "dve": nc.vector,
                     "pool": nc.gpsimd, "sp": nc.sync}
        self.sem = {}
        self.cnt = {}
        for e in self.engs:
            self.sem[e] = nc.alloc_semaphore("s_" + e)
            self.cnt[e] = 0
        self.dsem = {}
        self.dcnt = {}
        self.drr = {}
        for q in ("sp", "pool"):
            self.dsem[q] = [nc.alloc_semaphore("d_%s%d" % (q, i)) for i in range(NDMA_SEMS)]
            self.dcnt[q] = [0] * NDMA_SEMS
            self.drr[q] = 0
        self.waited = {e: {} for e in self.engs}
        self.lastw = {}
        self.readers = {}
        self.nins = 0

    def _semof(self, sk):
        if sk[0] == "c":
            return self.sem[sk[1]]
        return self.dsem[sk[1]][sk[2]]

    def _wait(self, eng, tok):
        sk, val = tok
        if eng == "pe" and sk == ("c", "pe"):
            return
        w = self.waited[eng]
        if w.get(sk, 0) >= val:
            return
        self.engs[eng].wait_ge(self._semof(sk), val)
        w[sk] = val
        self.nins += 1

    def _deps(self, eng, reads, writes):
        for k in reads:
            t = self.lastw.get(k)
            if t is not None:
                self._wait(eng, t)
        for k in writes:
            t = self.lastw.get(k)
            if t is not None:
                self._wait(eng, t)
            for t in self.readers.get(k, ()):
                self._wait(eng, t)

    def _record(self, tok, reads, writes):
        for k in reads:
            lst = self.readers.setdefault(k, [])
            lst[:] = [t for t in lst if t[0] != tok[0]]
            lst.append(tok)
        for k in writes:
            self.lastw[k] = tok
            self.readers[k] = []

    def op(self, eng, fn, reads=(), writes=()):
        self.nops = getattr(self, "nops", 0) + 1
        if self.nops > MAXOPS:
            return None
        pr = [k for k in reads if k.startswith("pb")]
        if pr:
            reads = [k for k in reads if not k.startswith("pb")]
            writes = list(writes) + pr
        self._deps(eng, reads, writes)
        ins = fn(self.engs[eng])
        self.cnt[eng] += 1
        ins.then_inc(self.sem[eng], 1)
        tok = (("c", eng), self.cnt[eng])
        self._record(tok, reads, writes)
        self.nins += 1
        return tok

    def dma(self, out, in_, reads=(), writes=(), q="sp", **kw):
        self.nops = getattr(self, "nops", 0) + 1
        if self.nops > MAXOPS:
            return None
        i = self.drr[q]
        self.drr[q] = (i + 1) % NDMA_SEMS
        sk = ("d", q, i)
        if self.dcnt[q][i] > 0:
            self._wait(q, (sk, self.dcnt[q][i]))
        self._deps(q, reads, writes)
        ins = self.engs[q].dma_start(out=out, in_=in_, **kw)
        self.dcnt[q][i] += 16
        ins.then_inc(self.dsem[q][i], 16)
        tok = (sk, self.dcnt[q][i])
        self._record(tok, reads, writes)
        self.nins += 1
        return tok

    def idma(self, out, out_off, in_, in_off, reads=(), writes=()):
        q = "pool"
        self.nops = getattr(self, "nops", 0) + 1
        if self.nops > MAXOPS:
            return None
        i = self.drr[q]
        self.drr[q] = (i + 1) % NDMA_SEMS
        sk = ("d", q, i)
        if self.dcnt[q][i] > 0:
            self._wait(q, (sk, self.dcnt[q][i]))
        self._deps(q, reads, writes)
        ins = self.nc.gpsimd.indirect_dma_start(out=out, out_offset=out_off, in_=in_, in_offset=in_off)
        self.dcnt[q][i] += 16
        ins.then_inc(self.dsem[q][i], 16)
        tok = (sk, self.dcnt[q][i])
        self._record(tok, reads, writes)
        self.nins += 1
        return tok

    def barrier(self):
        toks = [(("c", e), self.cnt[e]) for e in self.engs if self.cnt[e] > 0]
        for q in self.dsem:
            for i in range(NDMA_SEMS):
                if self.dcnt[q][i] > 0:
                    toks.append((("d", q, i), self.dcnt[q][i]))
        for e in self.engs:
            for t in toks:
                if t[0] == ("c", e):
                    continue
                self._wait(e, t)
        self.lastw = {}
        self.readers = {}

    def dram(self, name, shape, dt=F32, kind="Internal"):
        return self.nc.dram_tensor(name, list(shape), dt, kind=kind).ap()


def _dtsize(dt):
    return 2 if dt == BF16 else 4


class Arena:
    def __init__(self, base_ap_f32, start, end):
        self.A = base_ap_f32
        self.cur = start
        self.end = end

    def t(self, shape, dt=F32, parts=128):
        n = 1
        for s in shape[1:]:
            n *= s
        nbytes = (n * _dtsize(dt) + 31) // 32 * 32
        off = self.cur
        self.cur += nbytes
        assert self.cur <= self.end, ("arena overflow", self.cur, self.end)
        v = self.A[0:shape[0], off // 4:(off + nbytes) // 4]
        if dt != F32:
            v = v.bitcast(dt)
        v = v[:, 0:n]
        if len(shape) == 3:
            v = v.rearrange("p (a b) -> p a b", a=shape[1])
        elif len(shape) == 4:
            v = v.rearrange("p (a b c) -> p a b c", a=shape[1], b=shape[2])
        return v


def make_consts():
    j = np.arange(128)[:, None]
    t = np.arange(128)[None, :]
    c = np.zeros((128, 8, 128), np.float32)
    c[:, 0] = np.eye(128)
    c[:, 1] = np.where(j >= t, -1.0, 0.0)
    c[:, 2] = -1.0
    c[:, 3] = np.where(j < t, 0.0, NEGM)
    c[:, 4] = np.where(j <= t, 0.0, NEGM)
    c[:, 5] = np.where(j < t, 1.0, 0.0)
    c[:, 6] = np.where(j <= t, 1.0, 0.0)
    c[:, 7] = np.where(t < j, 1.0, 0.0)
    ex = np.zeros((128, 256), np.float32)
    ex[:, 0:128] = 128.0 * np.arange(128)[None, :]
    ex[:, 128] = np.arange(128)
    return np.concatenate([c.reshape(128, 1024), ex], axis=1)


class Prog:
    def __init__(self, NL=4, NB=2, dbg=(), stop=None, moe_experts=NE, sparse=True):
        self.NL, self.NB = NL, NB
        self.stop = stop
        self.moe_experts = moe_experts
        self.sparse = sparse
        self.NTT = NB * NT
        self.S = 256
        self.NBLK = NB * T * 2 // self.S + 32
        kb = self.kb = KB()
        nc = self.nc = kb.nc
        dk = lambda n: ("ExternalOutput" if n in dbg else "Internal")
        I = lambda n, s: kb.dram(n, s, F32, kind="ExternalInput")
        self.x_in = I("x", [NB, T, D])
        self.consts_d = I("consts", [128, 1280])
        self.norm1_g = I("norm1_g", [NL, D])
        self.w_in = I("w_in", [NL, D, DIN])
        self.gm_wT = I("gm_wT", [NL, 4, 128, 128])
        self.gm_bT = I("gm_bT", [NL, 128, 4])
        self.rw_mu = I("rw_mu", [NL, 1024])
        self.rw_pv = I("rw_pv", [NL, 64, 32])
        self.rw_w2 = I("rw_w2", [NL, 64, 256])
        self.rw_a2 = I("rw_a2", [NL, 64, 256])
        self.rw_g2 = I("rw_g2", [NL, 128, 256])
        self.rw_gn_w = I("rw_gn_w", [NL, 256])
        self.rw_gn_b = I("rw_gn_b", [NL, 256])
        self.fx_b_f = I("fx_b_f", [NL, 4])
        self.w_out = I("w_out", [NL, D, D])
        self.norm2_g = I("norm2_g", [NL, D])
        self.router_w = I("router_w", [NL, D, 36])
        self.router_b = I("router_b", [NL, 36])
        if sparse:
            self.w_gate = I("exp_w_gate", [NL, NE * 128, 4096])
            self.w_up = I("exp_w_up", [NL, NE * 128, 4096])
            self.w_down = I("exp_w_down", [NL, NE * 128, 4096])
        else:
            self.w_gate = I("exp_w_gate", [NL, NE, D, EH])
            self.w_up = I("exp_w_up", [NL, NE, D, EH])
            self.w_down = I("exp_w_down", [NL, NE, EH, D])
        self.final_g = I("final_norm_g", [1, D])
        self.out = kb.dram("out", [NB, T, D], F32, kind="ExternalOutput")
        S = lambda n, s, dt=F32: kb.dram(n, s, dt, kind=dk(n))
        self.xres = S("xres", [NB, T, D])
        self.qkT = S("qkT", [NB, 16, 64, T], BF16)
        self.vtok = S("vtok", [NB, T, 512], BF16)
        self.rwT = S("rwT", [NB, 1024, T])
        self.fgT = S("fgT", [NB, 4, T])
        self.cumd = S("cumd", [NB, 4, T])
        self.mixT = S("mixT", [NB, 1024, T], BF16)
        self.h2T = S("h2T", [NB, 1024, T], BF16)
        self.gates = S("gates", [NB, T, 32])
        self.h2tok = S("h2tok", [NB, T, D], BF16)
        self.route = S("route", [NB, T, 66])
        self.xb = S("xb", [self.NBLK * self.S, D], BF16)
        self.yb = S("yb", [self.NBLK * self.S, D])
        self.dbg_sl = S("dbg_sl", [128, self.NTT * 2], mybir.dt.int32)
        self.dbg_widx = S("dbg_widx", [128, self.NBLK], mybir.dt.int32)
        self.dbg_misc = S("dbg_misc", [128, 4, 32])
        arena = nc.alloc_sbuf_tensor("arena", [128, 52000], F32)
        self.arena_ap = arena[:]
        self.pb = [nc.alloc_psum_tensor("pb%d" % i, [128, 512], F32)[:] for i in range(8)]
        self.R1 = Arena(self.arena_ap, 0, 40000)
        self.setup_consts()

    def P(self, eng, meth, *a, r=(), w=(), **kw):
        return self.kb.op(eng, lambda e: getattr(e, meth)(*a, **kw), r, w)

    def mm(self, out, lhsT, rhs, start, stop, r=(), w=(), sgc=False):
        if sgc:
            return self.kb.op("pe", lambda e: e.matmul(out, lhsT, rhs, start=start, stop=stop, skip_group_check=True), r, w)
        return self.kb.op("pe", lambda e: e.matmul(out, lhsT, rhs, start=start, stop=stop), r, w)

    def actf(self, out, in_, func, r=(), w=(), eng="act", **kw):
        return self.kb.op("act", lambda e: e.activation(out, in_, func, **kw), r, w)

    def col(self, dram_row):
        return dram_row.rearrange("(p o) -> p o", o=1)

    def setup_consts(self):
        kb = self.kb
        a = self.R1
        self.cst = a.t([128, 8, 128])
        kb.dma(self.cst.rearrange("p a b -> p (a b)"), self.consts_d[:, 0:1024], writes=["cst"])
        self.cex = a.t([128, 256])
        kb.dma(self.cex, self.consts_d[:, 1024:1280], writes=["cex"])
        self.thr = self.cex[:, 0:128]
        self.iota_p = self.cex[:, 128:129]
        self.m_strict = self.cst[:, 5, :]
        c = self.cst
        self.ident = c[:, 0, :]
        self.ntri = c[:, 1, :]
        self.negones = c[:, 2, :]
        self.mask2 = c[:, 5:7, :]
        self.m_incl = c[:, 6, :]
        self.m_lows = c[:, 7, :]
        self.ident_bf = a.t([128, 128], BF16)
        self.mbs_bf = a.t([128, 128], BF16)
        self.mbi_bf = a.t([128, 128], BF16)
        self.P("dve", "tensor_copy", self.ident_bf, c[:, 0, :], r=["cst"], w=["ident_bf"])
        self.P("dve", "tensor_copy", self.mbs_bf, c[:, 3, :], r=["cst"], w=["mbs_bf"])
        self.P("dve", "tensor_copy", self.mbi_bf, c[:, 4, :], r=["cst"], w=["mbi_bf"])
        self.ones_f = a.t([128, 128])
        self.P("pool", "memset", self.ones_f, 1.0, w=["ones_f"])
        self.ones_bf = a.t([128, 64], BF16)
        self.P("pool", "memset", self.ones_bf, 1.0, w=["ones_bf"])
        self.reset = a.t([64, 512])
        self.P("pool", "memset", self.reset, 1.0, w=["reset"])
        self.P("pool", "memset", self.reset.rearrange("p (c t) -> p c t", c=4)[:, :, 0:1], 0.0, w=["reset"])
        self.CK = ["cst", "ident_bf", "mbs_bf", "mbi_bf", "ones_f", "ones_bf", "reset", "ones4"]
        self.g1bc = a.t([128, D])
        self.g2bc = a.t([128, D])
        self.rbbc = a.t([128, 36])
        self.wr = a.t([128, 8, 36])
        self.gmw = a.t([128, 4, 128])
        self.gmb = a.t([128, 4])
        self.gnw = a.t([128, 256])
        self.gnb = a.t([128, 256])
        self.w2s = a.t([64, 256])
        self.a2s = a.t([64, 256])
        self.g2s = a.t([128, 256])
        self.pv = a.t([64, 40])
        self.mu_w = a.t([64, 1])
        self.mu_a = a.t([64, 1])
        self.mu_g = a.t([128, 1])
        self.fbf = a.t([4, 1])
        self.nfbf = a.t([4, 1])
        self.Mst = a.t([64, 4, 64])
        self.r1_end = a.cur

    def keepconsts(self):
        pass

    def load_layer(self, l):
        kb = self.kb
        bc = lambda row: row.partition_broadcast(128)
        kb.dma(self.g1bc, bc(self.norm1_g[l:l + 1, :]), writes=["g1bc"])
        kb.dma(self.g2bc, bc(self.norm2_g[l:l + 1, :]), writes=["g2bc"])
        kb.dma(self.rbbc, bc(self.router_b[l:l + 1, :]), writes=["rbbc"])
        kb.dma(self.wr, self.router_w[l].rearrange("(k p) n -> p k n", p=128), writes=["wr"])
        kb.dma(self.gmw, self.gm_wT[l].rearrange("g s t -> s g t"), writes=["gmw"])
        self.P("dve", "tensor_tensor", self.gmw, self.gmw, self.m_incl.unsqueeze(1).to_broadcast([128, 4, 128]),
               ALU.mult, r=["gmw"], w=["gmw"])
        kb.dma(self.gmb, self.gm_bT[l], writes=["gmb"])
        kb.dma(self.gnw, bc(self.rw_gn_w[l:l + 1, :]), writes=["gnw"])
        kb.dma(self.gnb, bc(self.rw_gn_b[l:l + 1, :]), writes=["gnb"])
        kb.dma(self.w2s, self.rw_w2[l], writes=["w2s"])
        kb.dma(self.a2s, self.rw_a2[l], writes=["a2s"])
        kb.dma(self.g2s, self.rw_g2[l], writes=["g2s"])
        kb.dma(self.pv[:, 0:32], self.rw_pv[l], writes=["pv"])
        self.P("dve", "tensor_scalar", self.pv[:, 32:36], self.pv[:, 24:28], -1.0, 1.0, ALU.mult, ALU.add,
               r=["pv"], w=["pv"])
        kb.dma(self.mu_w, self.col(self.rw_mu[l, 768:832]), writes=["mu_w"])
        kb.dma(self.mu_a, self.col(self.rw_mu[l, 832:896]), writes=["mu_a"])
        kb.dma(self.mu_g, self.col(self.rw_mu[l, 896:1024]), writes=["mu_g"])
        kb.dma(self.fbf, self.col(self.fx_b_f[l]), writes=["fbf"])
        self.P("dve", "tensor_single_scalar", self.nfbf, self.fbf, -1.0, ALU.mult, r=["fbf"], w=["nfbf"])

    def rmsnorm(self, xt, xk, gbc, gk, out, outk, tmp, tmpk, st, stk):
        self.actf(tmp, xt, AF.Square, r=[xk], w=[tmpk, stk], accum_out=st[:, 0:1])
        self.actf(st[:, 1:2], st[:, 0:1], AF.Ln, r=[stk], w=[stk], scale=1.0 / D, bias=RMS_EPS)
        self.actf(st[:, 2:3], st[:, 1:2], AF.Exp, r=[stk], w=[stk], scale=-0.5)
        self.P("dve", "scalar_tensor_tensor", out, xt, st[:, 2:3], gbc, ALU.mult, ALU.mult,
               r=[xk, stk, gk], w=[outk])

    def phaseA(self, l, b, src):
        kb = self.kb
        a = Arena(self.arena_ap, self.r1_end, 208000)
        win = a.t([128, 8, DIN], BF16)
        wstg = [a.t([128, DIN]) for _ in range(2)]
        for kc in range(8):
            si = kc % 2
            sk = "wstg%d" % si
            kb.dma(wstg[si], self.w_in[l, kc * 128:(kc + 1) * 128, :], writes=[sk])
            self.actf(win[:, kc, 0:1024], wstg[si][:, 0:1024], AF.Copy, r=[sk], w=["win%d" % kc])
            self.P("dve", "tensor_copy", win[:, kc, 1024:2048], wstg[si][:, 1024:2048], r=[sk], w=["win%d" % kc])
            self.P("pool", "tensor_copy", win[:, kc, 2048:DIN], wstg[si][:, 2048:DIN], r=[sk], w=["win%d" % kc])
        WK = ["win%d" % kc for kc in range(8)]
        xt = [a.t([128, D]) for _ in range(2)]
        junk = a.t([128, D])
        hb = [a.t([128, D], BF16) for _ in range(2)]
        hT4 = [a.t([128, 8, 512], BF16) for _ in range(2)]
        st = [a.t([128, 4]) for _ in range(2)]
        vsb = [a.t([128, 512], BF16) for _ in range(2)]
        xg = a.t([128, 512]); x2 = a.t([128, 512]); u1 = a.t([128, 512]); sg = a.t([128, 512]); hid = a.t([128, 512])
        gst = a.t([128, 16])
        vc = a.t([128, 256]); sq = a.t([128, 256]); vn = a.t([128, 256]); tmpg = a.t([128, 256]); ygm = a.t([128, 256])
        ygT = [a.t([128, 2, 128], BF16) for _ in range(2)]
        stg_bf = [a.t([64, 512], BF16) for _ in range(4)]
        stg_f = [a.t([128, 512]) for _ in range(4)]
        pb = self.pb
        pbT = pb[7][:, 0:512].bitcast(BF16)
        fm = []
        for h in range(4):
            fm.append((h * 64, 64, "qk", h, 1.0))
            fm.append((256 + h * 64, 64, "qk", 4 + h, 0.125))
            fm.append((2304 + h * 64, 64, "qk", 8 + h, 1.0))
            fm.append((2560 + h * 64, 64, "qk", 12 + h, 0.125))
        for j in range(12):
            fm.append((1280 + j * 64, 64, "rw", j * 64, 1.0))
        fm.append((2048, 64, "rw", 768, 1.0))
        fm.append((2112, 64, "rw", 832, 1.0))
        fm.append((2176, 128, "rw", 896, 1.0))
        fm.append((3072, 4, "fg", 0, 1.0))
        ci = 0
        for g4 in range(4):
            hT = hT4[g4 % 2]
            hTk = "hT4_%d" % (g4 % 2)
            for tt in range(4):
                t = g4 * 4 + tt
                p = t % 2
                rows = slice(t * 128, (t + 1) * 128)
                kb.dma(xt[p], src[rows, :], writes=["xt%d" % p])
                self.rmsnorm(xt[p], "xt%d" % p, self.g1bc, "g1bc", hb[p], "hb%d" % p, junk, "junk", st[p], "st%d" % p)
                for kc in range(8):
                    self.P("pe", "transpose", pbT[:, kc * 128:(kc + 1) * 128], hb[p][:, kc * 128:(kc + 1) * 128],
                           self.ident_bf, r=["hb%d" % p, "ident_bf"], w=["pb7"])
                self.actf(hT[:, :, tt * 128:(tt + 1) * 128], pbT.rearrange("p (k n) -> p k n", k=8), AF.Copy,
                          r=["pb7"], w=[hTk])
                for (pbi, pc0, c0, n) in ((0, 0, 512, 512), (1, 0, 1024, 256), (1, 256, 2816, 256)):
                    for kc in range(8):
                        self.mm(pb[pbi][:, pc0:pc0 + n], hT[:, kc, tt * 128:(tt + 1) * 128], win[:, kc, c0:c0 + n],
                                kc == 0, kc == 7, r=[hTk, WK[kc]], w=["pb%d" % pbi])
                self.actf(vsb[p][:, 0:256], pb[0][:, 0:256], AF.Copy, r=["pb0"], w=["vsb%d" % p])
                self.actf(vsb[p][:, 256:512], pb[1][:, 256:512], AF.Copy, r=["pb1"], w=["vsb%d" % p])
                kb.dma(self.vtok[b, rows, :], vsb[p], reads=["vsb%d" % p], writes=["vtok"])
                self.P("dve", "tensor_copy", xg[:, 0:256], pb[0][:, 256:512], r=["pb0"], w=["xg"])
                self.P("dve", "tensor_copy", xg[:, 256:512], pb[1][:, 0:256], r=["pb1"], w=["xg"])
                self.actf(x2, xg, AF.Square, r=["xg"], w=["x2"])
                self.P("dve", "tensor_scalar", u1, x2, 0.044715, 1.0, ALU.mult, ALU.add, r=["x2"], w=["u1"])
                self.P("dve", "tensor_tensor", u1, u1, xg, ALU.mult, r=["u1", "xg"], w=["u1"])
                self.actf(sg, u1, AF.Sigmoid, r=["u1"], w=["sg"], scale=1.5957691216057308)
                self.P("dve", "tensor_tensor", hid, sg, xg, ALU.mult, r=["sg", "xg"], w=["hid"])
                hv = hid[:, 256:512].rearrange("p (g d) -> p g d", g=4)
                v3 = lambda ap: ap.rearrange("p (g d) -> p g d", g=4)
                bc4 = lambda ap: ap.unsqueeze(2).to_broadcast([128, 4, 64])
                self.P("dve", "tensor_reduce", gst[:, 0:4], hv, AX.X, ALU.add, r=["hid"], w=["gst"])
                self.P("dve", "tensor_single_scalar", gst[:, 4:8], gst[:, 0:4], -1.0 / 64, ALU.mult, r=["gst"], w=["gst"])
                self.P("dve", "tensor_tensor", v3(vc), hv, bc4(gst[:, 4:8]), ALU.add, r=["hid", "gst"], w=["vc"])
                self.actf(sq, vc, AF.Square, r=["vc"], w=["sq"])
                self.P("dve", "tensor_reduce", gst[:, 8:12], v3(sq), AX.X, ALU.add, r=["sq"], w=["gst"])
                self.actf(gst[:, 8:12], gst[:, 8:12], AF.Ln, r=["gst"], w=["gst"], scale=1.0 / 64, bias=LN_EPS)
                self.actf(gst[:, 12:16], gst[:, 8:12], AF.Exp, r=["gst"], w=["gst"], scale=-0.5)
                self.P("dve", "tensor_tensor", v3(vn), v3(vc), bc4(gst[:, 12:16]), ALU.mult, r=["vc", "gst"], w=["vn"])
                for g in range(4):
                    self.mm(pb[2][:, g * 64:(g + 1) * 64], self.gmw[:, g, :], vn[:, g * 64:(g + 1) * 64], True, True,
                            r=["gmw", "vn"], w=["pb2"])
                self.P("dve", "tensor_tensor", v3(tmpg), v3(pb[2][:, 0:256]), bc4(self.gmb), ALU.add,
                       r=["pb2", "gmb"], w=["tmpg"])
                self.P("dve", "tensor_tensor", ygm, tmpg, hid[:, 0:256], ALU.mult, r=["tmpg", "hid"], w=["ygm"])
                for c in range(2):
                    self.P("pe", "transpose", pb[2][:, 256 + c * 128:256 + (c + 1) * 128], ygm[:, c * 128:(c + 1) * 128],
                           self.ident, r=["ygm", "cst"], w=["pb2"])
                self.actf(ygT[p].rearrange("p c n -> p (c n)"), pb[2][:, 256:512], AF.Copy, r=["pb2"], w=["ygT%d" % p])
                kb.dma(self.mixT[b, 256:512, rows].rearrange("(c p) n -> p c n", p=128), ygT[p],
                       reads=["ygT%d" % p], writes=["mixT"])
            cols = slice(g4 * 512, (g4 + 1) * 512)
            for (c0, M, kind, dest, scale) in fm:
                pi = 3 + ci % 4
                si = ci % 4
                ci += 1
                pk = "pb%d" % pi
                for kc in range(8):
                    self.mm(pb[pi][0:M, :], win[:, kc, c0:c0 + M], hT[:, kc, :], kc == 0, kc == 7,
                            r=[hTk, WK[kc]], w=[pk])
                if kind == "qk":
                    self.actf(stg_bf[si], pb[pi][0:64, :], AF.Copy, r=[pk], w=["stgb%d" % si], scale=scale)
                    kb.dma(self.qkT[b, dest, :, cols], stg_bf[si], reads=["stgb%d" % si], writes=["qkT"])
                elif kind == "rw":
                    self.P("dve", "tensor_copy", stg_f[si][0:M, :], pb[pi][0:M, :], r=[pk], w=["stgf%d" % si])
                    kb.dma(self.rwT[b, dest:dest + M, cols], stg_f[si][0:M, :], reads=["stgf%d" % si], writes=["rwT"])
                else:
                    self.P("dve", "tensor_copy", stg_f[si][0:M, :], pb[pi][0:M, :], r=[pk], w=["stgf%d" % si])
                    kb.dma(self.fgT[b, :, cols], stg_f[si][0:M, :], reads=["stgf%d" % si], writes=["fgT"])

    def phaseB_attn(self, l, b):
        kb = self.kb
        pball = self.pb
        a = Arena(self.arena_ap, self.r1_end, 208000)
        fg = a.t([4, T]); fe = a.t([4, T]); cum = a.t([4, T])
        self.ones4 = a.t([4, T])
        self.P("pool", "memset", self.ones4, 1.0, w=["ones4"])
        ncum_all = a.t([128, 16, 4])
        NS = 2
        slots = []
        for s in range(NS):
            B = dict(qT=a.t([64, T], BF16), kT=a.t([64, T], BF16), V=a.t([128, 16, 64], BF16),
                     E=[a.t([128, 512]) for _ in range(2)], Lp=[a.t([128, 512]) for _ in range(2)],
                     At=[a.t([128, 512], BF16) for _ in range(2)], Slp=a.t([128, 512]),
                     ob=[a.t([64, 512], BF16) for _ in range(2)], rec=a.t([64, 512]), cumrow=a.t([1, T]))
            slots.append(B)
        kb.dma(fg, self.fgT[b], reads=["fgT"], writes=["fg"])
        self.actf(fe, fg, AF.Exp, r=["fg", "nfbf"], w=["fe"], scale=-1.0, bias=self.nfbf)
        self.actf(fe, fe, AF.Ln, r=["fe"], w=["fe"], bias=1.0)
        self.P("dve", "tensor_tensor_scan", cum, self.ones4, fe, 0.0, ALU.mult, ALU.subtract,
               r=["fe", "ones4"], w=["cum"])
        kb.dma(self.cumd[b], cum, reads=["cum"], writes=["cumd"])
        for kbk in range(16):
            self.P("pe", "transpose", pball[6][:, kbk * 4:(kbk + 1) * 4], cum[0:4, kbk * 128:(kbk + 1) * 128],
                   self.ident[0:4, 0:4], r=["cum", "cst"], w=["pb6"])
        self.actf(ncum_all.rearrange("p a b -> p (a b)"), pball[6][:, 0:64], AF.Copy, r=["pb6"], w=["ncum"], scale=-1.0)

        def head_gen(kind, h, s):
            B = slots[s]
            K = lambda n: "%s_s%d" % (n, s)
            pb = pball[4 * s:4 * s + 4]
            pk = ["pb%d" % (4 * s + i) for i in range(4)]
            qT, kT, V, Slp, rec, cumrow = B["qT"], B["kT"], B["V"], B["Slp"], B["rec"], B["cumrow"]
            qk_q = h if kind == "sb" else 8 + h
            qk_k = 4 + h if kind == "sb" else 12 + h
            vc0 = h * 64 if kind == "sb" else 256 + h * 64
            mrow = h * 64 if kind == "sb" else 768 + h * 64
            kb.dma(qT, self.qkT[b, qk_q], reads=["qkT"], writes=[K("qT")])
            kb.dma(kT, self.qkT[b, qk_k], reads=["qkT"], writes=[K("kT")])
            kb.dma(V, self.vtok[b, :, vc0:vc0 + 64].rearrange("(kb p) d -> p kb d", p=128),
                   reads=["vtok"], writes=[K("V")])
            if kind == "fx":
                kb.dma(cumrow, self.cumd[b, h:h + 1, :], reads=["cumd"], writes=[K("cumrow")])
            yield
            it = 0
            for c in range(4):
                Q0 = c * 512
                last = 4 * c + 3
                if kind == "sb":
                    self.P("pool", "memset", Slp, 0.0, w=[K("Slp")])
                    order = list(range(last, -1, -1))
                else:
                    order = list(range(0, last + 1))
                first_pv = True
                for kbk in order:
                    r_ = kbk - 4 * c
                    diag = r_ >= 0
                    col0 = max(r_, 0) * 128
                    i2 = it % 2
                    it += 1
                    E, Lp, At = B["E"][i2], B["Lp"][i2], B["At"][i2]
                    Ek, Lk, Ak = K("E%d" % i2), K("Lp%d" % i2), K("At%d" % i2)
                    kblk = kT[:, kbk * 128:(kbk + 1) * 128]
                    qs = lambda c_a, c_b: qT[:, Q0 + c_a:Q0 + c_b]
                    blocks = []
                    if diag:
                        blocks.append((col0, col0 + 128, True))
                        if col0 + 128 < 512:
                            blocks.append((col0 + 128, 512, False))
                    else:
                        blocks.append((0, 512, False))
                    if kind == "sb":
                        pz, pzk = (pb[0], pk[0]) if i2 == 0 else (pb[3], pk[3])
                        pa, pak = pb[1], pk[1]
                        for (lo, hi, dg) in blocks:
                            if dg:
                                self.mm(pz[:, lo:hi], self.ident_bf, self.mbs_bf, True, False, r=["ident_bf", "mbs_bf"], w=[pzk])
                            self.mm(pz[:, lo:hi], kblk, qs(lo, hi), not dg, True, r=[K("kT"), K("qT")], w=[pzk])
                        yield
                        self.actf(E[:, col0:512], pz[:, col0:512], AF.Exp, r=[pzk], w=[Ek])
                        yield
                        self.actf(Lp[:, col0:512], E[:, col0:512], AF.Ln, r=[Ek], w=[Lk], bias=1.0)
                        yield
                        for (lo, hi, dg) in blocks:
                            if dg:
                                self.mm(pa[:, lo:hi], self.ident_bf, self.mbs_bf, True, False, r=["ident_bf", "mbs_bf"], w=[pak])
                            self.mm(pa[:, lo:hi], kblk, qs(lo, hi), not dg, False, r=[K("kT"), K("qT")], w=[pak])
                            self.mm(pa[:, lo:hi], self.ntri, Lp[:, lo:hi], False, False, r=["cst", Lk], w=[pak])
                            self.mm(pa[:, lo:hi], self.negones, Slp[:, lo:hi], False, True, r=["cst", K("Slp")], w=[pak])
                        yield
                        self.actf(At[:, col0:512], pa[:, col0:512], AF.Exp, r=[pak], w=[Ak])
                        self.P("pool", "tensor_tensor", Slp[:, col0:512], Slp[:, col0:512], Lp[:, col0:512], ALU.add,
                               r=[K("Slp"), Lk], w=[K("Slp")])
                        yield
                        for (lo, hi, dg) in blocks:
                            self.mm(pb[2][0:64, lo:hi], V[:, kbk, :], At[:, lo:hi], first_pv, kbk == 0,
                                    r=[K("V"), Ak], w=[pk[2]], sgc=True)
                            first_pv = False
                        yield
                    else:
                        pz, pzk = (pb[0], pk[0]) if i2 == 0 else (pb[1], pk[1])
                        for (lo, hi, dg) in blocks:
                            if dg:
                                self.mm(pz[:, lo:hi], self.ident_bf, self.mbi_bf, True, False, r=["ident_bf", "mbi_bf"], w=[pzk])
                            self.mm(pz[:, lo:hi], kblk, qs(lo, hi), not dg, False, r=[K("kT"), K("qT")], w=[pzk])
                            self.mm(pz[:, lo:hi], self.ones_f[0:1, :], cumrow[0:1, Q0 + lo:Q0 + hi], False, True,
                                    r=["ones_f", K("cumrow")], w=[pzk])
                        yield
                        self.actf(At[:, col0:512], pz[:, col0:512], AF.Exp, r=[pzk, "ncum"], w=[Ak],
                                  bias=ncum_all[:, kbk, h:h + 1])
                        yield
                        for (lo, hi, dg) in blocks:
                            self.mm(pb[2][0:64, lo:hi], V[:, kbk, :], At[:, lo:hi], first_pv, dg,
                                    r=[K("V"), Ak], w=[pk[2]], sgc=True)
                            self.mm(pb[3][0:64, lo:hi], self.ones_bf, At[:, lo:hi], first_pv, dg,
                                    r=["ones_bf", Ak], w=[pk[3]], sgc=True)
                            first_pv = False
                        yield
                oi = c % 2
                ob, obk = B["ob"][oi], K("ob%d" % oi)
                if kind == "sb":
                    self.actf(ob, pb[2][0:64, :], AF.Copy, r=[pk[2]], w=[obk])
                else:
                    self.P("dve", "reciprocal", rec, pb[3][0:64, :], r=[pk[3]], w=[K("rec")])
                    yield
                    self.P("dve", "tensor_tensor", ob, pb[2][0:64, :], rec, ALU.mult, r=[pk[2], K("rec")], w=[obk])
                yield
                kb.dma(self.mixT[b, mrow:mrow + 64, Q0:Q0 + 512], ob, reads=[obk], writes=["mixT"])

        for kind in ("sb", "fx"):
            for hp in range(2):
                gens = [head_gen(kind, 2 * hp + s, s) for s in range(NS)]
                while gens:
                    for g in list(gens):
                        try:
                            next(g)
                        except StopIteration:
                            gens.remove(g)

    def phaseB_rwkv(self, l, b):
        kb = self.kb
        pball = self.pb
        a = Arena(self.arena_ap, self.r1_end, 208000)
        F = lambda parts=64: a.t([parts, 512])
        xw, xwp, xa, xap = F(), F(), F(), F()
        xgt, xgp = F(128), F(128)
        pv = self.pv
        NS = 2
        slots = []
        for s in range(NS):
            B = {}
            for nm in ("raw0", "raw1", "raw2", "prv0", "prv1", "prv2", "rm", "km", "vm", "sgd", "logd", "G", "av",
                       "kk", "kk2", "nrm", "kap", "tmpa", "kp", "bbv", "eG", "eGn", "eGx", "ktT", "btT", "kdT", "rtT", "rkr"):
                B[nm] = F()
            B["tok3"] = [a.t([128, 3, 64]) for _ in range(4)]
            B["sgt"] = [a.t([128, 72]) for _ in range(4)]
            B["Yall"] = a.t([128, 4, 64])
            B["yc"] = a.t([128, 4, 64])
            B["ysq"] = a.t([128, 4, 64])
            B["yst4"] = a.t([128, 16])
            B["ABm"] = [a.t([128, 4, 128]) for _ in range(2)]
            B["X"] = [a.t([128, 128]) for _ in range(2)]
            B["Y"] = [a.t([128, 128]) for _ in range(2)]
            B["R"] = [a.t([128, 128]) for _ in range(2)]
            B["XYR"] = [a.t([128, 3, 128]) for _ in range(2)]
            for nm in ("Wn", "U", "ysb", "yn", "yj"):
                B[nm] = a.t([128, 64])
            B["mt"] = a.t([64, 64])
            B["yst"] = a.t([128, 8])
            B["ymix"] = [a.t([64, 512], BF16) for _ in range(2)]
            slots.append(B)
        for h in range(4):
            self.P("pool", "memset", self.Mst[:, h, :], 0.0, w=["M%d" % h])

        def load_shift(g4, dst, dstp, rows, nm, parts):
            c0 = g4 * 512
            kb.dma(dst[0:parts, :], self.rwT[b, rows, c0:c0 + 512], reads=["rwT"], writes=[nm])
            if g4 == 0:
                self.P("pool", "memset", dstp[0:parts, 0:1], 0.0, w=[nm + "p"])
                kb.dma(dstp[0:parts, 1:512], self.rwT[b, rows, 0:511], reads=["rwT"], writes=[nm + "p"])
            else:
                kb.dma(dstp[0:parts, :], self.rwT[b, rows, c0 - 1:c0 + 511], reads=["rwT"], writes=[nm + "p"])

        def mix(dst, x_, xp_, mu, nm, dk, parts=64):
            self.P("pool", "tensor_tensor", xp_[0:parts, :], xp_[0:parts, :], x_[0:parts, :], ALU.subtract,
                   r=[nm, nm + "p"], w=[nm + "p"])
            self.P("dve", "scalar_tensor_tensor", dst[0:parts, :], xp_[0:parts, :], mu, x_[0:parts, :], ALU.mult, ALU.add,
                   r=[nm, nm + "p", "pv", "mu_w", "mu_a", "mu_g"], w=[dk])

        def head_gen(g4, h, s):
            B = slots[s]
            K = lambda n: "%s_s%d" % (n, s)
            pb = pball[4 * s:4 * s + 4]
            pk = ["pb%d" % (4 * s + i) for i in range(4)]
            c0 = g4 * 512
            cols = slice(c0, c0 + 512)
            hc = slice(h * 64, (h + 1) * 64)
            Mh = self.Mst[:, h, :]
            Mk = "M%d" % h
            rm, km, vm = B["rm"], B["km"], B["vm"]
            sgd, logd, G, av = B["sgd"], B["logd"], B["G"], B["av"]
            kk, kk2, nrm, kap, tmpa, kp, bbv = B["kk"], B["kk2"], B["nrm"], B["kap"], B["tmpa"], B["kp"], B["bbv"]
            eG, eGn, eGx = B["eG"], B["eGn"], B["eGx"]
            ktT, btT, kdT, rtT, rkr = B["ktT"], B["btT"], B["kdT"], B["rtT"], B["rkr"]
            Wn, U, ysb, yn, yj, mt, yst = B["Wn"], B["U"], B["ysb"], B["yn"], B["yj"], B["mt"], B["yst"]
            X, Y, R = B["X"], B["Y"], B["R"]
            for j, (nm, dst) in enumerate((("r", rm), ("k", km), ("v", vm))):
                rows = slice((j * 4 + h) * 64, (j * 4 + h + 1) * 64)
                load_shift(g4, B["raw%d" % j], B["prv%d" % j], rows, K("raw%d" % j), 64)
                mix(dst, B["raw%d" % j], B["prv%d" % j], pv[:, j * 4 + h:j * 4 + h + 1], K("raw%d" % j), K(nm + "m"))
            yield
            self.mm(pb[0][0:64, :], self.w2s[:, hc], xw, True, True, r=["w2s", "xw"], w=[pk[0]])
            self.mm(pb[1][0:64, :], self.a2s[:, hc], xa, True, True, r=["a2s", "xa"], w=[pk[1]])
            self.P("dve", "tensor_single_scalar", kk, km, pv[:, 20 + h:21 + h], ALU.mult, r=[K("km"), "pv"], w=[K("kk")])
            self.P("pool", "tensor_tensor", kk2, kk, kk, ALU.mult, r=[K("kk")], w=[K("kk2")])
            yield
            self.actf(sgd, pb[0][0:64, :], AF.Sigmoid, r=[pk[0], "pv"], w=[K("sgd")], bias=pv[:, 12 + h:13 + h])
            self.actf(av, pb[1][0:64, :], AF.Sigmoid, r=[pk[1], "pv"], w=[K("av")], bias=pv[:, 16 + h:17 + h])
            self.mm(pb[2][0:64, :], self.ones_f[0:64, 0:64], kk2, True, True, r=["ones_f", K("kk2")], w=[pk[2]])
            yield
            self.P("dve", "tensor_single_scalar", logd, sgd, -0.6065306597126334, ALU.mult, r=[K("sgd")], w=[K("logd")])
            self.actf(nrm, pb[2][0:64, :], AF.Sqrt, r=[pk[2]], w=[K("nrm")])
            yield
            self.P("dve", "tensor_tensor_scan", G, self.reset, logd, 0.0, ALU.mult, ALU.add, r=["reset", K("logd")], w=[K("G")])
            self.P("dve", "tensor_single_scalar", nrm, nrm, 1e-12, ALU.max, r=[K("nrm")], w=[K("nrm")])
            yield
            self.P("dve", "reciprocal", nrm, nrm, r=[K("nrm")], w=[K("nrm")])
            self.actf(eG, G, AF.Exp, r=[K("G")], w=[K("eG")])
            self.actf(eGn, G, AF.Exp, r=[K("G")], w=[K("eGn")], scale=-1.0)
            self.P("dve", "tensor_tensor", logd, G, logd, ALU.subtract, r=[K("G"), K("logd")], w=[K("logd")])
            yield
            self.P("dve", "tensor_tensor", kap, kk, nrm, ALU.mult, r=[K("kk"), K("nrm")], w=[K("kap")])
            self.actf(eGx, logd, AF.Exp, r=[K("logd")], w=[K("eGx")])
            self.P("dve", "tensor_scalar", tmpa, av, pv[:, 24 + h:25 + h], pv[:, 32 + h:33 + h], ALU.mult, ALU.add,
                   r=[K("av"), "pv"], w=[K("tmpa")])
            yield
            self.P("pool", "tensor_tensor", kp, km, tmpa, ALU.mult, r=[K("km"), K("tmpa")], w=[K("kp")])
            self.P("pool", "tensor_tensor", bbv, kap, av, ALU.mult, r=[K("kap"), K("av")], w=[K("bbv")])
            self.P("dve", "tensor_tensor", ktT, kap, eGx, ALU.mult, r=[K("kap"), K("eGx")], w=[K("ktT")])
            self.P("pool", "tensor_tensor", rtT, rm, eG, ALU.mult, r=[K("rm"), K("eG")], w=[K("rtT")])
            yield
            self.P("pool", "tensor_tensor", btT, bbv, eGn, ALU.mult, r=[K("bbv"), K("eGn")], w=[K("btT")])
            self.P("dve", "tensor_tensor", kdT, kp, eGn, ALU.mult, r=[K("kp"), K("eGn")], w=[K("kdT")])
            self.P("dve", "scalar_tensor_tensor", rkr, rm, pv[:, 28 + h:29 + h], kp, ALU.mult, ALU.mult,
                   r=[K("rm"), K("kp"), "pv"], w=[K("rkr")])
            yield
            for cc in range(4):
                u2 = cc % 2
                cs = slice(cc * 128, (cc + 1) * 128)
                t3, t3k = B["tok3"][cc], K("tok3_%d" % cc)
                sgtk = K("sgt%d" % cc)
                for j, (srcT, sk) in enumerate(((btT, K("btT")), (kdT, K("kdT")), (vm, K("vm")))):
                    self.P("pe", "transpose", pb[0][:, j * 64:(j + 1) * 64], srcT[:, cs], self.ident[0:64, 0:64],
                           r=[sk, "cst"], w=[pk[0]])
                self.mm(pb[0][:, 192:193], rkr[:, cs], self.ones_f[0:64, 0:1], True, True, r=[K("rkr"), "ones_f"], w=[pk[0]])
                self.mm(pb[0][:, 200:264], xgt[:, cs], self.g2s[:, hc], True, True, r=["xg", "g2s"], w=[pk[0]])
                for j, (lT, lk, rT, rk_) in enumerate(((btT, K("btT"), ktT, K("ktT")), (btT, K("btT"), rtT, K("rtT")),
                                                       (kdT, K("kdT"), ktT, K("ktT")), (kdT, K("kdT"), rtT, K("rtT")))):
                    self.mm(pb[1][:, j * 128:(j + 1) * 128], lT[:, cs], rT[:, cs], True, True, r=[lk, rk_], w=[pk[1]])
                self.mm(pb[2][:, 0:128], ktT[:, cs], btT[:, cs], True, True, r=[K("ktT"), K("btT")], w=[pk[2]])
                yield
                self.actf(t3.rearrange("p a b -> p (a b)"), pb[0][:, 0:192], AF.Copy, r=[pk[0]], w=[t3k])
                self.P("dve", "tensor_copy", B["sgt"][cc], pb[0][:, 192:264], r=[pk[0]], w=[sgtk])
                bt_tok, kd_tok, v_tok = t3[:, 0, :], t3[:, 1, :], t3[:, 2, :]
                s_tok = B["sgt"][cc][:, 0:1]
                g_tok = B["sgt"][cc][:, 8:72]
                AB, ABk = B["ABm"][u2], K("AB%d" % u2)
                self.P("dve", "tensor_tensor", AB.rearrange("p (x y) t -> p x y t", x=2),
                       pb[1].rearrange("p (x y t) -> p x y t", x=2, y=2),
                       self.mask2.unsqueeze(1).to_broadcast([128, 2, 2, 128]), ALU.mult, r=[pk[1], "cst"], w=[ABk])
                Nm, BbT, AkT, BkT = AB[:, 0, :], AB[:, 1, :], AB[:, 2, :], AB[:, 3, :]
                self.P("dve", "tensor_tensor", Y[0], pb[2][:, 0:128], self.m_lows, ALU.mult, r=[pk[2], "cst"], w=[K("Y0")])
                yield
                self.P("pool", "tensor_tensor", R[0], self.ident, Nm, ALU.subtract, r=["cst", ABk], w=[K("R0")])
                Xc, Xk = Nm, ABk
                Yc, Yk = Y[0], K("Y0")
                Rc, Rk = R[0], K("R0")
                for i in range(1, 8):
                    xi = i % 2
                    T3, T3k = B["XYR"][xi], K("XYR%d" % xi)
                    if i <= 5:
                        self.mm(pb[2][:, 128:256], Yc, Xc, True, True, r=[Yk, Xk], w=[pk[2]])
                    if i <= 6:
                        self.mm(pb[2][:, 256:384], Xc, Yc, True, True, r=[Xk, Yk], w=[pk[2]])
                    if i >= 2:
                        self.mm(pb[2][:, 384:512], self.ident, Rc, True, False, r=["cst", Rk], w=[pk[2]])
                        self.mm(pb[2][:, 384:512], Yc, Rc, False, True, r=[Yk, Rk], w=[pk[2]])
                    yield
                    c_lo = 128 if i <= 5 else (256 if i <= 6 else 384)
                    c_hi = 512 if i >= 2 else 384
                    self.actf(T3.rearrange("p a b -> p (a b)")[:, c_lo - 128:c_hi - 128], pb[2][:, c_lo:c_hi], AF.Copy,
                              r=[pk[2]], w=[T3k])
                    if i <= 5:
                        Xc, Xk = T3[:, 0, :], T3k
                    if i <= 6:
                        Yc, Yk = T3[:, 1, :], T3k
                    if i >= 2:
                        Rc, Rk = T3[:, 2, :], T3k
                    yield
                self.mm(pb[3][:, 128:192], ktT[:, cs], Mh, True, False, r=[K("ktT"), Mk], w=[pk[3]])
                self.mm(pb[3][:, 128:192], AkT, v_tok, False, True, r=[ABk, t3k], w=[pk[3]])
                yield
                self.actf(Wn, pb[3][:, 128:192], AF.Copy, r=[pk[3]], w=[K("Wn")], scale=-1.0)
                yield
                self.mm(pb[3][:, 192:256], Rc, Wn, True, True, r=[Rk, K("Wn")], w=[pk[3]])
                yield
                self.actf(U, pb[3][:, 192:256], AF.Copy, r=[pk[3]], w=[K("U")])
                yield
                self.mm(pb[3][:, 256:320], rtT[:, cs], Mh, True, False, r=[K("rtT"), Mk], w=[pk[3]])
                self.mm(pb[3][:, 256:320], BbT, U, False, False, r=[ABk, K("U")], w=[pk[3]])
                self.mm(pb[3][:, 256:320], BkT, v_tok, False, True, r=[ABk, t3k], w=[pk[3]])
                self.mm(pb[3][0:64, 320:384], bt_tok, U, True, False, r=[t3k, K("U")], w=[pk[3]])
                self.mm(pb[3][0:64, 320:384], kd_tok, v_tok, False, True, r=[t3k], w=[pk[3]])
                yield
                self.P("dve", "tensor_tensor", mt, pb[3][0:64, 320:384], Mh, ALU.add, r=[pk[3], Mk], w=[K("mt")])
                self.actf(B["Yall"][:, cc, :], pb[3][:, 256:320], AF.Identity, r=[pk[3]], w=[K("Yall"), K("yst4")],
                          accum_out=B["yst4"][:, cc:cc + 1])
                yield
                self.P("dve", "tensor_single_scalar", Mh, mt, eG[:, cc * 128 + 127:cc * 128 + 128], ALU.mult,
                       r=[K("mt"), K("eG")], w=[Mk])
                yield
            Yall, yc, ysq, y4 = B["Yall"], B["yc"], B["ysq"], B["yst4"]
            bc4 = lambda ap: ap.unsqueeze(2).to_broadcast([128, 4, 64])
            bch = lambda ap: ap.unsqueeze(1).to_broadcast([128, 4, 64])
            self.P("dve", "tensor_single_scalar", y4[:, 4:8], y4[:, 0:4], -1.0 / 64, ALU.mult, r=[K("yst4")], w=[K("yst4")])
            yield
            self.P("dve", "tensor_tensor", yc, Yall, bc4(y4[:, 4:8]), ALU.add, r=[K("Yall"), K("yst4")], w=[K("yc")])
            yield
            self.actf(ysq, yc, AF.Square, r=[K("yc")], w=[K("ysq")])
            yield
            self.P("dve", "tensor_reduce", y4[:, 8:12], ysq, AX.X, ALU.add, r=[K("ysq")], w=[K("yst4")])
            yield
            self.actf(y4[:, 8:12], y4[:, 8:12], AF.Ln, r=[K("yst4")], w=[K("yst4")], scale=1.0 / 64, bias=GN_EPS)
            yield
            self.actf(y4[:, 12:16], y4[:, 8:12], AF.Exp, r=[K("yst4")], w=[K("yst4")], scale=-0.5)
            yield
            self.P("dve", "tensor_tensor", yc, yc, bc4(y4[:, 12:16]), ALU.mult, r=[K("yc"), K("yst4")], w=[K("yc")])
            yield
            self.P("dve", "tensor_tensor", yc, yc, bch(self.gnw[:, hc]), ALU.mult, r=[K("yc"), "gnw"], w=[K("yc")])
            yield
            self.P("dve", "tensor_tensor", yc, yc, bch(self.gnb[:, hc]), ALU.add, r=[K("yc"), "gnb"], w=[K("yc")])
            yield
            for cc in range(4):
                self.P("dve", "scalar_tensor_tensor", yc[:, cc, :], B["tok3"][cc][:, 2, :], B["sgt"][cc][:, 0:1], yc[:, cc, :],
                       ALU.mult, ALU.add, r=[K("yc"), K("tok3_%d" % cc), K("sgt%d" % cc)], w=[K("yc")])
            yield
            for cc in range(4):
                self.P("dve", "tensor_tensor", yc[:, cc, :], yc[:, cc, :], B["sgt"][cc][:, 8:72], ALU.mult,
                       r=[K("yc"), K("sgt%d" % cc)], w=[K("yc")])
            yield
            for cc in range(4):
                self.P("pe", "transpose", pb[0][0:64, cc * 128:(cc + 1) * 128], yc[:, cc, :], self.ident, r=[K("yc"), "cst"], w=[pk[0]])
            yield
            ym, ymk = B["ymix"][g4 % 2], K("ymix%d" % (g4 % 2))
            self.actf(ym, pb[0][0:64, :], AF.Copy, r=[pk[0]], w=[ymk])
            yield
            kb.dma(self.mixT[b, 512 + h * 64:512 + (h + 1) * 64, cols], ym, reads=[ymk], writes=["mixT"])

        for g4 in range(4):
            load_shift(g4, xw, xwp, slice(768, 832), "xw", 64)
            load_shift(g4, xa, xap, slice(832, 896), "xa", 64)
            load_shift(g4, xgt, xgp, slice(896, 1024), "xg", 128)
            mix(xw, xw, xwp, self.mu_w, "xw", "xw")
            mix(xa, xa, xap, self.mu_a, "xa", "xa")
            mix(xgt, xgt, xgp, self.mu_g, "xg", "xg", 128)
            self.actf(xw, xw, AF.Tanh, r=["xw"], w=["xw"])
            self.actf(xgt, xgt, AF.Sigmoid, r=["xg"], w=["xg"])
            for hp in range(2):
                gens = [head_gen(g4, 2 * hp + s, s) for s in range(NS)]
                while gens:
                    for g in list(gens):
                        try:
                            next(g)
                        except StopIteration:
                            gens.remove(g)

    def phaseD(self, l, b, src):
        kb = self.kb
        pball = self.pb
        a = Arena(self.arena_ap, self.r1_end, 208000)
        wo = a.t([128, 8, D], BF16)
        wostg = [a.t([128, D]) for _ in range(2)]
        for kc in range(8):
            si = kc % 2
            sk = "wostg%d" % si
            kb.dma(wostg[si], self.w_out[l, kc * 128:(kc + 1) * 128, :], writes=[sk])
            self.actf(wo[:, kc, 0:512], wostg[si][:, 0:512], AF.Copy, r=[sk], w=["wo%d" % kc])
            self.P("pool", "tensor_copy", wo[:, kc, 512:1024], wostg[si][:, 512:1024], r=[sk], w=["wo%d" % kc])
        chains = []
        for q in range(2):
            chains.append(dict(mx=a.t([128, 8, 128], BF16), xt=a.t([128, D]), xn=a.t([128, D]), h2=a.t([128, D]),
                               junk=a.t([128, D]), st=a.t([128, 4]), hTf=a.t([128, 8, 128]), h2b=a.t([128, D], BF16),
                               lg=a.t([128, 36]), lm=a.t([128, 32]), l2=a.t([128, 32]), ohg=a.t([128, 4]), ge=a.t([128, 4]),
                               rs=a.t([128, 16]), rt=a.t([128, 66])))

        def tile_gen(t, q):
            C = chains[q]
            K = lambda n: "%s_c%d" % (n, q)
            mx, xt, xn, h2, junk, st, hTf, h2b = C["mx"], C["xt"], C["xn"], C["h2"], C["junk"], C["st"], C["hTf"], C["h2b"]
            lg, lm, l2, ohg, ge, rs, rt = C["lg"], C["lm"], C["l2"], C["ohg"], C["ge"], C["rs"], C["rt"]
            oh1, oh2 = rt[:, 0:32], rt[:, 32:64]
            po, pok = pball[q], "pb%d" % q
            pt, ptk = pball[2 + q], "pb%d" % (2 + q)
            pr, prk = pball[4 + q], "pb%d" % (4 + q)
            rows = slice(t * 128, (t + 1) * 128)
            kb.dma(mx, self.mixT[b, :, rows].rearrange("(k p) n -> p k n", p=128), reads=["mixT"], writes=[K("mx")])
            kb.dma(xt, src[rows, :], reads=["xres"], writes=[K("xt")])
            yield
            for hf in range(2):
                for kc in range(8):
                    self.mm(po, mx[:, kc, :], wo[:, kc, hf * 512:(hf + 1) * 512], kc == 0, kc == 7,
                            r=[K("mx"), "wo%d" % kc], w=[pok])
                yield
                self.P("dve", "tensor_tensor", xn[:, hf * 512:(hf + 1) * 512], po, xt[:, hf * 512:(hf + 1) * 512],
                       ALU.add, r=[pok, K("xt")], w=[K("xn")])
                yield
            kb.dma(self.xres[b, rows, :], xn, reads=[K("xn")], writes=["xres_w"])
            self.actf(junk, xn, AF.Square, r=[K("xn")], w=[K("junk"), K("st")], accum_out=st[:, 0:1])
            yield
            self.actf(st[:, 1:2], st[:, 0:1], AF.Ln, r=[K("st")], w=[K("st")], scale=1.0 / D, bias=RMS_EPS)
            yield
            self.actf(st[:, 2:3], st[:, 1:2], AF.Exp, r=[K("st")], w=[K("st")], scale=-0.5)
            yield
            self.P("dve", "scalar_tensor_tensor", h2, xn, st[:, 2:3], self.g2bc, ALU.mult, ALU.mult,
                   r=[K("xn"), K("st"), "g2bc"], w=[K("h2")])
            yield
            self.P("pool", "tensor_copy", h2b, h2, r=[K("h2")], w=[K("h2b")])
            kb.dma(self.h2tok[b, rows, :], h2b, reads=[K("h2b")], writes=["h2tok"])
            for hh in range(2):
                for k4 in range(4):
                    kc = hh * 4 + k4
                    self.P("pe", "transpose", pt[:, k4 * 128:(k4 + 1) * 128], h2[:, kc * 128:(kc + 1) * 128],
                           self.ident, r=[K("h2"), "cst"], w=[ptk])
                yield
                self.actf(hTf[:, hh * 4:(hh + 1) * 4, :].rearrange("p k n -> p (k n)"), pt, AF.Copy, r=[ptk], w=[K("hTf")])
                yield
            for kc in range(8):
                self.mm(pr[:, 0:36], hTf[:, kc, :], self.wr[:, kc, :], kc == 0, kc == 7, r=[K("hTf"), "wr"], w=[prk])
            yield
            rk_ = K("rs")
            self.P("dve", "tensor_tensor", lg, pr[:, 0:36], self.rbbc, ALU.add, r=[prk, "rbbc"], w=[K("lg")])
            yield
            self.P("dve", "tensor_reduce", rs[:, 0:1], lg[:, 0:4], AX.X, ALU.max, r=[K("lg")], w=[rk_])
            yield
            self.P("dve", "tensor_scalar", ohg, lg[:, 0:4], rs[:, 0:1], None, ALU.is_equal, r=[K("lg"), rk_], w=[K("ohg")])
            self.P("dve", "tensor_single_scalar", rs[:, 1:2], rs[:, 0:1], -1.0, ALU.mult, r=[rk_], w=[rk_])
            yield
            self.actf(ge, lg[:, 0:4], AF.Exp, r=[K("lg"), rk_], w=[K("ge"), rk_], bias=rs[:, 1:2], accum_out=rs[:, 2:3])
            self.P("dve", "tensor_scalar", ohg, ohg, -1.0, 1e30, ALU.add, ALU.mult, r=[K("ohg")], w=[K("ohg")])
            yield
            self.P("dve", "reciprocal", rs[:, 3:4], rs[:, 2:3], r=[rk_], w=[rk_])
            self.P("dve", "tensor_tensor", lm.rearrange("p (g e) -> p g e", g=4), lg[:, 4:36].rearrange("p (g e) -> p g e", g=4),
                   ohg.unsqueeze(2).to_broadcast([128, 4, 8]), ALU.add, r=[K("lg"), K("ohg")], w=[K("lm")])
            yield
            self.P("dve", "tensor_reduce", rs[:, 4:5], lm, AX.X, ALU.max, r=[K("lm")], w=[rk_])
            yield
            self.P("dve", "tensor_scalar", oh1, lm, rs[:, 4:5], None, ALU.is_equal, r=[K("lm"), rk_], w=[K("rt")])
            yield
            self.P("dve", "scalar_tensor_tensor", l2, oh1, -1e30, lm, ALU.mult, ALU.add, r=[K("rt"), K("lm")], w=[K("l2")])
            yield
            self.P("dve", "tensor_reduce", rs[:, 5:6], l2, AX.X, ALU.max, r=[K("l2")], w=[rk_])
            yield
            self.P("dve", "tensor_scalar", oh2, l2, rs[:, 5:6], None, ALU.is_equal, r=[K("l2"), rk_], w=[K("rt")])
            self.P("dve", "tensor_tensor", rs[:, 6:7], rs[:, 5:6], rs[:, 4:5], ALU.subtract, r=[rk_], w=[rk_])
            yield
            self.actf(rs[:, 7:8], rs[:, 6:7], AF.Exp, r=[rk_], w=[rk_])
            yield
            self.P("dve", "tensor_single_scalar", rs[:, 8:9], rs[:, 7:8], 1.0, ALU.add, r=[rk_], w=[rk_])
            yield
            self.P("dve", "reciprocal", rs[:, 9:10], rs[:, 8:9], r=[rk_], w=[rk_])
            yield
            self.P("dve", "tensor_tensor", rs[:, 10:11], rs[:, 9:10], rs[:, 7:8], ALU.mult, r=[rk_], w=[rk_])
            yield
            self.P("dve", "tensor_tensor", rt[:, 64:65], rs[:, 9:10], rs[:, 3:4], ALU.mult, r=[rk_], w=[K("rt")])
            self.P("dve", "tensor_tensor", rt[:, 65:66], rs[:, 10:11], rs[:, 3:4], ALU.mult, r=[rk_], w=[K("rt")])
            yield
            kb.dma(self.route[b, rows, :], rt, reads=[K("rt")], writes=["route"])

        for tp in range(NT // 2):
            gens = [tile_gen(2 * tp + q, q) for q in range(2)]
            while gens:
                for g in list(gens):
                    try:
                        next(g)
                    except StopIteration:
                        gens.remove(g)

    def phaseE(self, l, b, final):
        kb = self.kb
        pb = self.pb
        a = Arena(self.arena_ap, self.r1_end, 208000)
        yacc = a.t([128, NT, D])
        hT = a.t([128, 8, T], BF16)
        gts = a.t([128, NT, 32])
        wg = [a.t([128, 8, EH], BF16) for _ in range(2)]
        wu = [a.t([128, 8, EH], BF16) for _ in range(2)]
        wd = [a.t([128, 4, D], BF16) for _ in range(2)]
        sl = [a.t([128, 512]) for _ in range(2)]
        act = [a.t([128, 4, 512], BF16) for _ in range(2)]
        for t in range(NT):
            kb.dma(yacc[:, t, :], self.xres[b, t * 128:(t + 1) * 128, :], writes=["y%d" % t])
        for kc in range(8):
            kb.dma(hT[:, kc, :], self.h2T[b, kc * 128:(kc + 1) * 128, :], writes=["hT"])
        kb.dma(gts, self.gates[b].rearrange("(t p) e -> p t e", p=128), writes=["gts"])
        it = 0
        nstage = 0
        stg = [a.t([128, 4, 512]) for _ in range(2)]
        for e in range(self.moe_experts):
            p = e % 2
            for wi, (wsrc, wdst, wkey, nh) in enumerate(((self.w_gate, wg[p], "wg%d" % p, 2), (self.w_up, wu[p], "wu%d" % p, 2),
                                                           (self.w_down, wd[p], "wd%d" % p, 1))):
                for hh in range(2):
                    si = nstage % 2
                    nstage += 1
                    if nh == 2:
                        src_ap = wsrc[l, e, hh * 512:(hh + 1) * 512, :].rearrange("(k p) n -> p k n", p=128)
                        dst_ap = wdst[:, hh * 4:(hh + 1) * 4, :]
                        stv = stg[si]
                    else:
                        src_ap = wsrc[l, e, hh * 256:(hh + 1) * 256, :].rearrange("(k p) n -> p k n", p=128)
                        dst_ap = wdst[:, hh * 2:(hh + 1) * 2, :]
                        stv = stg[si].rearrange("p k n -> p (k n)").rearrange("p (k n) -> p k n", k=2)
                    kb.dma(stv, src_ap, writes=["stg%d" % si])
                    if nstage % 2 == 0:
                        self.actf(dst_ap, stv, AF.Copy, r=["stg%d" % si], w=[wkey + "_%d" % hh])
                    else:
                        self.P("pool", "tensor_copy", dst_ap, stv, r=["stg%d" % si], w=[wkey + "_%d" % hh])
            for g in range(4):
                ap_ = it % 2
                it += 1
                tcols = slice(g * 512, (g + 1) * 512)
                for m in range(4):
                    pg = pb[(2 * m) % 4]; pgk = "pb%d" % ((2 * m) % 4)
                    pu = pb[(2 * m) % 4 + 1]; puk = "pb%d" % ((2 * m) % 4 + 1)
                    for kc in range(8):
                        self.mm(pg, wg[p][:, kc, m * 128:(m + 1) * 128], hT[:, kc, tcols], kc == 0, kc == 7,
                                r=["wg%d_0" % p, "wg%d_1" % p, "hT"], w=[pgk])
                    for kc in range(8):
                        self.mm(pu, wu[p][:, kc, m * 128:(m + 1) * 128], hT[:, kc, tcols], kc == 0, kc == 7,
                                r=["wu%d_0" % p, "wu%d_1" % p, "hT"], w=[puk])
                    s_ = sl[m % 2]; sk = "sl%d" % (m % 2)
                    self.actf(s_, pg, AF.Silu, r=[pgk], w=[sk])
                    self.P("dve", "tensor_tensor", act[ap_][:, m, :], s_, pu, ALU.mult, r=[sk, puk], w=["act%d_%d" % (ap_, m)])
                for tt in range(4):
                    t = g * 4 + tt
                    for hf in range(2):
                        po = pb[4 + (tt * 2 + hf) % 4]; pok = "pb%d" % (4 + (tt * 2 + hf) % 4)
                        for m in range(4):
                            self.mm(po, act[ap_][:, m, tt * 128:(tt + 1) * 128], wd[p][:, m, hf * 512:(hf + 1) * 512],
                                    m == 0, m == 3, r=["act%d_%d" % (ap_, m), "wd%d_0" % p, "wd%d_1" % p], w=[pok])
                        ys = yacc[:, t, hf * 512:(hf + 1) * 512]
                        self.P("dve", "scalar_tensor_tensor", ys, po, gts[:, t, e:e + 1], ys, ALU.mult, ALU.add,
                               r=[pok, "gts", "y%d" % t], w=["y%d" % t])
        if not final:
            for t in range(NT):
                kb.dma(self.xres[b, t * 128:(t + 1) * 128, :], yacc[:, t, :], reads=["y%d" % t], writes=["xres"])
        else:
            kb.barrier()
            gf = wg[0].rearrange("p k n -> p (k n)").bitcast(F32)[:, 0:D]
            junk = wu[0].rearrange("p k n -> p (k n)").bitcast(F32)[:, 0:D]
            ot = [wd[0].rearrange("p k n -> p (k n)").bitcast(F32)[:, 0:D], wd[1].rearrange("p k n -> p (k n)").bitcast(F32)[:, 0:D]]
            stf = sl[0]
            kb.dma(gf, self.final_g.partition_broadcast(128), writes=["wg0_0", "wg0_1", "gf"])
            for t in range(NT):
                p = t % 2
                self.rmsnorm(yacc[:, t, :], "y%d" % t, gf, "gf", ot[p], "ot%d" % p, junk, "fjunk",
                             stf[:, 4 * t:4 * t + 4], "sl0")
                kb.dma(self.out[b, t * 128:(t + 1) * 128, :], ot[p], reads=["ot%d" % p], writes=["out"])

    def phaseE_sparse(self, l, final):
        kb = self.kb
        pb = self.pb
        I32 = mybir.dt.int32
        NTT, NBLK = self.NTT, self.NBLK
        IO = bass.IndirectOffsetOnAxis
        a = Arena(self.arena_ap, self.r1_end, 208000)
        R = a.t([128, NTT, 66])
        OHs = a.t([128, NTT, 32])
        rk = a.t([128, NTT, 32])
        tmp = a.t([128, NTT, 32])
        cnt = a.t([128, 32]); nblk = a.t([128, 32]); pend = a.t([128, 32]); pstart = a.t([128, 32])
        cmp_ = a.t([128, max(NBLK, 32), 32])
        sl_f = a.t([128, NTT, 2])
        sl_i = a.t([128, NTT, 2], I32)
        be_f = a.t([128, NBLK])
        widx = a.t([128, NBLK], I32)
        stg = [a.t([128, 4096]) for _ in range(3)]
        wgb = [a.t([128, 8, EH], BF16) for _ in range(2)]
        wub = [a.t([128, 8, EH], BF16) for _ in range(2)]
        wdb = [a.t([128, 4, D], BF16) for _ in range(2)]
        xblk = [a.t([128, D], BF16) for _ in range(2)]
        xT = [a.t([128, 8, 128], BF16) for _ in range(2)]
        ssb = a.t([128, 512]); actb = a.t([128, 512], BF16); actT = a.t([128, 4, 128], BF16)
        yblk = [a.t([128, D]) for _ in range(2)]
        pbT = pb[7][:, 0:512].bitcast(BF16)
        for b in range(self.NB):
            kb.dma(R[:, b * NT:(b + 1) * NT, :], self.route[b].rearrange("(t p) c -> p t c", p=128), writes=["R"])
        self.P("dve", "tensor_tensor", OHs, R[:, :, 0:32], R[:, :, 32:64], ALU.add, r=["R"], w=["OHs"])
        for t in range(NTT):
            bank = pb[t // 16]
            bk = "pb%d" % (t // 16)
            cs = slice((t % 16) * 32, (t % 16 + 1) * 32)
            for t2 in range(t):
                self.mm(bank[:, cs], self.ones_f, OHs[:, t2, :], t2 == 0, False, r=["ones_f", "OHs"], w=[bk])
            self.mm(bank[:, cs], self.m_strict, OHs[:, t, :], t == 0, True, r=["cst", "OHs"], w=[bk])
        for t in range(NTT):
            self.mm(pb[2][:, 0:32], self.ones_f, OHs[:, t, :], t == 0, t == NTT - 1, r=["ones_f", "OHs"], w=["pb2"])
        for g in range(NTT // 16):
            self.actf(rk[:, g * 16:(g + 1) * 16, :].rearrange("p t e -> p (t e)"), pb[g], AF.Copy, r=["pb%d" % g], w=["rk"])
        self.P("dve", "tensor_copy", cnt, pb[2][:, 0:32], r=["pb2"], w=["cnt"])
        SB_ = self.S
        self.P("dve", "tensor_single_scalar", cnt, cnt, 128.0 / SB_, ALU.mult, r=["cnt"], w=["cnt"])
        self.P("dve", "tensor_tensor", cmp_[:, 0:32, :], cnt.unsqueeze(2).to_broadcast([128, 32, 32]),
               self.thr[:, 0:32].unsqueeze(1).to_broadcast([128, 32, 32]), ALU.is_gt, r=["cnt", "cex"], w=["cmp"])
        self.P("dve", "tensor_reduce", nblk, cmp_[:, 0:32, :], AX.X, ALU.add, r=["cmp"], w=["nblk"])
        self.P("dve", "tensor_single_scalar", nblk, nblk, float(SB_), ALU.mult, r=["nblk"], w=["nblk"])
        self.P("dve", "tensor_tensor_scan", pend, self.ones_f[:, 0:32], nblk, 0.0, ALU.mult, ALU.add, r=["ones_f", "nblk"], w=["pend"])
        self.P("dve", "tensor_tensor", pstart, pend, nblk, ALU.subtract, r=["pend", "nblk"], w=["pstart"])
        self.P("dve", "tensor_tensor", rk, rk, pstart.unsqueeze(1).to_broadcast([128, NTT, 32]), ALU.add, r=["rk", "pstart"], w=["rk"])
        for k in range(2):
            self.P("dve", "tensor_tensor", tmp, rk, R[:, :, k * 32:(k + 1) * 32], ALU.mult, r=["rk", "R"], w=["tmp"])
            self.P("dve", "tensor_reduce", sl_f[:, :, k], tmp, AX.X, ALU.add, r=["tmp"], w=["sl_f"])
        self.P("dve", "tensor_copy", sl_i, sl_f, r=["sl_f"], w=["sl_i"])
        self.P("dve", "tensor_single_scalar", pstart, pend, 128.0 / SB_, ALU.mult, r=["pend", "rk"], w=["pstart"])
        self.P("dve", "tensor_tensor", cmp_[:, 0:NBLK, :], self.thr[:, 0:NBLK].unsqueeze(2).to_broadcast([128, NBLK, 32]),
               pstart.unsqueeze(1).to_broadcast([128, NBLK, 32]), ALU.is_ge, r=["cex", "pstart"], w=["cmp"])
        self.P("dve", "tensor_reduce", be_f, cmp_[:, 0:NBLK, :], AX.X, ALU.add, r=["cmp"], w=["be_f"])
        self.P("dve", "tensor_scalar", be_f, be_f, 31.0, 128.0, ALU.min, ALU.mult, r=["be_f"], w=["be_f"])
        self.P("dve", "tensor_scalar", be_f, be_f, self.iota_p, float(l * NE * 128), ALU.add, ALU.add, r=["be_f", "cex"], w=["be_f"])
        self.P("dve", "tensor_copy", widx, be_f, r=["be_f"], w=["widx"])
        if self.stop in ("E0", "E1", "E2"):
            kb.dma(self.dbg_sl, sl_i.rearrange("p t k -> p (t k)"), reads=["sl_i"], writes=["dbg_sl"])
            kb.dma(self.dbg_widx, widx, reads=["widx"], writes=["dbg_widx"])
            for i_, (tl, tk) in enumerate(((cnt, "cnt"), (nblk, "nblk"), (pend, "pend"), (pstart, "pstart"))):
                kb.dma(self.dbg_misc[:, i_, :], tl, reads=[tk], writes=["dbg_misc"])
        if self.stop == "E0":
            return
        for t in range(NTT):
            b, tt = t // NT, t % NT
            p = t % 2
            kb.dma(xblk[p], self.h2tok[b, tt * 128:(tt + 1) * 128, :], writes=["xblk%d" % p])
            for k in range(2):
                kb.idma(self.xb, IO(ap=sl_i[:, t, k:k + 1], axis=0), xblk[p], None,
                        reads=["xblk%d" % p, "sl_i"], writes=["xb"])
        if self.stop == "E1":
            return
        ssb2 = [ssb, a.t([128, 512])]
        actb2 = [actb, a.t([128, 512], BF16)]
        actT2 = [actT, a.t([128, 4, 128], BF16)]

        def load_weights(j):
            p = j % 2
            for wi, (wsrc, wdst, wk) in enumerate(((self.w_gate, wgb[p], "wgb%d" % p), (self.w_up, wub[p], "wub%d" % p),
                                                   (self.w_down, wdb[p], "wdb%d" % p))):
                kb.idma(stg[wi], None, wsrc.rearrange("l r c -> (l r) c"), IO(ap=widx[:, j:j + 1], axis=0), reads=["widx"], writes=["stg%d" % wi])
                dstv = wdst.rearrange("p k n -> p (k n)")
                if wi == 0:
                    self.actf(dstv, stg[wi], AF.Copy, r=["stg%d" % wi], w=[wk])
                elif wi == 1:
                    self.P("dve", "tensor_copy", dstv, stg[wi], r=["stg%d" % wi], w=[wk])
                else:
                    self.actf(dstv[:, 0:2048], stg[wi][:, 0:2048], AF.Copy, r=["stg%d" % wi], w=[wk])
                    self.P("dve", "tensor_copy", dstv[:, 2048:4096], stg[wi][:, 2048:4096], r=["stg%d" % wi], w=[wk])

        def sub_gen(j, sub, q):
            p = j % 2
            row0 = j * self.S + sub * 128
            pT = pb[6 + q][:, 0:512].bitcast(BF16)
            pTk = "pb%d" % (6 + q)
            pg, pu, po = pb[2 * q], pb[2 * q + 1], pb[4 + q]
            pgk, puk, pok = "pb%d" % (2 * q), "pb%d" % (2 * q + 1), "pb%d" % (4 + q)
            kb.dma(xblk[q], self.xb[row0:row0 + 128, :], reads=["xb"], writes=["xblk%d" % q])
            yield
            for kc in range(8):
                self.P("pe", "transpose", pT[:, kc * 128:(kc + 1) * 128], xblk[q][:, kc * 128:(kc + 1) * 128],
                       self.ident_bf, r=["xblk%d" % q, "ident_bf"], w=[pTk])
            yield
            self.actf(xT[q].rearrange("p k n -> p (k n)"), pT, AF.Copy, r=[pTk], w=["xT%d" % q])
            yield
            for kc in range(8):
                self.mm(pg, xT[q][:, kc, :], wgb[p][:, kc, :], kc == 0, kc == 7, r=["xT%d" % q, "wgb%d" % p], w=[pgk])
            for kc in range(8):
                self.mm(pu, xT[q][:, kc, :], wub[p][:, kc, :], kc == 0, kc == 7, r=["xT%d" % q, "wub%d" % p], w=[puk])
            yield
            self.actf(ssb2[q], pg, AF.Silu, r=[pgk], w=["ssb%d" % q])
            yield
            self.P("dve", "tensor_tensor", actb2[q], ssb2[q], pu, ALU.mult, r=["ssb%d" % q, puk], w=["actb%d" % q])
            yield
            for m in range(4):
                self.P("pe", "transpose", pT[:, m * 128:(m + 1) * 128], actb2[q][:, m * 128:(m + 1) * 128],
                       self.ident_bf, r=["actb%d" % q, "ident_bf"], w=[pTk])
            yield
            self.actf(actT2[q].rearrange("p k n -> p (k n)"), pT[:, 0:512], AF.Copy, r=[pTk], w=["actT%d" % q])
            yield
            for hf in range(2):
                for m in range(4):
                    self.mm(po, actT2[q][:, m, :], wdb[p][:, m, hf * 512:(hf + 1) * 512], m == 0, m == 3,
                            r=["actT%d" % q, "wdb%d" % p], w=[pok])
                yield
                if hf == 0:
                    self.actf(yblk[q][:, 0:512], po, AF.Copy, r=[pok], w=["yblk%d" % q])
                else:
                    self.P("dve", "tensor_copy", yblk[q][:, 512:1024], po, r=[pok], w=["yblk%d" % q])
                yield
            kb.dma(self.yb[row0:row0 + 128, :], yblk[q], reads=["yblk%d" % q], writes=["yb"])

        def run2(gens):
            gens = list(gens)
            while gens:
                for g in list(gens):
                    try:
                        next(g)
                    except StopIteration:
                        gens.remove(g)

        load_weights(0)
        nsub = self.S // 128
        for j in range(NBLK):
            for pr in range(nsub // 2):
                run2([sub_gen(j, 2 * pr + q, q) for q in range(2)])
                if pr == 0 and j + 1 < NBLK:
                    load_weights(j + 1)
        if self.stop == "E2":
            return
        xt = [stg[0][:, 0:D], stg[0][:, D:2 * D]]
        y1 = [stg[1][:, 0:D], stg[1][:, D:2 * D]]
        y2 = [stg[2][:, 0:D], stg[2][:, D:2 * D]]
        ot = [stg[0][:, 2 * D:3 * D], stg[0][:, 3 * D:4 * D]]
        junk = stg[1][:, 2 * D:3 * D]
        gf = stg[2][:, 2 * D:3 * D]
        stf = a.t([128, 4 * NTT])
        kb.barrier()
        if final:
            kb.dma(gf, self.final_g.partition_broadcast(128), writes=["gf"])
        for t in range(NTT):
            b, tt = t // NT, t % NT
            p = t % 2
            rows = slice(tt * 128, (tt + 1) * 128)
            kb.dma(xt[p], self.xres[b, rows, :], writes=["cxt%d" % p])
            kb.idma(y1[p], None, self.yb, IO(ap=sl_i[:, t, 0:1], axis=0), reads=["sl_i"], writes=["cy1%d" % p])
            kb.idma(y2[p], None, self.yb, IO(ap=sl_i[:, t, 1:2], axis=0), reads=["sl_i"], writes=["cy2%d" % p])
            self.P("dve", "scalar_tensor_tensor", xt[p], y1[p], R[:, t, 64:65], xt[p], ALU.mult, ALU.add,
                   r=["cy1%d" % p, "R", "cxt%d" % p], w=["cxt%d" % p])
            self.P("dve", "scalar_tensor_tensor", xt[p], y2[p], R[:, t, 65:66], xt[p], ALU.mult, ALU.add,
                   r=["cy2%d" % p, "R", "cxt%d" % p], w=["cxt%d" % p])
            if not final:
                kb.dma(self.xres[b, rows, :], xt[p], reads=["cxt%d" % p], writes=["xres"])
            else:
                self.rmsnorm(xt[p], "cxt%d" % p, gf, "gf", ot[p], "cot%d" % p, junk, "cjunk", stf[:, 4 * t:4 * t + 4], "stf")
                kb.dma(self.out[b, rows, :], ot[p], reads=["cot%d" % p], writes=["out"])

    def build(self):
        kb = self.kb
        if self.sparse:
            z = self.arena_ap[:, 45000:45000 + 512].bitcast(BF16)
            self.P("pool", "memset", z, 0.0, w=["zz"])
            for j in range(self.NBLK * self.S // 128):
                kb.dma(self.xb[j * 128:(j + 1) * 128, :], z, reads=["zz"], writes=["xb"])
            kb.barrier()
        for l in range(self.NL):
            self.load_layer(l)
            for b in range(self.NB):
                src = self.x_in[b] if l == 0 else self.xres[b]
                self.phaseA(l, b, src)
                kb.barrier()
                if self.stop == "A":
                    return self.finish()
                self.phaseB_attn(l, b)
                kb.barrier()
                if self.stop == "B1":
                    return self.finish()
                self.phaseB_rwkv(l, b)
                kb.barrier()
                if self.stop == "B2":
                    return self.finish()
                self.phaseD(l, b, src)
                kb.barrier()
                if self.stop == "D":
                    return self.finish()
                if not self.sparse:
                    self.phaseE(l, b, final=(l == self.NL - 1))
                    kb.barrier()
            if self.sparse:
                self.phaseE_sparse(l, final=(l == self.NL - 1))
                kb.barrier()
        return self.finish()

    def finish(self):
        self.kb.barrier()
        return self


def prep_inputs(inp, b0, nb, NL=4):
    f = lambda a: np.ascontiguousarray(a, dtype=np.float32)

    def pm(w, k, n):
        L = w.shape[0]
        w5 = np.reshape(np.asarray(w, dtype=np.float32), (L, NE, k, 128, n))
        return np.ascontiguousarray(np.transpose(w5, (0, 1, 3, 2, 4))).reshape(L, NE * 128, k * n)
    m = {
        "x": f(inp["x"][b0:b0 + nb]),
        "consts": make_consts(),
        "norm1_g": f(inp["norm1_g"][:NL]),
        "w_in": f(inp["w_in"][:NL]),
        "gm_wT": f(np.transpose(inp["gm_w_s"][:NL], (0, 1, 3, 2))),
        "gm_bT": f(np.transpose(inp["gm_b"][:NL], (0, 2, 1))),
        "rw_mu": f(inp["rw_mu"][:NL]),
        "rw_pv": f(np.concatenate([
            np.transpose(np.reshape(inp["rw_mu"][:NL, 0:768], (NL, 12, 64)), (0, 2, 1))] + [
            np.transpose(np.reshape(np.reshape(inp[k][:NL], (NL, 256)), (NL, 4, 64)), (0, 2, 1))
            for k in ("rw_w0", "rw_a0", "rw_k_k", "rw_k_a", "rw_r_k")], axis=2)),
        "rw_w2": f(inp["rw_w2"][:NL]),
        "rw_a2": f(inp["rw_a2"][:NL]),
        "rw_g2": f(inp["rw_g2"][:NL]),
        "rw_gn_w": f(inp["rw_gn_w"][:NL]),
        "rw_gn_b": f(inp["rw_gn_b"][:NL]),
        "fx_b_f": f(inp["fx_b_f"][:NL]),
        "w_out": f(inp["w_out"][:NL]),
        "norm2_g": f(inp["norm2_g"][:NL]),
        "router_w": f(np.concatenate([inp["router_group_w"][:NL], inp["router_expert_w"][:NL]], axis=2)),
        "router_b": f(np.concatenate([inp["router_group_b"][:NL], inp["router_expert_b"][:NL]], axis=1)),
        "exp_w_gate": pm(inp["exp_w_gate"][:NL], 8, EH),
        "exp_w_up": pm(inp["exp_w_up"][:NL], 8, EH),
        "exp_w_down": pm(inp["exp_w_down"][:NL], 4, D),
        "final_norm_g": f(np.reshape(inp["final_norm_g"], (1, D))),
    }
    return m


def kernel(**inputs):
    inp = {k: np.asarray(v) for k, v in inputs.items()}
    n = 8
    nb = 2
    prog = Prog(NL=4, NB=nb).build()
    shared = prep_inputs(inp, 0, nb)
    in_maps = []
    for c in range(n):
        m = dict(shared)
        m["x"] = np.ascontiguousarray(inp["x"][c * nb:(c + 1) * nb], dtype=np.float32)
        in_maps.append(m)
    res = run_bass_kernel_spmd(prog.nc, in_maps, core_ids=list(range(n)))
    out = np.concatenate([np.asarray(r["out"]) for r in res.results], axis=0)
    return out.astype(np.float32)
```

```python
import numpy as np
import concourse.bass as bass
import concourse.mybir as mybir
from concourse.bass_utils import run_bass_kernel_spmd

F32 = mybir.dt.float32
BF16 = mybir.dt.bfloat16
AF = mybir.ActivationFunctionType
ALU = mybir.AluOpType
AX = mybir.AxisListType

T = 2048
D = 1024
NT = 16
DIN = 3076
NE = 32
EH = 512
NDMA_SEMS = 12
RMS_EPS = 1e-6
LN_EPS = 1e-5
GN_EPS = 64e-5
NEGM = -30000.0
import os
MAXOPS = int(os.environ.get("KB_MAXOPS", "1000000000"))


class KB:
    def __init__(self):
        self.nc = bass.Bass("TRN2", target_bir_lowering=False)
        nc = self.nc
        self.engs = {"pe": nc.tensor, "act": nc.scalar, "dve": nc.vector,
                     "pool": nc.gpsimd, "sp": nc.sync}
        self.sem = {}
        self.cnt = {}
        for e in self.engs:
            self.sem[e] = nc.alloc_semaphore("s_" + e)
            self.cnt[e] = 0
        self.dsem = {}
        self.dcnt = {}
        self.drr = {}
        for q in ("sp", "pool"):
            self.dsem[q] = [nc.alloc_semaphore("d_%s%d" % (q, i)) for i in range(NDMA_SEMS)]
            self.dcnt[q] = [0] * NDMA_SEMS
            self.drr[q] = 0
        self.waited = {e: {} for e in self.engs}
        self.lastw = {}
        self.readers = {}
        self.nins = 0

    def _semof(self, sk):
        if sk[0] == "c":
            return self.sem[sk[1]]
        return self.dsem[sk[1]][sk[2]]

    def _wait(self, eng, tok):
        sk, val = tok
        if eng == "pe" and sk == ("c", "pe"):
            return
        w = self.waited[eng]
        if w.get(sk, 0) >= val:
            return
        self.engs[eng].wait_ge(self._semof(sk), val)
        w[sk] = val
        self.nins += 1

    def _deps(self, eng, reads, writes):
        for k in reads:
            t = self.lastw.get(k)
            if t is not None:
                self._wait(eng, t)
        for k in writes:
            t = self.lastw.get(k)
            if t is not None:
                self._wait(eng, t)
            for t in self.readers.get(k, ()):
                self._wait(eng, t)

    def _record(self, tok, reads, writes):
        for k in reads:
            lst = self.readers.setdefault(k, [])
            lst[:] = [t for t in lst if t[0] != tok[0]]
            lst.append(tok)
        for k in writes:
            self.lastw[k] = tok
            self.readers[k] = []

    def op(self, eng, fn, reads=(), writes=()):
        self.nops = getattr(self, "nops", 0) + 1
        if self.nops > MAXOPS:
            return None
        pr = [k for k in reads if k.startswith("pb")]
        if pr:
            reads = [k for k in reads if not k.startswith("pb")]
            writes = list(writes) + pr
        self._deps(eng, reads, writes)
        ins = fn(self.engs[eng])
        self.cnt[eng] += 1
        ins.then_inc(self.sem[eng], 1)
        tok = (("c", eng), self.cnt[eng])
        self._record(tok, reads, writes)
        self.nins += 1
        return tok

    def dma(self, out, in_, reads=(), writes=(), q="sp", **kw):
        self.nops = getattr(self, "nops", 0) + 1
        if self.nops > MAXOPS:
            return None
        i = self.drr[q]
        self.drr[q] = (i + 1) % NDMA_SEMS
        sk = ("d", q, i)
        if self.dcnt[q][i] > 0:
            self._wait(q, (sk, self.dcnt[q][i]))
        self._deps(q, reads, writes)
        ins = self.engs[q].dma_start(out=out, in_=in_, **kw)
        self.dcnt[q][i] += 16
        ins.then_inc(self.dsem[q][i], 16)
        tok = (sk, self.dcnt[q][i])
        self._record(tok, reads, writes)
        self.nins += 1
        return tok

    def idma(self, out, out_off, in_, in_off, reads=(), writes=()):
        q = "pool"
        self.nops = getattr(self, "nops", 0) + 1
        if self.nops > MAXOPS:
            return None
        i = self.drr[q]
        self.drr[q] = (i + 1) % NDMA_SEMS
        sk = ("d", q, i)
        if self.dcnt[q][i] > 0:
            self._wait(q, (sk, self.dcnt[q][i]))
        self._deps(q, reads, writes)
        ins = self.nc.gpsimd.indirect_dma_start(out=out, out_offset=out_off, in_=in_, in_offset=in_off)
        self.dcnt[q][i] += 16
        ins.then_inc(self.dsem[q][i], 16)
        tok = (sk, self.dcnt[q][i])
        self._record(tok, reads, writes)
        self.nins += 1
        return tok

    def barrier(self):
        toks = [(("c", e), self.cnt[e]) for e in self.engs if self.cnt[e] > 0]
        for q in self.dsem:
            for i in range(NDMA_SEMS):
                if self.dcnt[q][i] > 0:
                    toks.append((("d", q, i), self.dcnt[q][i]))
        for e in self.engs:
            for t in toks:
                if t[0] == ("c", e):
                    continue
                self._wait(e, t)
        self.lastw = {}
        self.readers = {}

    def dram(self, name, shape, dt=F32, kind="Internal"):
        return self.nc.dram_tensor(name, list(shape), dt, kind=kind).ap()


def _dtsize(dt):
    return 2 if dt == BF16 else 4


class Arena:
    def __init__(self, base_ap_f32, start, end):
        self.A = base_ap_f32
        self.cur = start
        self.end = end

    def t(self, shape, dt=F32, parts=128):
        n = 1
        for s in shape[1:]:
            n *= s
        nbytes = (n * _dtsize(dt) + 31) // 32 * 32
        off = self.cur
        self.cur += nbytes
        assert self.cur <= self.end, ("arena overflow", self.cur, self.end)
        v = self.A[0:shape[0], off // 4:(off + nbytes) // 4]
        if dt != F32:
            v = v.bitcast(dt)
        v = v[:, 0:n]
        if len(shape) == 3:
            v = v.rearrange("p (a b) -> p a b", a=shape[1])
        elif len(shape) == 4:
            v = v.rearrange("p (a b c) -> p a b c", a=shape[1], b=shape[2])
        return v


def make_consts():
    j = np.arange(128)[:, None]
    t = np.arange(128)[None, :]
    c = np.zeros((128, 8, 128), np.float32)
    c[:, 0] = np.eye(128)
    c[:, 1] = np.where(j >= t, -1.0, 0.0)
    c[:, 2] = -1.0
    c[:, 3] = np.where(j < t, 0.0, NEGM)
    c[:, 4] = np.where(j <= t, 0.0, NEGM)
    c[:, 5] = np.where(j < t, 1.0, 0.0)
    c[:, 6] = np.where(j <= t, 1.0, 0.0)
    c[:, 7] = np.where(t < j, 1.0, 0.0)
    ex = np.zeros((128, 256), np.float32)
    ex[:, 0:128] = 128.0 * np.arange(128)[None, :]
    ex[:, 128] = np.arange(128)
    return np.concatenate([c.reshape(128, 1024), ex], axis=1)


class Prog:
    def __init__(self, NL=4, NB=2, dbg=(), stop=None, moe_experts=NE, sparse=True):
        self.NL, self.NB = NL, NB
        self.stop = stop
        self.moe_experts = moe_experts
        self.sparse = sparse
        self.NTT = NB * NT
        self.S = 256
        self.NBLK = NB * T * 2 // self.S + 32
        kb = self.kb = KB()
        nc = self.nc = kb.nc
        dk = lambda n: ("ExternalOutput" if n in dbg else "Internal")
        I = lambda n, s: kb.dram(n, s, F32, kind="ExternalInput")
        self.x_in = I("x", [NB, T, D])
        self.consts_d = I("consts", [128, 1280])
        self.norm1_g = I("norm1_g", [NL, D])
        self.w_in = I("w_in", [NL, D, DIN])
        self.gm_wT = I("gm_wT", [NL, 4, 128, 128])
        self.gm_bT = I("gm_bT", [NL, 128, 4])
        self.rw_mu = I("rw_mu", [NL, 1024])
        self.rw_pv = I("rw_pv", [NL, 64, 32])
        self.rw_w2 = I("rw_w2", [NL, 64, 256])
        self.rw_a2 = I("rw_a2", [NL, 64, 256])
        self.rw_g2 = I("rw_g2", [NL, 128, 256])
        self.rw_gn_w = I("rw_gn_w", [NL, 256])
        self.rw_gn_b = I("rw_gn_b", [NL, 256])
        self.fx_b_f = I("fx_b_f", [NL, 4])
        self.w_out = I("w_out", [NL, D, D])
        self.norm2_g = I("norm2_g", [NL, D])
        self.router_w = I("router_w", [NL, D, 36])
        self.router_b = I("router_b", [NL, 36])
        if sparse:
            self.w_gate = I("exp_w_gate", [NL, NE * 128, 4096])
            self.w_up = I("exp_w_up", [NL, NE * 128, 4096])
            self.w_down = I("exp_w_down", [NL, NE * 128, 4096])
        else:
            self.w_gate = I("exp_w_gate", [NL, NE, D, EH])
            self.w_up = I("exp_w_up", [NL, NE, D, EH])
            self.w_down = I("exp_w_down", [NL, NE, EH, D])
        self.final_g = I("final_norm_g", [1, D])
        self.out = kb.dram("out", [NB, T, D], F32, kind="ExternalOutput")
        S = lambda n, s, dt=F32: kb.dram(n, s, dt, kind=dk(n))
        self.xres = S("xres", [NB, T, D])
        self.qkT = S("qkT", [NB, 16, 64, T], BF16)
        self.vtok = S("vtok", [NB, T, 512], BF16)
        self.rwT = S("rwT", [NB, 1024, T])
        self.fgT = S("fgT", [NB, 4, T])
        self.cumd = S("cumd", [NB, 4, T])
        self.mixT = S("mixT", [NB, 1024, T], BF16)
        self.h2T = S("h2T", [NB, 1024, T], BF16)
        self.gates = S("gates", [NB, T, 32])
        self.h2tok = S("h2tok", [NB, T, D], BF16)
        self.route = S("route", [NB, T, 66])
        self.xb = S("xb", [self.NBLK * self.S, D], BF16)
        self.yb = S("yb", [self.NBLK * self.S, D])
        self.dbg_sl = S("dbg_sl", [128, self.NTT * 2], mybir.dt.int32)
        self.dbg_widx = S("dbg_widx", [128, self.NBLK], mybir.dt.int32)
        self.dbg_misc = S("dbg_misc", [128, 4, 32])
        arena = nc.alloc_sbuf_tensor("arena", [128, 52000], F32)
        self.arena_ap = arena[:]
        self.pb = [nc.alloc_psum_tensor("pb%d" % i, [128, 512], F32)[:] for i in range(8)]
        self.R1 = Arena(self.arena_ap, 0, 40000)
        self.setup_consts()

    def P(self, eng, meth, *a, r=(), w=(), **kw):
        return self.kb.op(eng, lambda e: getattr(e, meth)(*a, **kw), r, w)

    def mm(self, out, lhsT, rhs, start, stop, r=(), w=(), sgc=False):
        if sgc:
            return self.kb.op("pe", lambda e: e.matmul(out, lhsT, rhs, start=start, stop=stop, skip_group_check=True), r, w)
        return self.kb.op("pe", lambda e: e.matmul(out, lhsT, rhs, start=start, stop=stop), r, w)

    def actf(self, out, in_, func, r=(), w=(), eng="act", **kw):
        return self.kb.op("act", lambda e: e.activation(out, in_, func, **kw), r, w)

    def col(self, dram_row):
        return dram_row.rearrange("(p o) -> p o", o=1)

    def setup_consts(self):
        kb = self.kb
        a = self.R1
        self.cst = a.t([128, 8, 128])
        kb.dma(self.cst.rearrange("p a b -> p (a b)"), self.consts_d[:, 0:1024], writes=["cst"])
        self.cex = a.t([128, 256])
        kb.dma(self.cex, self.consts_d[:, 1024:1280], writes=["cex"])
        self.thr = self.cex[:, 0:128]
        self.iota_p = self.cex[:, 128:129]
        self.m_strict = self.cst[:, 5, :]
        c = self.cst
        self.ident = c[:, 0, :]
        self.ntri = c[:, 1, :]
        self.negones = c[:, 2, :]
        self.mask2 = c[:, 5:7, :]
        self.m_incl = c[:, 6, :]
        self.m_lows = c[:, 7, :]
        self.ident_bf = a.t([128, 128], BF16)
        self.mbs_bf = a.t([128, 128], BF16)
        self.mbi_bf = a.t([128, 128], BF16)
        self.P("dve", "tensor_copy", self.ident_bf, c[:, 0, :], r=["cst"], w=["ident_bf"])
        self.P("dve", "tensor_copy", self.mbs_bf, c[:, 3, :], r=["cst"], w=["mbs_bf"])
        self.P("dve", "tensor_copy", self.mbi_bf, c[:, 4, :], r=["cst"], w=["mbi_bf"])
        self.ones_f = a.t([128, 128])
        self.P("pool", "memset", self.ones_f, 1.0, w=["ones_f"])
        self.ones_bf = a.t([128, 64], BF16)
        self.P("pool", "memset", self.ones_bf, 1.0, w=["ones_bf"])
        self.reset = a.t([64, 512])
        self.P("pool", "memset", self.reset, 1.0, w=["reset"])
        self.P("pool", "memset", self.reset.rearrange("p (c t) -> p c t", c=4)[:, :, 0:1], 0.0, w=["reset"])
        self.CK = ["cst", "ident_bf", "mbs_bf", "mbi_bf", "ones_f", "ones_bf", "reset", "ones4"]
        self.g1bc = a.t([128, D])
        self.g2bc = a.t([128, D])
        self.rbbc = a.t([128, 36])
        self.wr = a.t([128, 8, 36])
        self.gmw = a.t([128, 4, 128])
        self.gmb = a.t([128, 4])
        self.gnw = a.t([128, 256])
        self.gnb = a.t([128, 256])
        self.w2s = a.t([64, 256])
        self.a2s = a.t([64, 256])
        self.g2s = a.t([128, 256])
        self.pv = a.t([64, 40])
        self.mu_w = a.t([64, 1])
        self.mu_a = a.t([64, 1])
        self.mu_g = a.t([128, 1])
        self.fbf = a.t([4, 1])
        self.nfbf = a.t([4, 1])
        self.Mst = a.t([64, 4, 64])
        self.r1_end = a.cur

    def keepconsts(self):
        pass

    def load_layer(self, l):
        kb = self.kb
        bc = lambda row: row.partition_broadcast(128)
        kb.dma(self.g1bc, bc(self.norm1_g[l:l + 1, :]), writes=["g1bc"])
        kb.dma(self.g2bc, bc(self.norm2_g[l:l + 1, :]), writes=["g2bc"])
        kb.dma(self.rbbc, bc(self.router_b[l:l + 1, :]), writes=["rbbc"])
        kb.dma(self.wr, self.router_w[l].rearrange("(k p) n -> p k n", p=128), writes=["wr"])
        kb.dma(self.gmw, self.gm_wT[l].rearrange("g s t -> s g t"), writes=["gmw"])
        self.P("dve", "tensor_tensor", self.gmw, self.gmw, self.m_incl.unsqueeze(1).to_broadcast([128, 4, 128]),
               ALU.mult, r=["gmw"], w=["gmw"])
        kb.dma(self.gmb, self.gm_bT[l], writes=["gmb"])
        kb.dma(self.gnw, bc(self.rw_gn_w[l:l + 1, :]), writes=["gnw"])
        kb.dma(self.gnb, bc(self.rw_gn_b[l:l + 1, :]), writes=["gnb"])
        kb.dma(self.w2s, self.rw_w2[l], writes=["w2s"])
        kb.dma(self.a2s, self.rw_a2[l], writes=["a2s"])
        kb.dma(self.g2s, self.rw_g2[l], writes=["g2s"])
        kb.dma(self.pv[:, 0:32], self.rw_pv[l], writes=["pv"])
        self.P("dve", "tensor_scalar", self.pv[:, 32:36], self.pv[:, 24:28], -1.0, 1.0, ALU.mult, ALU.add,
               r=["pv"], w=["pv"])
        kb.dma(self.mu_w, self.col(self.rw_mu[l, 768:832]), writes=["mu_w"])
        kb.dma(self.mu_a, self.col(self.rw_mu[l, 832:896]), writes=["mu_a"])
        kb.dma(self.mu_g, self.col(self.rw_mu[l, 896:1024]), writes=["mu_g"])
        kb.dma(self.fbf, self.col(self.fx_b_f[l]), writes=["fbf"])
        self.P("dve", "tensor_single_scalar", self.nfbf, self.fbf, -1.0, ALU.mult, r=["fbf"], w=["nfbf"])

    def rmsnorm(self, xt, xk, gbc, gk, out, outk, tmp, tmpk, st, stk):
        self.actf(tmp, xt, AF.Square, r=[xk], w=[tmpk, stk], accum_out=st[:, 0:1])
        self.actf(st[:, 1:2], st[:, 0:1], AF.Ln, r=[stk], w=[stk], scale=1.0 / D, bias=RMS_EPS)
        self.actf(st[:, 2:3], st[:, 1:2], AF.Exp, r=[stk], w=[stk], scale=-0.5)
        self.P("dve", "scalar_tensor_tensor", out, xt, st[:, 2:3], gbc, ALU.mult, ALU.mult,
               r=[xk, stk, gk], w=[outk])

    def phaseA(self, l, b, src):
        kb = self.kb
        pb = self.pb
        a = Arena(self.arena_ap, self.r1_end, 208000)
        win = a.t([128, 8, DIN], BF16)
        wstg = [a.t([128, DIN]) for _ in range(2)]
        for kc in range(8):
            si = kc % 2
            sk = "wstg%d" % si
            kb.dma(wstg[si], self.w_in[l, kc * 128:(kc + 1) * 128, :], writes=[sk])
            self.actf(win[:, kc, 0:1024], wstg[si][:, 0:1024], AF.Copy, r=[sk], w=["win%d" % kc])
            self.P("dve", "tensor_copy", win[:, kc, 1024:2048], wstg[si][:, 1024:2048], r=[sk], w=["win%d" % kc])
            self.P("pool", "tensor_copy", win[:, kc, 2048:DIN], wstg[si][:, 2048:DIN], r=[sk], w=["win%d" % kc])
        WK = ["win%d" % kc for kc in range(8)]
        hT4 = [a.t([128, 8, 512], BF16) for _ in range(2)]
        chains = []
        for q in range(2):
            C = dict(xt=a.t([128, D]), junk=a.t([128, D]), hb=a.t([128, D], BF16), st=a.t([128, 4]), vsb=a.t([128, 512], BF16),
                     xg=a.t([128, 512]), x2=a.t([128, 512]), u1=a.t([128, 512]), sg=a.t([128, 512]), hid=a.t([128, 512]),
                     gst=a.t([128, 16]), vc=a.t([128, 256]), sq=a.t([128, 256]), vn=a.t([128, 256]), tmpg=a.t([128, 256]),
                     ygm=a.t([128, 256]), ygT=a.t([128, 2, 128], BF16))
            chains.append(C)
        stg_bf = [a.t([64, 512], BF16) for _ in range(2)]
        stg_f = [a.t([128, 512]) for _ in range(2)]
        fm = []
        for h in range(4):
            fm.append((h * 64, 64, "qk", h, 1.0))
            fm.append((256 + h * 64, 64, "qk", 4 + h, 0.125))
            fm.append((2304 + h * 64, 64, "qk", 8 + h, 1.0))
            fm.append((2560 + h * 64, 64, "qk", 12 + h, 0.125))
        for j in range(12):
            fm.append((1280 + j * 64, 64, "rw", j * 64, 1.0))
        fm.append((2048, 64, "rw", 768, 1.0))
        fm.append((2112, 64, "rw", 832, 1.0))
        fm.append((2176, 128, "rw", 896, 1.0))
        fm.append((3072, 4, "fg", 0, 1.0))

        def tile_gen(t, q):
            C = chains[q]
            K = lambda n: "%s_c%d" % (n, q)
            g4, tt = t // 4, t % 4
            hT = hT4[g4 % 2]
            hTk = "hT4_%d" % (g4 % 2)
            xt, junk, hb, st, vsb = C["xt"], C["junk"], C["hb"], C["st"], C["vsb"]
            xg, x2, u1, sg, hid, gst = C["xg"], C["x2"], C["u1"], C["sg"], C["hid"], C["gst"]
            vc, sq, vn, tmpg, ygm, ygT = C["vc"], C["sq"], C["vn"], C["tmpg"], C["ygm"], C["ygT"]
            ptk_, pgk_, pTk_ = "pb%d" % q, "pb%d" % (2 + q), "pb%d" % (4 + q)
            ptok, pgm = pb[q], pb[2 + q]
            pT = pb[4 + q][:, 0:512].bitcast(BF16)
            rows = slice(t * 128, (t + 1) * 128)
            kb.dma(xt, src[rows, :], writes=[K("xt")])
            yield
            self.actf(junk, xt, AF.Square, r=[K("xt")], w=[K("junk"), K("st")], accum_out=st[:, 0:1])
            yield
            self.actf(st[:, 1:2], st[:, 0:1], AF.Ln, r=[K("st")], w=[K("st")], scale=1.0 / D, bias=RMS_EPS)
            yield
            self.actf(st[:, 2:3], st[:, 1:2], AF.Exp, r=[K("st")], w=[K("st")], scale=-0.5)
            yield
            self.P("dve", "scalar_tensor_tensor", hb, xt, st[:, 2:3], self.g1bc, ALU.mult, ALU.mult,
                   r=[K("xt"), K("st"), "g1bc"], w=[K("hb")])
            yield
            for kc in range(8):
                self.P("pe", "transpose", pT[:, kc * 128:(kc + 1) * 128], hb[:, kc * 128:(kc + 1) * 128],
                       self.ident_bf, r=[K("hb"), "ident_bf"], w=[pTk_])
            yield
            self.actf(hT[:, :, tt * 128:(tt + 1) * 128], pT.rearrange("p (k n) -> p k n", k=8), AF.Copy,
                      r=[pTk_], w=[hTk])
            yield
            for kc in range(8):
                self.mm(ptok, hT[:, kc, tt * 128:(tt + 1) * 128], win[:, kc, 512:1024], kc == 0, kc == 7,
                        r=[hTk, WK[kc]], w=[ptk_])
            yield
            self.actf(vsb[:, 0:256], ptok[:, 0:256], AF.Copy, r=[ptk_], w=[K("vsb")])
            self.P("dve", "tensor_copy", xg[:, 0:256], ptok[:, 256:512], r=[ptk_], w=[K("xg")])
            yield
            for (pc0, c0) in ((0, 1024), (256, 2816)):
                for kc in range(8):
                    self.mm(ptok[:, pc0:pc0 + 256], hT[:, kc, tt * 128:(tt + 1) * 128], win[:, kc, c0:c0 + 256], kc == 0, kc == 7,
                            r=[hTk, WK[kc]], w=[ptk_])
            yield
            self.actf(vsb[:, 256:512], ptok[:, 256:512], AF.Copy, r=[ptk_], w=[K("vsb")])
            self.P("dve", "tensor_copy", xg[:, 256:512], ptok[:, 0:256], r=[ptk_], w=[K("xg")])
            yield
            kb.dma(self.vtok[b, rows, :], vsb, reads=[K("vsb")], writes=["vtok"])
            self.actf(x2, xg, AF.Square, r=[K("xg")], w=[K("x2")])
            yield
            self.P("dve", "tensor_scalar", u1, x2, 0.044715, 1.0, ALU.mult, ALU.add, r=[K("x2")], w=[K("u1")])
            yield
            self.P("dve", "tensor_tensor", u1, u1, xg, ALU.mult, r=[K("u1"), K("xg")], w=[K("u1")])
            yield
            self.actf(sg, u1, AF.Sigmoid, r=[K("u1")], w=[K("sg")], scale=1.5957691216057308)
            yield
            self.P("dve", "tensor_tensor", hid, sg, xg, ALU.mult, r=[K("sg"), K("xg")], w=[K("hid")])
            yield
            hv = hid[:, 256:512].rearrange("p (g d) -> p g d", g=4)
            v3 = lambda ap: ap.rearrange("p (g d) -> p g d", g=4)
            bc4 = lambda ap: ap.unsqueeze(2).to_broadcast([128, 4, 64])
            self.P("dve", "tensor_reduce", gst[:, 0:4], hv, AX.X, ALU.add, r=[K("hid")], w=[K("gst")])
            yield
            self.P("dve", "tensor_single_scalar", gst[:, 4:8], gst[:, 0:4], -1.0 / 64, ALU.mult, r=[K("gst")], w=[K("gst")])
            yield
            self.P("dve", "tensor_tensor", v3(vc), hv, bc4(gst[:, 4:8]), ALU.add, r=[K("hid"), K("gst")], w=[K("vc")])
            yield
            self.actf(sq, vc, AF.Square, r=[K("vc")], w=[K("sq")])
            yield
            self.P("dve", "tensor_reduce", gst[:, 8:12], v3(sq), AX.X, ALU.add, r=[K("sq")], w=[K("gst")])
            yield
            self.actf(gst[:, 8:12], gst[:, 8:12], AF.Ln, r=[K("gst")], w=[K("gst")], scale=1.0 / 64, bias=LN_EPS)
            yield
            self.actf(gst[:, 12:16], gst[:, 8:12], AF.Exp, r=[K("gst")], w=[K("gst")], scale=-0.5)
            yield
            self.P("dve", "tensor_tensor", v3(vn), v3(vc), bc4(gst[:, 12:16]), ALU.mult, r=[K("vc"), K("gst")], w=[K("vn")])
            yield
            for g in range(4):
                self.mm(pgm[:, g * 64:(g + 1) * 64], self.gmw[:, g, :], vn[:, g * 64:(g + 1) * 64], True, True,
                        r=["gmw", K("vn")], w=[pgk_])
            yield
            self.P("dve", "tensor_tensor", v3(tmpg), v3(pgm[:, 0:256]), bc4(self.gmb), ALU.add,
                   r=[pgk_, "gmb"], w=[K("tmpg")])
            yield
            self.P("dve", "tensor_tensor", ygm, tmpg, hid[:, 0:256], ALU.mult, r=[K("tmpg"), K("hid")], w=[K("ygm")])
            yield
            for c in range(2):
                self.P("pe", "transpose", pgm[:, 256 + c * 128:256 + (c + 1) * 128], ygm[:, c * 128:(c + 1) * 128],
                       self.ident, r=[K("ygm"), "cst"], w=[pgk_])
            yield
            self.actf(ygT.rearrange("p c n -> p (c n)"), pgm[:, 256:512], AF.Copy, r=[pgk_], w=[K("ygT")])
            yield
            kb.dma(self.mixT[b, 256:512, rows].rearrange("(c p) n -> p c n", p=128), ygT,
                   reads=[K("ygT")], writes=["mixT"])

        def fm_gen(g4):
            hT = hT4[g4 % 2]
            hTk = "hT4_%d" % (g4 % 2)
            cols = slice(g4 * 512, (g4 + 1) * 512)
            for ci, (c0, M, kind, dest, scale) in enumerate(fm):
                pi = 6 + ci % 2
                si = ci % 2
                pk = "pb%d" % pi
                for kc in range(8):
                    self.mm(pb[pi][0:M, :], win[:, kc, c0:c0 + M], hT[:, kc, :], kc == 0, kc == 7,
                            r=[hTk, WK[kc]], w=[pk])
                yield
                if kind == "qk":
                    self.actf(stg_bf[si], pb[pi][0:64, :], AF.Copy, r=[pk], w=["stgb%d" % si], scale=scale)
                    kb.dma(self.qkT[b, dest, :, cols], stg_bf[si], reads=["stgb%d" % si], writes=["qkT"])
                elif kind == "rw":
                    self.P("dve", "tensor_copy", stg_f[si][0:M, :], pb[pi][0:M, :], r=[pk], w=["stgf%d" % si])
                    kb.dma(self.rwT[b, dest:dest + M, cols], stg_f[si][0:M, :], reads=["stgf%d" % si], writes=["rwT"])
                else:
                    self.P("dve", "tensor_copy", stg_f[si][0:M, :], pb[pi][0:M, :], r=[pk], w=["stgf%d" % si])
                    kb.dma(self.fgT[b, :, cols], stg_f[si][0:M, :], reads=["stgf%d" % si], writes=["fgT"])
                yield

        def run(gens):
            gens = list(gens)
            while gens:
                for g in list(gens):
                    try:
                        next(g)
                    except StopIteration:
                        gens.remove(g)

        for g4 in range(5):
            streams = []
            if g4 >= 1:
                streams.append(fm_gen(g4 - 1))
            if g4 < 4:
                run(streams + [tile_gen(g4 * 4 + q, q) for q in range(2)])
                run([tile_gen(g4 * 4 + 2 + q, q) for q in range(2)])
            else:
                run(streams)

    def phaseB_attn(self, l, b):
        kb = self.kb
        pball = self.pb
        a = Arena(self.arena_ap, self.r1_end, 208000)
        fg = a.t([4, T]); fe = a.t([4, T]); cum = a.t([4, T])
        self.ones4 = a.t([4, T])
        self.P("pool", "memset", self.ones4, 1.0, w=["ones4"])
        ncum_all = a.t([128, 16, 4])
        NS = 2
        slots = []
        for s in range(NS):
            B = dict(qT=a.t([64, T], BF16), kT=a.t([64, T], BF16), V=a.t([128, 16, 64], BF16),
                     E=[a.t([128, 512]) for _ in range(2)], Lp=[a.t([128, 512]) for _ in range(2)],
                     At=[a.t([128, 512], BF16) for _ in range(2)], Slp=a.t([128, 512]),
                     ob=[a.t([64, 512], BF16) for _ in range(2)], rec=a.t([64, 512]), cumrow=a.t([1, T]))
            slots.append(B)
        kb.dma(fg, self.fgT[b], reads=["fgT"], writes=["fg"])
        self.actf(fe, fg, AF.Exp, r=["fg", "nfbf"], w=["fe"], scale=-1.0, bias=self.nfbf)
        self.actf(fe, fe, AF.Ln, r=["fe"], w=["fe"], bias=1.0)
        self.P("dve", "tensor_tensor_scan", cum, self.ones4, fe, 0.0, ALU.mult, ALU.subtract,
               r=["fe", "ones4"], w=["cum"])
        kb.dma(self.cumd[b], cum, reads=["cum"], writes=["cumd"])
        for kbk in range(16):
            self.P("pe", "transpose", pball[6][:, kbk * 4:(kbk + 1) * 4], cum[0:4, kbk * 128:(kbk + 1) * 128],
                   self.ident[0:4, 0:4], r=["cum", "cst"], w=["pb6"])
        self.actf(ncum_all.rearrange("p a b -> p (a b)"), pball[6][:, 0:64], AF.Copy, r=["pb6"], w=["ncum"], scale=-1.0)

        def head_gen(kind, h, s):
            B = slots[s]
            K = lambda n: "%s_s%d" % (n, s)
            pb = pball[4 * s:4 * s + 4]
            pk = ["pb%d" % (4 * s + i) for i in range(4)]
            qT, kT, V, Slp, rec, cumrow = B["qT"], B["kT"], B["V"], B["Slp"], B["rec"], B["cumrow"]
            qk_q = h if kind == "sb" else 8 + h
            qk_k = 4 + h if kind == "sb" else 12 + h
            vc0 = h * 64 if kind == "sb" else 256 + h * 64
            mrow = h * 64 if kind == "sb" else 768 + h * 64
            kb.dma(qT, self.qkT[b, qk_q], reads=["qkT"], writes=[K("qT")])
            kb.dma(kT, self.qkT[b, qk_k], reads=["qkT"], writes=[K("kT")])
            kb.dma(V, self.vtok[b, :, vc0:vc0 + 64].rearrange("(kb p) d -> p kb d", p=128),
                   reads=["vtok"], writes=[K("V")])
            if kind == "fx":
                kb.dma(cumrow, self.cumd[b, h:h + 1, :], reads=["cumd"], writes=[K("cumrow")])
            yield
            it = 0
            for c in range(4):
                Q0 = c * 512
                last = 4 * c + 3
                if kind == "sb":
                    self.P("pool", "memset", Slp, 0.0, w=[K("Slp")])
                    order = list(range(last, -1, -1))
                else:
                    order = list(range(0, last + 1))
                first_pv = True
                for kbk in order:
                    r_ = kbk - 4 * c
                    diag = r_ >= 0
                    col0 = max(r_, 0) * 128
                    i2 = it % 2
                    it += 1
                    E, Lp, At = B["E"][i2], B["Lp"][i2], B["At"][i2]
                    Ek, Lk, Ak = K("E%d" % i2), K("Lp%d" % i2), K("At%d" % i2)
                    kblk = kT[:, kbk * 128:(kbk + 1) * 128]
                    qs = lambda c_a, c_b: qT[:, Q0 + c_a:Q0 + c_b]
                    blocks = []
                    if diag:
                        blocks.append((col0, col0 + 128, True))
                        if col0 + 128 < 512:
                            blocks.append((col0 + 128, 512, False))
                    else:
                        blocks.append((0, 512, False))
                    if kind == "sb":
                        pz, pzk = (pb[0], pk[0]) if i2 == 0 else (pb[3], pk[3])
                        pa, pak = pb[1], pk[1]
                        for (lo, hi, dg) in blocks:
                            if dg:
                                self.mm(pz[:, lo:hi], self.ident_bf, self.mbs_bf, True, False, r=["ident_bf", "mbs_bf"], w=[pzk])
                            self.mm(pz[:, lo:hi], kblk, qs(lo, hi), not dg, True, r=[K("kT"), K("qT")], w=[pzk])
                        yield
                        self.actf(E[:, col0:512], pz[:, col0:512], AF.Exp, r=[pzk], w=[Ek])
                        yield
                        self.actf(Lp[:, col0:512], E[:, col0:512], AF.Ln, r=[Ek], w=[Lk], bias=1.0)
                        yield
                        for (lo, hi, dg) in blocks:
                            if dg:
                                self.mm(pa[:, lo:hi], self.ident_bf, self.mbs_bf, True, False, r=["ident_bf", "mbs_bf"], w=[pak])
                            self.mm(pa[:, lo:hi], kblk, qs(lo, hi), not dg, False, r=[K("kT"), K("qT")], w=[pak])
                            self.mm(pa[:, lo:hi], self.ntri, Lp[:, lo:hi], False, False, r=["cst", Lk], w=[pak])
                            self.mm(pa[:, lo:hi], self.negones, Slp[:, lo:hi], False, True, r=["cst", K("Slp")], w=[pak])
                        yield
                        self.actf(At[:, col0:512], pa[:, col0:512], AF.Exp, r=[pak], w=[Ak])
                        self.P("pool", "tensor_tensor", Slp[:, col0:512], Slp[:, col0:512], Lp[:, col0:512], ALU.add,
                               r=[K("Slp"), Lk], w=[K("Slp")])
                        yield
                        for (lo, hi, dg) in blocks:
                            self.mm(pb[2][0:64, lo:hi], V[:, kbk, :], At[:, lo:hi], first_pv, kbk == 0,
                                    r=[K("V"), Ak], w=[pk[2]], sgc=True)
                            first_pv = False
                        yield
                    else:
                        pz, pzk = (pb[0], pk[0]) if i2 == 0 else (pb[1], pk[1])
                        for (lo, hi, dg) in blocks:
                            if dg:
                                self.mm(pz[:, lo:hi], self.ident_bf, self.mbi_bf, True, False, r=["ident_bf", "mbi_bf"], w=[pzk])
                            self.mm(pz[:, lo:hi], kblk, qs(lo, hi), not dg, False, r=[K("kT"), K("qT")], w=[pzk])
                            self.mm(pz[:, lo:hi], self.ones_f[0:1, :], cumrow[0:1, Q0 + lo:Q0 + hi], False, True,
                                    r=["ones_f", K("cumrow")], w=[pzk])
                        yield
                        self.actf(At[:, col0:512], pz[:, col0:512], AF.Exp, r=[pzk, "ncum"], w=[Ak],
                                  bias=ncum_all[:, kbk, h:h + 1])
                        yield
                        for (lo, hi, dg) in blocks:
                            self.mm(pb[2][0:64, lo:hi], V[:, kbk, :], At[:, lo:hi], first_pv, dg,
                                    r=[K("V"), Ak], w=[pk[2]], sgc=True)
                            self.mm(pb[3][0:64, lo:hi], self.ones_bf, At[:, lo:hi], first_pv, dg,
                                    r=["ones_bf", Ak], w=[pk[3]], sgc=True)
                            first_pv = False
                        yield
                oi = c % 2
                ob, obk = B["ob"][oi], K("ob%d" % oi)
                if kind == "sb":
                    self.actf(ob, pb[2][0:64, :], AF.Copy, r=[pk[2]], w=[obk])
                else:
                    self.P("dve", "reciprocal", rec, pb[3][0:64, :], r=[pk[3]], w=[K("rec")])
                    yield
                    self.P("dve", "tensor_tensor", ob, pb[2][0:64, :], rec, ALU.mult, r=[pk[2], K("rec")], w=[obk])
                yield
                kb.dma(self.mixT[b, mrow:mrow + 64, Q0:Q0 + 512], ob, reads=[obk], writes=["mixT"])

        for kind in ("sb", "fx"):
            for hp in range(2):
                gens = [head_gen(kind, 2 * hp + s, s) for s in range(NS)]
                while gens:
                    for g in list(gens):
                        try:
                            next(g)
                        except StopIteration:
                            gens.remove(g)

    def phaseB_rwkv(self, l, b):
        kb = self.kb
        pball = self.pb
        a = Arena(self.arena_ap, self.r1_end, 208000)
        F = lambda parts=64: a.t([parts, 512])
        xw, xwp, xa, xap = F(), F(), F(), F()
        xgt, xgp = F(128), F(128)
        pv = self.pv
        NS = 2
        slots = []
        for s in range(NS):
            B = {}
            for nm in ("raw0", "raw1", "raw2", "prv0", "prv1", "prv2", "rm", "km", "vm", "sgd", "logd", "G", "av",
                       "kk", "kk2", "nrm", "kap", "tmpa", "kp", "bbv", "eG", "eGn", "eGx", "ktT", "btT", "kdT", "rtT", "rkr"):
                B[nm] = F()
            B["tok3"] = [a.t([128, 3, 64]) for _ in range(4)]
            B["sgt"] = [a.t([128, 72]) for _ in range(4)]
            B["Yall"] = a.t([128, 4, 64])
            B["yc"] = a.t([128, 4, 64])
            B["ysq"] = a.t([128, 4, 64])
            B["yst4"] = a.t([128, 16])
            B["ABm"] = [a.t([128, 4, 128]) for _ in range(2)]
            B["X"] = [a.t([128, 128]) for _ in range(2)]
            B["Y"] = [a.t([128, 128]) for _ in range(2)]
            B["R"] = [a.t([128, 128]) for _ in range(2)]
            B["XYR"] = [a.t([128, 3, 128]) for _ in range(2)]
            for nm in ("Wn", "U", "ysb", "yn", "yj"):
                B[nm] = a.t([128, 64])
            B["mt"] = a.t([64, 64])
            B["yst"] = a.t([128, 8])
            B["ymix"] = [a.t([64, 512], BF16) for _ in range(2)]
            slots.append(B)
        for h in range(4):
            self.P("pool", "memset", self.Mst[:, h, :], 0.0, w=["M%d" % h])

        def load_shift(g4, dst, dstp, rows, nm, parts):
            c0 = g4 * 512
            kb.dma(dst[0:parts, :], self.rwT[b, rows, c0:c0 + 512], reads=["rwT"], writes=[nm])
            if g4 == 0:
                self.P("pool", "memset", dstp[0:parts, 0:1], 0.0, w=[nm + "p"])
                kb.dma(dstp[0:parts, 1:512], self.rwT[b, rows, 0:511], reads=["rwT"], writes=[nm + "p"])
            else:
                kb.dma(dstp[0:parts, :], self.rwT[b, rows, c0 - 1:c0 + 511], reads=["rwT"], writes=[nm + "p"])

        def mix(dst, x_, xp_, mu, nm, dk, parts=64):
            self.P("pool", "tensor_tensor", xp_[0:parts, :], xp_[0:parts, :], x_[0:parts, :], ALU.subtract,
                   r=[nm, nm + "p"], w=[nm + "p"])
            self.P("dve", "scalar_tensor_tensor", dst[0:parts, :], xp_[0:parts, :], mu, x_[0:parts, :], ALU.mult, ALU.add,
                   r=[nm, nm + "p", "pv", "mu_w", "mu_a", "mu_g"], w=[dk])

        def head_gen(g4, h, s):
            B = slots[s]
            K = lambda n: "%s_s%d" % (n, s)
            pb = pball[4 * s:4 * s + 4]
            pk = ["pb%d" % (4 * s + i) for i in range(4)]
            c0 = g4 * 512
            cols = slice(c0, c0 + 512)
            hc = slice(h * 64, (h + 1) * 64)
            Mh = self.Mst[:, h, :]
            Mk = "M%d" % h
            rm, km, vm = B["rm"], B["km"], B["vm"]
            sgd, logd, G, av = B["sgd"], B["logd"], B["G"], B["av"]
            kk, kk2, nrm, kap, tmpa, kp, bbv = B["kk"], B["kk2"], B["nrm"], B["kap"], B["tmpa"], B["kp"], B["bbv"]
            eG, eGn, eGx = B["eG"], B["eGn"], B["eGx"]
            ktT, btT, kdT, rtT, rkr = B["ktT"], B["btT"], B["kdT"], B["rtT"], B["rkr"]
            Wn, U, ysb, yn, yj, mt, yst = B["Wn"], B["U"], B["ysb"], B["yn"], B["yj"], B["mt"], B["yst"]
            X, Y, R = B["X"], B["Y"], B["R"]
            for j, (nm, dst) in enumerate((("r", rm), ("k", km), ("v", vm))):
                rows = slice((j * 4 + h) * 64, (j * 4 + h + 1) * 64)
                load_shift(g4, B["raw%d" % j], B["prv%d" % j], rows, K("raw%d" % j), 64)
                mix(dst, B["raw%d" % j], B["prv%d" % j], pv[:, j * 4 + h:j * 4 + h + 1], K("raw%d" % j), K(nm + "m"))
            yield
            self.mm(pb[0][0:64, :], self.w2s[:, hc], xw, True, True, r=["w2s", "xw"], w=[pk[0]])
            self.mm(pb[1][0:64, :], self.a2s[:, hc], xa, True, True, r=["a2s", "xa"], w=[pk[1]])
            self.P("dve", "tensor_single_scalar", kk, km, pv[:, 20 + h:21 + h], ALU.mult, r=[K("km"), "pv"], w=[K("kk")])
            self.P("pool", "tensor_tensor", kk2, kk, kk, ALU.mult, r=[K("kk")], w=[K("kk2")])
            yield
            self.actf(sgd, pb[0][0:64, :], AF.Sigmoid, r=[pk[0], "pv"], w=[K("sgd")], bias=pv[:, 12 + h:13 + h])
            self.actf(av, pb[1][0:64, :], AF.Sigmoid, r=[pk[1], "pv"], w=[K("av")], bias=pv[:, 16 + h:17 + h])
            self.mm(pb[2][0:64, :], self.ones_f[0:64, 0:64], kk2, True, True, r=["ones_f", K("kk2")], w=[pk[2]])
            yield
            self.P("dve", "tensor_single_scalar", logd, sgd, -0.6065306597126334, ALU.mult, r=[K("sgd")], w=[K("logd")])
            self.actf(nrm, pb[2][0:64, :], AF.Sqrt, r=[pk[2]], w=[K("nrm")])
            yield
            self.P("dve", "tensor_tensor_scan", G, self.reset, logd, 0.0, ALU.mult, ALU.add, r=["reset", K("logd")], w=[K("G")])
            self.P("dve", "tensor_single_scalar", nrm, nrm, 1e-12, ALU.max, r=[K("nrm")], w=[K("nrm")])
            yield
            self.P("dve", "reciprocal", nrm, nrm, r=[K("nrm")], w=[K("nrm")])
            self.actf(eG, G, AF.Exp, r=[K("G")], w=[K("eG")])
            self.actf(eGn, G, AF.Exp, r=[K("G")], w=[K("eGn")], scale=-1.0)
            self.P("dve", "tensor_tensor", logd, G, logd, ALU.subtract, r=[K("G"), K("logd")], w=[K("logd")])
            yield
            self.P("dve", "tensor_tensor", kap, kk, nrm, ALU.mult, r=[K("kk"), K("nrm")], w=[K("kap")])
            self.actf(eGx, logd, AF.Exp, r=[K("logd")], w=[K("eGx")])
            self.P("dve", "tensor_scalar", tmpa, av, pv[:, 24 + h:25 + h], pv[:, 32 + h:33 + h], ALU.mult, ALU.add,
                   r=[K("av"), "pv"], w=[K("tmpa")])
            yield
            self.P("pool", "tensor_tensor", kp, km, tmpa, ALU.mult, r=[K("km"), K("tmpa")], w=[K("kp")])
            self.P("pool", "tensor_tensor", bbv, kap, av, ALU.mult, r=[K("kap"), K("av")], w=[K("bbv")])
            self.P("dve", "tensor_tensor", ktT, kap, eGx, ALU.mult, r=[K("kap"), K("eGx")], w=[K("ktT")])
            self.P("pool", "tensor_tensor", rtT, rm, eG, ALU.mult, r=[K("rm"), K("eG")], w=[K("rtT")])
            yield
            self.P("pool", "tensor_tensor", btT, bbv, eGn, ALU.mult, r=[K("bbv"), K("eGn")], w=[K("btT")])
            self.P("dve", "tensor_tensor", kdT, kp, eGn, ALU.mult, r=[K("kp"), K("eGn")], w=[K("kdT")])
            self.P("dve", "scalar_tensor_tensor", rkr, rm, pv[:, 28 + h:29 + h], kp, ALU.mult, ALU.mult,
                   r=[K("rm"), K("kp"), "pv"], w=[K("rkr")])
            yield
            for cc in range(4):
                u2 = cc % 2
                cs = slice(cc * 128, (cc + 1) * 128)
                t3, t3k = B["tok3"][cc], K("tok3_%d" % cc)
                sgtk = K("sgt%d" % cc)
                for j, (srcT, sk) in enumerate(((btT, K("btT")), (kdT, K("kdT")), (vm, K("vm")))):
                    self.P("pe", "transpose", pb[0][:, j * 64:(j + 1) * 64], srcT[:, cs], self.ident[0:64, 0:64],
                           r=[sk, "cst"], w=[pk[0]])
                self.mm(pb[0][:, 192:193], rkr[:, cs], self.ones_f[0:64, 0:1], True, True, r=[K("rkr"), "ones_f"], w=[pk[0]])
                self.mm(pb[0][:, 200:264], xgt[:, cs], self.g2s[:, hc], True, True, r=["xg", "g2s"], w=[pk[0]])
                for j, (lT, lk, rT, rk_) in enumerate(((btT, K("btT"), ktT, K("ktT")), (btT, K("btT"), rtT, K("rtT")),
                                                       (kdT, K("kdT"), ktT, K("ktT")), (kdT, K("kdT"), rtT, K("rtT")))):
                    self.mm(pb[1][:, j * 128:(j + 1) * 128], lT[:, cs], rT[:, cs], True, True, r=[lk, rk_], w=[pk[1]])
                self.mm(pb[2][:, 0:128], ktT[:, cs], btT[:, cs], True, True, r=[K("ktT"), K("btT")], w=[pk[2]])
                yield
                self.actf(t3.rearrange("p a b -> p (a b)"), pb[0][:, 0:192], AF.Copy, r=[pk[0]], w=[t3k])
                self.P("dve", "tensor_copy", B["sgt"][cc], pb[0][:, 192:264], r=[pk[0]], w=[sgtk])
                bt_tok, kd_tok, v_tok = t3[:, 0, :], t3[:, 1, :], t3[:, 2, :]
                s_tok = B["sgt"][cc][:, 0:1]
                g_tok = B["sgt"][cc][:, 8:72]
                AB, ABk = B["ABm"][u2], K("AB%d" % u2)
                self.P("dve", "tensor_tensor", AB.rearrange("p (x y) t -> p x y t", x=2),
                       pb[1].rearrange("p (x y t) -> p x y t", x=2, y=2),
                       self.mask2.unsqueeze(1).to_broadcast([128, 2, 2, 128]), ALU.mult, r=[pk[1], "cst"], w=[ABk])
                Nm, BbT, AkT, BkT = AB[:, 0, :], AB[:, 1, :], AB[:, 2, :], AB[:, 3, :]
                self.P("dve", "tensor_tensor", Y[0], pb[2][:, 0:128], self.m_lows, ALU.mult, r=[pk[2], "cst"], w=[K("Y0")])
                yield
                self.P("pool", "tensor_tensor", R[0], self.ident, Nm, ALU.subtract, r=["cst", ABk], w=[K("R0")])
                Xc, Xk = Nm, ABk
                Yc, Yk = Y[0], K("Y0")
                Rc, Rk = R[0], K("R0")
                for i in range(1, 8):
                    xi = i % 2
                    T3, T3k = B["XYR"][xi], K("XYR%d" % xi)
                    if i <= 5:
                        self.mm(pb[2][:, 128:256], Yc, Xc, True, True, r=[Yk, Xk], w=[pk[2]])
                    if i <= 6:
                        self.mm(pb[2][:, 256:384], Xc, Yc, True, True, r=[Xk, Yk], w=[pk[2]])
                    if i >= 2:
                        self.mm(pb[2][:, 384:512], self.ident, Rc, True, False, r=["cst", Rk], w=[pk[2]])
                        self.mm(pb[2][:, 384:512], Yc, Rc, False, True, r=[Yk, Rk], w=[pk[2]])
                    yield
                    c_lo = 128 if i <= 5 else (256 if i <= 6 else 384)
                    c_hi = 512 if i >= 2 else 384
                    self.actf(T3.rearrange("p a b -> p (a b)")[:, c_lo - 128:c_hi - 128], pb[2][:, c_lo:c_hi], AF.Copy,
                              r=[pk[2]], w=[T3k])
                    if i <= 5:
                        Xc, Xk = T3[:, 0, :], T3k
                    if i <= 6:
                        Yc, Yk = T3[:, 1, :], T3k
                    if i >= 2:
                        Rc, Rk = T3[:, 2, :], T3k
                    yield
                self.mm(pb[3][:, 128:192], ktT[:, cs], Mh, True, False, r=[K("ktT"), Mk], w=[pk[3]])
                self.mm(pb[3][:, 128:192], AkT, v_tok, False, True, r=[ABk, t3k], w=[pk[3]])
                yield
                self.actf(Wn, pb[3][:, 128:192], AF.Copy, r=[pk[3]], w=[K("Wn")], scale=-1.0)
                yield
                self.mm(pb[3][:, 192:256], Rc, Wn, True, True, r=[Rk, K("Wn")], w=[pk[3]])
                yield
                self.actf(U, pb[3][:, 192:256], AF.Copy, r=[pk[3]], w=[K("U")])
                yield
                self.mm(pb[3][:, 256:320], rtT[:, cs], Mh, True, False, r=[K("rtT"), Mk], w=[pk[3]])
                self.mm(pb[3][:, 256:320], BbT, U, False, False, r=[ABk, K("U")], w=[pk[3]])
                self.mm(pb[3][:, 256:320], BkT, v_tok, False, True, r=[ABk, t3k], w=[pk[3]])
                self.mm(pb[3][0:64, 320:384], bt_tok, U, True, False, r=[t3k, K("U")], w=[pk[3]])
                self.mm(pb[3][0:64, 320:384], kd_tok, v_tok, False, True, r=[t3k], w=[pk[3]])
                yield
                self.P("dve", "tensor_tensor", mt, pb[3][0:64, 320:384], Mh, ALU.add, r=[pk[3], Mk], w=[K("mt")])
                self.actf(B["Yall"][:, cc, :], pb[3][:, 256:320], AF.Identity, r=[pk[3]], w=[K("Yall"), K("yst4")],
                          accum_out=B["yst4"][:, cc:cc + 1])
                yield
                self.P("dve", "tensor_single_scalar", Mh, mt, eG[:, cc * 128 + 127:cc * 128 + 128], ALU.mult,
                       r=[K("mt"), K("eG")], w=[Mk])
                yield
            Yall, yc, ysq, y4 = B["Yall"], B["yc"], B["ysq"], B["yst4"]
            bc4 = lambda ap: ap.unsqueeze(2).to_broadcast([128, 4, 64])
            bch = lambda ap: ap.unsqueeze(1).to_broadcast([128, 4, 64])
            self.P("dve", "tensor_single_scalar", y4[:, 4:8], y4[:, 0:4], -1.0 / 64, ALU.mult, r=[K("yst4")], w=[K("yst4")])
            yield
            self.P("dve", "tensor_tensor", yc, Yall, bc4(y4[:, 4:8]), ALU.add, r=[K("Yall"), K("yst4")], w=[K("yc")])
            yield
            self.actf(ysq, yc, AF.Square, r=[K("yc")], w=[K("ysq")])
            yield
            self.P("dve", "tensor_reduce", y4[:, 8:12], ysq, AX.X, ALU.add, r=[K("ysq")], w=[K("yst4")])
            yield
            self.actf(y4[:, 8:12], y4[:, 8:12], AF.Ln, r=[K("yst4")], w=[K("yst4")], scale=1.0 / 64, bias=GN_EPS)
            yield
            self.actf(y4[:, 12:16], y4[:, 8:12], AF.Exp, r=[K("yst4")], w=[K("yst4")], scale=-0.5)
            yield
            self.P("dve", "tensor_tensor", yc, yc, bc4(y4[:, 12:16]), ALU.mult, r=[K("yc"), K("yst4")], w=[K("yc")])
            yield
            self.P("dve", "tensor_tensor", yc, yc, bch(self.gnw[:, hc]), ALU.mult, r=[K("yc"), "gnw"], w=[K("yc")])
            yield
            self.P("dve", "tensor_tensor", yc, yc, bch(self.gnb[:, hc]), ALU.add, r=[K("yc"), "gnb"], w=[K("yc")])
            yield
            for cc in range(4):
                self.P("dve", "scalar_tensor_tensor", yc[:, cc, :], B["tok3"][cc][:, 2, :], B["sgt"][cc][:, 0:1], yc[:, cc, :],
                       ALU.mult, ALU.add, r=[K("yc"), K("tok3_%d" % cc), K("sgt%d" % cc)], w=[K("yc")])
            yield
            for cc in range(4):
                self.P("dve", "tensor_tensor", yc[:, cc, :], yc[:, cc, :], B["sgt"][cc][:, 8:72], ALU.mult,
                       r=[K("yc"), K("sgt%d" % cc)], w=[K("yc")])
            yield
            for cc in range(4):
                self.P("pe", "transpose", pb[0][0:64, cc * 128:(cc + 1) * 128], yc[:, cc, :], self.ident, r=[K("yc"), "cst"], w=[pk[0]])
            yield
            ym, ymk = B["ymix"][g4 % 2], K("ymix%d" % (g4 % 2))
            self.actf(ym, pb[0][0:64, :], AF.Copy, r=[pk[0]], w=[ymk])
            yield
            kb.dma(self.mixT[b, 512 + h * 64:512 + (h + 1) * 64, cols], ym, reads=[ymk], writes=["mixT"])

        for g4 in range(4):
            load_shift(g4, xw, xwp, slice(768, 832), "xw", 64)
            load_shift(g4, xa, xap, slice(832, 896), "xa", 64)
            load_shift(g4, xgt, xgp, slice(896, 1024), "xg", 128)
            mix(xw, xw, xwp, self.mu_w, "xw", "xw")
            mix(xa, xa, xap, self.mu_a, "xa", "xa")
            mix(xgt, xgt, xgp, self.mu_g, "xg", "xg", 128)
            self.actf(xw, xw, AF.Tanh, r=["xw"], w=["xw"])
            self.actf(xgt, xgt, AF.Sigmoid, r=["xg"], w=["xg"])
            for hp in range(2):
                gens = [head_gen(g4, 2 * hp + s, s) for s in range(NS)]
                while gens:
                    for g in list(gens):
                        try:
                            next(g)
                        except StopIteration:
                            gens.remove(g)

    def phaseD(self, l, b, src):
        kb = self.kb
        pball = self.pb
        a = Arena(self.arena_ap, self.r1_end, 208000)
        wo = a.t([128, 8, D], BF16)
        wostg = [a.t([128, D]) for _ in range(2)]
        for kc in range(8):
            si = kc % 2
            sk = "wostg%d" % si
            kb.dma(wostg[si], self.w_out[l, kc * 128:(kc + 1) * 128, :], writes=[sk])
            self.actf(wo[:, kc, 0:512], wostg[si][:, 0:512], AF.Copy, r=[sk], w=["wo%d" % kc])
            self.P("pool", "tensor_copy", wo[:, kc, 512:1024], wostg[si][:, 512:1024], r=[sk], w=["wo%d" % kc])
        chains = []
        for q in range(2):
            chains.append(dict(mx=a.t([128, 8, 128], BF16), xt=a.t([128, D]), xn=a.t([128, D]), h2=a.t([128, D]),
                               junk=a.t([128, D]), st=a.t([128, 4]), hTf=a.t([128, 8, 128]), h2b=a.t([128, D], BF16),
                               lg=a.t([128, 36]), lm=a.t([128, 32]), l2=a.t([128, 32]), ohg=a.t([128, 4]), ge=a.t([128, 4]),
                               rs=a.t([128, 16]), rt=a.t([128, 66])))

        def tile_gen(t, q):
            C = chains[q]
            K = lambda n: "%s_c%d" % (n, q)
            mx, xt, xn, h2, junk, st, hTf, h2b = C["mx"], C["xt"], C["xn"], C["h2"], C["junk"], C["st"], C["hTf"], C["h2b"]
            lg, lm, l2, ohg, ge, rs, rt = C["lg"], C["lm"], C["l2"], C["ohg"], C["ge"], C["rs"], C["rt"]
            oh1, oh2 = rt[:, 0:32], rt[:, 32:64]
            po, pok = pball[q], "pb%d" % q
            pt, ptk = pball[2 + q], "pb%d" % (2 + q)
            pr, prk = pball[4 + q], "pb%d" % (4 + q)
            rows = slice(t * 128, (t + 1) * 128)
            kb.dma(mx, self.mixT[b, :, rows].rearrange("(k p) n -> p k n", p=128), reads=["mixT"], writes=[K("mx")])
            kb.dma(xt, src[rows, :], reads=["xres"], writes=[K("xt")])
            yield
            for hf in range(2):
                for kc in range(8):
                    self.mm(po, mx[:, kc, :], wo[:, kc, hf * 512:(hf + 1) * 512], kc == 0, kc == 7,
                            r=[K("mx"), "wo%d" % kc], w=[pok])
                yield
                self.P("dve", "tensor_tensor", xn[:, hf * 512:(hf + 1) * 512], po, xt[:, hf * 512:(hf + 1) * 512],
                       ALU.add, r=[pok, K("xt")], w=[K("xn")])
                yield
            kb.dma(self.xres[b, rows, :], xn, reads=[K("xn")], writes=["xres_w"])
            self.actf(junk, xn, AF.Square, r=[K("xn")], w=[K("junk"), K("st")], accum_out=st[:, 0:1])
            yield
            self.actf(st[:, 1:2], st[:, 0:1], AF.Ln, r=[K("st")], w=[K("st")], scale=1.0 / D, bias=RMS_EPS)
            yield
            self.actf(st[:, 2:3], st[:, 1:2], AF.Exp, r=[K("st")], w=[K("st")], scale=-0.5)
            yield
            self.P("dve", "scalar_tensor_tensor", h2, xn, st[:, 2:3], self.g2bc, ALU.mult, ALU.mult,
                   r=[K("xn"), K("st"), "g2bc"], w=[K("h2")])
            yield
            self.P("pool", "tensor_copy", h2b, h2, r=[K("h2")], w=[K("h2b")])
            kb.dma(self.h2tok[b, rows, :], h2b, reads=[K("h2b")], writes=["h2tok"])
            for hh in range(2):
                for k4 in range(4):
                    kc = hh * 4 + k4
                    self.P("pe", "transpose", pt[:, k4 * 128:(k4 + 1) * 128], h2[:, kc * 128:(kc + 1) * 128],
                           self.ident, r=[K("h2"), "cst"], w=[ptk])
                yield
                self.actf(hTf[:, hh * 4:(hh + 1) * 4, :].rearrange("p k n -> p (k n)"), pt, AF.Copy, r=[ptk], w=[K("hTf")])
                yield
            for kc in range(8):
                self.mm(pr[:, 0:36], hTf[:, kc, :], self.wr[:, kc, :], kc == 0, kc == 7, r=[K("hTf"), "wr"], w=[prk])
            yield
            rk_ = K("rs")
            self.P("dve", "tensor_tensor", lg, pr[:, 0:36], self.rbbc, ALU.add, r=[prk, "rbbc"], w=[K("lg")])
            yield
            self.P("dve", "tensor_reduce", rs[:, 0:1], lg[:, 0:4], AX.X, ALU.max, r=[K("lg")], w=[rk_])
            yield
            self.P("dve", "tensor_scalar", ohg, lg[:, 0:4], rs[:, 0:1], None, ALU.is_equal, r=[K("lg"), rk_], w=[K("ohg")])
            self.P("dve", "tensor_single_scalar", rs[:, 1:2], rs[:, 0:1], -1.0, ALU.mult, r=[rk_], w=[rk_])
            yield
            self.actf(ge, lg[:, 0:4], AF.Exp, r=[K("lg"), rk_], w=[K("ge"), rk_], bias=rs[:, 1:2], accum_out=rs[:, 2:3])
            self.P("dve", "tensor_scalar", ohg, ohg, -1.0, 1e30, ALU.add, ALU.mult, r=[K("ohg")], w=[K("ohg")])
            yield
            self.P("dve", "reciprocal", rs[:, 3:4], rs[:, 2:3], r=[rk_], w=[rk_])
            self.P("dve", "tensor_tensor", lm.rearrange("p (g e) -> p g e", g=4), lg[:, 4:36].rearrange("p (g e) -> p g e", g=4),
                   ohg.unsqueeze(2).to_broadcast([128, 4, 8]), ALU.add, r=[K("lg"), K("ohg")], w=[K("lm")])
            yield
            self.P("dve", "tensor_reduce", rs[:, 4:5], lm, AX.X, ALU.max, r=[K("lm")], w=[rk_])
            yield
            self.P("dve", "tensor_scalar", oh1, lm, rs[:, 4:5], None, ALU.is_equal, r=[K("lm"), rk_], w=[K("rt")])
            yield
            self.P("dve", "scalar_tensor_tensor", l2, oh1, -1e30, lm, ALU.mult, ALU.add, r=[K("rt"), K("lm")], w=[K("l2")])
            yield
            self.P("dve", "tensor_reduce", rs[:, 5:6], l2, AX.X, ALU.max, r=[K("l2")], w=[rk_])
            yield
            self.P("dve", "tensor_scalar", oh2, l2, rs[:, 5:6], None, ALU.is_equal, r=[K("l2"), rk_], w=[K("rt")])
            self.P("dve", "tensor_tensor", rs[:, 6:7], rs[:, 5:6], rs[:, 4:5], ALU.subtract, r=[rk_], w=[rk_])
            yield
            self.actf(rs[:, 7:8], rs[:, 6:7], AF.Exp, r=[rk_], w=[rk_])
            yield
            self.P("dve", "tensor_single_scalar", rs[:, 8:9], rs[:, 7:8], 1.0, ALU.add, r=[rk_], w=[rk_])
            yield
            self.P("dve", "reciprocal", rs[:, 9:10], rs[:, 8:9], r=[rk_], w=[rk_])
            yield
            self.P("dve", "tensor_tensor", rs[:, 10:11], rs[:, 9:10], rs[:, 7:8], ALU.mult, r=[rk_], w=[rk_])
            yield
            self.P("dve", "tensor_tensor", rt[:, 64:65], rs[:, 9:10], rs[:, 3:4], ALU.mult, r=[rk_], w=[K("rt")])
            self.P("dve", "tensor_tensor", rt[:, 65:66], rs[:, 10:11], rs[:, 3:4], ALU.mult, r=[rk_], w=[K("rt")])
            yield
            kb.dma(self.route[b, rows, :], rt, reads=[K("rt")], writes=["route"])

        for tp in range(NT // 2):
            gens = [tile_gen(2 * tp + q, q) for q in range(2)]
            while gens:
                for g in list(gens):
                    try:
                        next(g)
                    except StopIteration:
                        gens.remove(g)

    def phaseE(self, l, b, final):
        kb = self.kb
        pb = self.pb
        a = Arena(self.arena_ap, self.r1_end, 208000)
        yacc = a.t([128, NT, D])
        hT = a.t([128, 8, T], BF16)
        gts = a.t([128, NT, 32])
        wg = [a.t([128, 8, EH], BF16) for _ in range(2)]
        wu = [a.t([128, 8, EH], BF16) for _ in range(2)]
        wd = [a.t([128, 4, D], BF16) for _ in range(2)]
        sl = [a.t([128, 512]) for _ in range(2)]
        act = [a.t([128, 4, 512], BF16) for _ in range(2)]
        for t in range(NT):
            kb.dma(yacc[:, t, :], self.xres[b, t * 128:(t + 1) * 128, :], writes=["y%d" % t])
        for kc in range(8):
            kb.dma(hT[:, kc, :], self.h2T[b, kc * 128:(kc + 1) * 128, :], writes=["hT"])
        kb.dma(gts, self.gates[b].rearrange("(t p) e -> p t e", p=128), writes=["gts"])
        it = 0
        nstage = 0
        stg = [a.t([128, 4, 512]) for _ in range(2)]
        for e in range(self.moe_experts):
            p = e % 2
            for wi, (wsrc, wdst, wkey, nh) in enumerate(((self.w_gate, wg[p], "wg%d" % p, 2), (self.w_up, wu[p], "wu%d" % p, 2),
                                                           (self.w_down, wd[p], "wd%d" % p, 1))):
                for hh in range(2):
                    si = nstage % 2
                    nstage += 1
                    if nh == 2:
                        src_ap = wsrc[l, e, hh * 512:(hh + 1) * 512, :].rearrange("(k p) n -> p k n", p=128)
                        dst_ap = wdst[:, hh * 4:(hh + 1) * 4, :]
                        stv = stg[si]
                    else:
                        src_ap = wsrc[l, e, hh * 256:(hh + 1) * 256, :].rearrange("(k p) n -> p k n", p=128)
                        dst_ap = wdst[:, hh * 2:(hh + 1) * 2, :]
                        stv = stg[si].rearrange("p k n -> p (k n)").rearrange("p (k n) -> p k n", k=2)
                    kb.dma(stv, src_ap, writes=["stg%d" % si])
                    if nstage % 2 == 0:
                        self.actf(dst_ap, stv, AF.Copy, r=["stg%d" % si], w=[wkey + "_%d" % hh])
                    else:
                        self.P("pool", "tensor_copy", dst_ap, stv, r=["stg%d" % si], w=[wkey + "_%d" % hh])
            for g in range(4):
                ap_ = it % 2
                it += 1
                tcols = slice(g * 512, (g + 1) * 512)
                for m in range(4):
                    pg = pb[(2 * m) % 4]; pgk = "pb%d" % ((2 * m) % 4)
                    pu = pb[(2 * m) % 4 + 1]; puk = "pb%d" % ((2 * m) % 4 + 1)
                    for kc in range(8):
                        self.mm(pg, wg[p][:, kc, m * 128:(m + 1) * 128], hT[:, kc, tcols], kc == 0, kc == 7,
                                r=["wg%d_0" % p, "wg%d_1" % p, "hT"], w=[pgk])
                    for kc in range(8):
                        self.mm(pu, wu[p][:, kc, m * 128:(m + 1) * 128], hT[:, kc, tcols], kc == 0, kc == 7,
                                r=["wu%d_0" % p, "wu%d_1" % p, "hT"], w=[puk])
                    s_ = sl[m % 2]; sk = "sl%d" % (m % 2)
                    self.actf(s_, pg, AF.Silu, r=[pgk], w=[sk])
                    self.P("dve", "tensor_tensor", act[ap_][:, m, :], s_, pu, ALU.mult, r=[sk, puk], w=["act%d_%d" % (ap_, m)])
                for tt in range(4):
                    t = g * 4 + tt
                    for hf in range(2):
                        po = pb[4 + (tt * 2 + hf) % 4]; pok = "pb%d" % (4 + (tt * 2 + hf) % 4)
                        for m in range(4):
                            self.mm(po, act[ap_][:, m, tt * 128:(tt + 1) * 128], wd[p][:, m, hf * 512:(hf + 1) * 512],
                                    m == 0, m == 3, r=["act%d_%d" % (ap_, m), "wd%d_0" % p, "wd%d_1" % p], w=[pok])
                        ys = yacc[:, t, hf * 512:(hf + 1) * 512]
                        self.P("dve", "scalar_tensor_tensor", ys, po, gts[:, t, e:e + 1], ys, ALU.mult, ALU.add,
                               r=[pok, "gts", "y%d" % t], w=["y%d" % t])
        if not final:
            for t in range(NT):
                kb.dma(self.xres[b, t * 128:(t + 1) * 128, :], yacc[:, t, :], reads=["y%d" % t], writes=["xres"])
        else:
            kb.barrier()
            gf = wg[0].rearrange("p k n -> p (k n)").bitcast(F32)[:, 0:D]
            junk = wu[0].rearrange("p k n -> p (k n)").bitcast(F32)[:, 0:D]
            ot = [wd[0].rearrange("p k n -> p (k n)").bitcast(F32)[:, 0:D], wd[1].rearrange("p k n -> p (k n)").bitcast(F32)[:, 0:D]]
            stf = sl[0]
            kb.dma(gf, self.final_g.partition_broadcast(128), writes=["wg0_0", "wg0_1", "gf"])
            for t in range(NT):
                p = t % 2
                self.rmsnorm(yacc[:, t, :], "y%d" % t, gf, "gf", ot[p], "ot%d" % p, junk, "fjunk",
                             stf[:, 4 * t:4 * t + 4], "sl0")
                kb.dma(self.out[b, t * 128:(t + 1) * 128, :], ot[p], reads=["ot%d" % p], writes=["out"])

    def phaseE_sparse(self, l, final):
        kb = self.kb
        pb = self.pb
        I32 = mybir.dt.int32
        NTT, NBLK = self.NTT, self.NBLK
        IO = bass.IndirectOffsetOnAxis
        a = Arena(self.arena_ap, self.r1_end, 208000)
        R = a.t([128, NTT, 66])
        OHs = a.t([128, NTT, 32])
        rk = a.t([128, NTT, 32])
        tmp = a.t([128, NTT, 32])
        cnt = a.t([128, 32]); nblk = a.t([128, 32]); pend = a.t([128, 32]); pstart = a.t([128, 32])
        cmp_ = a.t([128, max(NBLK, 32), 32])
        sl_f = a.t([128, NTT, 2])
        sl_i = a.t([128, NTT, 2], I32)
        be_f = a.t([128, NBLK])
        widx = a.t([128, NBLK], I32)
        stg = [a.t([128, 4096]) for _ in range(3)]
        wgb = [a.t([128, 8, EH], BF16) for _ in range(2)]
        wub = [a.t([128, 8, EH], BF16) for _ in range(2)]
        wdb = [a.t([128, 4, D], BF16) for _ in range(2)]
        xblk = [a.t([128, D], BF16) for _ in range(2)]
        xT = [a.t([128, 8, 128], BF16) for _ in range(2)]
        ssb = a.t([128, 512]); actb = a.t([128, 512], BF16); actT = a.t([128, 4, 128], BF16)
        yblk = [a.t([128, D]) for _ in range(2)]
        pbT = pb[7][:, 0:512].bitcast(BF16)
        for b in range(self.NB):
            kb.dma(R[:, b * NT:(b + 1) * NT, :], self.route[b].rearrange("(t p) c -> p t c", p=128), writes=["R"])
        self.P("dve", "tensor_tensor", OHs, R[:, :, 0:32], R[:, :, 32:64], ALU.add, r=["R"], w=["OHs"])
        for t in range(NTT):
            bank = pb[t // 16]
            bk = "pb%d" % (t // 16)
            cs = slice((t % 16) * 32, (t % 16 + 1) * 32)
            for t2 in range(t):
                self.mm(bank[:, cs], self.ones_f, OHs[:, t2, :], t2 == 0, False, r=["ones_f", "OHs"], w=[bk])
            self.mm(bank[:, cs], self.m_strict, OHs[:, t, :], t == 0, True, r=["cst", "OHs"], w=[bk])
        for t in range(NTT):
            self.mm(pb[2][:, 0:32], self.ones_f, OHs[:, t, :], t == 0, t == NTT - 1, r=["ones_f", "OHs"], w=["pb2"])
        for g in range(NTT // 16):
            self.actf(rk[:, g * 16:(g + 1) * 16, :].rearrange("p t e -> p (t e)"), pb[g], AF.Copy, r=["pb%d" % g], w=["rk"])
        self.P("dve", "tensor_copy", cnt, pb[2][:, 0:32], r=["pb2"], w=["cnt"])
        SB_ = self.S
        self.P("dve", "tensor_single_scalar", cnt, cnt, 128.0 / SB_, ALU.mult, r=["cnt"], w=["cnt"])
        self.P("dve", "tensor_tensor", cmp_[:, 0:32, :], cnt.unsqueeze(2).to_broadcast([128, 32, 32]),
               self.thr[:, 0:32].unsqueeze(1).to_broadcast([128, 32, 32]), ALU.is_gt, r=["cnt", "cex"], w=["cmp"])
        self.P("dve", "tensor_reduce", nblk, cmp_[:, 0:32, :], AX.X, ALU.add, r=["cmp"], w=["nblk"])
        self.P("dve", "tensor_single_scalar", nblk, nblk, float(SB_), ALU.mult, r=["nblk"], w=["nblk"])
        self.P("dve", "tensor_tensor_scan", pend, self.ones_f[:, 0:32], nblk, 0.0, ALU.mult, ALU.add, r=["ones_f", "nblk"], w=["pend"])
        self.P("dve", "tensor_tensor", pstart, pend, nblk, ALU.subtract, r=["pend", "nblk"], w=["pstart"])
        self.P("dve", "tensor_tensor", rk, rk, pstart.unsqueeze(1).to_broadcast([128, NTT, 32]), ALU.add, r=["rk", "pstart"], w=["rk"])
        for k in range(2):
            self.P("dve", "tensor_tensor", tmp, rk, R[:, :, k * 32:(k + 1) * 32], ALU.mult, r=["rk", "R"], w=["tmp"])
            self.P("dve", "tensor_reduce", sl_f[:, :, k], tmp, AX.X, ALU.add, r=["tmp"], w=["sl_f"])
        self.P("dve", "tensor_copy", sl_i, sl_f, r=["sl_f"], w=["sl_i"])
        self.P("dve", "tensor_single_scalar", pstart, pend, 128.0 / SB_, ALU.mult, r=["pend", "rk"], w=["pstart"])
        self.P("dve", "tensor_tensor", cmp_[:, 0:NBLK, :], self.thr[:, 0:NBLK].unsqueeze(2).to_broadcast([128, NBLK, 32]),
               pstart.unsqueeze(1).to_broadcast([128, NBLK, 32]), ALU.is_ge, r=["cex", "pstart"], w=["cmp"])
        self.P("dve", "tensor_reduce", be_f, cmp_[:, 0:NBLK, :], AX.X, ALU.add, r=["cmp"], w=["be_f"])
        self.P("dve", "tensor_scalar", be_f, be_f, 31.0, 128.0, ALU.min, ALU.mult, r=["be_f"], w=["be_f"])
        self.P("dve", "tensor_scalar", be_f, be_f, self.iota_p, float(l * NE * 128), ALU.add, ALU.add, r=["be_f", "cex"], w=["be_f"])
        self.P("dve", "tensor_copy", widx, be_f, r=["be_f"], w=["widx"])
        if self.stop in ("E0", "E1", "E2"):
            kb.dma(self.dbg_sl, sl_i.rearrange("p t k -> p (t k)"), reads=["sl_i"], writes=["dbg_sl"])
            kb.dma(self.dbg_widx, widx, reads=["widx"], writes=["dbg_widx"])
            for i_, (tl, tk) in enumerate(((cnt, "cnt"), (nblk, "nblk"), (pend, "pend"), (pstart, "pstart"))):
                kb.dma(self.dbg_misc[:, i_, :], tl, reads=[tk], writes=["dbg_misc"])
        if self.stop == "E0":
            return
        for t in range(NTT):
            b, tt = t // NT, t % NT
            p = t % 2
            kb.dma(xblk[p], self.h2tok[b, tt * 128:(tt + 1) * 128, :], writes=["xblk%d" % p])
            for k in range(2):
                kb.idma(self.xb, IO(ap=sl_i[:, t, k:k + 1], axis=0), xblk[p], None,
                        reads=["xblk%d" % p, "sl_i"], writes=["xb"])
        if self.stop == "E1":
            return
        ssb2 = [ssb, a.t([128, 512])]
        actb2 = [actb, a.t([128, 512], BF16)]
        actT2 = [actT, a.t([128, 4, 128], BF16)]

        def load_weights(j):
            p = j % 2
            for wi, (wsrc, wdst, wk) in enumerate(((self.w_gate, wgb[p], "wgb%d" % p), (self.w_up, wub[p], "wub%d" % p),
                                                   (self.w_down, wdb[p], "wdb%d" % p))):
                kb.idma(stg[wi], None, wsrc.rearrange("l r c -> (l r) c"), IO(ap=widx[:, j:j + 1], axis=0), reads=["widx"], writes=["stg%d" % wi])
                dstv = wdst.rearrange("p k n -> p (k n)")
                if wi == 0:
                    self.actf(dstv, stg[wi], AF.Copy, r=["stg%d" % wi], w=[wk])
                elif wi == 1:
                    self.P("dve", "tensor_copy", dstv, stg[wi], r=["stg%d" % wi], w=[wk])
                else:
                    self.actf(dstv[:, 0:2048], stg[wi][:, 0:2048], AF.Copy, r=["stg%d" % wi], w=[wk])
                    self.P("dve", "tensor_copy", dstv[:, 2048:4096], stg[wi][:, 2048:4096], r=["stg%d" % wi], w=[wk])

        def sub_gen(j, sub, q):
            p = j % 2
            row0 = j * self.S + sub * 128
            pT = pb[6 + q][:, 0:512].bitcast(BF16)
            pTk = "pb%d" % (6 + q)
            pg, pu, po = pb[2 * q], pb[2 * q + 1], pb[4 + q]
            pgk, puk, pok = "pb%d" % (2 * q), "pb%d" % (2 * q + 1), "pb%d" % (4 + q)
            kb.dma(xblk[q], self.xb[row0:row0 + 128, :], reads=["xb"], writes=["xblk%d" % q])
            yield
            for kc in range(8):
                self.P("pe", "transpose", pT[:, kc * 128:(kc + 1) * 128], xblk[q][:, kc * 128:(kc + 1) * 128],
                       self.ident_bf, r=["xblk%d" % q, "ident_bf"], w=[pTk])
            yield
            self.actf(xT[q].rearrange("p k n -> p (k n)"), pT, AF.Copy, r=[pTk], w=["xT%d" % q])
            yield
            for kc in range(8):
                self.mm(pg, xT[q][:, kc, :], wgb[p][:, kc, :], kc == 0, kc == 7, r=["xT%d" % q, "wgb%d" % p], w=[pgk])
            for kc in range(8):
                self.mm(pu, xT[q][:, kc, :], wub[p][:, kc, :], kc == 0, kc == 7, r=["xT%d" % q, "wub%d" % p], w=[puk])
            yield
            self.actf(ssb2[q], pg, AF.Silu, r=[pgk], w=["ssb%d" % q])
            yield
            self.P("dve", "tensor_tensor", actb2[q], ssb2[q], pu, ALU.mult, r=["ssb%d" % q, puk], w=["actb%d" % q])
            yield
            for m in range(4):
                self.P("pe", "transpose", pT[:, m * 128:(m + 1) * 128], actb2[q][:, m * 128:(m + 1) * 128],
                       self.ident_bf, r=["actb%d" % q, "ident_bf"], w=[pTk])
            yield
            self.actf(actT2[q].rearrange("p k n -> p (k n)"), pT[:, 0:512], AF.Copy, r=[pTk], w=["actT%d" % q])
            yield
            for hf in range(2):
                for m in range(4):
                    self.mm(po, actT2[q][:, m, :], wdb[p][:, m, hf * 512:(hf + 1) * 512], m == 0, m == 3,
                            r=["actT%d" % q, "wdb%d" % p], w=[pok])
                yield
                if hf == 0:
                    self.actf(yblk[q][:, 0:512], po, AF.Copy, r=[pok], w=["yblk%d" % q])
                else:
                    self.P("dve", "tensor_copy", yblk[q][:, 512:1024], po, r=[pok], w=["yblk%d" % q])
                yield
            kb.dma(self.yb[row0:row0 + 128, :], yblk[q], reads=["yblk%d" % q], writes=["yb"])

        def run2(gens):
            gens = list(gens)
            while gens:
                for g in list(gens):
                    try:
                        next(g)
                    except StopIteration:
                        gens.remove(g)

        load_weights(0)
        nsub = self.S // 128
        for j in range(NBLK):
            for pr in range(nsub // 2):
                run2([sub_gen(j, 2 * pr + q, q) for q in range(2)])
                if pr == 0 and j + 1 < NBLK:
                    load_weights(j + 1)
        if self.stop == "E2":
            return
        xt = [stg[0][:, 0:D], stg[0][:, D:2 * D]]
        y1 = [stg[1][:, 0:D], stg[1][:, D:2 * D]]
        y2 = [stg[2][:, 0:D], stg[2][:, D:2 * D]]
        ot = [stg[0][:, 2 * D:3 * D], stg[0][:, 3 * D:4 * D]]
        junk = stg[1][:, 2 * D:3 * D]
        gf = stg[2][:, 2 * D:3 * D]
        stf = a.t([128, 4 * NTT])
        kb.barrier()
        if final:
            kb.dma(gf, self.final_g.partition_broadcast(128), writes=["gf"])
        for t in range(NTT):
            b, tt = t // NT, t % NT
            p = t % 2
            rows = slice(tt * 128, (tt + 1) * 128)
            kb.dma(xt[p], self.xres[b, rows, :], writes=["cxt%d" % p])
            kb.idma(y1[p], None, self.yb, IO(ap=sl_i[:, t, 0:1], axis=0), reads=["sl_i"], writes=["cy1%d" % p])
            kb.idma(y2[p], None, self.yb, IO(ap=sl_i[:, t, 1:2], axis=0), reads=["sl_i"], writes=["cy2%d" % p])
            self.P("dve", "scalar_tensor_tensor", xt[p], y1[p], R[:, t, 64:65], xt[p], ALU.mult, ALU.add,
                   r=["cy1%d" % p, "R", "cxt%d" % p], w=["cxt%d" % p])
            self.P("dve", "scalar_tensor_tensor", xt[p], y2[p], R[:, t, 65:66], xt[p], ALU.mult, ALU.add,
                   r=["cy2%d" % p, "R", "cxt%d" % p], w=["cxt%d" % p])
            if not final:
                kb.dma(self.xres[b, rows, :], xt[p], reads=["cxt%d" % p], writes=["xres"])
            else:
                self.rmsnorm(xt[p], "cxt%d" % p, gf, "gf", ot[p], "cot%d" % p, junk, "cjunk", stf[:, 4 * t:4 * t + 4], "stf")
                kb.dma(self.out[b, rows, :], ot[p], reads=["cot%d" % p], writes=["out"])

    def build(self):
        kb = self.kb
        if self.sparse:
            z = self.arena_ap[:, 45000:45000 + 512].bitcast(BF16)
            self.P("pool", "memset", z, 0.0, w=["zz"])
            for j in range(self.NBLK * self.S // 128):
                kb.dma(self.xb[j * 128:(j + 1) * 128, :], z, reads=["zz"], writes=["xb"])
            kb.barrier()
        for l in range(self.NL):
            self.load_layer(l)
            for b in range(self.NB):
                src = self.x_in[b] if l == 0 else self.xres[b]
                self.phaseA(l, b, src)
                kb.barrier()
                if self.stop == "A":
                    return self.finish()
                self.phaseB_attn(l, b)
                kb.barrier()
                if self.stop == "B1":
                    return self.finish()
                self.phaseB_rwkv(l, b)
                kb.barrier()
                if self.stop == "B2":
                    return self.finish()
                self.phaseD(l, b, src)
                kb.barrier()
                if self.stop == "D":
                    return self.finish()
                if not self.sparse:
                    self.phaseE(l, b, final=(l == self.NL - 1))
                    kb.barrier()
            if self.sparse:
                self.phaseE_sparse(l, final=(l == self.NL - 1))
                kb.barrier()
        return self.finish()

    def finish(self):
        self.kb.barrier()
        return self


def prep_inputs(inp, b0, nb, NL=4):
    f = lambda a: np.ascontiguousarray(a, dtype=np.float32)

    def pm(w, k, n):
        L = w.shape[0]
        w5 = np.reshape(np.asarray(w, dtype=np.float32), (L, NE, k, 128, n))
        return np.ascontiguousarray(np.transpose(w5, (0, 1, 3, 2, 4))).reshape(L, NE * 128, k * n)
    m = {
        "x": f(inp["x"][b0:b0 + nb]),
        "consts": make_consts(),
        "norm1_g": f(inp["norm1_g"][:NL]),
        "w_in": f(inp["w_in"][:NL]),
        "gm_wT": f(np.transpose(inp["gm_w_s"][:NL], (0, 1, 3, 2))),
        "gm_bT": f(np.transpose(inp["gm_b"][:NL], (0, 2, 1))),
        "rw_mu": f(inp["rw_mu"][:NL]),
        "rw_pv": f(np.concatenate([
            np.transpose(np.reshape(inp["rw_mu"][:NL, 0:768], (NL, 12, 64)), (0, 2, 1))] + [
            np.transpose(np.reshape(np.reshape(inp[k][:NL], (NL, 256)), (NL, 4, 64)), (0, 2, 1))
            for k in ("rw_w0", "rw_a0", "rw_k_k", "rw_k_a", "rw_r_k")], axis=2)),
        "rw_w2": f(inp["rw_w2"][:NL]),
        "rw_a2": f(inp["rw_a2"][:NL]),
        "rw_g2": f(inp["rw_g2"][:NL]),
        "rw_gn_w": f(inp["rw_gn_w"][:NL]),
        "rw_gn_b": f(inp["rw_gn_b"][:NL]),
        "fx_b_f": f(inp["fx_b_f"][:NL]),
        "w_out": f(inp["w_out"][:NL]),
        "norm2_g": f(inp["norm2_g"][:NL]),
        "router_w": f(np.concatenate([inp["router_group_w"][:NL], inp["router_expert_w"][:NL]], axis=2)),
        "router_b": f(np.concatenate([inp["router_group_b"][:NL], inp["router_expert_b"][:NL]], axis=1)),
        "exp_w_gate": pm(inp["exp_w_gate"][:NL], 8, EH),
        "exp_w_up": pm(inp["exp_w_up"][:NL], 8, EH),
        "exp_w_down": pm(inp["exp_w_down"][:NL], 4, D),
        "final_norm_g": f(np.reshape(inp["final_norm_g"], (1, D))),
    }
    return m


def kernel(**inputs):
    inp = {k: np.asarray(v) for k, v in inputs.items()}
    n = 8
    nb = 2
    prog = Prog(NL=4, NB=nb).build()
    shared = prep_inputs(inp, 0, nb)
    in_maps = []
    for c in range(n):
        m = dict(shared)
        m["x"] = np.ascontiguousarray(inp["x"][c * nb:(c + 1) * nb], dtype=np.float32)
        in_maps.append(m)
    res = run_bass_kernel_spmd(prog.nc, in_maps, core_ids=list(range(n)))
    out = np.concatenate([np.asarray(r["out"]) for r in res.results], axis=0)
    return out.astype(np.float32)
```
